# Optimizing a Trainium2 kernel written in Bass

```python
import math
import jax, jax.numpy as jnp
from jax import lax
import numpy as np


D_MODEL = 1024
BATCH = 2
SEQ = 8192
DEPTH = 2

PLE_DIM = 256
CONV_W = 5
LN_EPS = 1e-5

GDN_HEADS = 4
GDN_DK = 128
GDN_DV = 128
GDN_CHUNK = 64
GDN_WIDTH = GDN_HEADS * GDN_DV
GDN_QKV = 2 * GDN_HEADS * GDN_DK + GDN_WIDTH
GDN_COLS = GDN_QKV + GDN_WIDTH + 2 * 2 * GDN_HEADS

RWKV_HEADS = 8
RWKV_HD = 64
RWKV_WIDTH = RWKV_HEADS * RWKV_HD
RWKV_DECAY_RANK = 64
RWKV_ICL_RANK = 64
RWKV_GATE_RANK = 128
RWKV_GN_EPS = 64e-5
RWKV_COLS = 3 * RWKV_WIDTH + 2 * RWKV_DECAY_RANK + 2 * RWKV_ICL_RANK + RWKV_GATE_RANK

SSD_HEADS = 16
SSD_HD = 64
SSD_WIDTH = SSD_HEADS * SSD_HD
SSD_GROUPS = 2
SSD_STATE = 128
SSD_CHUNK = 128
SSD_XBC = SSD_WIDTH + 2 * SSD_GROUPS * SSD_STATE
SSD_COLS = SSD_WIDTH + SSD_XBC + 2 * SSD_HEADS

N_BRANCH = 3
MIX_WIDTH = GDN_WIDTH + RWKV_WIDTH + SSD_WIDTH
GATE_COLS = N_BRANCH * D_MODEL
IN_COLS = GDN_COLS + RWKV_COLS + SSD_COLS + GATE_COLS

N_EXPERTS = 32
TOP_K = 4
D_EXPERT = 1024
SWIGLU_LIMIT = 7.0
SWIGLU_ALPHA = 1.702
MOE_BLOCK = 256

DEEPNORM_ALPHA = (2 * DEPTH) ** 0.25
DEEPNORM_BETA = (8 * DEPTH) ** -0.25

kernel_name = 'bidir_hybrid_gdn_rwkv7_ssd_moe'


def split_last(t, sizes):
    return jnp.split(t, np.cumsum(sizes)[:-1].tolist(), axis=-1)


def layer_norm(x, g, b):
    xf = x.astype(jnp.float32)
    mu = xf.mean(-1, keepdims=True)
    var = jnp.square(xf - mu).mean(-1, keepdims=True)
    return ((xf - mu) * lax.rsqrt(var + LN_EPS) * g + b).astype(x.dtype)


def rms_norm(x, w, eps):
    return x * lax.rsqrt(jnp.mean(jnp.square(x), -1, keepdims=True) + eps) * w


def l2_normalize(x, eps=1e-6):
    return x * lax.rsqrt(jnp.sum(jnp.square(x), -1, keepdims=True) + eps)


def centred_dwconv(x, w):
    return lax.conv_general_dilated(
        x, w[:, None, :].astype(x.dtype), window_strides=(1,),
        padding=[(CONV_W // 2, CONV_W // 2)], dimension_numbers=('NWC', 'WIO', 'NWC'),
        feature_group_count=x.shape[-1])


def gdn_chunked(q, k, v, g, beta):
    b, s, h, dk = q.shape
    dv = v.shape[-1]
    c = GDN_CHUNK
    n = s // c

    def to_chunks(t):
        t = jnp.moveaxis(t, 2, 1)
        return t.reshape(b, h, n, c, *t.shape[3:])

    qc, kc, vc = to_chunks(q), to_chunks(k), to_chunks(v)
    gc = jnp.cumsum(to_chunks(g), axis=-1)
    bc = to_chunks(beta)
    incl = jnp.tril(jnp.ones((c, c), bool))
    strict = jnp.tril(jnp.ones((c, c), bool), -1)
    decay = jnp.exp(jnp.where(incl, gc[..., :, None] - gc[..., None, :], -jnp.inf))
    kb = kc * bc[..., None]
    kkt = jnp.einsum('bhnid,bhnjd->bhnij', kb, kc) * decay
    lhs = jnp.where(strict, kkt, 0.0) + jnp.eye(c, dtype=kkt.dtype)
    rhs = jnp.concatenate([vc * bc[..., None], kb * jnp.exp(gc)[..., None]], -1)
    sol = lax.linalg.triangular_solve(lhs, rhs, left_side=True, lower=True, unit_diagonal=True)
    u, w = sol[..., :dv], sol[..., dv:]
    qk = jnp.einsum('bhnid,bhnjd->bhnij', qc, kc) * decay
    q_dec = qc * jnp.exp(gc)[..., None]
    k_dec = kc * jnp.exp(gc[..., -1:] - gc)[..., None]
    g_end = jnp.exp(gc[..., -1])

    def step(state, inp):
        u_i, w_i, qk_i, qd_i, kd_i, ge_i = inp
        v_new = u_i - jnp.einsum('bhcd,bhde->bhce', w_i, state)
        o = jnp.einsum('bhcd,bhde->bhce', qd_i, state) + jnp.einsum('bhij,bhje->bhie', qk_i, v_new)
        state = state * ge_i[..., None, None] + jnp.einsum('bhcd,bhce->bhde', kd_i, v_new)
        return state, o

    xs = tuple(jnp.moveaxis(t, 2, 0) for t in (u, w, qk, q_dec, k_dec, g_end))
    _, o = lax.scan(step, jnp.zeros((b, h, dk, dv), u.dtype), xs)
    return jnp.moveaxis(o, 0, 2).reshape(b, h, s, dv).transpose(0, 2, 1, 3)


def gdn_branch(cols, conv_w, a_log, dt_bias, norm_w):
    bs = cols.shape[:2]
    qkv, z, beta_raw, a_raw = split_last(cols, [GDN_QKV, GDN_WIDTH, 2 * GDN_HEADS, 2 * GDN_HEADS])
    qkv = jax.nn.silu(centred_dwconv(qkv, conv_w))
    q, k, v = split_last(qkv, [GDN_HEADS * GDN_DK, GDN_HEADS * GDN_DK, GDN_WIDTH])
    q = l2_normalize(q.reshape(*bs, GDN_HEADS, GDN_DK)) * GDN_DK ** -0.5
    k = l2_normalize(k.reshape(*bs, GDN_HEADS, GDN_DK))
    v = v.reshape(*bs, GDN_HEADS, GDN_DV)
    beta = jax.nn.sigmoid(beta_raw.reshape(*bs, 2, GDN_HEADS))
    g = -jnp.exp(a_log) * jax.nn.softplus(a_raw.reshape(*bs, 2, GDN_HEADS) + dt_bias)
    fwd = gdn_chunked(q, k, v, g[:, :, 0], beta[:, :, 0])
    bwd = gdn_chunked(*(jnp.flip(t, 1) for t in (q, k, v, g[:, :, 1], beta[:, :, 1])))
    o = fwd + jnp.flip(bwd, 1)
    o = rms_norm(o, norm_w, 1e-6) * jax.nn.silu(z.reshape(*bs, GDN_HEADS, GDN_DV))
    return o.reshape(*bs, GDN_WIDTH)


def token_shift(t, mu_prev, mu_next):
    prev = jnp.pad(t[:, :-1], ((0, 0), (1, 0), (0, 0)))
    nxt = jnp.pad(t[:, 1:], ((0, 0), (0, 1), (0, 0)))
    return t + mu_prev * (prev - t) + mu_next * (nxt - t)


def rwkv7_scan(r, w, k, v, kk, a):
    d, b, s, h, n = r.shape

    def step(state, inp):
        r_t, w_t, k_t, v_t, kk_t, a_t = inp
        sa = jnp.einsum('dbhvk,dbhk->dbhv', state, kk_t)
        state = (state * w_t[..., None, :] - sa[..., :, None] * (kk_t * a_t)[..., None, :]
                 + v_t[..., :, None] * k_t[..., None, :])
        return state, jnp.einsum('dbhvk,dbhk->dbhv', state, r_t)

    xs = tuple(jnp.moveaxis(t, 2, 0) for t in (r, w, k, v, kk, a))
    _, y = lax.scan(step, jnp.zeros((d, b, h, n, n), r.dtype), xs)
    return jnp.moveaxis(y, 0, 2)


def rwkv7_branch(cols, mu_prev, mu_next, w0, w_up, a0, a_up, g_up, k_k, k_a, r_k, ln_g, ln_b):
    bs = cols.shape[:2]
    cols = token_shift(cols, mu_prev, mu_next)
    r, k, v, wl, al, gl = split_last(cols, [RWKV_WIDTH] * 3 + [2 * RWKV_DECAY_RANK, 2 * RWKV_ICL_RANK, RWKV_GATE_RANK])
    w_raw = w0 + jnp.einsum('bsdr,drc->bsdc', jnp.tanh(wl.reshape(*bs, 2, RWKV_DECAY_RANK)), w_up)
    decay = jnp.exp(-jnp.exp(-jax.nn.softplus(-w_raw) - 0.5))
    a = jax.nn.sigmoid(a0 + jnp.einsum('bsdr,drc->bsdc', al.reshape(*bs, 2, RWKV_ICL_RANK), a_up))
    g = jax.nn.sigmoid(gl) @ g_up

    def heads(t):
        return t.reshape(*t.shape[:-1], RWKV_HEADS, RWKV_HD)

    def both(t):
        return jnp.stack([t, jnp.flip(t, 1)])

    def per_dir(t):
        return jnp.stack([t[:, :, 0], jnp.flip(t[:, :, 1], 1)])

    kk = l2_normalize(heads(k * k_k))
    k_dir = k[:, :, None] * (1.0 + (a - 1.0) * k_a)
    r_h, v_h = heads(r), heads(v)
    y = rwkv7_scan(both(r_h), heads(per_dir(decay)), heads(per_dir(k_dir)), both(v_h), both(kk),
                   heads(per_dir(a)))
    y = y[0] + jnp.flip(y[1], 1)
    mu = y.mean(-1, keepdims=True)
    var = jnp.square(y - mu).mean(-1, keepdims=True)
    y = ((y - mu) * lax.rsqrt(var + RWKV_GN_EPS)).reshape(*bs, RWKV_WIDTH) * ln_g + ln_b
    bonus = jnp.sum(r_h[:, :, None] * heads(k_dir) * r_k, -1, keepdims=True).sum(2)
    y = y + (bonus * v_h).reshape(*bs, RWKV_WIDTH)
    return y * g


def ssd_chunked(x, a, bm, cm):
    b, s, g, hg, p = x.shape
    n = bm.shape[-1]
    q = SSD_CHUNK
    nc = s // q
    xc = x.reshape(b, nc, q, g, hg, p)
    bc = bm.reshape(b, nc, q, g, n)
    cc = cm.reshape(b, nc, q, g, n)
    acum = jnp.cumsum(a.reshape(b, nc, q, g, hg), axis=2)
    incl = jnp.tril(jnp.ones((q, q), bool))[:, :, None, None]
    seg = jnp.exp(jnp.where(incl, acum[:, :, :, None] - acum[:, :, None], -jnp.inf))
    scores = jnp.einsum('bclgn,bcsgn->bclsg', cc, bc)[..., None] * seg
    y_diag = jnp.einsum('bclsgh,bcsghp->bclghp', scores, xc)
    x_end = xc * jnp.exp(acum[:, :, -1:] - acum)[..., None]
    chunk_states = jnp.einsum('bcsgn,bcsghp->bcghpn', bc, x_end)
    chunk_decay = jnp.exp(acum[:, :, -1])

    def step(h, inp):
        st, dec = inp
        return h * dec[..., None, None] + st, h

    _, h_in = lax.scan(step, jnp.zeros((b, g, hg, p, n), x.dtype),
                       (jnp.moveaxis(chunk_states, 1, 0), jnp.moveaxis(chunk_decay, 1, 0)))
    y_off = jnp.einsum('bclgn,cbghpn->bclghp', cc, h_in) * jnp.exp(acum)[..., None]
    return (y_diag + y_off).reshape(b, s, g, hg, p)


def ssd_branch(cols, conv_w, conv_b, a_log, dt_bias, d_skip, norm_w):
    bs = cols.shape[:2]
    hg = SSD_HEADS // SSD_GROUPS
    z, xbc, dt_raw = split_last(cols, [SSD_WIDTH, SSD_XBC, 2 * SSD_HEADS])
    xbc = jax.nn.silu(centred_dwconv(xbc, conv_w) + conv_b)
    x, bm, cm = split_last(xbc, [SSD_WIDTH, SSD_GROUPS * SSD_STATE, SSD_GROUPS * SSD_STATE])
    x = x.reshape(*bs, SSD_GROUPS, hg, SSD_HD)
    bm = bm.reshape(*bs, SSD_GROUPS, SSD_STATE)
    cm = cm.reshape(*bs, SSD_GROUPS, SSD_STATE)
    dt = jax.nn.softplus(dt_raw.reshape(*bs, 2, SSD_HEADS) + dt_bias)
    log_a = (dt * -jnp.exp(a_log)).reshape(*bs, 2, SSD_GROUPS, hg)
    dt = dt.reshape(*bs, 2, SSD_GROUPS, hg)
    fwd = ssd_chunked(x * dt[:, :, 0, ..., None], log_a[:, :, 0], bm, cm)
    bwd = ssd_chunked(*(jnp.flip(t, 1) for t in (x * dt[:, :, 1, ..., None], log_a[:, :, 1], bm, cm)))
    y = fwd + jnp.flip(bwd, 1) + x * d_skip.reshape(SSD_GROUPS, hg, 1)
    y = y.reshape(*bs, SSD_WIDTH) * jax.nn.silu(z)
    y = rms_norm(y.reshape(*bs, SSD_GROUPS, SSD_WIDTH // SSD_GROUPS),
                 norm_w.reshape(SSD_GROUPS, SSD_WIDTH // SSD_GROUPS), 1e-5)
    return y.reshape(*bs, SSD_WIDTH)


def moe_ffn(h, w_router, b_router, w_gu, b_gu, w_down, b_down):
    t, d = h.shape
    logits = jnp.dot(h, w_router).astype(jnp.float32) + b_router
    top_val, top_idx = lax.top_k(logits, TOP_K)
    gate = jax.nn.softmax(top_val, axis=-1)
    n_slots = t * TOP_K
    n_blocks = -(-n_slots // MOE_BLOCK) + N_EXPERTS
    flat_e = top_idx.reshape(-1)
    order = jnp.argsort(flat_e)
    sorted_e = flat_e[order]
    counts = jnp.bincount(flat_e, length=N_EXPERTS)
    padded = (counts + MOE_BLOCK - 1) // MOE_BLOCK * MOE_BLOCK
    pad_end = jnp.cumsum(padded)
    grp_start = jnp.cumsum(counts) - counts
    dest = (pad_end - padded)[sorted_e] + jnp.arange(n_slots) - grp_start[sorted_e]
    row_tok = jnp.full((n_blocks * MOE_BLOCK,), t, jnp.int32).at[dest].set((order // TOP_K).astype(jnp.int32))
    row_gate = jnp.zeros((n_blocks * MOE_BLOCK,), jnp.float32).at[dest].set(gate.reshape(-1)[order])
    block_e = jnp.minimum(jnp.searchsorted(pad_end, jnp.arange(n_blocks) * MOE_BLOCK, side='right'),
                          N_EXPERTS - 1)
    x_rows = h[jnp.minimum(row_tok, t - 1)].reshape(n_blocks, MOE_BLOCK, d)

    def expert_block(args):
        xb, e = args
        gu = xb @ w_gu[e] + b_gu[e]
        glu, up = gu[:, :D_EXPERT], gu[:, D_EXPERT:]
        glu = jnp.minimum(glu, SWIGLU_LIMIT)
        up = jnp.clip(up, -SWIGLU_LIMIT, SWIGLU_LIMIT)
        return ((up + 1.0) * glu * jax.nn.sigmoid(SWIGLU_ALPHA * glu)) @ w_down[e] + b_down[e]

    y_rows = lax.map(expert_block, (x_rows, block_e)).reshape(-1, d)
    out = jnp.zeros((t, d), jnp.float32).at[row_tok].add(y_rows * row_gate[:, None], mode='drop')
    return out.astype(h.dtype)


def setup_inputs(seed: int = 0) -> dict:
    key = jax.random.key(seed)
    keys = iter(jax.random.split(key, 64))

    def normal(shape, scale):
        return jax.random.normal(next(keys), shape, jnp.float32) * scale

    def uniform(shape, lo, hi):
        return jax.random.uniform(next(keys), shape, jnp.float32, lo, hi)

    def gain(shape):
        return 1.0 + normal(shape, 0.02)

    def dt_bias(shape):
        dt = jnp.exp(uniform(shape, math.log(1e-3), math.log(1e-1)))
        return dt + jnp.log(-jnp.expm1(-dt))

    L, D, beta = DEPTH, D_MODEL, DEEPNORM_BETA
    return {
        'x': normal((BATCH, SEQ, D), 1.0),
        'p': normal((L, BATCH, SEQ, PLE_DIM), 1.0),
        'ln_in_g': gain((D,)),
        'ln_in_b': normal((D,), 0.02),
        'w_in': normal((L, D, IN_COLS), D ** -0.5),
        'gdn_conv': normal((L, CONV_W, GDN_QKV), CONV_W ** -0.5),
        'gdn_a_log': jnp.log(uniform((L, 2, GDN_HEADS), 1.0, 16.0)),
        'gdn_dt_bias': dt_bias((L, 2, GDN_HEADS)),
        'gdn_norm': gain((L, GDN_DV)),
        'rwkv_mu_prev': uniform((L, RWKV_COLS), 0.0, 0.5),
        'rwkv_mu_next': uniform((L, RWKV_COLS), 0.0, 0.5),
        'rwkv_w0': uniform((L, 2, RWKV_WIDTH), -6.5, -1.5),
        'rwkv_w_up': normal((L, 2, RWKV_DECAY_RANK, RWKV_WIDTH), 0.3 * RWKV_DECAY_RANK ** -0.5),
        'rwkv_a0': normal((L, 2, RWKV_WIDTH), 0.1),
        'rwkv_a_up': normal((L, 2, RWKV_ICL_RANK, RWKV_WIDTH), 0.3 * RWKV_ICL_RANK ** -0.5),
        'rwkv_g_up': normal((L, RWKV_GATE_RANK, RWKV_WIDTH), RWKV_GATE_RANK ** -0.5),
        'rwkv_k_k': 0.85 + normal((L, RWKV_WIDTH), 0.02),
        'rwkv_k_a': gain((L, RWKV_WIDTH)),
        'rwkv_r_k': normal((L, RWKV_HEADS, RWKV_HD), 0.1),
        'rwkv_ln_g': gain((L, RWKV_WIDTH)),
        'rwkv_ln_b': normal((L, RWKV_WIDTH), 0.02),
        'ssd_conv': normal((L, CONV_W, SSD_XBC), CONV_W ** -0.5),
        'ssd_conv_b': normal((L, SSD_XBC), 0.02),
        'ssd_a_log': jnp.log(uniform((L, 2, SSD_HEADS), 1.0, 16.0)),
        'ssd_dt_bias': dt_bias((L, 2, SSD_HEADS)),
        'ssd_d': gain((L, SSD_HEADS)),
        'ssd_norm': gain((L, SSD_WIDTH)),
        'w_branch': jnp.concatenate([normal((L, GDN_WIDTH, D), beta * GDN_WIDTH ** -0.5),
                                     normal((L, RWKV_WIDTH, D), beta * RWKV_WIDTH ** -0.5),
                                     normal((L, SSD_WIDTH, D), beta * SSD_WIDTH ** -0.5)], axis=1),
        'w_o': normal((L, D, D), beta * D ** -0.5),
        'ln1_g': gain((L, D)),
        'ln1_b': normal((L, D), 0.02),
        'w_router': normal((L, D, N_EXPERTS), D ** -0.5),
        'b_router': normal((L, N_EXPERTS), 0.01),
        'w_gu': normal((L, N_EXPERTS, D, 2 * D_EXPERT), D ** -0.5),
        'b_gu': normal((L, N_EXPERTS, 2 * D_EXPERT), 0.01),
        'w_down': normal((L, N_EXPERTS, D_EXPERT, D), beta * D_EXPERT ** -0.5),
        'b_down': normal((L, N_EXPERTS, D), 0.01),
        'w_pl': normal((L, PLE_DIM, D), beta * PLE_DIM ** -0.5),
        'w_pl_gate': normal((L, D, D), D ** -0.5),
        'ln2_g': gain((L, D)),
        'ln2_b': normal((L, D), 0.02),
    }


def reference(x, p, ln_in_g, ln_in_b, w_in, gdn_conv, gdn_a_log, gdn_dt_bias, gdn_norm,
              rwkv_mu_prev, rwkv_mu_next, rwkv_w0, rwkv_w_up, rwkv_a0, rwkv_a_up, rwkv_g_up,
              rwkv_k_k, rwkv_k_a, rwkv_r_k, rwkv_ln_g, rwkv_ln_b, ssd_conv, ssd_conv_b, ssd_a_log,
              ssd_dt_bias, ssd_d, ssd_norm, w_branch, w_o, ln1_g, ln1_b, w_router, b_router,
              w_gu, b_gu, w_down, b_down, w_pl, w_pl_gate, ln2_g, ln2_b):
    b, s, d = x.shape
    f32 = jnp.float32
    x = layer_norm(x, ln_in_g, ln_in_b)
    for i in range(DEPTH):
        cols = x @ w_in[i]
        c_gdn, c_rwkv, c_ssd, c_gate = split_last(cols, [GDN_COLS, RWKV_COLS, SSD_COLS, GATE_COLS])
        y_a = gdn_branch(c_gdn.astype(f32), gdn_conv[i], gdn_a_log[i], gdn_dt_bias[i], gdn_norm[i])
        y_b = rwkv7_branch(c_rwkv.astype(f32), rwkv_mu_prev[i], rwkv_mu_next[i], rwkv_w0[i],
                           rwkv_w_up[i], rwkv_a0[i], rwkv_a_up[i], rwkv_g_up[i], rwkv_k_k[i],
                           rwkv_k_a[i], rwkv_r_k[i], rwkv_ln_g[i], rwkv_ln_b[i])
        y_c = ssd_branch(c_ssd.astype(f32), ssd_conv[i], ssd_conv_b[i], ssd_a_log[i],
                         ssd_dt_bias[i], ssd_d[i], ssd_norm[i])
        wb = w_branch[i]
        gates = jax.nn.sigmoid(c_gate.reshape(b, s, N_BRANCH, d))
        merged = (gates[:, :, 0] * (y_a.astype(x.dtype) @ wb[:GDN_WIDTH])
                  + gates[:, :, 1] * (y_b.astype(x.dtype) @ wb[GDN_WIDTH:GDN_WIDTH + RWKV_WIDTH])
                  + gates[:, :, 2] * (y_c.astype(x.dtype) @ wb[GDN_WIDTH + RWKV_WIDTH:]))
        x = layer_norm(DEEPNORM_ALPHA * x + merged @ w_o[i], ln1_g[i], ln1_b[i])
        ffn = moe_ffn(x.reshape(b * s, d), w_router[i], b_router[i], w_gu[i], b_gu[i],
                      w_down[i], b_down[i]).reshape(b, s, d)
        ple = (p[i] @ w_pl[i]) * jax.nn.sigmoid(x @ w_pl_gate[i])
        x = layer_norm(DEEPNORM_ALPHA * x + ffn + ple, ln2_g[i], ln2_b[i])
    return x
```

```python
import os
import contextlib, time
import numpy as np
import concourse.bass as bass
import concourse.mybir as mybir
from concourse.bass_utils import run_bass_kernel_spmd

F32 = mybir.dt.float32
BF16 = mybir.dt.bfloat16
I32 = mybir.dt.int32
ALU = mybir.AluOpType
AF = mybir.ActivationFunctionType
AX = mybir.AxisListType

ENG = ["sync", "gpsimd", "scalar", "vector", "tensor"]
NDMASEM = 6
import os as _os
ATTACH = bool(int(_os.environ.get('FW_ATTACH', '1')))


class Prog:
    def __init__(self, immediate=True):
        self.immediate = immediate
        self.nc = bass.Bass("TRN2", target_bir_lowering=False)
        try:
            self.nc.allow_low_precision("bf16 matmul operands with fp32 accumulation")
            self.nc.allow_non_contiguous_dma("strided layouts")
        except Exception as ex:
            print("allow_* failed", ex)
        self.st = contextlib.ExitStack()
        self.ops = {e: [] for e in ENG}
        self.cnt = {}
        self.sems = {}
        self.lastw = {}
        self.reads = {}
        self.seen = {e: {} for e in ENG}
        self.dma_i = {e: 0 for e in ENG}
        self.dma_last = {}
        self.ninstr = 0
        for e in ["gpsimd", "scalar", "vector", "tensor"]:
            self._sem("c_" + e)
        for e in ["sync", "gpsimd", "scalar"]:
            for i in range(NDMASEM):
                self._sem(f"d_{e}_{i}")

    def _sem(self, name):
        self.sems[name] = self.st.enter_context(self.nc.semaphore(name))
        self.cnt[name] = 0

    def dram(self, name, shape, dt=F32, kind="ExternalInput"):
        return self.nc.dram_tensor(name, list(shape), dt, kind=kind).ap()

    def sb(self, name, shape, dt=F32):
        return self.st.enter_context(self.nc.sbuf_tensor("s_" + name, list(shape), dt))

    def ps(self, name, shape, dt=F32):
        return self.st.enter_context(self.nc.psum_tensor("p_" + name, list(shape), dt))

    def _deps(self, eng, reads, writes):
        need = {}
        def add(tok):
            if tok is None:
                return
            s, v = tok
            if need.get(s, 0) < v:
                need[s] = v
        for k in reads:
            add(self.lastw.get(k))
        for k in writes:
            add(self.lastw.get(k))
            for t in self.reads.get(k, ()):
                add(t)
        out = []
        for s, v in need.items():
            if self.seen[eng].get(s, 0) < v:
                self.seen[eng][s] = v
                out.append((s, v))
        return out

    def _commit(self, tok, reads, writes):
        for k in reads:
            self.reads.setdefault(k, []).append(tok)
        for k in writes:
            self.lastw[k] = tok
            self.reads[k] = []

    def op(self, eng, fn, reads=(), writes=()):
        psr = [k for k in reads if isinstance(k, str) and k.startswith("ps")]
        if psr:
            reads = [k for k in reads if k not in psr]
            writes = list(writes) + psr
        waits = self._deps(eng, reads, writes)
        s = "c_" + eng
        self.cnt[s] += 1
        tok = (s, self.cnt[s])
        self._commit(tok, reads, writes)
        self._emit(eng, waits, fn, s, 1)
        self.ninstr += 1
        return tok

    def dma(self, eng, out, in_, reads=(), writes=(), **kw):
        slot = self.dma_i[eng] % NDMASEM
        self.dma_i[eng] += 1
        s = f"d_{eng}_{slot}"
        waits = self._deps(eng, reads, writes)
        prev = self.cnt[s]
        if prev > 0 and self.seen[eng].get(s, 0) < prev:
            self.seen[eng][s] = prev
            waits.append((s, prev))
        self.cnt[s] += 16
        tok = (s, self.cnt[s])
        self._commit(tok, reads, writes)
        fn = lambda e, out=out, in_=in_, kw=kw: e.dma_start(out=out, in_=in_, **kw)
        self._emit(eng, waits, fn, s, 16)
        self.ninstr += 1
        return tok

    def coll(self, kind, in_ap, out_ap, groups, reads=(), writes=()):
        eng = "gpsimd"
        slot = self.dma_i[eng] % NDMASEM
        self.dma_i[eng] += 1
        s = f"d_{eng}_{slot}"
        waits = self._deps(eng, reads, writes)
        prev = self.cnt[s]
        if prev > 0 and self.seen[eng].get(s, 0) < prev:
            self.seen[eng][s] = prev
            waits.append((s, prev))
        self.cnt[s] += 16
        tok = (s, self.cnt[s])
        self._commit(tok, reads, writes)
        fn = lambda e: e.collective_compute(kind, ALU.bypass, replica_groups=groups, ins=[in_ap], outs=[out_ap])
        self._emit(eng, waits, fn, s, 16)
        self.ninstr += 1
        return tok

    def barrier(self):
        for eng in ENG:
            waits = []
            for sname, v in self.cnt.items():
                if v > 0 and self.seen[eng].get(sname, 0) < v:
                    self.seen[eng][sname] = v
                    waits.append((sname, v))
            self._emit(eng, waits, None, None, 0)

    def finish_wait(self, eng, keys):
        waits = self._deps(eng, keys, ())
        self._emit(eng, waits, None, None, 0)

    def _emit(self, eng, waits, fn, s, inc):
        if not self.immediate:
            self.ops[eng].append((waits, fn, s, inc)); return
        engobj = getattr(self.nc, eng)
        if fn is None or not ATTACH:
            for (ws, wv) in waits:
                engobj.wait_ge(self.sems[ws], wv)
            if fn is not None:
                fn(engobj).then_inc(self.sems[s], inc)
            return
        for (ws, wv) in waits[1:]:
            engobj.wait_ge(self.sems[ws], wv)
        ins = fn(engobj)
        if waits:
            ins._wait_ge(self.sems[waits[0][0]], waits[0][1])
        ins.then_inc(self.sems[s], inc)

    def build(self):
        if self.immediate:
            self.st.close(); return self.nc
        nc = self.nc
        with nc.Block() as block:
            def mk(e):
                def body(engobj):
                    for waits, fn, s, inc in self.ops[e]:
                        for (ws, wv) in waits:
                            engobj.wait_ge(self.sems[ws], wv)
                        if fn is not None:
                            fn(engobj).then_inc(self.sems[s], inc)
                return body
            block.sync(mk("sync"))
            block.gpsimd(mk("gpsimd"))
            block.scalar(mk("scalar"))
            block.vector(mk("vector"))
            block.tensor(mk("tensor"))
        self.st.close()
        return nc


NCOL = 2060
NEG = -30000.0
RV = {}
_o = 0
for _n, _l in [("gconv", 5 * 384), ("sconv", 5 * 512), ("sconvb", 512), ("mup", 768), ("mun", 768), ("spb", 10), ("alog", 10),
               ("gnorm", 128), ("w0", 256), ("a0", 256), ("kk", 128), ("ka", 128), ("rk", 128), ("lng", 128), ("lnb", 128), ("dsk", 256)]:
    RV[_n] = (_o, _l); _o += _l
NV = _o


def host_inputs_A(z, L, stream_b, j):
    f = lambda a: np.ascontiguousarray(a, dtype=np.float32)
    w_in = z['w_in'][L]
    g = j // 2
    GD0, RW0, SS0 = 0, 2064, 2064 + 1920
    r = lambda a, n: list(range(a, a + n))
    cols = (r(GD0 + j * 128, 128) + r(GD0 + 512 + j * 128, 128) + r(GD0 + 1024 + j * 128, 128) + r(GD0 + 1536 + j * 128, 128)
            + r(RW0 + j * 128, 128) + r(RW0 + 512 + j * 128, 128) + r(RW0 + 1024 + j * 128, 128) + r(RW0 + 1536, 384)
            + r(SS0 + j * 256, 256) + r(SS0 + 1024 + j * 256, 256) + r(SS0 + 2048 + g * 128, 128) + r(SS0 + 2304 + g * 128, 128)
            + [GD0 + 2048 + d * 4 + j for d in range(2)] + [GD0 + 2056 + d * 4 + j for d in range(2)]
            + [SS0 + 2560 + d * 16 + 4 * j + i for d in range(2) for i in range(4)])
    assert len(cols) == NCOL
    rv = np.zeros(NV, np.float32)
    def put(n, a):
        o, l = RV[n]; a = np.asarray(a, np.float32).reshape(-1); assert a.size == l, (n, a.size, l); rv[o:o + l] = a
    qkv_idx = r(j * 128, 128) + r(512 + j * 128, 128) + r(1024 + j * 128, 128)
    xbc_idx = r(j * 256, 256) + r(1024 + g * 128, 128) + r(1280 + g * 128, 128)
    rw_idx = r(j * 128, 128) + r(512 + j * 128, 128) + r(1024 + j * 128, 128) + r(1536, 384)
    put("gconv", z['gdn_conv'][L][:, qkv_idx]); put("sconv", z['ssd_conv'][L][:, xbc_idx]); put("sconvb", z['ssd_conv_b'][L][xbc_idx])
    put("mup", z['rwkv_mu_prev'][L][rw_idx]); put("mun", z['rwkv_mu_next'][L][rw_idx])
    put("spb", np.concatenate([z['gdn_dt_bias'][L][:, j], z['ssd_dt_bias'][L][:, 4 * j:4 * j + 4].reshape(-1)]))
    put("alog", np.concatenate([z['gdn_a_log'][L][:, j], z['ssd_a_log'][L][:, 4 * j:4 * j + 4].reshape(-1)]))
    put("gnorm", z['gdn_norm'][L]); put("w0", z['rwkv_w0'][L][:, j * 128:(j + 1) * 128]); put("a0", z['rwkv_a0'][L][:, j * 128:(j + 1) * 128])
    put("kk", z['rwkv_k_k'][L][j * 128:(j + 1) * 128]); put("ka", z['rwkv_k_a'][L][j * 128:(j + 1) * 128])
    put("rk", z['rwkv_r_k'][L][2 * j:2 * j + 2]); put("lng", z['rwkv_ln_g'][L][j * 128:(j + 1) * 128]); put("lnb", z['rwkv_ln_b'][L][j * 128:(j + 1) * 128])
    put("dsk", np.repeat(z['ssd_d'][L][4 * j:4 * j + 4], 64))
    k = np.arange(128)
    UT = (k[:, None] <= k[None, :]).astype(np.float32); LT = (k[:, None] >= k[None, :]).astype(np.float32)
    blk = (k[:, None] // 64 == k[None, :] // 64).astype(np.float32)
    bdUT = UT * blk; bdLT = LT * blk
    msk = np.stack([UT, LT, np.where(UT > 0, 0.0, NEG), np.where(LT > 0, 0.0, NEG), np.eye(128), np.ones((128, 128)), bdUT, bdLT, -(bdUT - np.eye(128)), -(bdLT - np.eye(128)), bdUT - np.eye(128), bdLT - np.eye(128)], axis=1)
    return {
        "xT": f(stream_b.T), "wc": f(w_in[:, cols]), "rowvec": f(rv[None]),
        "wup": f(z['rwkv_w_up'][L][:, :, j * 128:(j + 1) * 128].reshape(128, 128)),
        "aup": f(z['rwkv_a_up'][L][:, :, j * 128:(j + 1) * 128].reshape(128, 128)),
        "gup": f(z['rwkv_g_up'][L][:, j * 128:(j + 1) * 128]), "msk": f(msk),
    }


def build_A(T=8192, dump=False, NS=16, phases=(1, 2, 3, 4)):
    p = Prog(); nc = p.nc
    NTL = T // 128
    dk = "ExternalOutput" if dump else "Internal"
    xT = p.dram("xT", [1024, T]); wc = p.dram("wc", [1024, NCOL]); rowvec = p.dram("rowvec", [1, NV])
    wup = p.dram("wup", [128, 128]); aup = p.dram("aup", [128, 128]); gup = p.dram("gup", [128, 128]); mskd = p.dram("msk", [128, 12, 128])
    ya_o = p.dram("ya", [T, 128], kind="ExternalOutput"); yb_o = p.dram("yb", [T, 128], kind="ExternalOutput"); yc_o = p.dram("yc", [T, 256], kind="ExternalOutput")
    cols_s = p.dram("cols_s", [T + 4, NCOL], kind=dk)
    gkq_s = p.dram("gkq_s", [T, 2, 128], kind=dk); gsc_s = p.dram("gsc_s", [T, 4], kind=dk); gbvT_s = p.dram("gbvT_s", [2, 128, T], kind=dk)
    rw_s = p.dram("rw_s", [2, 2, T, 5, 64], kind=dk); rvT_s = p.dram("rvT_s", [128, T], kind=dk); rpost_s = p.dram("rpost_s", [T, 258], kind=dk)
    ssd_s = p.dram("ssd_s", [T, 528], kind=dk)
    gy_s = p.dram("gy_s", [2, T, 128], kind=dk); gkqv_s = p.dram("gkqv_s", [T, 3, 128], kind=dk); gbg_s = p.dram("gbg_s", [T, 4], kind=dk); ry_s = p.dram("ry_s", [2, T, 128], kind=dk); sy_s = p.dram("sy_s", [T, 256], kind=dk)

    V = lambda fn, r=(), w=(): p.op("vector", fn, r, w)
    G = lambda fn, r=(), w=(): p.op("gpsimd", fn, r, w)
    S = lambda fn, r=(), w=(): p.op("scalar", fn, r, w)
    PE = lambda fn, r=(), w=(): p.op("tensor", fn, r, w)

    msk = p.sb("msk", [128, 12, 128]); rvs = p.sb("rvs", [128, NV])
    p.dma("sync", msk[:], mskd[:, :, :], writes=["msk"])
    p.dma("sync", rvs[:], rowvec.partition_broadcast(128)[:, 0, :], writes=["rvs"])
    UT, LT, NEGf, NEGb, ident, ones, bdUT, bdLT, nbdUTs, nbdLTs, sbdUT, sbdLT = (msk[:, i, :] for i in range(12))
    def rv(n, a=0, l=None):
        o, ln = RV[n]
        return rvs[:, o + a:o + a + (ln - a if l is None else l)]
    negexp = p.sb("negexp", [128, 10])
    S(lambda e: e.activation(out=negexp[:], in_=rv("alog"), func=AF.Exp), ["rvs"], ["negexp"])
    V(lambda e: e.tensor_scalar(out=negexp[:], in0=negexp[:], scalar1=-1.0, scalar2=None, op0=ALU.mult), ["negexp"], ["negexp"])
    PS = [p.ps(f"ps{i}", [128, 512]) for i in range(8)]

    if 1 in phases:
        with contextlib.ExitStack() as sc:
            sb = lambda name, shape, dt=F32: sc.enter_context(nc.sbuf_tensor("s_" + name, list(shape), dt))
            W_bf = sb("W_bf", [128, 8, 2048], BF16); w_sm = sb("w_sm", [128, 8, 12])
            zt = sb("zt", [2, NCOL])
            xb = [sb(f"xb{i}", [128, 8, 128], BF16) for i in range(2)]; xf = [sb(f"xf{i}", [128, 8, 128]) for i in range(2)]
            ct = [sb(f"ct{i}", [128, NCOL]) for i in range(2)]
            wcv = wc.rearrange("(kc p) n -> p kc n", p=128)
            for kc in range(8):
                p.dma("gpsimd", W_bf[:, kc, :], wc[kc * 128:(kc + 1) * 128, 0:2048], writes=["W_bf"])
            p.dma("sync", w_sm[:], wcv[:, :, 2048:2060], writes=["w_sm"])
            V(lambda e: e.memset(zt[:], 0.0), [], ["zt"])
            p.dma("sync", cols_s[0:2, :], zt[:], reads=["zt"], writes=["cols_pad"])
            p.dma("sync", cols_s[T + 2:T + 4, :], zt[:], reads=["zt"], writes=["cols_pad"])
            xTv = xT.rearrange("(kc p) n -> p kc n", p=128)
            for tt in range(NTL):
                t0 = tt * 128; i = tt % 2
                p.dma("gpsimd", xb[i][:], xTv[:, :, t0:t0 + 128], writes=[f"xb{i}"])
                p.dma("sync", xf[i][:], xTv[:, :, t0:t0 + 128], writes=[f"xf{i}"])
                for gq in range(4):
                    pp = PS[gq]
                    for kc in range(8):
                        PE(lambda e, kc=kc, gq=gq, pp=pp, i=i: e.matmul(pp[:, :], lhsT=xb[i][:, kc, :], rhs=W_bf[:, kc, gq * 512:(gq + 1) * 512], start=(kc == 0), stop=(kc == 7)),
                           [f"xb{i}", "W_bf"], [f"ps{gq}"])
                    if gq % 2 == 0:
                        S(lambda e, gq=gq, pp=pp, i=i: e.copy(out=ct[i][:, gq * 512:(gq + 1) * 512], in_=pp[:, :]), [f"ps{gq}"], [f"ct{i}"])
                    else:
                        V(lambda e, gq=gq, pp=pp, i=i: e.tensor_copy(out=ct[i][:, gq * 512:(gq + 1) * 512], in_=pp[:, :]), [f"ps{gq}"], [f"ct{i}"])
                for kc in range(8):
                    PE(lambda e, kc=kc, i=i: e.matmul(PS[4][:, :12], lhsT=xf[i][:, kc, :], rhs=w_sm[:, kc, :], start=(kc == 0), stop=(kc == 7)),
                       [f"xf{i}", "w_sm"], ["ps4"])
                V(lambda e, i=i: e.tensor_copy(out=ct[i][:, 2048:2060], in_=PS[4][:, :12]), ["ps4"], [f"ct{i}"])
                p.dma("sync", cols_s[2 + t0:2 + t0 + 128, :], ct[i][:], reads=[f"ct{i}"], writes=["cols_s"])
        p.barrier()

    if 2 in phases:
        with contextlib.ExitStack() as sc:
            sb = lambda name, shape, dt=F32: sc.enter_context(nc.sbuf_tensor("s_" + name, list(shape), dt))
            win = [sb(f"win{j}", [128, NCOL]) for j in range(5)]
            wup_sb = sb("wup_sb", [128, 128]); aup_sb = sb("aup_sb", [128, 128]); gup_sb = sb("gup_sb", [128, 128])
            p.dma("sync", wup_sb[:], wup[:, :], writes=["wup_sb"]); p.dma("sync", aup_sb[:], aup[:, :], writes=["aup_sb"]); p.dma("sync", gup_sb[:], gup[:, :], writes=["gup_sb"])
            cacc = sb("cacc", [128, 896]); ctmp = sb("ctmp", [128, 896]); qkv = sb("qkv", [128, 384]); xbc = sb("xbc", [128, 528])
            junk = sb("junk", [128, 128]); ssq = sb("ssq", [128, 4]); kq = sb("kq", [128, 2, 128])
            spx = sb("spx", [128, 10]); spa = sb("spa", [128, 10]); spl = sb("spl", [128, 10]); beta = sb("beta", [128, 2]); gsc = sb("gsc", [128, 4])
            bv = sb("bv", [128, 2, 128]); trs = sb("trs", [128, 128]); bg = sb("bg", [128, 4])
            sh = sb("sh", [128, 768]); d1 = sb("d1", [128, 768]); d2 = sb("d2", [128, 768])
            tw = sb("tw", [128, 128]); twT = sb("twT", [128, 128]); alT = sb("alT", [128, 128]); sgl = sb("sgl", [128, 128]); sgT = sb("sgT", [128, 128])
            RW = [sb(f"RW{d}", [128, 5, 128]) for d in range(2)]; ad = [sb(f"ad{d}", [128, 128]) for d in range(2)]
            wraw = sb("wraw", [128, 128]); kx = sb("kx", [128, 128]); sqk = sb("sqk", [128, 128]); rkk = sb("rkk", [128, 2]); kkn = sb("kkn", [128, 128])
            rkr = sb("rkr", [128, 128]); prod = sb("prod", [128, 128]); bon = sb("bon", [128, 2, 2]); rpost = sb("rpost", [128, 258]); t128 = sb("t128", [128, 128])
            pT1, pT2, pM1, pM2, pT3 = PS[0], PS[1], PS[2], PS[3], PS[4]
            for tt in range(NTL):
                t0 = tt * 128
                for j in range(5):
                    p.dma("sync" if j % 2 == 0 else "scalar", win[j][:], cols_s[t0 + j:t0 + j + 128, :], reads=["cols_s", "cols_pad"], writes=[f"win{j}"])
                cur = win[2]
                for (c0, c1, o0, cname, cw) in [(0, 384, 0, "gconv", 384), (1536, 2048, 384, "sconv", 512)]:
                    for j in range(5):
                        wj = rv(cname, j * cw, cw)
                        if j == 0:
                            V(lambda e, c0=c0, c1=c1, o0=o0, wj=wj, cw=cw: e.tensor_tensor(out=cacc[:, o0:o0 + cw], in0=win[0][:, c0:c1], in1=wj, op=ALU.mult), ["win0", "rvs"], [f"cacc{o0}"])
                        else:
                            G(lambda e, c0=c0, c1=c1, o0=o0, wj=wj, cw=cw, j=j: e.tensor_tensor(out=ctmp[:, o0:o0 + cw], in0=win[j][:, c0:c1], in1=wj, op=ALU.mult), [f"win{j}", "rvs"], [f"ctmp{o0}"])
                            V(lambda e, o0=o0, cw=cw: e.tensor_tensor(out=cacc[:, o0:o0 + cw], in0=cacc[:, o0:o0 + cw], in1=ctmp[:, o0:o0 + cw], op=ALU.add), [f"cacc{o0}", f"ctmp{o0}"], [f"cacc{o0}"])
                V(lambda e: e.tensor_tensor(out=cacc[:, 384:896], in0=cacc[:, 384:896], in1=rv("sconvb"), op=ALU.add), ["cacc384", "rvs"], ["cacc384"])
                S(lambda e: e.activation(out=qkv[:], in_=cacc[:, 0:384], func=AF.Silu), ["cacc0"], ["qkv"])
                S(lambda e: e.activation(out=xbc[:, 0:512], in_=cacc[:, 384:896], func=AF.Silu), ["cacc384"], ["xbc"])
                V(lambda e: e.tensor_tensor(out=spx[:], in0=cur[:, 2050:2060], in1=rv("spb"), op=ALU.add), ["win2", "rvs"], ["spx"])
                S(lambda e: e.activation(out=spa[:], in_=spx[:], func=AF.Abs), ["spx"], ["spa"])
                S(lambda e: e.activation(out=spa[:], in_=spa[:], func=AF.Exp, scale=-1.0), ["spa"], ["spa"])
                S(lambda e: e.activation(out=spl[:], in_=spa[:], func=AF.Ln, bias=1.0), ["spa"], ["spl"])
                V(lambda e: e.tensor_scalar(out=spx[:], in0=spx[:], scalar1=0.0, scalar2=None, op0=ALU.max), ["spx"], ["spx"])
                V(lambda e: e.tensor_tensor(out=spx[:], in0=spx[:], in1=spl[:], op=ALU.add), ["spx", "spl"], ["spx"])
                V(lambda e: e.tensor_tensor(out=spl[:], in0=spx[:], in1=negexp[:], op=ALU.mult), ["spx", "negexp"], ["spl"])
                V(lambda e: e.tensor_copy(out=xbc[:, 512:520], in_=spx[:, 2:10]), ["spx"], ["xbc"])
                V(lambda e: e.tensor_copy(out=xbc[:, 520:528], in_=spl[:, 2:10]), ["spl"], ["xbc"])
                p.dma("gpsimd", ssd_s[t0:t0 + 128, :], xbc[:], reads=["xbc"], writes=["ssd_s"])
                S(lambda e: e.activation(out=beta[:], in_=cur[:, 2048:2050], func=AF.Sigmoid), ["win2"], ["beta"])
                S(lambda e: e.activation(out=gsc[:, 0:2], in_=spl[:, 0:2], func=AF.Exp), ["spl"], ["gsc"])
                V(lambda e: e.scalar_tensor_tensor(out=gsc[:, 2:4], in0=gsc[:, 0:2], scalar=-1.0, in1=beta[:], op0=ALU.mult, op1=ALU.mult), ["gsc", "beta"], ["gsc"])
                for qi in range(2):
                    src = qkv[:, qi * 128:(qi + 1) * 128]
                    V(lambda e, src=src, qi=qi: e.scalar_tensor_tensor(out=junk[:], in0=src, scalar=1.0, in1=src, op0=ALU.mult, op1=ALU.mult, accum_out=ssq[:, qi:qi + 1]), ["qkv"], ["junk", "ssq"])
                V(lambda e: e.tensor_scalar(out=ssq[:, 0:2], in0=ssq[:, 0:2], scalar1=1e-6, scalar2=None, op0=ALU.add), ["ssq"], ["ssq"])
                S(lambda e: e.activation(out=ssq[:, 0:2], in_=ssq[:, 0:2], func=AF.Sqrt), ["ssq"], ["ssq"])
                V(lambda e: e.reciprocal(out=ssq[:, 0:2], in_=ssq[:, 0:2]), ["ssq"], ["ssq"])
                V(lambda e: e.tensor_scalar(out=kq[:, 0, :], in0=qkv[:, 128:256], scalar1=ssq[:, 1:2], scalar2=None, op0=ALU.mult), ["qkv", "ssq"], ["kq"])
                V(lambda e: e.tensor_scalar(out=kq[:, 1, :], in0=qkv[:, 0:128], scalar1=ssq[:, 0:1], scalar2=128 ** -0.5, op0=ALU.mult, op1=ALU.mult), ["qkv", "ssq"], ["kq"])
                p.dma("gpsimd", gkqv_s[t0:t0 + 128, 0:2, :], kq[:], reads=["kq"], writes=["gkqv_s"])
                p.dma("gpsimd", gkqv_s[t0:t0 + 128, 2, :], qkv[:, 256:384], reads=["qkv"], writes=["gkqv_s"])
                G(lambda e: e.tensor_copy(out=bg[:, 0:2], in_=beta[:]), ["beta"], ["bg"])
                G(lambda e: e.tensor_copy(out=bg[:, 2:4], in_=spl[:, 0:2]), ["spl"], ["bg"])
                p.dma("gpsimd", gbg_s[t0:t0 + 128, :], bg[:], reads=["bg"], writes=["gbg_s"])
                c_, pv, nx = cur[:, 512:1280], win[1][:, 512:1280], win[3][:, 512:1280]
                V(lambda e: e.tensor_tensor(out=d1[:], in0=pv, in1=c_, op=ALU.subtract), ["win1", "win2"], ["d1"])
                G(lambda e: e.tensor_tensor(out=d1[:], in0=d1[:], in1=rv("mup"), op=ALU.mult), ["d1", "rvs"], ["d1"])
                V(lambda e: e.tensor_tensor(out=d2[:], in0=nx, in1=c_, op=ALU.subtract), ["win3", "win2"], ["d2"])
                G(lambda e: e.tensor_tensor(out=d2[:], in0=d2[:], in1=rv("mun"), op=ALU.mult), ["d2", "rvs"], ["d2"])
                V(lambda e: e.tensor_tensor(out=sh[:], in0=c_, in1=d1[:], op=ALU.add), ["win2", "d1"], ["sh"])
                V(lambda e: e.tensor_tensor(out=sh[:], in0=sh[:], in1=d2[:], op=ALU.add), ["sh", "d2"], ["sh"])
                r_, k_, v_, wl, al, gl = (sh[:, i * 128:(i + 1) * 128] for i in range(6))
                S(lambda e: e.activation(out=tw[:], in_=wl, func=AF.Tanh), ["sh"], ["tw"])
                PE(lambda e: e.transpose(out=pT1[:, :128], in_=tw[:], identity=ident), ["tw", "msk"], ["ps0"])
                S(lambda e: e.copy(out=twT[:], in_=pT1[:, :128]), ["ps0"], ["twT"])
                PE(lambda e: e.transpose(out=pT2[:, :128], in_=al, identity=ident), ["sh", "msk"], ["ps1"])
                V(lambda e: e.tensor_copy(out=alT[:], in_=pT2[:, :128]), ["ps1"], ["alT"])
                S(lambda e: e.activation(out=sgl[:], in_=gl, func=AF.Sigmoid), ["sh"], ["sgl"])
                PE(lambda e: e.transpose(out=pT3[:, :128], in_=sgl[:], identity=ident), ["sgl", "msk"], ["ps4"])
                V(lambda e: e.tensor_copy(out=sgT[:], in_=pT3[:, :128]), ["ps4"], ["sgT"])
                PE(lambda e: e.matmul(pT3[:, 128:256], lhsT=sgT[:], rhs=gup_sb[:], start=True, stop=True), ["sgT", "gup_sb"], ["ps4"])
                S(lambda e: e.copy(out=rpost[:, 128:256], in_=pT3[:, 128:256]), ["ps4"], ["rpost"])
                V(lambda e: e.tensor_tensor(out=kx[:], in0=k_, in1=rv("kk"), op=ALU.mult), ["sh", "rvs"], ["kx"])
                S(lambda e: e.activation(out=sqk[:], in_=kx[:], func=AF.Square), ["kx"], ["sqk"])
                V(lambda e: e.tensor_reduce(out=rkk[:], in_=sqk[:].rearrange("p (h n) -> p h n", h=2), axis=AX.X, op=ALU.add), ["sqk"], ["rkk"])
                V(lambda e: e.tensor_scalar(out=rkk[:], in0=rkk[:], scalar1=1e-6, scalar2=None, op0=ALU.add), ["rkk"], ["rkk"])
                S(lambda e: e.activation(out=rkk[:], in_=rkk[:], func=AF.Sqrt), ["rkk"], ["rkk"])
                V(lambda e: e.reciprocal(out=rkk[:], in_=rkk[:]), ["rkk"], ["rkk"])
                for h in range(2):
                    V(lambda e, h=h: e.tensor_scalar(out=kkn[:, h * 64:(h + 1) * 64], in0=kx[:, h * 64:(h + 1) * 64], scalar1=rkk[:, h:h + 1], scalar2=None, op0=ALU.mult), ["kx", "rkk"], ["kkn"])
                G(lambda e: e.tensor_tensor(out=rkr[:], in0=r_, in1=rv("rk"), op=ALU.mult), ["sh", "rvs"], ["rkr"])
                for d in range(2):
                    hs = slice(d * 64, (d + 1) * 64)
                    PE(lambda e, hs=hs: e.matmul(pM1[:, :128], lhsT=twT[hs, :], rhs=wup_sb[hs, :], start=True, stop=True), ["twT", "wup_sb"], ["ps2"])
                    V(lambda e, d=d: e.tensor_tensor(out=wraw[:], in0=pM1[:, :128], in1=rv("w0", d * 128, 128), op=ALU.add), ["ps2", "rvs"], ["wraw"])
                    S(lambda e: e.activation(out=wraw[:], in_=wraw[:], func=AF.Sigmoid), ["wraw"], ["wraw"])
                    S(lambda e, d=d: e.activation(out=RW[d][:, 0, :], in_=wraw[:], func=AF.Exp, scale=-0.6065306597126334), ["wraw"], [f"RW{d}"])
                    PE(lambda e, hs=hs: e.matmul(pM2[:, :128], lhsT=alT[hs, :], rhs=aup_sb[hs, :], start=True, stop=True), ["alT", "aup_sb"], ["ps3"])
                    V(lambda e, d=d: e.tensor_tensor(out=ad[d][:], in0=pM2[:, :128], in1=rv("a0", d * 128, 128), op=ALU.add), ["ps3", "rvs"], [f"ad{d}"])
                    S(lambda e, d=d: e.activation(out=ad[d][:], in_=ad[d][:], func=AF.Sigmoid), [f"ad{d}"], [f"ad{d}"])
                    G(lambda e, d=d: e.tensor_copy(out=RW[d][:, 1, :], in_=kkn[:]), ["kkn"], [f"RW{d}"])
                    V(lambda e, d=d: e.scalar_tensor_tensor(out=RW[d][:, 2, :], in0=kkn[:], scalar=-1.0, in1=ad[d][:], op0=ALU.mult, op1=ALU.mult), ["kkn", f"ad{d}"], [f"RW{d}"])
                    V(lambda e, d=d: e.scalar_tensor_tensor(out=t128[:], in0=ad[d][:], scalar=-1.0, in1=rv("ka"), op0=ALU.add, op1=ALU.mult), [f"ad{d}", "rvs"], ["t128"])
                    V(lambda e, d=d: e.scalar_tensor_tensor(out=RW[d][:, 3, :], in0=t128[:], scalar=1.0, in1=k_, op0=ALU.add, op1=ALU.mult), ["t128", "sh"], [f"RW{d}"])
                    G(lambda e, d=d: e.tensor_copy(out=RW[d][:, 4, :], in_=r_), ["sh"], [f"RW{d}"])
                    V(lambda e, d=d: e.tensor_tensor(out=prod[:], in0=rkr[:], in1=RW[d][:, 3, :], op=ALU.mult), ["rkr", f"RW{d}"], ["prod"])
                    V(lambda e, d=d: e.tensor_reduce(out=bon[:, d, :], in_=prod[:].rearrange("p (h n) -> p h n", h=2), axis=AX.X, op=ALU.add), ["prod"], ["bon"])
                    for h in range(2):
                        p.dma("gpsimd", rw_s[d, h, t0:t0 + 128, :, :], RW[d][:, :, h * 64:(h + 1) * 64], reads=[f"RW{d}"], writes=["rw_s"])
                V(lambda e: e.tensor_tensor(out=rpost[:, 256:258], in0=bon[:, 0, :], in1=bon[:, 1, :], op=ALU.add), ["bon"], ["rpost"])
                G(lambda e: e.tensor_copy(out=rpost[:, 0:128], in_=v_), ["sh"], ["rpost"])
                p.dma("gpsimd", rpost_s[t0:t0 + 128, :], rpost[:], reads=["rpost"], writes=["rpost_s"])
                PE(lambda e: e.transpose(out=pT2[:, :128], in_=v_, identity=ident), ["sh", "msk"], ["ps1"])
                V(lambda e: e.tensor_copy(out=t128[:], in_=pT2[:, :128]), ["ps1"], ["t128"])
                p.dma("gpsimd", rvT_s[:, t0:t0 + 128], t128[:], reads=["t128"], writes=["rvT_s"])
        p.barrier()
    fin = ["ya", "yb", "yc"]
    if dump:
        fin += ["cols_s", "gkqv_s", "gbg_s", "rw_s", "rvT_s", "rpost_s", "ssd_s", "gy_s", "ry_s"]
    if 3 in phases:
        phase3(p, T, NS, locals())
    if 4 in phases:
        phase4(p, T, locals())
    p.finish_wait("sync", [k for k in fin if k in p.lastw])
    return p.build()


def phase3(p, T, NS, L):
    SSDSTOP = int(os.environ.get('A_SSDSTOP', '99'))
    GSTOP = int(os.environ.get('A_GSTOP', '99'))
    nc = p.nc
    V = L["V"]; G = L["G"]; S = L["S"]; PE = L["PE"]; PS = L["PS"]
    UT, LT, ident, ones = L["UT"], L["LT"], L["ident"], L["ones"]
    gkqv_s, gbg_s, rw_s, rvT_s, ssd_s, gy_s, ry_s, sy_s, cols_s, yc_o = (L[k] for k in
        ["gkqv_s", "gbg_s", "rw_s", "rvT_s", "ssd_s", "gy_s", "ry_s", "sy_s", "cols_s", "yc_o"])
    bdUT, bdLT, nbdUTs, nbdLTs, sbdUT, sbdLT = (L[k_] for k_ in ["bdUT", "bdLT", "nbdUTs", "nbdLTs", "sbdUT", "sbdLT"])
    rpost_s = L["rpost_s"]
    rv = L["rv"]
    NTL = T // 128; NCH = T // NS
    with contextlib.ExitStack() as sc:
        sb = lambda name, shape, dt=F32: sc.enter_context(nc.sbuf_tensor("s_" + name, list(shape), dt))
        RB = []
        for d in range(2):
            r_ = {}
            for nm, shp in [("rwt", [128, 5, 128]), ("vt", [128, 128]), ("lw", [128, 128]), ("lp", [128, 128]), ("Pt", [128, 128]), ("iP", [128, 128]), ("Pm", [128, 128]),
                            ("rt", [128, 128]), ("kt", [128, 128]), ("nbt", [128, 128]), ("ct", [128, 128]), ("rtF", [128, 128]), ("nbF", [128, 128]), ("cF", [128, 128]), ("PF", [128, 128]),
                            ("X", [128, 128]), ("XT", [128, 128]), ("AckT", [128, 128]), ("ArkT", [128, 128]), ("AnrbT", [128, 128]),
                            ("P0", [128, 128]), ("P1", [128, 128]), ("PT0", [128, 128]), ("PT1", [128, 128]), ("TT", [128, 128]),
                            ("Zs", [128, 64]), ("Ms", [128, 64]), ("Yt", [128, 128]), ("H", [128, 64])]:
                r_[nm] = sb(f"r{d}_{nm}", shp)
            for nm in ["cFh", "nbFh", "ktFh", "rtFh"]:
                for h in range(2):
                    r_[f"{nm}{h}"] = sb(f"r{d}_{nm}{h}", [128, 128])
                    V(lambda e, t_=r_[f"{nm}{h}"]: e.memset(t_[:], 0.0), [], [f"r{d}_{nm}{h}"])
            for nm in ["ktS", "nbS"]:
                for ch in range(2):
                    for h in range(2):
                        r_[f"{nm}{ch}{h}"] = sb(f"r{d}_{nm}{ch}{h}", [128, 128])
                        V(lambda e, t_=r_[f"{nm}{ch}{h}"]: e.memset(t_[:], 0.0), [], [f"r{d}_{nm}{ch}{h}"])
            V(lambda e, r_=r_: e.memset(r_["H"][:], 0.0), [], [f"r{d}_H"])
            V(lambda e, r_=r_: e.memset(r_["Zs"][:], 0.0), [], [f"r{d}_Zs"])
            V(lambda e, r_=r_: e.memset(r_["Ms"][:], 0.0), [], [f"r{d}_Ms"])
            RB.append(r_)
        GB = []
        for d in range(2):
            g_ = {}
            for nm, shp in [("kqv", [128, 3, 128]), ("bg", [128, 4]), ("kF", [128, 128]), ("qF", [128, 128]), ("Gs", [128, 128]), ("QKs", [128, 128]),
                            ("gcc", [128, 1]), ("ngcc", [128, 1]), ("R", [128, 128]), ("grow", [128, 128]), ("egrow", [128, 128]), ("dT", [128, 128]), ("dN", [128, 128]),
                            ("Bd", [128, 128]), ("brow", [128, 128]), ("X", [128, 128]), ("XT", [128, 128]), ("P0", [128, 128]), ("P1", [128, 128]),
                            ("PT0", [128, 128]), ("PT1", [128, 128]), ("TT", [128, 128]), ("vb", [128, 128]), ("kbg", [128, 128]), ("be", [128, 1]), ("eg", [128, 1]),
                            ("u", [128, 128]), ("wF", [128, 128]), ("qdF", [128, 128]), ("QKm", [128, 128]), ("vnew", [128, 128]), ("kdA", [128, 128]), ("kdB", [128, 128]),
                            ("dl", [128, 1]), ("dlA", [128, 1]), ("dlB", [128, 1]), ("S", [128, 128]), ("o", [128, 128]), ("t1", [128, 128]), ("t2", [128, 128])]:
                g_[nm] = sb(f"g{d}_{nm}", shp)
            GB.append(g_)
            V(lambda e, g_=g_: e.memset(g_["S"][:], 0.0), [], [f"g{d}_S"])
            V(lambda e, g_=g_: e.memset(g_["vnew"][:], 0.0), [], [f"g{d}_vnew"])
        sxt = sb("sxt", [128, 528]); BCF = sb("BCF", [128, 256]); scT = sb("scT", [128, 128]); acol = sb("acol", [128, 4]); nacol = sb("nacol", [128, 4])
        Rm = sb("Rm", [128, 4, 128]); E = sb("E", [128, 4, 128]); Dm = sb("Dm", [128, 128]); M = sb("M", [128, 4, 128]); CdF = sb("CdF", [128, 4, 128])
        xdt = sb("xdt", [128, 256]); xend = sb("xend", [128, 256]); dend = sb("dend", [128, 4]); H = sb("H", [128, 256]); yt = sb("yt", [128, 256])
        syf = sb("syf", [128, 256]); zt = sb("zts", [128, 256]); xd = sb("xd", [128, 256]); szs = sb("szs", [128, 256])
        pA, pSc, pAc, pRow, pY, pSt = PS[0][:, 0:256], PS[0][:, 256:384], PS[0][:, 384:512], PS[1], PS[2][:, 0:256], PS[2][:, 256:512]
        print('phase3 sbuf remaining', nc.sbuf_bytes_remaining, flush=True)

        def gdn_chunk(d, c):
            t0 = c * 128
            B_ = GB[d]; K = lambda n: f"g{d}_{n}"
            bank = PS[6 + d]; kb = f"ps{6 + d}"
            regs = [bank[:, i * 128:(i + 1) * 128] for i in range(4)] + [PS[3][:, d * 256:d * 256 + 128], PS[3][:, d * 256 + 128:d * 256 + 256]]
            keys = [kb] * 4 + ["ps3", "ps3"]
            (rA, rB, rC, rD, rE, rF), (kA, kB, kC, kD, kE, kF_) = regs, keys
            fwd = (d == 0)
            Mtri = bdUT if fwd else bdLT
            MaskT = Mtri
            MaskN = bdLT if fwd else bdUT
            nST = nbdUTs if fwd else nbdLTs
            nSN = nbdLTs if fwd else nbdUTs
            selA = bdLT[:, 0:1]; selB = bdUT[:, 127:128]
            hA, hB = slice(0, 64), slice(64, 128)
            if fwd:
                first, second, lastF, lastS = hA, hB, 63, 127
            else:
                first, second, lastF, lastS = hB, hA, 64, 0
            kqv, bg = B_["kqv"], B_["bg"]
            p.dma("sync", kqv[:], gkqv_s[t0:t0 + 128, :, :], reads=["gkqv_s"], writes=[K("kqv")])
            p.dma("sync", bg[:], gbg_s[t0:t0 + 128, :], reads=["gbg_s"], writes=[K("bg")])
            kc, qc, vc = kqv[:, 0, :], kqv[:, 1, :], kqv[:, 2, :]
            beta = bg[:, d:d + 1]; g = bg[:, 2 + d:3 + d]
            kF, qF, Gs, QKs = B_["kF"], B_["qF"], B_["Gs"], B_["QKs"]
            PE(lambda e: e.transpose(out=rA, in_=kc, identity=ident), [K("kqv"), "msk"], [kA])
            S(lambda e: e.copy(out=kF[:], in_=rA), [kA], [K("kF")])
            PE(lambda e: e.transpose(out=rB, in_=qc, identity=ident), [K("kqv"), "msk"], [kB])
            S(lambda e: e.copy(out=qF[:], in_=rB), [kB], [K("qF")])
            PE(lambda e: e.matmul(rC, lhsT=kF[:], rhs=kF[:], start=True, stop=True), [K("kF")], [kC])
            S(lambda e: e.copy(out=Gs[:], in_=rC), [kC], [K("Gs")])
            PE(lambda e: e.matmul(rD, lhsT=kF[:], rhs=qF[:], start=True, stop=True), [K("kF"), K("qF")], [kD])
            S(lambda e: e.copy(out=QKs[:], in_=rD), [kD], [K("QKs")])
            G(lambda e: e.tensor_scalar(out=B_["R"][:], in0=Mtri, scalar1=g, scalar2=None, op0=ALU.mult), [K("bg"), "msk"], [K("R")])
            PE(lambda e: e.matmul(rE, lhsT=ones, rhs=B_["R"][:], start=True, stop=True), [K("R"), "msk"], [kE])
            S(lambda e: e.copy(out=B_["grow"][:], in_=rE), [kE], [K("grow")])
            S(lambda e: e.activation(out=B_["egrow"][:], in_=rE, func=AF.Exp), [kE], [K("egrow")])
            V(lambda e: e.scalar_tensor_tensor(out=B_["t1"][:], in0=B_["grow"][:], scalar=1.0, in1=ident, op0=ALU.mult, op1=ALU.mult, accum_out=B_["gcc"][:, 0:1]), [K("grow"), "msk"], [K("t1"), K("gcc")])
            S(lambda e: e.mul(out=B_["ngcc"][:], in_=B_["gcc"][:], mul=-1.0), [K("gcc")], [K("ngcc")])
            for nm, bias_k, sc, Mk in [("dT", "ngcc", 1.0, MaskT), ("dN", "gcc", -1.0, MaskN)]:
                S(lambda e, nm=nm, bias_k=bias_k, sc=sc: e.activation(out=B_[nm][:], in_=B_["grow"][:], func=AF.Identity, bias=B_[bias_k][:, 0:1], scale=sc), [K("grow"), K(bias_k)], [K(nm)])
                G(lambda e, nm=nm: e.tensor_scalar(out=B_[nm][:], in0=B_[nm][:], scalar1=0.0, scalar2=None, op0=ALU.min), [K(nm)], [K(nm)])
                S(lambda e, nm=nm: e.activation(out=B_[nm][:], in_=B_[nm][:], func=AF.Exp), [K(nm)], [K(nm)])
                G(lambda e, nm=nm, Mk=Mk: e.tensor_tensor(out=B_[nm][:], in0=B_[nm][:], in1=Mk, op=ALU.mult), [K(nm), "msk"], [K(nm)])
            G(lambda e: e.tensor_scalar(out=B_["Bd"][:], in0=ident, scalar1=beta, scalar2=None, op0=ALU.mult), [K("bg"), "msk"], [K("Bd")])
            PE(lambda e: e.matmul(rF, lhsT=ones, rhs=B_["Bd"][:], start=True, stop=True), [K("Bd"), "msk"], [kF_])
            S(lambda e: e.copy(out=B_["brow"][:], in_=rF), [kF_], [K("brow")])
            G(lambda e: e.tensor_tensor(out=B_["t1"][:], in0=Gs[:], in1=B_["dT"][:], op=ALU.mult), [K("Gs"), K("dT"), K("t1")], [K("t1")])
            G(lambda e: e.tensor_tensor(out=B_["t1"][:], in0=B_["t1"][:], in1=B_["brow"][:], op=ALU.mult), [K("t1"), K("brow")], [K("t1")])
            G(lambda e: e.tensor_tensor(out=B_["XT"][:], in0=B_["t1"][:], in1=nST, op=ALU.mult), [K("t1"), "msk"], [K("XT")])
            G(lambda e: e.tensor_tensor(out=B_["t2"][:], in0=Gs[:], in1=B_["dN"][:], op=ALU.mult), [K("Gs"), K("dN")], [K("t2")])
            G(lambda e: e.tensor_scalar(out=B_["t2"][:], in0=B_["t2"][:], scalar1=beta, scalar2=None, op0=ALU.mult), [K("t2"), K("bg")], [K("t2")])
            G(lambda e: e.tensor_tensor(out=B_["X"][:], in0=B_["t2"][:], in1=nSN, op=ALU.mult), [K("t2"), "msk"], [K("X")])
            G(lambda e: e.tensor_tensor(out=B_["TT"][:], in0=B_["XT"][:], in1=ident, op=ALU.add), [K("XT"), "msk"], [K("TT")])
            if GSTOP == 1: return
            Pc, PTc, kP, kPT = B_["X"], B_["XT"], K("X"), K("XT")
            for lv in range(1, 6):
                Pn, kPn = B_[f"P{lv % 2}"], K(f"P{lv % 2}")
                PE(lambda e, Pc=Pc, PTc=PTc: e.matmul(rA, lhsT=PTc[:], rhs=Pc[:], start=True, stop=True), [kP, kPT], [kA])
                S(lambda e, Pn=Pn: e.copy(out=Pn[:], in_=rA), [kA], [kPn])
                if lv < 5:
                    PTn, kPTn = B_[f"PT{lv % 2}"], K(f"PT{lv % 2}")
                    PE(lambda e, Pc=Pc, PTc=PTc: e.matmul(rB, lhsT=Pc[:], rhs=PTc[:], start=True, stop=True), [kP, kPT], [kB])
                    S(lambda e, PTn=PTn: e.copy(out=PTn[:], in_=rB), [kB], [kPTn])
                PE(lambda e, Pn=Pn: e.matmul(rC, lhsT=Pn[:], rhs=B_["TT"][:], start=True, stop=True), [kPn, K("TT")], [kC])
                V(lambda e: e.tensor_tensor(out=B_["TT"][:], in0=B_["TT"][:], in1=rC, op=ALU.add), [K("TT"), kC], [K("TT")])
                Pc, kP = Pn, kPn
                if lv < 5:
                    PTc, kPT = PTn, kPTn
            if GSTOP == 2: return
            S(lambda e: e.activation(out=B_["eg"][:], in_=B_["gcc"][:], func=AF.Exp), [K("gcc")], [K("eg")])
            G(lambda e: e.tensor_tensor(out=B_["be"][:], in0=B_["eg"][:], in1=beta, op=ALU.mult), [K("eg"), K("bg")], [K("be")])
            G(lambda e: e.tensor_scalar(out=B_["vb"][:], in0=vc, scalar1=beta, scalar2=None, op0=ALU.mult), [K("kqv"), K("bg")], [K("vb")])
            G(lambda e: e.tensor_scalar(out=B_["kbg"][:], in0=kc, scalar1=B_["be"][:, 0:1], scalar2=None, op0=ALU.mult), [K("kqv"), K("be")], [K("kbg")])
            PE(lambda e: e.matmul(rD, lhsT=B_["TT"][:], rhs=B_["vb"][:], start=True, stop=True), [K("TT"), K("vb")], [kD])
            S(lambda e: e.copy(out=B_["u"][:], in_=rD), [kD], [K("u")])
            PE(lambda e: e.matmul(rE, lhsT=B_["kbg"][:], rhs=B_["TT"][:], start=True, stop=True), [K("kbg"), K("TT")], [kE])
            S(lambda e: e.copy(out=B_["wF"][:], in_=rE), [kE], [K("wF")])
            G(lambda e: e.tensor_tensor(out=B_["qdF"][:], in0=qF[:], in1=B_["egrow"][:], op=ALU.mult), [K("qF"), K("egrow")], [K("qdF")])
            G(lambda e: e.tensor_tensor(out=B_["QKm"][:], in0=QKs[:], in1=B_["dT"][:], op=ALU.mult), [K("QKs"), K("dT")], [K("QKm")])
            for hs, lst in [(first, lastF), (second, lastS)]:
                S(lambda e, hs=hs, lst=lst: e.activation(out=B_["dl"][hs, :], in_=B_["gcc"][hs, :], func=AF.Exp, bias=B_["grow"][hs, lst:lst + 1], scale=-1.0), [K("gcc"), K("grow")], [K("dl")])
            G(lambda e: e.tensor_tensor(out=B_["dlA"][:], in0=B_["dl"][:], in1=selA, op=ALU.mult), [K("dl"), "msk"], [K("dlA")])
            G(lambda e: e.tensor_tensor(out=B_["dlB"][:], in0=B_["dl"][:], in1=selB, op=ALU.mult), [K("dl"), "msk"], [K("dlB")])
            G(lambda e: e.tensor_scalar(out=B_["kdA"][:], in0=kc, scalar1=B_["dlA"][:, 0:1], scalar2=None, op0=ALU.mult), [K("kqv"), K("dlA")], [K("kdA")])
            G(lambda e: e.tensor_scalar(out=B_["kdB"][:], in0=kc, scalar1=B_["dlB"][:, 0:1], scalar2=None, op0=ALU.mult), [K("kqv"), K("dlB")], [K("kdB")])
            kdF, kdS, kkF, kkS = (B_["kdA"], B_["kdB"], K("kdA"), K("kdB")) if fwd else (B_["kdB"], B_["kdA"], K("kdB"), K("kdA"))
            if GSTOP == 3: return
            Sst = B_["S"]
            for (hs, lst, kd_, kkd, rW, kW, rO, kO) in [(first, lastF, kdF, kkF, rA, kA, rC, kC), (second, lastS, kdS, kkS, rB, kB, rD, kD)]:
                PE(lambda e, rW=rW: e.matmul(rW, lhsT=B_["wF"][:], rhs=Sst[:], start=True, stop=True), [K("wF"), K("S")], [kW])
                V(lambda e, hs=hs, rW=rW: e.tensor_tensor(out=B_["vnew"][hs, :], in0=B_["u"][hs, :], in1=rW[hs, :], op=ALU.subtract), [K("u"), kW], [K("vnew")])
                PE(lambda e, rO=rO: e.matmul(rO, lhsT=B_["qdF"][:], rhs=Sst[:], start=True, stop=True), [K("qdF"), K("S")], [kO])
                S(lambda e, hs=hs, rO=rO: e.copy(out=B_["o"][hs, :], in_=rO[hs, :]), [kO], [K("o")])
                PE(lambda e, kd_=kd_: e.matmul(rF, lhsT=kd_[:], rhs=B_["vnew"][:], start=True, stop=True), [kkd, K("vnew")], [kF_])
                V(lambda e, lst=lst: e.scalar_tensor_tensor(out=Sst[:], in0=Sst[:], scalar=B_["egrow"][:, lst:lst + 1], in1=rF, op0=ALU.mult, op1=ALU.add), [K("S"), K("egrow"), kF_], [K("S")])
            if GSTOP == 5: return
            PE(lambda e: e.matmul(rE, lhsT=B_["QKm"][:], rhs=B_["vnew"][:], start=True, stop=True), [K("QKm"), K("vnew")], [kE])
            V(lambda e: e.tensor_tensor(out=B_["o"][:], in0=B_["o"][:], in1=rE, op=ALU.add), [K("o"), kE], [K("o")])
            p.dma("gpsimd", gy_s[d, t0:t0 + 128, :], B_["o"][:], reads=[K("o")], writes=["gy_s"])

        def rwkv_block(d, c):
            t0 = c * 128
            B_ = RB[d]; K = lambda n: f"r{d}_{n}"
            bank = PS[4 + d]; kb = f"ps{4 + d}"
            rA, rB, rC, rD = (bank[:, i * 128:(i + 1) * 128] for i in range(4))
            fwd = (d == 0)
            Mtri = bdUT if fwd else bdLT
            mS_N = sbdLT if fwd else sbdUT
            mS_T = sbdUT if fwd else sbdLT
            mI_T = bdUT if fwd else bdLT
            selc = [bdLT[:, 0:1], bdUT[:, 127:128]]
            hA, hB = slice(0, 64), slice(64, 128)
            order = [(0, hA, 63), (1, hB, 127)] if fwd else [(1, hB, 64), (0, hA, 0)]
            rwt, vt = B_["rwt"], B_["vt"]
            for h in range(2):
                p.dma("sync", rwt[:, :, h * 64:(h + 1) * 64], rw_s[d, h, t0:t0 + 128, :, :], reads=["rw_s"], writes=[K("rwt")])
            p.dma("sync", vt[:], rpost_s[t0:t0 + 128, 0:128], reads=["rpost_s"], writes=[K("vt")])
            w_, kk_, nkka_, kd_, r_ = (rwt[:, i, :] for i in range(5))
            S(lambda e: e.activation(out=B_["lw"][:], in_=w_, func=AF.Ln), [K("rwt")], [K("lw")])
            PE(lambda e: e.matmul(rA, lhsT=Mtri, rhs=B_["lw"][:], start=True, stop=True), [K("lw"), "msk"], [kb])
            S(lambda e: e.copy(out=B_["lp"][:], in_=rA), [kb], [K("lp")])
            S(lambda e: e.activation(out=B_["Pt"][:], in_=rA, func=AF.Exp), [kb], [K("Pt")])
            S(lambda e: e.activation(out=B_["iP"][:], in_=rA, func=AF.Exp, scale=-1.0), [kb], [K("iP")])
            G(lambda e: e.tensor_tensor(out=B_["Pm"][:], in0=B_["lp"][:], in1=B_["lw"][:], op=ALU.subtract), [K("lp"), K("lw")], [K("Pm")])
            S(lambda e: e.activation(out=B_["Pm"][:], in_=B_["Pm"][:], func=AF.Exp), [K("Pm")], [K("Pm")])
            G(lambda e: e.tensor_tensor(out=B_["rt"][:], in0=r_, in1=B_["Pt"][:], op=ALU.mult), [K("rwt"), K("Pt")], [K("rt")])
            G(lambda e: e.tensor_tensor(out=B_["kt"][:], in0=kd_, in1=B_["iP"][:], op=ALU.mult), [K("rwt"), K("iP")], [K("kt")])
            G(lambda e: e.tensor_tensor(out=B_["nbt"][:], in0=nkka_, in1=B_["iP"][:], op=ALU.mult), [K("rwt"), K("iP")], [K("nbt")])
            G(lambda e: e.tensor_tensor(out=B_["ct"][:], in0=kk_, in1=B_["Pm"][:], op=ALU.mult), [K("rwt"), K("Pm")], [K("ct")])
            for src, full, hm in [("rt", "rtF", "rtFh"), ("nbt", "nbF", "nbFh"), ("ct", "cF", "cFh"), ("kt", None, "ktFh"), ("Pt", "PF", None)]:
                PE(lambda e, src=src: e.transpose(out=rB, in_=B_[src][:], identity=ident), [K(src), "msk"], [kb])
                if full is not None:
                    S(lambda e, full=full: e.copy(out=B_[full][:], in_=rB), [kb], [K(full)])
                if hm is not None:
                    for h, hs in [(0, hA), (1, hB)]:
                        S(lambda e, hm=hm, h=h, hs=hs: e.copy(out=B_[f"{hm}{h}"][hs, :], in_=rB[hs, :]), [kb], [K(f"{hm}{h}")])
            for ch in range(2):
                for h in range(2):
                    cs = slice(h * 64, (h + 1) * 64)
                    G(lambda e, ch=ch, h=h, cs=cs: e.tensor_scalar(out=B_[f"ktS{ch}{h}"][:, cs], in0=B_["kt"][:, cs], scalar1=selc[ch], scalar2=None, op0=ALU.mult), [K("kt"), "msk"], [K(f"ktS{ch}{h}")])
                    G(lambda e, ch=ch, h=h, cs=cs: e.tensor_scalar(out=B_[f"nbS{ch}{h}"][:, cs], in0=B_["nbt"][:, cs], scalar1=selc[ch], scalar2=None, op0=ALU.mult), [K("nbt"), "msk"], [K(f"nbS{ch}{h}")])
            for h in range(2):
                hr = hA if h == 0 else hB
                cFh, nbFh, ktFh, rtFh = (B_[f"{n}{h}"] for n in ["cFh", "nbFh", "ktFh", "rtFh"])
                kcF, knbF, kktF, krtF = (K(f"{n}{h}") for n in ["cFh", "nbFh", "ktFh", "rtFh"])
                for (nm, l_, kl, r__, kr, mk) in [("X", cFh, kcF, "nbF", K("nbF"), mS_N), ("XT", nbFh, knbF, "cF", K("cF"), mS_T), ("AckT", ktFh, kktF, "cF", K("cF"), mS_T),
                                                   ("ArkT", ktFh, kktF, "rtF", K("rtF"), mI_T), ("AnrbT", nbFh, knbF, "rtF", K("rtF"), mI_T)]:
                    PE(lambda e, l_=l_, r__=r__: e.matmul(rC, lhsT=l_[:], rhs=B_[r__][:], start=True, stop=True), [kl, kr], [kb])
                    V(lambda e, nm=nm, mk=mk: e.tensor_tensor(out=B_[nm][:], in0=rC, in1=mk, op=ALU.mult), [kb, "msk"], [K(nm)])
                G(lambda e: e.tensor_tensor(out=B_["TT"][:], in0=B_["XT"][:], in1=ident, op=ALU.add), [K("XT"), "msk"], [K("TT")])
                Pc, PTc, kP, kPT = B_["X"], B_["XT"], K("X"), K("XT")
                for lv in range(1, 6):
                    Pn, kPn = B_[f"P{lv % 2}"], K(f"P{lv % 2}")
                    PE(lambda e, Pc=Pc, PTc=PTc: e.matmul(rA, lhsT=PTc[:], rhs=Pc[:], start=True, stop=True), [kP, kPT], [kb])
                    S(lambda e, Pn=Pn: e.copy(out=Pn[:], in_=rA), [kb], [kPn])
                    if lv < 5:
                        PTn, kPTn = B_[f"PT{lv % 2}"], K(f"PT{lv % 2}")
                        PE(lambda e, Pc=Pc, PTc=PTc: e.matmul(rB, lhsT=Pc[:], rhs=PTc[:], start=True, stop=True), [kP, kPT], [kb])
                        S(lambda e, PTn=PTn: e.copy(out=PTn[:], in_=rB), [kb], [kPTn])
                    PE(lambda e, Pn=Pn: e.matmul(rC, lhsT=Pn[:], rhs=B_["TT"][:], start=True, stop=True), [kPn, K("TT")], [kb])
                    V(lambda e: e.tensor_tensor(out=B_["TT"][:], in0=B_["TT"][:], in1=rC, op=ALU.add), [K("TT"), kb], [K("TT")])
                    Pc, kP = Pn, kPn
                    if lv < 5:
                        PTc, kPT = PTn, kPTn
                Vh = vt[:, hr]
                H = B_["H"]
                for (ch, hs, lst) in order:
                    PE(lambda e: e.matmul(rD[:, 0:64], lhsT=cFh[:], rhs=H[:], start=True, stop=False), [kcF, K("H")], [kb])
                    PE(lambda e: e.matmul(rD[:, 0:64], lhsT=B_["AckT"][:], rhs=Vh, start=False, stop=True), [K("AckT"), K("vt")], [kb])
                    S(lambda e, hs=hs: e.copy(out=B_["Zs"][hs, :], in_=rD[hs, 0:64]), [kb], [K("Zs")])
                    PE(lambda e: e.matmul(rD[:, 64:128], lhsT=B_["TT"][:], rhs=B_["Zs"][:], start=True, stop=True), [K("TT"), K("Zs")], [kb])
                    S(lambda e, hs=hs: e.copy(out=B_["Ms"][hs, :], in_=rD[hs, 64:128]), [kb], [K("Ms")])
                    PE(lambda e: e.matmul(rA[:, 0:64], lhsT=rtFh[:], rhs=H[:], start=True, stop=True), [krtF, K("H")], [kb])
                    S(lambda e, hs=hs, hr=hr: e.copy(out=B_["Yt"][hs, hr], in_=rA[hs, 0:64]), [kb], [K("Yt")])
                    PE(lambda e, ch=ch, h=h: e.matmul(rB[:, 0:64], lhsT=B_[f"ktS{ch}{h}"][:], rhs=Vh, start=True, stop=False), [K(f"ktS{ch}{h}"), K("vt")], [kb])
                    PE(lambda e, ch=ch, h=h: e.matmul(rB[:, 0:64], lhsT=B_[f"nbS{ch}{h}"][:], rhs=B_["Ms"][:], start=False, stop=True), [K(f"nbS{ch}{h}"), K("Ms")], [kb])
                    V(lambda e, hr=hr, lst=lst: e.tensor_scalar(out=H[hr, :], in0=H[hr, :], scalar1=B_["PF"][hr, lst:lst + 1], scalar2=None, op0=ALU.mult), [K("H"), K("PF")], [K("H")])
                    V(lambda e, hr=hr, lst=lst: e.scalar_tensor_tensor(out=H[hr, :], in0=rB[hr, 0:64], scalar=B_["PF"][hr, lst:lst + 1], in1=H[hr, :], op0=ALU.mult, op1=ALU.add), [K("H"), K("PF"), kb], [K("H")])
                PE(lambda e: e.matmul(rC[:, 0:64], lhsT=B_["ArkT"][:], rhs=Vh, start=True, stop=False), [K("ArkT"), K("vt")], [kb])
                PE(lambda e: e.matmul(rC[:, 0:64], lhsT=B_["AnrbT"][:], rhs=B_["Ms"][:], start=False, stop=True), [K("AnrbT"), K("Ms")], [kb])
                V(lambda e, hr=hr: e.tensor_tensor(out=B_["Yt"][:, hr], in0=B_["Yt"][:, hr], in1=rC[:, 0:64], op=ALU.add), [K("Yt"), kb], [K("Yt")])
            p.dma("gpsimd", ry_s[d, t0:t0 + 128, :], B_["Yt"][:], reads=[K("Yt")], writes=["ry_s"])

        def ssd_chunk(d, c):
            t0 = c * 128
            Mk = UT if d == 0 else LT
            last = 127 if d == 0 else 0
            p.dma("gpsimd", sxt[:], ssd_s[t0:t0 + 128, :], reads=["ssd_s"], writes=["sxt"])
            if d == 1:
                p.dma("gpsimd", syf[:], sy_s[t0:t0 + 128, :], reads=["sy_s"], writes=["syf"])
                p.dma("gpsimd", zt[:], cols_s[2 + t0:2 + t0 + 128, 1280:1536], reads=["cols_s"], writes=["zts"])
            sx = sxt[:, 0:256]; sB = sxt[:, 256:384]; sC = sxt[:, 384:512]
            dt_d = sxt[:, 512 + d * 4:516 + d * 4]; a_d = sxt[:, 520 + d * 4:524 + d * 4]
            PE(lambda e: e.transpose(out=pA[:, 0:128], in_=sB, identity=ident), ["sxt", "msk"], ["ps0"])
            PE(lambda e: e.transpose(out=pA[:, 128:256], in_=sC, identity=ident), ["sxt", "msk"], ["ps0"])
            S(lambda e: e.copy(out=BCF[:], in_=pA[:, 0:256]), ["ps0"], ["BCF"])
            if SSDSTOP == 1: return
            PE(lambda e: e.matmul(pSc[:, :128], lhsT=BCF[:, 0:128], rhs=BCF[:, 128:256], start=True, stop=True), ["BCF"], ["ps0"])
            S(lambda e: e.copy(out=scT[:], in_=pSc[:, :128]), ["ps0"], ["scT"])
            PE(lambda e: e.matmul(pAc[:, :4], lhsT=Mk, rhs=a_d, start=True, stop=True), ["sxt", "msk"], ["ps0"])
            S(lambda e: e.copy(out=acol[:], in_=pAc[:, :4]), ["ps0"], ["acol"])
            S(lambda e: e.mul(out=nacol[:], in_=pAc[:, :4], mul=-1.0), ["ps0"], ["nacol"])
            if SSDSTOP == 2: return
            for h in range(4):
                (V if os.environ.get('A_V1') else G)(lambda e, h=h: e.tensor_scalar(out=Rm[:, h, :], in0=Mk, scalar1=a_d[:, h:h + 1], scalar2=None, op0=ALU.mult), ["sxt", "msk"], ["Rm"])
            PE(lambda e: e.matmul(pRow[:, :512], lhsT=ones, rhs=Rm[:].rearrange("p h n -> p (h n)"), start=True, stop=True), ["Rm", "msk"], ["ps1"])
            S(lambda e: e.activation(out=E[:].rearrange("p h n -> p (h n)"), in_=pRow[:, :512], func=AF.Exp), ["ps1"], ["E"])
            for h in range(4):
                S(lambda e, h=h: e.activation(out=dend[:, h:h + 1], in_=pRow[:, h * 128 + last:h * 128 + last + 1], func=AF.Exp, bias=nacol[:, h:h + 1], scale=1.0), ["ps1", "nacol"], ["dend"])
            if SSDSTOP == 3: return
            for h in range(4):
                hs = slice(h * 64, (h + 1) * 64)
                S(lambda e, h=h: e.activation(out=Dm[:], in_=pRow[:, h * 128:(h + 1) * 128], func=AF.Identity, bias=nacol[:, h:h + 1], scale=1.0), ["ps1", "nacol"], ["Dm"])
                G(lambda e: e.tensor_scalar(out=Dm[:], in0=Dm[:], scalar1=0.0, scalar2=None, op0=ALU.min), ["Dm"], ["Dm"])
                S(lambda e: e.activation(out=Dm[:], in_=Dm[:], func=AF.Exp), ["Dm"], ["Dm"])
                G(lambda e: e.tensor_tensor(out=Dm[:], in0=Dm[:], in1=Mk, op=ALU.mult), ["Dm", "msk"], ["Dm"])
                G(lambda e, h=h: e.tensor_tensor(out=M[:, h, :], in0=Dm[:], in1=scT[:], op=ALU.mult), ["Dm", "scT"], ["M"])
                G(lambda e, h=h: e.tensor_tensor(out=CdF[:, h, :], in0=E[:, h, :], in1=BCF[:, 128:256], op=ALU.mult), ["E", "BCF"], ["CdF"])
                G(lambda e, h=h, hs=hs: e.tensor_scalar(out=xdt[:, hs], in0=sx[:, hs], scalar1=dt_d[:, h:h + 1], scalar2=None, op0=ALU.mult), ["sxt"], ["xdt"])
                G(lambda e, h=h, hs=hs: e.tensor_scalar(out=xend[:, hs], in0=xdt[:, hs], scalar1=dend[:, h:h + 1], scalar2=None, op0=ALU.mult), ["xdt", "dend"], ["xend"])
            if SSDSTOP == 4: return
            for h in range(4):
                hs = slice(h * 64, (h + 1) * 64)
                PE(lambda e, h=h, hs=hs: e.matmul(pY[:, hs], lhsT=M[:, h, :], rhs=xdt[:, hs], start=True, stop=False), ["M", "xdt"], ["ps2"])
                PE(lambda e, h=h, hs=hs: e.matmul(pY[:, hs], lhsT=CdF[:, h, :], rhs=H[:, hs], start=False, stop=True), ["CdF", "H"], ["ps2"])
            PE(lambda e: e.matmul(pSt[:, :256], lhsT=sB, rhs=xend[:], start=True, stop=True), ["sxt", "xend"], ["ps2"])
            if SSDSTOP == 5: return
            for h in range(4):
                hs = slice(h * 64, (h + 1) * 64)
                V(lambda e, h=h, hs=hs: e.scalar_tensor_tensor(out=H[:, hs], in0=H[:, hs], scalar=E[:, h, last:last + 1], in1=pSt[:, hs], op0=ALU.mult, op1=ALU.add), ["H", "E", "ps2"], ["H"])
            if SSDSTOP == 6: return
            if d == 0:
                S(lambda e: e.copy(out=yt[:], in_=pY[:, :256]), ["ps2"], ["yt"])
                p.dma("gpsimd", sy_s[t0:t0 + 128, :], yt[:], reads=["yt"], writes=["sy_s"])
            else:
                V(lambda e: e.tensor_tensor(out=yt[:], in0=pY[:, :256], in1=syf[:], op=ALU.add), ["ps2", "syf"], ["yt"])
                G(lambda e: e.tensor_tensor(out=xd[:], in0=sx, in1=rv("dsk"), op=ALU.mult), ["sxt", "rvs"], ["xd"])
                G(lambda e: e.tensor_tensor(out=yt[:], in0=yt[:], in1=xd[:], op=ALU.add), ["yt", "xd"], ["yt"])
                S(lambda e: e.activation(out=szs[:], in_=zt[:], func=AF.Silu), ["zts"], ["szs"])
                G(lambda e: e.tensor_tensor(out=yt[:], in0=yt[:], in1=szs[:], op=ALU.mult), ["yt", "szs"], ["yt"])
                p.dma("gpsimd", yc_o[t0:t0 + 128, :], yt[:], reads=["yt"], writes=["yc"])

        ssd_list = [(0, c) for c in range(NTL)] + [(1, c) for c in range(NTL - 1, -1, -1)]
        V(lambda e: e.memset(H[:], 0.0), [], ["H"])
        for it in range(NTL):
            if not os.environ.get("A_NOGDN"):
                gdn_chunk(0, it); gdn_chunk(1, NTL - 1 - it)
            if not os.environ.get("A_NORWKV"):
                rwkv_block(0, it); rwkv_block(1, NTL - 1 - it)
            if not os.environ.get("A_NOSSD"):
                for si in (2 * it, 2 * it + 1):
                    dd, cc = ssd_list[si]
                    if dd == 1 and cc == NTL - 1:
                        V(lambda e: e.memset(H[:], 0.0), [], ["H"])
                    ssd_chunk(dd, cc)
    p.barrier()


def phase4(p, T, L):
    nc = p.nc
    V = L["V"]; G = L["G"]; S = L["S"]; PE = L["PE"]; PS = L["PS"]; ident = L["ident"]; rv = L["rv"]
    gy_s, ry_s, cols_s, rpost_s, ya_o, yb_o = (L[k] for k in ["gy_s", "ry_s", "cols_s", "rpost_s", "ya_o", "yb_o"])
    NTL = T // 128
    with contextlib.ExitStack() as sc:
        sb = lambda name, shape, dt=F32: sc.enter_context(nc.sbuf_tensor("s_" + name, list(shape), dt))
        g0 = sb("g0", [128, 128]); g1 = sb("g1", [128, 128]); o = sb("o", [128, 128]); jk = sb("jk", [128, 128]); ss = sb("ss", [128, 1])
        zt = sb("zt4", [128, 128]); ya = sb("ya_t", [128, 128])
        r0 = sb("r0", [128, 128]); r1 = sb("r1", [128, 128]); y = sb("y4", [128, 128]); st = sb("st4", [128, 2]); sq = sb("sq4", [128, 128]); vr = sb("vr4", [128, 2])
        rp = sb("rp4", [128, 258]); yb = sb("yb_t", [128, 128])
        pT, pU = PS[0], PS[1]
        for tt in range(NTL):
            t0 = tt * 128
            p.dma("sync", g0[:], gy_s[0, t0:t0 + 128, :], reads=["gy_s"], writes=["g0"])
            p.dma("sync", g1[:], gy_s[1, t0:t0 + 128, :], reads=["gy_s"], writes=["g1"])
            p.dma("scalar", zt[:], cols_s[2 + t0:2 + t0 + 128, 384:512], reads=["cols_s"], writes=["zt4"])
            p.dma("sync", r0[:], ry_s[0, t0:t0 + 128, :], reads=["ry_s"], writes=["r0"])
            p.dma("sync", r1[:], ry_s[1, t0:t0 + 128, :], reads=["ry_s"], writes=["r1"])
            p.dma("scalar", rp[:], rpost_s[t0:t0 + 128, :], reads=["rpost_s"], writes=["rp4"])
            V(lambda e: e.tensor_tensor(out=o[:], in0=g0[:], in1=g1[:], op=ALU.add), ["g0", "g1"], ["o"])
            V(lambda e: e.scalar_tensor_tensor(out=jk[:], in0=o[:], scalar=1.0, in1=o[:], op0=ALU.mult, op1=ALU.mult, accum_out=ss[:, 0:1]), ["o"], ["jk", "ss"])
            V(lambda e: e.tensor_scalar(out=ss[:], in0=ss[:], scalar1=1.0 / 128, scalar2=1e-6, op0=ALU.mult, op1=ALU.add), ["ss"], ["ss"])
            S(lambda e: e.activation(out=ss[:], in_=ss[:], func=AF.Sqrt), ["ss"], ["ss"])
            V(lambda e: e.reciprocal(out=ss[:], in_=ss[:]), ["ss"], ["ss"])
            V(lambda e: e.scalar_tensor_tensor(out=o[:], in0=o[:], scalar=ss[:, 0:1], in1=rv("gnorm"), op0=ALU.mult, op1=ALU.mult), ["o", "ss", "rvs"], ["o"])
            S(lambda e: e.activation(out=zt[:], in_=zt[:], func=AF.Silu), ["zt4"], ["zt4"])
            V(lambda e: e.tensor_tensor(out=ya[:], in0=o[:], in1=zt[:], op=ALU.mult), ["o", "zt4"], ["ya_t"])
            p.dma("gpsimd", ya_o[t0:t0 + 128, :], ya[:], reads=["ya_t"], writes=["ya"])
            V(lambda e: e.tensor_tensor(out=y[:], in0=r0[:], in1=r1[:], op=ALU.add), ["r0", "r1"], ["y4"])
            V(lambda e: e.tensor_reduce(out=st[:], in_=y[:].rearrange("p (h n) -> p h n", h=2), axis=AX.X, op=ALU.add), ["y4"], ["st4"])
            V(lambda e: e.tensor_scalar(out=st[:], in0=st[:], scalar1=1.0 / 64, scalar2=None, op0=ALU.mult), ["st4"], ["st4"])
            for h in range(2):
                hs = slice(h * 64, (h + 1) * 64)
                V(lambda e, h=h, hs=hs: e.tensor_scalar(out=y[:, hs], in0=y[:, hs], scalar1=st[:, h:h + 1], scalar2=None, op0=ALU.subtract), ["y4", "st4"], ["y4"])
            S(lambda e: e.activation(out=sq[:], in_=y[:], func=AF.Square), ["y4"], ["sq4"])
            V(lambda e: e.tensor_reduce(out=vr[:], in_=sq[:].rearrange("p (h n) -> p h n", h=2), axis=AX.X, op=ALU.add), ["sq4"], ["vr4"])
            V(lambda e: e.tensor_scalar(out=vr[:], in0=vr[:], scalar1=1.0 / 64, scalar2=64e-5, op0=ALU.mult, op1=ALU.add), ["vr4"], ["vr4"])
            S(lambda e: e.activation(out=vr[:], in_=vr[:], func=AF.Sqrt), ["vr4"], ["vr4"])
            V(lambda e: e.reciprocal(out=vr[:], in_=vr[:]), ["vr4"], ["vr4"])
            for h in range(2):
                hs = slice(h * 64, (h + 1) * 64)
                V(lambda e, h=h, hs=hs: e.tensor_scalar(out=y[:, hs], in0=y[:, hs], scalar1=vr[:, h:h + 1], scalar2=None, op0=ALU.mult), ["y4", "vr4"], ["y4"])
            V(lambda e: e.tensor_tensor(out=y[:], in0=y[:], in1=rv("lng"), op=ALU.mult), ["y4", "rvs"], ["y4"])
            V(lambda e: e.tensor_tensor(out=y[:], in0=y[:], in1=rv("lnb"), op=ALU.add), ["y4", "rvs"], ["y4"])
            for h in range(2):
                hs = slice(h * 64, (h + 1) * 64)
                V(lambda e, h=h, hs=hs: e.scalar_tensor_tensor(out=y[:, hs], in0=rp[:, hs], scalar=rp[:, 256 + h:257 + h], in1=y[:, hs], op0=ALU.mult, op1=ALU.add), ["y4", "rp4"], ["y4"])
            V(lambda e: e.tensor_tensor(out=yb[:], in0=y[:], in1=rp[:, 128:256], op=ALU.mult), ["y4", "rp4"], ["yb_t"])
            p.dma("gpsimd", yb_o[t0:t0 + 128, :], yb[:], reads=["yb_t"], writes=["yb"])


ALPHA = 4 ** 0.25
NTOK = 2048
T1 = 256
T2 = 512
NE = 32


def ln_fm(p, h, hk, nch, NT, gcol, bcol, ones_mean, sq, pS, pQ, tmp, kS, kQ, eps=1e-5):
    for oc in range(nch):
        p.op("tensor", lambda e, oc=oc: e.matmul(pS[:, :NT], lhsT=ones_mean[:], rhs=h[:, oc, :], start=(oc == 0), stop=(oc == nch - 1)),
             reads=[hk, "ones_mean"], writes=[kS])
    for oc in range(nch):
        s = oc % 2
        p.op("scalar", lambda e, oc=oc, s=s: e.activation(out=sq[:, s, :], in_=h[:, oc, :], func=AF.Square), reads=[hk], writes=[f"sq{s}"])
        p.op("tensor", lambda e, oc=oc, s=s: e.matmul(pQ[:, :NT], lhsT=ones_mean[:], rhs=sq[:, s, :], start=(oc == 0), stop=(oc == nch - 1)),
             reads=[f"sq{s}", "ones_mean"], writes=[kQ])
    mean, rstd, t = tmp["mean"], tmp["rstd"], tmp["t"]
    p.op("scalar", lambda e: e.copy(out=mean[:], in_=pS[:, :NT]), reads=[kS], writes=["ln_mean"])
    p.op("vector", lambda e: e.tensor_tensor(out=t[:], in0=mean[:], in1=mean[:], op=ALU.mult), reads=["ln_mean"], writes=["ln_t"])
    p.op("vector", lambda e: e.tensor_tensor(out=t[:], in0=pQ[:, :NT], in1=t[:], op=ALU.subtract), reads=[kQ, "ln_t"], writes=["ln_t"])
    p.op("vector", lambda e: e.tensor_scalar(out=t[:], in0=t[:], scalar1=eps, scalar2=None, op0=ALU.add), reads=["ln_t"], writes=["ln_t"])
    p.op("scalar", lambda e: e.activation(out=t[:], in_=t[:], func=AF.Sqrt), reads=["ln_t"], writes=["ln_t"])
    p.op("vector", lambda e: e.reciprocal(out=rstd[:], in_=t[:]), reads=["ln_t"], writes=["ln_rstd"])
    for oc in range(nch):
        p.op("vector", lambda e, oc=oc: e.tensor_tensor(out=h[:, oc, :], in0=h[:, oc, :], in1=mean[:], op=ALU.subtract), reads=[hk, "ln_mean"], writes=[hk])
        p.op("vector", lambda e, oc=oc: e.tensor_tensor(out=h[:, oc, :], in0=h[:, oc, :], in1=rstd[:], op=ALU.mult), reads=[hk, "ln_rstd"], writes=[hk])
        p.op("scalar", lambda e, oc=oc: e.activation(out=h[:, oc, :], in_=h[:, oc, :], func=AF.Identity, scale=gcol(oc), bias=bcol(oc)),
             reads=[hk, "vec"], writes=[hk])


def build_B(ntok=NTOK, ne=NE, dump=False):
    p = Prog(); nc = p.nc
    D = 1024
    xT = p.dram("xT", [D, ntok]); yT = p.dram("yT", [2048, ntok]); pT = p.dram("pT", [256, ntok])
    wg = p.dram("wg", [D, 3072]); wb = p.dram("wb", [2048, D]); wo = p.dram("wo", [D, D]); wplg = p.dram("wplg", [D, D])
    wpl = p.dram("wpl", [256, D]); wr = p.dram("wr", [D, 32]); br = p.dram("br", [1, 32])
    wgu = p.dram("wgu", [32, D, 2048]); wd = p.dram("wd", [32, D, D])
    bgu = p.dram("bgu", [128, 32, 16]); bd = p.dram("bd", [32, D]); vec = p.dram("vec", [128, 5, 8]); ident_d = p.dram("ident", [128, 128])
    outT = p.dram("outT", [D, ntok], kind="ExternalOutput")
    x1bf_s = p.dram("x1bf_s", [128, 8, ntok], BF16, kind="Internal")
    acc_s = p.dram("acc_s", [128, 8, ntok], F32, kind="Internal")
    if dump:
        x1_d = p.dram("x1_d", [D, ntok], kind="ExternalOutput")
        gate_d = p.dram("gate_d", [32, ntok], kind="ExternalOutput")

    ident = p.sb("ident", [128, 128]); ones_mean = p.sb("ones_mean", [128, 128]); vecs = p.sb("vecs", [128, 5, 8])
    gateT = p.sb("gateT", [32, ntok]); ones_row = p.sb("ones_row", [1, 128]); brow = p.sb("brow", [1, 32])
    PS = [p.ps(f"ps{i}", [128, 512]) for i in range(8)]
    p.dma("sync", ident[:], ident_d[:, :], writes=["ident"])
    p.dma("sync", vecs[:], vec[:, :, :], writes=["vec"])
    p.dma("sync", brow[:], br[:, :], writes=["brow"])
    p.op("vector", lambda e: e.memset(ones_mean[:], 1.0 / 1024), writes=["ones_mean"])
    p.op("vector", lambda e: e.memset(ones_row[:], 1.0), writes=["ones_row"])

    with contextlib.ExitStack() as sc:
        def sb(name, shape, dt=F32):
            return sc.enter_context(nc.sbuf_tensor("s_" + name, list(shape), dt))
        wg_bf = sb("wg_bf", [128, 8, 3072], BF16); wb_bf = sb("wb_bf", [128, 16, 1024], BF16)
        wo_bf = sb("wo_bf", [128, 8, 1024], BF16); wplg_bf = sb("wplg_bf", [128, 8, 1024], BF16); wpl_bf = sb("wpl_bf", [128, 2, 1024], BF16)
        wr_sb = sb("wr_sb", [128, 8, 32]); ones512 = sb("ones512", [128, 128])
        for kc in range(8):
            p.dma("gpsimd", wg_bf[:, kc, :], wg[kc * 128:(kc + 1) * 128, :], writes=["wg_bf"])
            p.dma("gpsimd", wo_bf[:, kc, :], wo[kc * 128:(kc + 1) * 128, :], writes=["wo_bf"])
            p.dma("gpsimd", wplg_bf[:, kc, :], wplg[kc * 128:(kc + 1) * 128, :], writes=["wplg_bf"])
            p.dma("sync", wr_sb[:, kc, :], wr[kc * 128:(kc + 1) * 128, :], writes=["wr_sb"])
        for kc in range(16):
            p.dma("gpsimd", wb_bf[:, kc, :], wb[kc * 128:(kc + 1) * 128, :], writes=["wb_bf"])
        for kc in range(2):
            p.dma("gpsimd", wpl_bf[:, kc, :], wpl[kc * 128:(kc + 1) * 128, :], writes=["wpl_bf"])
        p.op("vector", lambda e: e.memset(ones512[:], 1.0 / 512), writes=["ones512"])
        xf = sb("xf", [128, 8, T1]); x_bf = sb("x_bf", [128, 8, T1], BF16); uf = sb("uf", [128, 8, T1])
        y_bf = sb("y_bf", [128, 16, T1], BF16); m_bf = sb("m_bf", [128, 8, T1], BF16); sq = sb("sq", [128, 2, T1])
        x1b = sb("x1b", [128, 8, T1], BF16); accb = sb("accb", [128, 8, T1])
        pf = sb("pf", [128, 2, T1], BF16)
        tm = {k: sb("tm_" + k, [128, T1]) for k in ["mean", "rstd", "t", "g", "mf", "t2", "rs"]}
        lg = sb("lg", [128, 32]); top8 = sb("top8", [128, 8]); nmx = sb("nmx", [128, 1]); msk = sb("msk", [128, 32])
        ex = sb("ex", [128, 32]); ssum = sb("ssum", [128, 1]); gt = sb("gt", [128, 32])
        pG, pB, pH, pS, pQ, pR, pT_, pP = PS
        xTv = xT.rearrange("(kc p) n -> p kc n", p=128); yTv = yT.rearrange("(kc p) n -> p kc n", p=128)
        pTv = pT.rearrange("(kc p) n -> p kc n", p=128)
        for t in range(ntok // T1):
            o = t * T1
            p.dma("sync", xf[:], xTv[:, :, o:o + T1], writes=["xf"])
            p.dma("gpsimd", x_bf[:], xTv[:, :, o:o + T1], writes=["x_bf"])
            p.dma("gpsimd", y_bf[:, 0:8, :], yTv[:, 0:8, o:o + T1], writes=["y_bf_a"])
            p.dma("sync", uf[:], yTv[:, 8:16, o:o + T1], writes=["uf"])
            p.dma("gpsimd", pf[:], pTv[:, :, o:o + T1], writes=["pf"])
            for g in range(2):
                for c in range(4):
                    cc = g * 4 + c; s = cc % 2
                    p.op("scalar", lambda e, cc=cc, s=s: e.activation(out=sq[:, s, :], in_=uf[:, cc, :], func=AF.Square), reads=["uf"], writes=[f"sq{s}"])
                    p.op("tensor", lambda e, c=c, s=s: e.matmul(pS[:, :T1], lhsT=ones512[:], rhs=sq[:, s, :], start=(c == 0), stop=(c == 3)),
                         reads=[f"sq{s}", "ones512"], writes=["pS"])
                rs = tm["rs"]
                p.op("vector", lambda e: e.tensor_scalar(out=rs[:], in0=pS[:, :T1], scalar1=1e-5, scalar2=None, op0=ALU.add), reads=["pS"], writes=["rs"])
                p.op("scalar", lambda e: e.activation(out=rs[:], in_=rs[:], func=AF.Sqrt), reads=["rs"], writes=["rs"])
                p.op("vector", lambda e: e.reciprocal(out=rs[:], in_=rs[:]), reads=["rs"], writes=["rs"])
                for c in range(4):
                    cc = g * 4 + c
                    p.op("vector", lambda e, cc=cc: e.scalar_tensor_tensor(out=y_bf[:, 8 + cc, :], in0=uf[:, cc, :], scalar=vecs[:, 4, cc:cc + 1], in1=rs[:], op0=ALU.mult, op1=ALU.mult),
                         reads=["uf", "rs", "vec"], writes=["y_bf_u"])
            brk = [(0, 4), (4, 8), (8, 16)]
            for oc in range(8):
                for b in range(3):
                    c0 = b * 1024 + oc * 128
                    for kc in range(8):
                        p.op("tensor", lambda e, kc=kc, c0=c0: e.matmul(pG[:, :T1], lhsT=wg_bf[:, kc, c0:c0 + 128], rhs=x_bf[:, kc, :], start=(kc == 0), stop=(kc == 7)),
                             reads=["wg_bf", "x_bf"], writes=["pG"])
                    p.op("scalar", lambda e: e.activation(out=tm["g"][:], in_=pG[:, :T1], func=AF.Sigmoid), reads=["pG"], writes=["tm_g"])
                    k0, k1 = brk[b]
                    for kc in range(k0, k1):
                        p.op("tensor", lambda e, kc=kc, k0=k0, k1=k1: e.matmul(pB[:, :T1], lhsT=wb_bf[:, kc, oc * 128:(oc + 1) * 128], rhs=y_bf[:, kc, :], start=(kc == k0), stop=(kc == k1 - 1)),
                             reads=["wb_bf", "y_bf_a", "y_bf_u"], writes=["pB"])
                    if b == 0:
                        p.op("vector", lambda e: e.tensor_tensor(out=tm["mf"][:], in0=tm["g"][:], in1=pB[:, :T1], op=ALU.mult), reads=["tm_g", "pB"], writes=["tm_mf"])
                    else:
                        p.op("vector", lambda e: e.tensor_tensor(out=tm["t2"][:], in0=tm["g"][:], in1=pB[:, :T1], op=ALU.mult), reads=["tm_g", "pB"], writes=["tm_t2"])
                        if b == 1:
                            p.op("vector", lambda e: e.tensor_tensor(out=tm["mf"][:], in0=tm["mf"][:], in1=tm["t2"][:], op=ALU.add), reads=["tm_mf", "tm_t2"], writes=["tm_mf"])
                        else:
                            p.op("vector", lambda e, oc=oc: e.tensor_tensor(out=m_bf[:, oc, :], in0=tm["mf"][:], in1=tm["t2"][:], op=ALU.add), reads=["tm_mf", "tm_t2"], writes=["m_bf"])
            for oc in range(8):
                for kc in range(8):
                    p.op("tensor", lambda e, kc=kc, oc=oc: e.matmul(pH[:, :T1], lhsT=wo_bf[:, kc, oc * 128:(oc + 1) * 128], rhs=m_bf[:, kc, :], start=(kc == 0), stop=(kc == 7)),
                         reads=["wo_bf", "m_bf"], writes=["pH"])
                p.op("vector", lambda e, oc=oc: e.scalar_tensor_tensor(out=xf[:, oc, :], in0=xf[:, oc, :], scalar=ALPHA, in1=pH[:, :T1], op0=ALU.mult, op1=ALU.add),
                     reads=["xf", "pH"], writes=["xf"])
            ln_fm(p, xf, "xf", 8, T1, lambda oc: vecs[:, 0, oc:oc + 1], lambda oc: vecs[:, 1, oc:oc + 1], ones_mean, sq, pS, pQ, tm, "pS", "pQ")
            p.op("scalar", lambda e: e.copy(out=x1b[:], in_=xf[:]), reads=["xf"], writes=["x1b"])
            p.dma("sync", x1bf_s[:, :, o:o + T1], x1b[:], reads=["x1b"], writes=["x1bf_s"])
            if dump:
                p.dma("sync", x1_d.rearrange("(kc p) n -> p kc n", p=128)[:, :, o:o + T1], xf[:], reads=["xf"], writes=["x1_d"])
            for s in range(T1 // 128):
                for kc in range(8):
                    p.op("tensor", lambda e, kc=kc, s=s: e.matmul(pR[:, :32], lhsT=xf[:, kc, s * 128:(s + 1) * 128], rhs=wr_sb[:, kc, :], start=(kc == 0), stop=False),
                         reads=["xf", "wr_sb"], writes=["pR"])
                p.op("tensor", lambda e: e.matmul(pR[:, :32], lhsT=ones_row[:, :], rhs=brow[:, :], start=False, stop=True), reads=["ones_row", "brow"], writes=["pR"])
                p.op("vector", lambda e: e.tensor_copy(out=lg[:], in_=pR[:, :32]), reads=["pR"], writes=["lg"])
                p.op("vector", lambda e: e.max(out=top8[:], in_=lg[:]), reads=["lg"], writes=["top8"])
                p.op("vector", lambda e: e.tensor_scalar(out=nmx[:], in0=top8[:, 0:1], scalar1=-1.0, scalar2=None, op0=ALU.mult), reads=["top8"], writes=["nmx"])
                p.op("vector", lambda e: e.tensor_scalar(out=msk[:], in0=lg[:], scalar1=top8[:, 3:4], scalar2=None, op0=ALU.is_ge), reads=["lg", "top8"], writes=["msk"])
                p.op("scalar", lambda e: e.activation(out=ex[:], in_=lg[:], func=AF.Exp, bias=nmx[:, 0:1], scale=1.0), reads=["lg", "nmx"], writes=["ex"])
                p.op("vector", lambda e: e.scalar_tensor_tensor(out=ex[:], in0=ex[:], scalar=1.0, in1=msk[:], op0=ALU.mult, op1=ALU.mult, accum_out=ssum[:, 0:1]),
                     reads=["ex", "msk"], writes=["ex", "ssum"])
                p.op("vector", lambda e: e.reciprocal(out=ssum[:], in_=ssum[:]), reads=["ssum"], writes=["ssum"])
                p.op("vector", lambda e: e.tensor_scalar(out=gt[:], in0=ex[:], scalar1=ssum[:, 0:1], scalar2=None, op0=ALU.mult), reads=["ex", "ssum"], writes=["gt"])
                p.op("tensor", lambda e: e.transpose(out=pT_[:32, :128], in_=gt[:], identity=ident[:]), reads=["gt", "ident"], writes=["pT"])
                oo = o + s * 128
                p.op("scalar", lambda e, oo=oo: e.copy(out=gateT[:, oo:oo + 128], in_=pT_[:32, :128]), reads=["pT"], writes=["gateT"])
            for oc in range(8):
                for kc in range(2):
                    p.op("tensor", lambda e, kc=kc, oc=oc: e.matmul(pP[:, :T1], lhsT=wpl_bf[:, kc, oc * 128:(oc + 1) * 128], rhs=pf[:, kc, :], start=(kc == 0), stop=(kc == 1)),
                         reads=["wpl_bf", "pf"], writes=["pP"])
                for kc in range(8):
                    p.op("tensor", lambda e, kc=kc, oc=oc: e.matmul(pG[:, :T1], lhsT=wplg_bf[:, kc, oc * 128:(oc + 1) * 128], rhs=x1b[:, kc, :], start=(kc == 0), stop=(kc == 7)),
                         reads=["wplg_bf", "x1b"], writes=["pG"])
                p.op("scalar", lambda e: e.activation(out=tm["g"][:], in_=pG[:, :T1], func=AF.Sigmoid), reads=["pG"], writes=["tm_g"])
                p.op("vector", lambda e: e.tensor_tensor(out=tm["t2"][:], in0=tm["g"][:], in1=pP[:, :T1], op=ALU.mult), reads=["tm_g", "pP"], writes=["tm_t2"])
                p.op("vector", lambda e, oc=oc: e.scalar_tensor_tensor(out=accb[:, oc, :], in0=xf[:, oc, :], scalar=ALPHA, in1=tm["t2"][:], op0=ALU.mult, op1=ALU.add),
                     reads=["xf", "tm_t2"], writes=["accb"])
            p.dma("sync", acc_s[:, :, o:o + T1], accb[:], reads=["accb"], writes=["acc_s"])
    if dump:
        p.dma("sync", gate_d[:, :], gateT[:], reads=["gateT"], writes=["gate_d"])
    p.barrier()

    H = ntok // 2
    with contextlib.ExitStack() as sc:
        def sb(name, shape, dt=F32):
            return sc.enter_context(nc.sbuf_tensor("s_" + name, list(shape), dt))
        x1h = sb("x1h", [128, 8, H], BF16); acc = sb("acc", [128, 8, H])
        wgu_b = [sb(f"wgu_b{i}", [128, 8, 2048], BF16) for i in range(2)]
        wd_b = [sb(f"wd_b{i}", [128, 8, 1024], BF16) for i in range(2)]
        bgu_sb = sb("bgu_sb", [128, 32, 16]); bd_sb = sb("bd_sb", [32, 1024]); ones32 = sb("ones32", [32, 128])
        act = sb("act", [128, 8, T2], BF16); gbc = sb("gbc", [128, T2]); gm = sb("gm", [32, T2])
        glu = sb("glu", [128, T2]); up1 = sb("up1", [128, T2]); sg = sb("sg", [128, T2]); t1 = sb("t1", [128, T2]); t2 = sb("t2", [128, T2])
        sq = sb("sq2", [128, 2, T2]); tm = {k: sb("tn_" + k, [128, T2]) for k in ["mean", "rstd", "t"]}
        p.dma("sync", bgu_sb[:], bgu[:, :, :], writes=["bgu_sb"])
        p.dma("sync", bd_sb[:], bd[:, :], writes=["bd_sb"])
        p.op("vector", lambda e: e.memset(ones32[:], 1.0), writes=["ones32"])
        pGl = [PS[0], PS[1]]; pUp = [PS[2], PS[3]]; pD = [PS[4], PS[5]]; pBC = PS[6]; pS = PS[7]; pQ = PS[6]
        outv = outT.rearrange("(kc p) n -> p kc n", p=128)
        cnt = 0
        for hf in range(2):
            ho = hf * H
            p.dma("sync", x1h[:], x1bf_s[:, :, ho:ho + H], reads=["x1bf_s"], writes=["x1h"])
            p.dma("sync", acc[:], acc_s[:, :, ho:ho + H], reads=["acc_s"], writes=["acc"])
            for tt in range(H // T2):
                to = tt * T2
                for oc in range(8):
                    j = cnt % 2; cnt += 1
                    p.op("tensor", lambda e, oc=oc, j=j, to=to: e.matmul(pD[j][:, :T2], lhsT=bd_sb[:, oc * 128:(oc + 1) * 128], rhs=gateT[:, ho + to:ho + to + T2], start=True, stop=True),
                         reads=["bd_sb", "gateT"], writes=[f"pD{j}"])
                    p.op("vector", lambda e, oc=oc, j=j, to=to: e.tensor_tensor(out=acc[:, oc, to:to + T2], in0=acc[:, oc, to:to + T2], in1=pD[j][:, :T2], op=ALU.add),
                         reads=["acc", f"pD{j}"], writes=["acc"])
            for ex_ in range(ne):
                wb_i = ex_ % 2
                wgs, wds = wgu_b[wb_i], wd_b[wb_i]
                for kc in range(0, 8, 2):
                    p.dma("gpsimd", wgs[:, kc:kc + 2, :], wgu[ex_, kc * 128:(kc + 2) * 128, :].rearrange("(k p) n -> p k n", p=128), writes=[f"wgu{wb_i}"])
                for kc in range(0, 8, 4):
                    p.dma("gpsimd", wds[:, kc:kc + 4, :], wd[ex_, kc * 128:(kc + 4) * 128, :].rearrange("(k p) n -> p k n", p=128), writes=[f"wd{wb_i}"])
                for tt in range(H // T2):
                    to = tt * T2
                    p.op("vector", lambda e, to=to, ex_=ex_: e.tensor_scalar(out=gm[:], in0=gateT[:, ho + to:ho + to + T2], scalar1=ident[0:32, ex_:ex_ + 1], scalar2=None, op0=ALU.mult),
                         reads=["gateT", "ident"], writes=["gm"])
                    p.op("tensor", lambda e: e.matmul(pBC[:, :T2], lhsT=ones32[:], rhs=gm[:], start=True, stop=True), reads=["ones32", "gm"], writes=["pBC"])
                    p.op("scalar", lambda e: e.copy(out=gbc[:], in_=pBC[:, :T2]), reads=["pBC"], writes=["gbc"])
                    for oc in range(8):
                        j = oc % 2
                        for kc in range(8):
                            p.op("tensor", lambda e, kc=kc, oc=oc, j=j, to=to: e.matmul(pGl[j][:, :T2], lhsT=wgs[:, kc, oc * 128:(oc + 1) * 128], rhs=x1h[:, kc, to:to + T2], start=(kc == 0), stop=(kc == 7)),
                                 reads=[f"wgu{wb_i}", "x1h"], writes=[f"pGl{j}"])
                        for kc in range(8):
                            p.op("tensor", lambda e, kc=kc, oc=oc, j=j, to=to: e.matmul(pUp[j][:, :T2], lhsT=wgs[:, kc, 1024 + oc * 128:1024 + (oc + 1) * 128], rhs=x1h[:, kc, to:to + T2], start=(kc == 0), stop=(kc == 7)),
                                 reads=[f"wgu{wb_i}", "x1h"], writes=[f"pUp{j}"])
                        p.op("vector", lambda e, oc=oc, j=j, ex_=ex_: e.tensor_scalar(out=glu[:], in0=pGl[j][:, :T2], scalar1=bgu_sb[:, ex_, oc:oc + 1], scalar2=7.0, op0=ALU.add, op1=ALU.min),
                             reads=[f"pGl{j}", "bgu_sb"], writes=["glu"])
                        p.op("vector", lambda e, oc=oc, j=j, ex_=ex_: e.tensor_scalar(out=up1[:], in0=pUp[j][:, :T2], scalar1=bgu_sb[:, ex_, 8 + oc:9 + oc], scalar2=7.0, op0=ALU.add, op1=ALU.min),
                             reads=[f"pUp{j}", "bgu_sb"], writes=["up1"])
                        p.op("gpsimd", lambda e: e.tensor_scalar(out=up1[:], in0=up1[:], scalar1=-7.0, scalar2=1.0, op0=ALU.max, op1=ALU.add), reads=["up1"], writes=["up1"])
                        p.op("scalar", lambda e: e.activation(out=sg[:], in_=glu[:], func=AF.Sigmoid, scale=1.702), reads=["glu"], writes=["sg"])
                        p.op("gpsimd", lambda e: e.tensor_tensor(out=t2[:], in0=up1[:], in1=gbc[:], op=ALU.mult), reads=["up1", "gbc"], writes=["t2"])
                        p.op("vector", lambda e: e.tensor_tensor(out=t1[:], in0=glu[:], in1=sg[:], op=ALU.mult), reads=["glu", "sg"], writes=["t1"])
                        p.op("vector", lambda e, oc=oc: e.tensor_tensor(out=act[:, oc, :], in0=t1[:], in1=t2[:], op=ALU.mult), reads=["t1", "t2"], writes=["act"])
                    for oc in range(8):
                        j = cnt % 2; cnt += 1
                        for kc in range(8):
                            p.op("tensor", lambda e, kc=kc, oc=oc, j=j: e.matmul(pD[j][:, :T2], lhsT=wds[:, kc, oc * 128:(oc + 1) * 128], rhs=act[:, kc, :], start=(kc == 0), stop=(kc == 7)),
                                 reads=[f"wd{wb_i}", "act"], writes=[f"pD{j}"])
                        p.op("vector", lambda e, oc=oc, j=j, to=to: e.tensor_tensor(out=acc[:, oc, to:to + T2], in0=acc[:, oc, to:to + T2], in1=pD[j][:, :T2], op=ALU.add),
                             reads=["acc", f"pD{j}"], writes=["acc"])
            for tt in range(H // T2):
                to = tt * T2
                hv = acc[:, :, to:to + T2]
                ln_fm(p, hv, "acc", 8, T2, lambda oc: vecs[:, 2, oc:oc + 1], lambda oc: vecs[:, 3, oc:oc + 1], ones_mean, sq, pS, pQ, tm, "pS7", "pBC")
                p.dma("sync", outv[:, :, ho + to:ho + to + T2], hv, reads=["acc"], writes=["outT"])
    p.finish_wait("sync", ["outT"] + (["x1_d", "gate_d"] if dump else []))
    return p.build()


def build_L0(ntok=2048):
    p = Prog(); nc = p.nc
    xT = p.dram("xT", [1024, ntok]); vec = p.dram("vec", [128, 2, 8])
    outT = p.dram("outT", [1024, ntok], kind="ExternalOutput")
    ones_mean = p.sb("ones_mean", [128, 128]); vecs = p.sb("vecs", [128, 2, 8])
    p.dma("sync", vecs[:], vec[:, :, :], writes=["vec"])
    p.op("vector", lambda e: e.memset(ones_mean[:], 1.0 / 1024), writes=["ones_mean"])
    TT = 512
    h = [p.sb(f"h{i}", [128, 8, TT]) for i in range(2)]
    sq = p.sb("sq", [128, 2, TT]); tm = {k: p.sb("tm_" + k, [128, TT]) for k in ["mean", "rstd", "t"]}
    pS = p.ps("pS", [128, 512]); pQ = p.ps("pQ", [128, 512])
    xv = xT.rearrange("(kc p) n -> p kc n", p=128); ov = outT.rearrange("(kc p) n -> p kc n", p=128)
    for t in range(ntok // TT):
        o = t * TT; i = t % 2
        p.dma("sync", h[i][:], xv[:, :, o:o + TT], writes=[f"h{i}"])
        ln_fm(p, h[i], f"h{i}", 8, TT, lambda oc: vecs[:, 0, oc:oc + 1], lambda oc: vecs[:, 1, oc:oc + 1], ones_mean, sq, pS, pQ, tm, "pS", "pQ")
        p.dma("gpsimd", ov[:, :, o:o + TT], h[i][:], reads=[f"h{i}"], writes=["outT"])
    p.finish_wait("sync", ["outT"])
    return p.build()


def host_inputs_B(L, stream, ya, yb, u, z, c):
    sl = slice(c * 2048, (c + 1) * 2048)
    f = lambda a: np.ascontiguousarray(a, dtype=np.float32)
    ycat = np.concatenate([ya[sl], yb[sl], u[sl]], axis=1)
    vec = np.stack([z['ln1_g'][L].reshape(8, 128).T, z['ln1_b'][L].reshape(8, 128).T, z['ln2_g'][L].reshape(8, 128).T,
                    z['ln2_b'][L].reshape(8, 128).T, z['ssd_norm'][L].reshape(8, 128).T], axis=1)
    return {
        "xT": f(stream[sl].T), "yT": f(ycat.T), "pT": f(z['p'][L].reshape(-1, 256)[sl].T),
        "wg": f(z['w_in'][L][:, 6576:]), "wb": f(z['w_branch'][L]), "wo": f(z['w_o'][L]), "wplg": f(z['w_pl_gate'][L]),
        "wpl": f(z['w_pl'][L]), "wr": f(z['w_router'][L]), "br": f(z['b_router'][L][None]),
        "wgu": f(z['w_gu'][L]), "wd": f(z['w_down'][L]), "bgu": f(z['b_gu'][L].reshape(32, 16, 128).transpose(2, 0, 1)),
        "bd": f(z['b_down'][L]), "vec": f(vec), "ident": np.eye(128, dtype=np.float32),
    }


def kernel(**inputs):
    z = {k: np.asarray(v) for k, v in inputs.items()}
    NCORE = 8
    cores = list(range(NCORE))
    xf = z['x'].reshape(-1, 1024).astype(np.float32)
    f = lambda a: np.ascontiguousarray(a, dtype=np.float32)
    vec0 = f(np.stack([z['ln_in_g'].reshape(8, 128).T, z['ln_in_b'].reshape(8, 128).T], axis=1))
    nc0 = build_L0()
    res = run_bass_kernel_spmd(nc0, [{"xT": f(xf[c * 2048:(c + 1) * 2048].T), "vec": vec0} for c in cores], core_ids=cores)
    stream = np.concatenate([r["outT"].T for r in res.results], axis=0)
    for L in range(2):
        ncA = build_A(T=8192)
        imA = [host_inputs_A(z, L, stream[b * 8192:(b + 1) * 8192], j) for b in range(2) for j in range(4)]
        resA = run_bass_kernel_spmd(ncA, imA, core_ids=cores).results
        ya = np.concatenate([np.concatenate([resA[b * 4 + j]["ya"] for j in range(4)], axis=1) for b in range(2)], axis=0)
        yb = np.concatenate([np.concatenate([resA[b * 4 + j]["yb"] for j in range(4)], axis=1) for b in range(2)], axis=0)
        u = np.concatenate([np.concatenate([resA[b * 4 + j]["yc"] for j in range(4)], axis=1) for b in range(2)], axis=0)
        del resA, imA
        ncB = build_B()
        imB = [host_inputs_B(L, stream, ya, yb, u, z, c) for c in cores]
        resB = run_bass_kernel_spmd(ncB, imB, core_ids=cores).results
        stream = np.concatenate([r["outT"].T for r in resB], axis=0)
        del resB, imB
    return np.ascontiguousarray(stream.reshape(2, 8192, 1024), dtype=np.float32)
```

```python
import os
import contextlib, time
import numpy as np
import concourse.bass as bass
import concourse.mybir as mybir
from concourse.bass_utils import run_bass_kernel_spmd

F32 = mybir.dt.float32
BF16 = mybir.dt.bfloat16
I32 = mybir.dt.int32
ALU = mybir.AluOpType
AF = mybir.ActivationFunctionType
AX = mybir.AxisListType

ENG = ["sync", "gpsimd", "scalar", "vector", "tensor"]
NDMASEM = 6
import os as _os
ATTACH = bool(int(_os.environ.get('FW_ATTACH', '1')))


class Prog:
    def __init__(self, immediate=True):
        self.immediate = immediate
        self.nc = bass.Bass("TRN2", target_bir_lowering=False)
        try:
            self.nc.allow_low_precision("bf16 matmul operands with fp32 accumulation")
            self.nc.allow_non_contiguous_dma("strided layouts")
        except Exception as ex:
            print("allow_* failed", ex)
        self.st = contextlib.ExitStack()
        self.ops = {e: [] for e in ENG}
        self.cnt = {}
        self.sems = {}
        self.lastw = {}
        self.reads = {}
        self.seen = {e: {} for e in ENG}
        self.dma_i = {e: 0 for e in ENG}
        self.dma_last = {}
        self.ninstr = 0
        for e in ["gpsimd", "scalar", "vector", "tensor"]:
            self._sem("c_" + e)
        for e in ["sync", "gpsimd", "scalar"]:
            for i in range(NDMASEM):
                self._sem(f"d_{e}_{i}")

    def _sem(self, name):
        self.sems[name] = self.st.enter_context(self.nc.semaphore(name))
        self.cnt[name] = 0

    def dram(self, name, shape, dt=F32, kind="ExternalInput"):
        return self.nc.dram_tensor(name, list(shape), dt, kind=kind).ap()

    def sb(self, name, shape, dt=F32):
        return self.st.enter_context(self.nc.sbuf_tensor("s_" + name, list(shape), dt))

    def ps(self, name, shape, dt=F32):
        return self.st.enter_context(self.nc.psum_tensor("p_" + name, list(shape), dt))

    def _deps(self, eng, reads, writes):
        need = {}
        def add(tok):
            if tok is None:
                return
            s, v = tok
            if need.get(s, 0) < v:
                need[s] = v
        for k in reads:
            add(self.lastw.get(k))
        for k in writes:
            add(self.lastw.get(k))
            for t in self.reads.get(k, ()):
                add(t)
        out = []
        for s, v in need.items():
            if self.seen[eng].get(s, 0) < v:
                self.seen[eng][s] = v
                out.append((s, v))
        return out

    def _commit(self, tok, reads, writes):
        for k in reads:
            self.reads.setdefault(k, []).append(tok)
        for k in writes:
            self.lastw[k] = tok
            self.reads[k] = []

    def capture(self, f, *a):
        self._buf = []
        try:
            f(*a)
        finally:
            buf, self._buf = self._buf, None
        return buf

    def emit_interleaved(self, chains):
        chains = [list(c) for c in chains if c]
        idx = [0] * len(chains)
        live = True
        while live:
            live = False
            for ci, c in enumerate(chains):
                if idx[ci] < len(c):
                    kind, a, kw = c[idx[ci]]; idx[ci] += 1; live = True
                    (self.op if kind == "op" else self.dma)(*a, **kw)

    def op(self, eng, fn, reads=(), writes=()):
        if getattr(self, "_buf", None) is not None:
            self._buf.append(("op", (eng, fn, reads, writes), {})); return None
        psr = [k for k in reads if isinstance(k, str) and k.startswith("ps")]
        if psr:
            reads = [k for k in reads if k not in psr]
            writes = list(writes) + psr
        waits = self._deps(eng, reads, writes)
        s = "c_" + eng
        self.cnt[s] += 1
        tok = (s, self.cnt[s])
        self._commit(tok, reads, writes)
        self._emit(eng, waits, fn, s, 1)
        self.ninstr += 1
        return tok

    def dma(self, eng, out, in_, reads=(), writes=(), **kw):
        if getattr(self, "_buf", None) is not None:
            self._buf.append(("dma", (eng, out, in_, reads, writes), kw)); return None
        slot = self.dma_i[eng] % NDMASEM
        self.dma_i[eng] += 1
        s = f"d_{eng}_{slot}"
        waits = self._deps(eng, reads, writes)
        prev = self.cnt[s]
        if prev > 0 and self.seen[eng].get(s, 0) < prev:
            self.seen[eng][s] = prev
            waits.append((s, prev))
        self.cnt[s] += 16
        tok = (s, self.cnt[s])
        self._commit(tok, reads, writes)
        fn = lambda e, out=out, in_=in_, kw=kw: e.dma_start(out=out, in_=in_, **kw)
        self._emit(eng, waits, fn, s, 16)
        self.ninstr += 1
        return tok

    def coll(self, kind, in_ap, out_ap, groups, reads=(), writes=()):
        eng = "gpsimd"
        slot = self.dma_i[eng] % NDMASEM
        self.dma_i[eng] += 1
        s = f"d_{eng}_{slot}"
        waits = self._deps(eng, reads, writes)
        prev = self.cnt[s]
        if prev > 0 and self.seen[eng].get(s, 0) < prev:
            self.seen[eng][s] = prev
            waits.append((s, prev))
        self.cnt[s] += 16
        tok = (s, self.cnt[s])
        self._commit(tok, reads, writes)
        fn = lambda e: e.collective_compute(kind, ALU.bypass, replica_groups=groups, ins=[in_ap], outs=[out_ap])
        self._emit(eng, waits, fn, s, 16)
        self.ninstr += 1
        return tok

    def barrier(self):
        for eng in ENG:
            waits = []
            for sname, v in self.cnt.items():
                if v > 0 and self.seen[eng].get(sname, 0) < v:
                    self.seen[eng][sname] = v
                    waits.append((sname, v))
            self._emit(eng, waits, None, None, 0)

    def finish_wait(self, eng, keys):
        waits = self._deps(eng, keys, ())
        self._emit(eng, waits, None, None, 0)

    def _emit(self, eng, waits, fn, s, inc):
        if not self.immediate:
            self.ops[eng].append((waits, fn, s, inc)); return
        engobj = getattr(self.nc, eng)
        if fn is None or not ATTACH:
            for (ws, wv) in waits:
                engobj.wait_ge(self.sems[ws], wv)
            if fn is not None:
                fn(engobj).then_inc(self.sems[s], inc)
            return
        for (ws, wv) in waits[1:]:
            engobj.wait_ge(self.sems[ws], wv)
        ins = fn(engobj)
        if waits:
            ins._wait_ge(self.sems[waits[0][0]], waits[0][1])
        ins.then_inc(self.sems[s], inc)

    def build(self):
        if self.immediate:
            self.st.close(); return self.nc
        nc = self.nc
        with nc.Block() as block:
            def mk(e):
                def body(engobj):
                    for waits, fn, s, inc in self.ops[e]:
                        for (ws, wv) in waits:
                            engobj.wait_ge(self.sems[ws], wv)
                        if fn is not None:
                            fn(engobj).then_inc(self.sems[s], inc)
                return body
            block.sync(mk("sync"))
            block.gpsimd(mk("gpsimd"))
            block.scalar(mk("scalar"))
            block.vector(mk("vector"))
            block.tensor(mk("tensor"))
        self.st.close()
        return nc


NCOL = 2060
NEG = -30000.0
RV = {}
_o = 0
for _n, _l in [("gconv", 5 * 384), ("sconv", 5 * 512), ("sconvb", 512), ("mup", 768), ("mun", 768), ("spb", 10), ("alog", 10),
               ("gnorm", 128), ("w0", 256), ("a0", 256), ("kk", 128), ("ka", 128), ("rk", 128), ("lng", 128), ("lnb", 128), ("dsk", 256)]:
    RV[_n] = (_o, _l); _o += _l
NV = _o


def host_inputs_A(z, L, stream_b, j):
    f = lambda a: np.ascontiguousarray(a, dtype=np.float32)
    w_in = z['w_in'][L]
    g = j // 2
    GD0, RW0, SS0 = 0, 2064, 2064 + 1920
    r = lambda a, n: list(range(a, a + n))
    cols = (r(GD0 + j * 128, 128) + r(GD0 + 512 + j * 128, 128) + r(GD0 + 1024 + j * 128, 128) + r(GD0 + 1536 + j * 128, 128)
            + r(RW0 + j * 128, 128) + r(RW0 + 512 + j * 128, 128) + r(RW0 + 1024 + j * 128, 128) + r(RW0 + 1536, 384)
            + r(SS0 + j * 256, 256) + r(SS0 + 1024 + j * 256, 256) + r(SS0 + 2048 + g * 128, 128) + r(SS0 + 2304 + g * 128, 128)
            + [GD0 + 2048 + d * 4 + j for d in range(2)] + [GD0 + 2056 + d * 4 + j for d in range(2)]
            + [SS0 + 2560 + d * 16 + 4 * j + i for d in range(2) for i in range(4)])
    assert len(cols) == NCOL
    rv = np.zeros(NV, np.float32)
    def put(n, a):
        o, l = RV[n]; a = np.asarray(a, np.float32).reshape(-1); assert a.size == l, (n, a.size, l); rv[o:o + l] = a
    qkv_idx = r(j * 128, 128) + r(512 + j * 128, 128) + r(1024 + j * 128, 128)
    xbc_idx = r(j * 256, 256) + r(1024 + g * 128, 128) + r(1280 + g * 128, 128)
    rw_idx = r(j * 128, 128) + r(512 + j * 128, 128) + r(1024 + j * 128, 128) + r(1536, 384)
    put("gconv", z['gdn_conv'][L][:, qkv_idx]); put("sconv", z['ssd_conv'][L][:, xbc_idx]); put("sconvb", z['ssd_conv_b'][L][xbc_idx])
    put("mup", z['rwkv_mu_prev'][L][rw_idx]); put("mun", z['rwkv_mu_next'][L][rw_idx])
    put("spb", np.concatenate([z['gdn_dt_bias'][L][:, j], z['ssd_dt_bias'][L][:, 4 * j:4 * j + 4].reshape(-1)]))
    put("alog", np.concatenate([z['gdn_a_log'][L][:, j], z['ssd_a_log'][L][:, 4 * j:4 * j + 4].reshape(-1)]))
    put("gnorm", z['gdn_norm'][L]); put("w0", z['rwkv_w0'][L][:, j * 128:(j + 1) * 128]); put("a0", z['rwkv_a0'][L][:, j * 128:(j + 1) * 128])
    put("kk", z['rwkv_k_k'][L][j * 128:(j + 1) * 128]); put("ka", z['rwkv_k_a'][L][j * 128:(j + 1) * 128])
    put("rk", z['rwkv_r_k'][L][2 * j:2 * j + 2]); put("lng", z['rwkv_ln_g'][L][j * 128:(j + 1) * 128]); put("lnb", z['rwkv_ln_b'][L][j * 128:(j + 1) * 128])
    put("dsk", np.repeat(z['ssd_d'][L][4 * j:4 * j + 4], 64))
    k = np.arange(128)
    UT = (k[:, None] <= k[None, :]).astype(np.float32); LT = (k[:, None] >= k[None, :]).astype(np.float32)
    blk = (k[:, None] // 64 == k[None, :] // 64).astype(np.float32)
    bdUT = UT * blk; bdLT = LT * blk
    msk = np.stack([UT, LT, np.where(UT > 0, 0.0, NEG), np.where(LT > 0, 0.0, NEG), np.eye(128), np.ones((128, 128)), bdUT, bdLT, -(bdUT - np.eye(128)), -(bdLT - np.eye(128)), bdUT - np.eye(128), bdLT - np.eye(128)], axis=1)
    return {
        "xT": f(stream_b.T), "wc": f(w_in[:, cols]), "rowvec": f(rv[None]),
        "wup": f(z['rwkv_w_up'][L][:, :, j * 128:(j + 1) * 128].reshape(128, 128)),
        "aup": f(z['rwkv_a_up'][L][:, :, j * 128:(j + 1) * 128].reshape(128, 128)),
        "gup": f(z['rwkv_g_up'][L][:, j * 128:(j + 1) * 128]), "msk": f(msk),
    }


def build_A(T=8192, dump=False, NS=16, phases=(1, 2, 3, 4)):
    p = Prog(); nc = p.nc
    NTL = T // 128
    dk = "ExternalOutput" if dump else "Internal"
    xT = p.dram("xT", [1024, T]); wc = p.dram("wc", [1024, NCOL]); rowvec = p.dram("rowvec", [1, NV])
    wup = p.dram("wup", [128, 128]); aup = p.dram("aup", [128, 128]); gup = p.dram("gup", [128, 128]); mskd = p.dram("msk", [128, 12, 128])
    ya_o = p.dram("ya", [T, 128], kind="ExternalOutput"); yb_o = p.dram("yb", [T, 128], kind="ExternalOutput"); yc_o = p.dram("yc", [T, 256], kind="ExternalOutput")
    cols_s = p.dram("cols_s", [T + 4, NCOL], kind=dk)
    gkq_s = p.dram("gkq_s", [T, 2, 128], kind=dk); gsc_s = p.dram("gsc_s", [T, 4], kind=dk); gbvT_s = p.dram("gbvT_s", [2, 128, T], kind=dk)
    rw_s = p.dram("rw_s", [2, 2, T, 5, 64], kind=dk); rvT_s = p.dram("rvT_s", [128, T], kind=dk); rpost_s = p.dram("rpost_s", [T, 258], kind=dk)
    ssd_s = p.dram("ssd_s", [T, 528], kind=dk)
    gy_s = p.dram("gy_s", [2, T, 128], kind=dk); gkqv_s = p.dram("gkqv_s", [T, 3, 128], kind=dk); gbg_s = p.dram("gbg_s", [T, 4], kind=dk); ry_s = p.dram("ry_s", [2, T, 128], kind=dk); sy_s = p.dram("sy_s", [T, 256], kind=dk)

    V = lambda fn, r=(), w=(): p.op("vector", fn, r, w)
    G = lambda fn, r=(), w=(): p.op("gpsimd", fn, r, w)
    S = lambda fn, r=(), w=(): p.op("scalar", fn, r, w)
    PE = lambda fn, r=(), w=(): p.op("tensor", fn, r, w)

    msk = p.sb("msk", [128, 12, 128]); rvs = p.sb("rvs", [128, NV])
    p.dma("sync", msk[:], mskd[:, :, :], writes=["msk"])
    p.dma("sync", rvs[:], rowvec.partition_broadcast(128)[:, 0, :], writes=["rvs"])
    UT, LT, NEGf, NEGb, ident, ones, bdUT, bdLT, nbdUTs, nbdLTs, sbdUT, sbdLT = (msk[:, i, :] for i in range(12))
    def rv(n, a=0, l=None):
        o, ln = RV[n]
        return rvs[:, o + a:o + a + (ln - a if l is None else l)]
    negexp = p.sb("negexp", [128, 10])
    S(lambda e: e.activation(out=negexp[:], in_=rv("alog"), func=AF.Exp), ["rvs"], ["negexp"])
    V(lambda e: e.tensor_scalar(out=negexp[:], in0=negexp[:], scalar1=-1.0, scalar2=None, op0=ALU.mult), ["negexp"], ["negexp"])
    PS = [p.ps(f"ps{i}", [128, 512]) for i in range(8)]

    if 1 in phases:
        with contextlib.ExitStack() as sc:
            sb = lambda name, shape, dt=F32: sc.enter_context(nc.sbuf_tensor("s_" + name, list(shape), dt))
            W_bf = sb("W_bf", [128, 8, 2048], BF16); w_sm = sb("w_sm", [128, 8, 12])
            zt = sb("zt", [2, NCOL])
            xb = [sb(f"xb{i}", [128, 8, 128], BF16) for i in range(2)]; xf = [sb(f"xf{i}", [128, 8, 128]) for i in range(2)]
            ct = [sb(f"ct{i}", [128, NCOL]) for i in range(2)]
            wcv = wc.rearrange("(kc p) n -> p kc n", p=128)
            for kc in range(8):
                p.dma("gpsimd", W_bf[:, kc, :], wc[kc * 128:(kc + 1) * 128, 0:2048], writes=["W_bf"])
            p.dma("sync", w_sm[:], wcv[:, :, 2048:2060], writes=["w_sm"])
            V(lambda e: e.memset(zt[:], 0.0), [], ["zt"])
            p.dma("sync", cols_s[0:2, :], zt[:], reads=["zt"], writes=["cols_pad"])
            p.dma("sync", cols_s[T + 2:T + 4, :], zt[:], reads=["zt"], writes=["cols_pad"])
            xTv = xT.rearrange("(kc p) n -> p kc n", p=128)
            for tt in range(NTL):
                t0 = tt * 128; i = tt % 2
                p.dma("gpsimd", xb[i][:], xTv[:, :, t0:t0 + 128], writes=[f"xb{i}"])
                p.dma("sync", xf[i][:], xTv[:, :, t0:t0 + 128], writes=[f"xf{i}"])
                for gq in range(4):
                    pp = PS[gq]
                    for kc in range(8):
                        PE(lambda e, kc=kc, gq=gq, pp=pp, i=i: e.matmul(pp[:, :], lhsT=xb[i][:, kc, :], rhs=W_bf[:, kc, gq * 512:(gq + 1) * 512], start=(kc == 0), stop=(kc == 7)),
                           [f"xb{i}", "W_bf"], [f"ps{gq}"])
                    if gq % 2 == 0:
                        S(lambda e, gq=gq, pp=pp, i=i: e.copy(out=ct[i][:, gq * 512:(gq + 1) * 512], in_=pp[:, :]), [f"ps{gq}"], [f"ct{i}"])
                    else:
                        V(lambda e, gq=gq, pp=pp, i=i: e.tensor_copy(out=ct[i][:, gq * 512:(gq + 1) * 512], in_=pp[:, :]), [f"ps{gq}"], [f"ct{i}"])
                for kc in range(8):
                    PE(lambda e, kc=kc, i=i: e.matmul(PS[4][:, :12], lhsT=xf[i][:, kc, :], rhs=w_sm[:, kc, :], start=(kc == 0), stop=(kc == 7)),
                       [f"xf{i}", "w_sm"], ["ps4"])
                V(lambda e, i=i: e.tensor_copy(out=ct[i][:, 2048:2060], in_=PS[4][:, :12]), ["ps4"], [f"ct{i}"])
                p.dma("sync", cols_s[2 + t0:2 + t0 + 128, :], ct[i][:], reads=[f"ct{i}"], writes=["cols_s"])
        p.barrier()

    if 2 in phases:
        with contextlib.ExitStack() as sc:
            sb = lambda name, shape, dt=F32: sc.enter_context(nc.sbuf_tensor("s_" + name, list(shape), dt))
            win = [sb(f"win{j}", [128, NCOL]) for j in range(5)]
            wup_sb = sb("wup_sb", [128, 128]); aup_sb = sb("aup_sb", [128, 128]); gup_sb = sb("gup_sb", [128, 128])
            p.dma("sync", wup_sb[:], wup[:, :], writes=["wup_sb"]); p.dma("sync", aup_sb[:], aup[:, :], writes=["aup_sb"]); p.dma("sync", gup_sb[:], gup[:, :], writes=["gup_sb"])
            cacc = sb("cacc", [128, 896]); ctmp = sb("ctmp", [128, 896]); qkv = sb("qkv", [128, 384]); xbc = sb("xbc", [128, 528])
            junk = sb("junk", [128, 128]); ssq = sb("ssq", [128, 4]); kq = sb("kq", [128, 2, 128])
            spx = sb("spx", [128, 10]); spa = sb("spa", [128, 10]); spl = sb("spl", [128, 10]); beta = sb("beta", [128, 2]); gsc = sb("gsc", [128, 4])
            bv = sb("bv", [128, 2, 128]); trs = sb("trs", [128, 128]); bg = sb("bg", [128, 4])
            sh = sb("sh", [128, 768]); d1 = sb("d1", [128, 768]); d2 = sb("d2", [128, 768])
            tw = sb("tw", [128, 128]); twT = sb("twT", [128, 128]); alT = sb("alT", [128, 128]); sgl = sb("sgl", [128, 128]); sgT = sb("sgT", [128, 128])
            RW = [sb(f"RW{d}", [128, 5, 128]) for d in range(2)]; ad = [sb(f"ad{d}", [128, 128]) for d in range(2)]
            wraw = sb("wraw", [128, 128]); kx = sb("kx", [128, 128]); sqk = sb("sqk", [128, 128]); rkk = sb("rkk", [128, 2]); kkn = sb("kkn", [128, 128])
            rkr = sb("rkr", [128, 128]); prod = sb("prod", [128, 128]); bon = sb("bon", [128, 2, 2]); rpost = sb("rpost", [128, 258]); t128 = sb("t128", [128, 128])
            pT1, pT2, pM1, pM2, pT3 = PS[0], PS[1], PS[2], PS[3], PS[4]
            for tt in range(NTL):
                t0 = tt * 128
                for j in range(5):
                    p.dma("sync" if j % 2 == 0 else "scalar", win[j][:], cols_s[t0 + j:t0 + j + 128, :], reads=["cols_s", "cols_pad"], writes=[f"win{j}"])
                cur = win[2]
                for (c0, c1, o0, cname, cw) in [(0, 384, 0, "gconv", 384), (1536, 2048, 384, "sconv", 512)]:
                    for j in range(5):
                        wj = rv(cname, j * cw, cw)
                        if j == 0:
                            V(lambda e, c0=c0, c1=c1, o0=o0, wj=wj, cw=cw: e.tensor_tensor(out=cacc[:, o0:o0 + cw], in0=win[0][:, c0:c1], in1=wj, op=ALU.mult), ["win0", "rvs"], [f"cacc{o0}"])
                        else:
                            G(lambda e, c0=c0, c1=c1, o0=o0, wj=wj, cw=cw, j=j: e.tensor_tensor(out=ctmp[:, o0:o0 + cw], in0=win[j][:, c0:c1], in1=wj, op=ALU.mult), [f"win{j}", "rvs"], [f"ctmp{o0}"])
                            V(lambda e, o0=o0, cw=cw: e.tensor_tensor(out=cacc[:, o0:o0 + cw], in0=cacc[:, o0:o0 + cw], in1=ctmp[:, o0:o0 + cw], op=ALU.add), [f"cacc{o0}", f"ctmp{o0}"], [f"cacc{o0}"])
                V(lambda e: e.tensor_tensor(out=cacc[:, 384:896], in0=cacc[:, 384:896], in1=rv("sconvb"), op=ALU.add), ["cacc384", "rvs"], ["cacc384"])
                S(lambda e: e.activation(out=qkv[:], in_=cacc[:, 0:384], func=AF.Silu), ["cacc0"], ["qkv"])
                S(lambda e: e.activation(out=xbc[:, 0:512], in_=cacc[:, 384:896], func=AF.Silu), ["cacc384"], ["xbc"])
                V(lambda e: e.tensor_tensor(out=spx[:], in0=cur[:, 2050:2060], in1=rv("spb"), op=ALU.add), ["win2", "rvs"], ["spx"])
                S(lambda e: e.activation(out=spa[:], in_=spx[:], func=AF.Abs), ["spx"], ["spa"])
                S(lambda e: e.activation(out=spa[:], in_=spa[:], func=AF.Exp, scale=-1.0), ["spa"], ["spa"])
                S(lambda e: e.activation(out=spl[:], in_=spa[:], func=AF.Ln, bias=1.0), ["spa"], ["spl"])
                V(lambda e: e.tensor_scalar(out=spx[:], in0=spx[:], scalar1=0.0, scalar2=None, op0=ALU.max), ["spx"], ["spx"])
                V(lambda e: e.tensor_tensor(out=spx[:], in0=spx[:], in1=spl[:], op=ALU.add), ["spx", "spl"], ["spx"])
                V(lambda e: e.tensor_tensor(out=spl[:], in0=spx[:], in1=negexp[:], op=ALU.mult), ["spx", "negexp"], ["spl"])
                V(lambda e: e.tensor_copy(out=xbc[:, 512:520], in_=spx[:, 2:10]), ["spx"], ["xbc"])
                V(lambda e: e.tensor_copy(out=xbc[:, 520:528], in_=spl[:, 2:10]), ["spl"], ["xbc"])
                p.dma("gpsimd", ssd_s[t0:t0 + 128, :], xbc[:], reads=["xbc"], writes=["ssd_s"])
                S(lambda e: e.activation(out=beta[:], in_=cur[:, 2048:2050], func=AF.Sigmoid), ["win2"], ["beta"])
                S(lambda e: e.activation(out=gsc[:, 0:2], in_=spl[:, 0:2], func=AF.Exp), ["spl"], ["gsc"])
                V(lambda e: e.scalar_tensor_tensor(out=gsc[:, 2:4], in0=gsc[:, 0:2], scalar=-1.0, in1=beta[:], op0=ALU.mult, op1=ALU.mult), ["gsc", "beta"], ["gsc"])
                for qi in range(2):
                    src = qkv[:, qi * 128:(qi + 1) * 128]
                    V(lambda e, src=src, qi=qi: e.scalar_tensor_tensor(out=junk[:], in0=src, scalar=1.0, in1=src, op0=ALU.mult, op1=ALU.mult, accum_out=ssq[:, qi:qi + 1]), ["qkv"], ["junk", "ssq"])
                V(lambda e: e.tensor_scalar(out=ssq[:, 0:2], in0=ssq[:, 0:2], scalar1=1e-6, scalar2=None, op0=ALU.add), ["ssq"], ["ssq"])
                S(lambda e: e.activation(out=ssq[:, 0:2], in_=ssq[:, 0:2], func=AF.Sqrt), ["ssq"], ["ssq"])
                V(lambda e: e.reciprocal(out=ssq[:, 0:2], in_=ssq[:, 0:2]), ["ssq"], ["ssq"])
                V(lambda e: e.tensor_scalar(out=kq[:, 0, :], in0=qkv[:, 128:256], scalar1=ssq[:, 1:2], scalar2=None, op0=ALU.mult), ["qkv", "ssq"], ["kq"])
                V(lambda e: e.tensor_scalar(out=kq[:, 1, :], in0=qkv[:, 0:128], scalar1=ssq[:, 0:1], scalar2=128 ** -0.5, op0=ALU.mult, op1=ALU.mult), ["qkv", "ssq"], ["kq"])
                p.dma("gpsimd", gkqv_s[t0:t0 + 128, 0:2, :], kq[:], reads=["kq"], writes=["gkqv_s"])
                p.dma("gpsimd", gkqv_s[t0:t0 + 128, 2, :], qkv[:, 256:384], reads=["qkv"], writes=["gkqv_s"])
                G(lambda e: e.tensor_copy(out=bg[:, 0:2], in_=beta[:]), ["beta"], ["bg"])
                G(lambda e: e.tensor_copy(out=bg[:, 2:4], in_=spl[:, 0:2]), ["spl"], ["bg"])
                p.dma("gpsimd", gbg_s[t0:t0 + 128, :], bg[:], reads=["bg"], writes=["gbg_s"])
                c_, pv, nx = cur[:, 512:1280], win[1][:, 512:1280], win[3][:, 512:1280]
                V(lambda e: e.tensor_tensor(out=d1[:], in0=pv, in1=c_, op=ALU.subtract), ["win1", "win2"], ["d1"])
                G(lambda e: e.tensor_tensor(out=d1[:], in0=d1[:], in1=rv("mup"), op=ALU.mult), ["d1", "rvs"], ["d1"])
                V(lambda e: e.tensor_tensor(out=d2[:], in0=nx, in1=c_, op=ALU.subtract), ["win3", "win2"], ["d2"])
                G(lambda e: e.tensor_tensor(out=d2[:], in0=d2[:], in1=rv("mun"), op=ALU.mult), ["d2", "rvs"], ["d2"])
                V(lambda e: e.tensor_tensor(out=sh[:], in0=c_, in1=d1[:], op=ALU.add), ["win2", "d1"], ["sh"])
                V(lambda e: e.tensor_tensor(out=sh[:], in0=sh[:], in1=d2[:], op=ALU.add), ["sh", "d2"], ["sh"])
                r_, k_, v_, wl, al, gl = (sh[:, i * 128:(i + 1) * 128] for i in range(6))
                S(lambda e: e.activation(out=tw[:], in_=wl, func=AF.Tanh), ["sh"], ["tw"])
                PE(lambda e: e.transpose(out=pT1[:, :128], in_=tw[:], identity=ident), ["tw", "msk"], ["ps0"])
                S(lambda e: e.copy(out=twT[:], in_=pT1[:, :128]), ["ps0"], ["twT"])
                PE(lambda e: e.transpose(out=pT2[:, :128], in_=al, identity=ident), ["sh", "msk"], ["ps1"])
                V(lambda e: e.tensor_copy(out=alT[:], in_=pT2[:, :128]), ["ps1"], ["alT"])
                S(lambda e: e.activation(out=sgl[:], in_=gl, func=AF.Sigmoid), ["sh"], ["sgl"])
                PE(lambda e: e.transpose(out=pT3[:, :128], in_=sgl[:], identity=ident), ["sgl", "msk"], ["ps4"])
                V(lambda e: e.tensor_copy(out=sgT[:], in_=pT3[:, :128]), ["ps4"], ["sgT"])
                PE(lambda e: e.matmul(pT3[:, 128:256], lhsT=sgT[:], rhs=gup_sb[:], start=True, stop=True), ["sgT", "gup_sb"], ["ps4"])
                S(lambda e: e.copy(out=rpost[:, 128:256], in_=pT3[:, 128:256]), ["ps4"], ["rpost"])
                V(lambda e: e.tensor_tensor(out=kx[:], in0=k_, in1=rv("kk"), op=ALU.mult), ["sh", "rvs"], ["kx"])
                S(lambda e: e.activation(out=sqk[:], in_=kx[:], func=AF.Square), ["kx"], ["sqk"])
                V(lambda e: e.tensor_reduce(out=rkk[:], in_=sqk[:].rearrange("p (h n) -> p h n", h=2), axis=AX.X, op=ALU.add), ["sqk"], ["rkk"])
                V(lambda e: e.tensor_scalar(out=rkk[:], in0=rkk[:], scalar1=1e-6, scalar2=None, op0=ALU.add), ["rkk"], ["rkk"])
                S(lambda e: e.activation(out=rkk[:], in_=rkk[:], func=AF.Sqrt), ["rkk"], ["rkk"])
                V(lambda e: e.reciprocal(out=rkk[:], in_=rkk[:]), ["rkk"], ["rkk"])
                for h in range(2):
                    V(lambda e, h=h: e.tensor_scalar(out=kkn[:, h * 64:(h + 1) * 64], in0=kx[:, h * 64:(h + 1) * 64], scalar1=rkk[:, h:h + 1], scalar2=None, op0=ALU.mult), ["kx", "rkk"], ["kkn"])
                G(lambda e: e.tensor_tensor(out=rkr[:], in0=r_, in1=rv("rk"), op=ALU.mult), ["sh", "rvs"], ["rkr"])
                for d in range(2):
                    hs = slice(d * 64, (d + 1) * 64)
                    PE(lambda e, hs=hs: e.matmul(pM1[:, :128], lhsT=twT[hs, :], rhs=wup_sb[hs, :], start=True, stop=True), ["twT", "wup_sb"], ["ps2"])
                    V(lambda e, d=d: e.tensor_tensor(out=wraw[:], in0=pM1[:, :128], in1=rv("w0", d * 128, 128), op=ALU.add), ["ps2", "rvs"], ["wraw"])
                    S(lambda e: e.activation(out=wraw[:], in_=wraw[:], func=AF.Sigmoid), ["wraw"], ["wraw"])
                    S(lambda e, d=d: e.activation(out=RW[d][:, 0, :], in_=wraw[:], func=AF.Exp, scale=-0.6065306597126334), ["wraw"], [f"RW{d}"])
                    PE(lambda e, hs=hs: e.matmul(pM2[:, :128], lhsT=alT[hs, :], rhs=aup_sb[hs, :], start=True, stop=True), ["alT", "aup_sb"], ["ps3"])
                    V(lambda e, d=d: e.tensor_tensor(out=ad[d][:], in0=pM2[:, :128], in1=rv("a0", d * 128, 128), op=ALU.add), ["ps3", "rvs"], [f"ad{d}"])
                    S(lambda e, d=d: e.activation(out=ad[d][:], in_=ad[d][:], func=AF.Sigmoid), [f"ad{d}"], [f"ad{d}"])
                    G(lambda e, d=d: e.tensor_copy(out=RW[d][:, 1, :], in_=kkn[:]), ["kkn"], [f"RW{d}"])
                    V(lambda e, d=d: e.scalar_tensor_tensor(out=RW[d][:, 2, :], in0=kkn[:], scalar=-1.0, in1=ad[d][:], op0=ALU.mult, op1=ALU.mult), ["kkn", f"ad{d}"], [f"RW{d}"])
                    V(lambda e, d=d: e.scalar_tensor_tensor(out=t128[:], in0=ad[d][:], scalar=-1.0, in1=rv("ka"), op0=ALU.add, op1=ALU.mult), [f"ad{d}", "rvs"], ["t128"])
                    V(lambda e, d=d: e.scalar_tensor_tensor(out=RW[d][:, 3, :], in0=t128[:], scalar=1.0, in1=k_, op0=ALU.add, op1=ALU.mult), ["t128", "sh"], [f"RW{d}"])
                    G(lambda e, d=d: e.tensor_copy(out=RW[d][:, 4, :], in_=r_), ["sh"], [f"RW{d}"])
                    V(lambda e, d=d: e.tensor_tensor(out=prod[:], in0=rkr[:], in1=RW[d][:, 3, :], op=ALU.mult), ["rkr", f"RW{d}"], ["prod"])
                    V(lambda e, d=d: e.tensor_reduce(out=bon[:, d, :], in_=prod[:].rearrange("p (h n) -> p h n", h=2), axis=AX.X, op=ALU.add), ["prod"], ["bon"])
                    for h in range(2):
                        p.dma("gpsimd", rw_s[d, h, t0:t0 + 128, :, :], RW[d][:, :, h * 64:(h + 1) * 64], reads=[f"RW{d}"], writes=["rw_s"])
                V(lambda e: e.tensor_tensor(out=rpost[:, 256:258], in0=bon[:, 0, :], in1=bon[:, 1, :], op=ALU.add), ["bon"], ["rpost"])
                G(lambda e: e.tensor_copy(out=rpost[:, 0:128], in_=v_), ["sh"], ["rpost"])
                p.dma("gpsimd", rpost_s[t0:t0 + 128, :], rpost[:], reads=["rpost"], writes=["rpost_s"])
                PE(lambda e: e.transpose(out=pT2[:, :128], in_=v_, identity=ident), ["sh", "msk"], ["ps1"])
                V(lambda e: e.tensor_copy(out=t128[:], in_=pT2[:, :128]), ["ps1"], ["t128"])
                p.dma("gpsimd", rvT_s[:, t0:t0 + 128], t128[:], reads=["t128"], writes=["rvT_s"])
        p.barrier()
    fin = ["ya", "yb", "yc"]
    if dump:
        fin += ["cols_s", "gkqv_s", "gbg_s", "rw_s", "rvT_s", "rpost_s", "ssd_s", "gy_s", "ry_s"]
    if 3 in phases:
        phase3(p, T, NS, locals())
    if 4 in phases:
        phase4(p, T, locals())
    p.finish_wait("sync", [k for k in fin if k in p.lastw])
    return p.build()


def phase3(p, T, NS, L):
    SSDSTOP = int(os.environ.get('A_SSDSTOP', '99'))
    GSTOP = int(os.environ.get('A_GSTOP', '99'))
    nc = p.nc
    V = L["V"]; G = L["G"]; S = L["S"]; PE = L["PE"]; PS = L["PS"]
    UT, LT, ident, ones = L["UT"], L["LT"], L["ident"], L["ones"]
    gkqv_s, gbg_s, rw_s, rvT_s, ssd_s, gy_s, ry_s, sy_s, cols_s, yc_o = (L[k] for k in
        ["gkqv_s", "gbg_s", "rw_s", "rvT_s", "ssd_s", "gy_s", "ry_s", "sy_s", "cols_s", "yc_o"])
    bdUT, bdLT, nbdUTs, nbdLTs, sbdUT, sbdLT = (L[k_] for k_ in ["bdUT", "bdLT", "nbdUTs", "nbdLTs", "sbdUT", "sbdLT"])
    rpost_s = L["rpost_s"]
    rv = L["rv"]
    NTL = T // 128; NCH = T // NS
    with contextlib.ExitStack() as sc:
        sb = lambda name, shape, dt=F32: sc.enter_context(nc.sbuf_tensor("s_" + name, list(shape), dt))
        RB = []
        for d in range(2):
            r_ = {}
            for nm, shp in [("rwt", [128, 5, 128]), ("vt", [128, 128]), ("lw", [128, 128]), ("lp", [128, 128]), ("Pt", [128, 128]), ("iP", [128, 128]), ("Pm", [128, 128]),
                            ("rt", [128, 128]), ("kt", [128, 128]), ("nbt", [128, 128]), ("ct", [128, 128]), ("rtF", [128, 128]), ("nbF", [128, 128]), ("cF", [128, 128]), ("PF", [128, 128]),
                            ("X", [128, 128]), ("XT", [128, 128]), ("AckT", [128, 128]), ("ArkT", [128, 128]), ("AnrbT", [128, 128]),
                            ("P0", [128, 128]), ("P1", [128, 128]), ("PT0", [128, 128]), ("PT1", [128, 128]), ("TT", [128, 128]),
                            ("Zs", [128, 64]), ("Ms", [128, 64]), ("Yt", [128, 128]), ("H", [128, 64])]:
                r_[nm] = sb(f"r{d}_{nm}", shp)
            for nm in ["cFh", "nbFh", "ktFh", "rtFh"]:
                for h in range(2):
                    r_[f"{nm}{h}"] = sb(f"r{d}_{nm}{h}", [128, 128])
                    V(lambda e, t_=r_[f"{nm}{h}"]: e.memset(t_[:], 0.0), [], [f"r{d}_{nm}{h}"])
            for nm in ["ktS", "nbS"]:
                for ch in range(2):
                    for h in range(2):
                        r_[f"{nm}{ch}{h}"] = sb(f"r{d}_{nm}{ch}{h}", [128, 128])
                        V(lambda e, t_=r_[f"{nm}{ch}{h}"]: e.memset(t_[:], 0.0), [], [f"r{d}_{nm}{ch}{h}"])
            V(lambda e, r_=r_: e.memset(r_["H"][:], 0.0), [], [f"r{d}_H"])
            V(lambda e, r_=r_: e.memset(r_["Zs"][:], 0.0), [], [f"r{d}_Zs"])
            V(lambda e, r_=r_: e.memset(r_["Ms"][:], 0.0), [], [f"r{d}_Ms"])
            RB.append(r_)
        GB = []
        for d in range(2):
            g_ = {}
            for nm, shp in [("kqv", [128, 3, 128]), ("bg", [128, 4]), ("kF", [128, 128]), ("qF", [128, 128]), ("Gs", [128, 128]), ("QKs", [128, 128]),
                            ("gcc", [128, 1]), ("ngcc", [128, 1]), ("R", [128, 128]), ("grow", [128, 128]), ("egrow", [128, 128]), ("dT", [128, 128]), ("dN", [128, 128]),
                            ("Bd", [128, 128]), ("brow", [128, 128]), ("X", [128, 128]), ("XT", [128, 128]), ("P0", [128, 128]), ("P1", [128, 128]),
                            ("PT0", [128, 128]), ("PT1", [128, 128]), ("TT", [128, 128]), ("vb", [128, 128]), ("kbg", [128, 128]), ("be", [128, 1]), ("eg", [128, 1]),
                            ("u", [128, 128]), ("wF", [128, 128]), ("qdF", [128, 128]), ("QKm", [128, 128]), ("vnew", [128, 128]), ("kdA", [128, 128]), ("kdB", [128, 128]),
                            ("dl", [128, 1]), ("dlA", [128, 1]), ("dlB", [128, 1]), ("S", [128, 128]), ("o", [128, 128]), ("t1", [128, 128]), ("t2", [128, 128])]:
                g_[nm] = sb(f"g{d}_{nm}", shp)
            GB.append(g_)
            V(lambda e, g_=g_: e.memset(g_["S"][:], 0.0), [], [f"g{d}_S"])
            V(lambda e, g_=g_: e.memset(g_["vnew"][:], 0.0), [], [f"g{d}_vnew"])
        sxt = sb("sxt", [128, 528]); BCF = sb("BCF", [128, 256]); scT = sb("scT", [128, 128]); acol = sb("acol", [128, 4]); nacol = sb("nacol", [128, 4])
        Rm = sb("Rm", [128, 4, 128]); E = sb("E", [128, 4, 128]); Dm = sb("Dm", [128, 128]); M = sb("M", [128, 4, 128]); CdF = sb("CdF", [128, 4, 128])
        xdt = sb("xdt", [128, 256]); xend = sb("xend", [128, 256]); dend = sb("dend", [128, 4]); H = sb("H", [128, 256]); yt = sb("yt", [128, 256])
        syf = sb("syf", [128, 256]); zt = sb("zts", [128, 256]); xd = sb("xd", [128, 256]); szs = sb("szs", [128, 256])
        pA, pSc, pAc, pRow, pY, pSt = PS[0][:, 0:256], PS[0][:, 256:384], PS[0][:, 384:512], PS[1], PS[2][:, 0:256], PS[2][:, 256:512]
        print('phase3 sbuf remaining', nc.sbuf_bytes_remaining, flush=True)

        def gdn_chunk(d, c):
            t0 = c * 128
            B_ = GB[d]; K = lambda n: f"g{d}_{n}"
            bank = PS[6 + d]; kb = f"ps{6 + d}"
            regs = [bank[:, i * 128:(i + 1) * 128] for i in range(4)] + [PS[3][:, d * 256:d * 256 + 128], PS[3][:, d * 256 + 128:d * 256 + 256]]
            keys = [kb] * 4 + ["ps3", "ps3"]
            (rA, rB, rC, rD, rE, rF), (kA, kB, kC, kD, kE, kF_) = regs, keys
            fwd = (d == 0)
            Mtri = bdUT if fwd else bdLT
            MaskT = Mtri
            MaskN = bdLT if fwd else bdUT
            nST = nbdUTs if fwd else nbdLTs
            nSN = nbdLTs if fwd else nbdUTs
            selA = bdLT[:, 0:1]; selB = bdUT[:, 127:128]
            hA, hB = slice(0, 64), slice(64, 128)
            if fwd:
                first, second, lastF, lastS = hA, hB, 63, 127
            else:
                first, second, lastF, lastS = hB, hA, 64, 0
            kqv, bg = B_["kqv"], B_["bg"]
            p.dma("sync", kqv[:], gkqv_s[t0:t0 + 128, :, :], reads=["gkqv_s"], writes=[K("kqv")])
            p.dma("sync", bg[:], gbg_s[t0:t0 + 128, :], reads=["gbg_s"], writes=[K("bg")])
            kc, qc, vc = kqv[:, 0, :], kqv[:, 1, :], kqv[:, 2, :]
            beta = bg[:, d:d + 1]; g = bg[:, 2 + d:3 + d]
            kF, qF, Gs, QKs = B_["kF"], B_["qF"], B_["Gs"], B_["QKs"]
            PE(lambda e: e.transpose(out=rA, in_=kc, identity=ident), [K("kqv"), "msk"], [kA])
            S(lambda e: e.copy(out=kF[:], in_=rA), [kA], [K("kF")])
            PE(lambda e: e.transpose(out=rB, in_=qc, identity=ident), [K("kqv"), "msk"], [kB])
            S(lambda e: e.copy(out=qF[:], in_=rB), [kB], [K("qF")])
            PE(lambda e: e.matmul(rC, lhsT=kF[:], rhs=kF[:], start=True, stop=True), [K("kF")], [kC])
            S(lambda e: e.copy(out=Gs[:], in_=rC), [kC], [K("Gs")])
            PE(lambda e: e.matmul(rD, lhsT=kF[:], rhs=qF[:], start=True, stop=True), [K("kF"), K("qF")], [kD])
            S(lambda e: e.copy(out=QKs[:], in_=rD), [kD], [K("QKs")])
            G(lambda e: e.tensor_scalar(out=B_["R"][:], in0=Mtri, scalar1=g, scalar2=None, op0=ALU.mult), [K("bg"), "msk"], [K("R")])
            PE(lambda e: e.matmul(rE, lhsT=ones, rhs=B_["R"][:], start=True, stop=True), [K("R"), "msk"], [kE])
            S(lambda e: e.copy(out=B_["grow"][:], in_=rE), [kE], [K("grow")])
            S(lambda e: e.activation(out=B_["egrow"][:], in_=rE, func=AF.Exp), [kE], [K("egrow")])
            V(lambda e: e.scalar_tensor_tensor(out=B_["t1"][:], in0=B_["grow"][:], scalar=1.0, in1=ident, op0=ALU.mult, op1=ALU.mult, accum_out=B_["gcc"][:, 0:1]), [K("grow"), "msk"], [K("t1"), K("gcc")])
            S(lambda e: e.mul(out=B_["ngcc"][:], in_=B_["gcc"][:], mul=-1.0), [K("gcc")], [K("ngcc")])
            for nm, bias_k, sc, Mk in [("dT", "ngcc", 1.0, MaskT), ("dN", "gcc", -1.0, MaskN)]:
                S(lambda e, nm=nm, bias_k=bias_k, sc=sc: e.activation(out=B_[nm][:], in_=B_["grow"][:], func=AF.Identity, bias=B_[bias_k][:, 0:1], scale=sc), [K("grow"), K(bias_k)], [K(nm)])
                G(lambda e, nm=nm: e.tensor_scalar(out=B_[nm][:], in0=B_[nm][:], scalar1=0.0, scalar2=None, op0=ALU.min), [K(nm)], [K(nm)])
                S(lambda e, nm=nm: e.activation(out=B_[nm][:], in_=B_[nm][:], func=AF.Exp), [K(nm)], [K(nm)])
                G(lambda e, nm=nm, Mk=Mk: e.tensor_tensor(out=B_[nm][:], in0=B_[nm][:], in1=Mk, op=ALU.mult), [K(nm), "msk"], [K(nm)])
            G(lambda e: e.tensor_scalar(out=B_["Bd"][:], in0=ident, scalar1=beta, scalar2=None, op0=ALU.mult), [K("bg"), "msk"], [K("Bd")])
            PE(lambda e: e.matmul(rF, lhsT=ones, rhs=B_["Bd"][:], start=True, stop=True), [K("Bd"), "msk"], [kF_])
            S(lambda e: e.copy(out=B_["brow"][:], in_=rF), [kF_], [K("brow")])
            G(lambda e: e.tensor_tensor(out=B_["t1"][:], in0=Gs[:], in1=B_["dT"][:], op=ALU.mult), [K("Gs"), K("dT"), K("t1")], [K("t1")])
            G(lambda e: e.tensor_tensor(out=B_["t1"][:], in0=B_["t1"][:], in1=B_["brow"][:], op=ALU.mult), [K("t1"), K("brow")], [K("t1")])
            G(lambda e: e.tensor_tensor(out=B_["XT"][:], in0=B_["t1"][:], in1=nST, op=ALU.mult), [K("t1"), "msk"], [K("XT")])
            G(lambda e: e.tensor_tensor(out=B_["t2"][:], in0=Gs[:], in1=B_["dN"][:], op=ALU.mult), [K("Gs"), K("dN")], [K("t2")])
            G(lambda e: e.tensor_scalar(out=B_["t2"][:], in0=B_["t2"][:], scalar1=beta, scalar2=None, op0=ALU.mult), [K("t2"), K("bg")], [K("t2")])
            G(lambda e: e.tensor_tensor(out=B_["X"][:], in0=B_["t2"][:], in1=nSN, op=ALU.mult), [K("t2"), "msk"], [K("X")])
            G(lambda e: e.tensor_tensor(out=B_["TT"][:], in0=B_["XT"][:], in1=ident, op=ALU.add), [K("XT"), "msk"], [K("TT")])
            if GSTOP == 1: return
            Pc, PTc, kP, kPT = B_["X"], B_["XT"], K("X"), K("XT")
            for lv in range(1, 6):
                Pn, kPn = B_[f"P{lv % 2}"], K(f"P{lv % 2}")
                PE(lambda e, Pc=Pc, PTc=PTc: e.matmul(rA, lhsT=PTc[:], rhs=Pc[:], start=True, stop=True), [kP, kPT], [kA])
                S(lambda e, Pn=Pn: e.copy(out=Pn[:], in_=rA), [kA], [kPn])
                if lv < 5:
                    PTn, kPTn = B_[f"PT{lv % 2}"], K(f"PT{lv % 2}")
                    PE(lambda e, Pc=Pc, PTc=PTc: e.matmul(rB, lhsT=Pc[:], rhs=PTc[:], start=True, stop=True), [kP, kPT], [kB])
                    S(lambda e, PTn=PTn: e.copy(out=PTn[:], in_=rB), [kB], [kPTn])
                PE(lambda e, Pn=Pn: e.matmul(rC, lhsT=Pn[:], rhs=B_["TT"][:], start=True, stop=True), [kPn, K("TT")], [kC])
                V(lambda e: e.tensor_tensor(out=B_["TT"][:], in0=B_["TT"][:], in1=rC, op=ALU.add), [K("TT"), kC], [K("TT")])
                Pc, kP = Pn, kPn
                if lv < 5:
                    PTc, kPT = PTn, kPTn
            if GSTOP == 2: return
            S(lambda e: e.activation(out=B_["eg"][:], in_=B_["gcc"][:], func=AF.Exp), [K("gcc")], [K("eg")])
            G(lambda e: e.tensor_tensor(out=B_["be"][:], in0=B_["eg"][:], in1=beta, op=ALU.mult), [K("eg"), K("bg")], [K("be")])
            G(lambda e: e.tensor_scalar(out=B_["vb"][:], in0=vc, scalar1=beta, scalar2=None, op0=ALU.mult), [K("kqv"), K("bg")], [K("vb")])
            G(lambda e: e.tensor_scalar(out=B_["kbg"][:], in0=kc, scalar1=B_["be"][:, 0:1], scalar2=None, op0=ALU.mult), [K("kqv"), K("be")], [K("kbg")])
            PE(lambda e: e.matmul(rD, lhsT=B_["TT"][:], rhs=B_["vb"][:], start=True, stop=True), [K("TT"), K("vb")], [kD])
            S(lambda e: e.copy(out=B_["u"][:], in_=rD), [kD], [K("u")])
            PE(lambda e: e.matmul(rE, lhsT=B_["kbg"][:], rhs=B_["TT"][:], start=True, stop=True), [K("kbg"), K("TT")], [kE])
            S(lambda e: e.copy(out=B_["wF"][:], in_=rE), [kE], [K("wF")])
            G(lambda e: e.tensor_tensor(out=B_["qdF"][:], in0=qF[:], in1=B_["egrow"][:], op=ALU.mult), [K("qF"), K("egrow")], [K("qdF")])
            G(lambda e: e.tensor_tensor(out=B_["QKm"][:], in0=QKs[:], in1=B_["dT"][:], op=ALU.mult), [K("QKs"), K("dT")], [K("QKm")])
            for hs, lst in [(first, lastF), (second, lastS)]:
                S(lambda e, hs=hs, lst=lst: e.activation(out=B_["dl"][hs, :], in_=B_["gcc"][hs, :], func=AF.Exp, bias=B_["grow"][hs, lst:lst + 1], scale=-1.0), [K("gcc"), K("grow")], [K("dl")])
            G(lambda e: e.tensor_tensor(out=B_["dlA"][:], in0=B_["dl"][:], in1=selA, op=ALU.mult), [K("dl"), "msk"], [K("dlA")])
            G(lambda e: e.tensor_tensor(out=B_["dlB"][:], in0=B_["dl"][:], in1=selB, op=ALU.mult), [K("dl"), "msk"], [K("dlB")])
            G(lambda e: e.tensor_scalar(out=B_["kdA"][:], in0=kc, scalar1=B_["dlA"][:, 0:1], scalar2=None, op0=ALU.mult), [K("kqv"), K("dlA")], [K("kdA")])
            G(lambda e: e.tensor_scalar(out=B_["kdB"][:], in0=kc, scalar1=B_["dlB"][:, 0:1], scalar2=None, op0=ALU.mult), [K("kqv"), K("dlB")], [K("kdB")])
            kdF, kdS, kkF, kkS = (B_["kdA"], B_["kdB"], K("kdA"), K("kdB")) if fwd else (B_["kdB"], B_["kdA"], K("kdB"), K("kdA"))
            if GSTOP == 3: return
            Sst = B_["S"]
            for (hs, lst, kd_, kkd, rW, kW, rO, kO) in [(first, lastF, kdF, kkF, rA, kA, rC, kC), (second, lastS, kdS, kkS, rB, kB, rD, kD)]:
                PE(lambda e, rW=rW: e.matmul(rW, lhsT=B_["wF"][:], rhs=Sst[:], start=True, stop=True), [K("wF"), K("S")], [kW])
                V(lambda e, hs=hs, rW=rW: e.tensor_tensor(out=B_["vnew"][hs, :], in0=B_["u"][hs, :], in1=rW[hs, :], op=ALU.subtract), [K("u"), kW], [K("vnew")])
                PE(lambda e, rO=rO: e.matmul(rO, lhsT=B_["qdF"][:], rhs=Sst[:], start=True, stop=True), [K("qdF"), K("S")], [kO])
                S(lambda e, hs=hs, rO=rO: e.copy(out=B_["o"][hs, :], in_=rO[hs, :]), [kO], [K("o")])
                PE(lambda e, kd_=kd_: e.matmul(rF, lhsT=kd_[:], rhs=B_["vnew"][:], start=True, stop=True), [kkd, K("vnew")], [kF_])
                V(lambda e, lst=lst: e.scalar_tensor_tensor(out=Sst[:], in0=Sst[:], scalar=B_["egrow"][:, lst:lst + 1], in1=rF, op0=ALU.mult, op1=ALU.add), [K("S"), K("egrow"), kF_], [K("S")])
            if GSTOP == 5: return
            PE(lambda e: e.matmul(rE, lhsT=B_["QKm"][:], rhs=B_["vnew"][:], start=True, stop=True), [K("QKm"), K("vnew")], [kE])
            V(lambda e: e.tensor_tensor(out=B_["o"][:], in0=B_["o"][:], in1=rE, op=ALU.add), [K("o"), kE], [K("o")])
            p.dma("gpsimd", gy_s[d, t0:t0 + 128, :], B_["o"][:], reads=[K("o")], writes=["gy_s"])

        def rwkv_block(d, c):
            t0 = c * 128
            B_ = RB[d]; K = lambda n: f"r{d}_{n}"
            bank = PS[4 + d]; kb = f"ps{4 + d}"
            rA, rB, rC, rD = (bank[:, i * 128:(i + 1) * 128] for i in range(4))
            fwd = (d == 0)
            Mtri = bdUT if fwd else bdLT
            mS_N = sbdLT if fwd else sbdUT
            mS_T = sbdUT if fwd else sbdLT
            mI_T = bdUT if fwd else bdLT
            selc = [bdLT[:, 0:1], bdUT[:, 127:128]]
            hA, hB = slice(0, 64), slice(64, 128)
            order = [(0, hA, 63), (1, hB, 127)] if fwd else [(1, hB, 64), (0, hA, 0)]
            rwt, vt = B_["rwt"], B_["vt"]
            for h in range(2):
                p.dma("sync", rwt[:, :, h * 64:(h + 1) * 64], rw_s[d, h, t0:t0 + 128, :, :], reads=["rw_s"], writes=[K("rwt")])
            p.dma("sync", vt[:], rpost_s[t0:t0 + 128, 0:128], reads=["rpost_s"], writes=[K("vt")])
            w_, kk_, nkka_, kd_, r_ = (rwt[:, i, :] for i in range(5))
            S(lambda e: e.activation(out=B_["lw"][:], in_=w_, func=AF.Ln), [K("rwt")], [K("lw")])
            PE(lambda e: e.matmul(rA, lhsT=Mtri, rhs=B_["lw"][:], start=True, stop=True), [K("lw"), "msk"], [kb])
            S(lambda e: e.copy(out=B_["lp"][:], in_=rA), [kb], [K("lp")])
            S(lambda e: e.activation(out=B_["Pt"][:], in_=rA, func=AF.Exp), [kb], [K("Pt")])
            S(lambda e: e.activation(out=B_["iP"][:], in_=rA, func=AF.Exp, scale=-1.0), [kb], [K("iP")])
            G(lambda e: e.tensor_tensor(out=B_["Pm"][:], in0=B_["lp"][:], in1=B_["lw"][:], op=ALU.subtract), [K("lp"), K("lw")], [K("Pm")])
            S(lambda e: e.activation(out=B_["Pm"][:], in_=B_["Pm"][:], func=AF.Exp), [K("Pm")], [K("Pm")])
            G(lambda e: e.tensor_tensor(out=B_["rt"][:], in0=r_, in1=B_["Pt"][:], op=ALU.mult), [K("rwt"), K("Pt")], [K("rt")])
            G(lambda e: e.tensor_tensor(out=B_["kt"][:], in0=kd_, in1=B_["iP"][:], op=ALU.mult), [K("rwt"), K("iP")], [K("kt")])
            G(lambda e: e.tensor_tensor(out=B_["nbt"][:], in0=nkka_, in1=B_["iP"][:], op=ALU.mult), [K("rwt"), K("iP")], [K("nbt")])
            G(lambda e: e.tensor_tensor(out=B_["ct"][:], in0=kk_, in1=B_["Pm"][:], op=ALU.mult), [K("rwt"), K("Pm")], [K("ct")])
            for src, full, hm in [("rt", "rtF", "rtFh"), ("nbt", "nbF", "nbFh"), ("ct", "cF", "cFh"), ("kt", None, "ktFh"), ("Pt", "PF", None)]:
                PE(lambda e, src=src: e.transpose(out=rB, in_=B_[src][:], identity=ident), [K(src), "msk"], [kb])
                if full is not None:
                    S(lambda e, full=full: e.copy(out=B_[full][:], in_=rB), [kb], [K(full)])
                if hm is not None:
                    for h, hs in [(0, hA), (1, hB)]:
                        S(lambda e, hm=hm, h=h, hs=hs: e.copy(out=B_[f"{hm}{h}"][hs, :], in_=rB[hs, :]), [kb], [K(f"{hm}{h}")])
            for ch in range(2):
                for h in range(2):
                    cs = slice(h * 64, (h + 1) * 64)
                    G(lambda e, ch=ch, h=h, cs=cs: e.tensor_scalar(out=B_[f"ktS{ch}{h}"][:, cs], in0=B_["kt"][:, cs], scalar1=selc[ch], scalar2=None, op0=ALU.mult), [K("kt"), "msk"], [K(f"ktS{ch}{h}")])
                    G(lambda e, ch=ch, h=h, cs=cs: e.tensor_scalar(out=B_[f"nbS{ch}{h}"][:, cs], in0=B_["nbt"][:, cs], scalar1=selc[ch], scalar2=None, op0=ALU.mult), [K("nbt"), "msk"], [K(f"nbS{ch}{h}")])
            def head(h):
                hr = hA if h == 0 else hB
                cFh, nbFh, ktFh, rtFh = (B_[f"{n}{h}"] for n in ["cFh", "nbFh", "ktFh", "rtFh"])
                kcF, knbF, kktF, krtF = (K(f"{n}{h}") for n in ["cFh", "nbFh", "ktFh", "rtFh"])
                for (nm, l_, kl, r__, kr, mk) in [("X", cFh, kcF, "nbF", K("nbF"), mS_N), ("XT", nbFh, knbF, "cF", K("cF"), mS_T), ("AckT", ktFh, kktF, "cF", K("cF"), mS_T),
                                                   ("ArkT", ktFh, kktF, "rtF", K("rtF"), mI_T), ("AnrbT", nbFh, knbF, "rtF", K("rtF"), mI_T)]:
                    PE(lambda e, l_=l_, r__=r__: e.matmul(rC, lhsT=l_[:], rhs=B_[r__][:], start=True, stop=True), [kl, kr], [kb])
                    V(lambda e, nm=nm, mk=mk: e.tensor_tensor(out=B_[nm][:], in0=rC, in1=mk, op=ALU.mult), [kb, "msk"], [K(nm)])
                G(lambda e: e.tensor_tensor(out=B_["TT"][:], in0=B_["XT"][:], in1=ident, op=ALU.add), [K("XT"), "msk"], [K("TT")])
                Pc, PTc, kP, kPT = B_["X"], B_["XT"], K("X"), K("XT")
                for lv in range(1, 6):
                    Pn, kPn = B_[f"P{lv % 2}"], K(f"P{lv % 2}")
                    PE(lambda e, Pc=Pc, PTc=PTc: e.matmul(rA, lhsT=PTc[:], rhs=Pc[:], start=True, stop=True), [kP, kPT], [kb])
                    S(lambda e, Pn=Pn: e.copy(out=Pn[:], in_=rA), [kb], [kPn])
                    if lv < 5:
                        PTn, kPTn = B_[f"PT{lv % 2}"], K(f"PT{lv % 2}")
                        PE(lambda e, Pc=Pc, PTc=PTc: e.matmul(rB, lhsT=Pc[:], rhs=PTc[:], start=True, stop=True), [kP, kPT], [kb])
                        S(lambda e, PTn=PTn: e.copy(out=PTn[:], in_=rB), [kb], [kPTn])
                    PE(lambda e, Pn=Pn: e.matmul(rC, lhsT=Pn[:], rhs=B_["TT"][:], start=True, stop=True), [kPn, K("TT")], [kb])
                    V(lambda e: e.tensor_tensor(out=B_["TT"][:], in0=B_["TT"][:], in1=rC, op=ALU.add), [K("TT"), kb], [K("TT")])
                    Pc, kP = Pn, kPn
                    if lv < 5:
                        PTc, kPT = PTn, kPTn
                Vh = vt[:, hr]
                H = B_["H"]
                for (ch, hs, lst) in order:
                    PE(lambda e: e.matmul(rD[:, 0:64], lhsT=cFh[:], rhs=H[:], start=True, stop=False), [kcF, K("H")], [kb])
                    PE(lambda e: e.matmul(rD[:, 0:64], lhsT=B_["AckT"][:], rhs=Vh, start=False, stop=True), [K("AckT"), K("vt")], [kb])
                    S(lambda e, hs=hs: e.copy(out=B_["Zs"][hs, :], in_=rD[hs, 0:64]), [kb], [K("Zs")])
                    PE(lambda e: e.matmul(rD[:, 64:128], lhsT=B_["TT"][:], rhs=B_["Zs"][:], start=True, stop=True), [K("TT"), K("Zs")], [kb])
                    S(lambda e, hs=hs: e.copy(out=B_["Ms"][hs, :], in_=rD[hs, 64:128]), [kb], [K("Ms")])
                    PE(lambda e: e.matmul(rA[:, 0:64], lhsT=rtFh[:], rhs=H[:], start=True, stop=True), [krtF, K("H")], [kb])
                    S(lambda e, hs=hs, hr=hr: e.copy(out=B_["Yt"][hs, hr], in_=rA[hs, 0:64]), [kb], [K("Yt")])
                    PE(lambda e, ch=ch, h=h: e.matmul(rB[:, 0:64], lhsT=B_[f"ktS{ch}{h}"][:], rhs=Vh, start=True, stop=False), [K(f"ktS{ch}{h}"), K("vt")], [kb])
                    PE(lambda e, ch=ch, h=h: e.matmul(rB[:, 0:64], lhsT=B_[f"nbS{ch}{h}"][:], rhs=B_["Ms"][:], start=False, stop=True), [K(f"nbS{ch}{h}"), K("Ms")], [kb])
                    V(lambda e, hr=hr, lst=lst: e.tensor_scalar(out=H[hr, :], in0=H[hr, :], scalar1=B_["PF"][hr, lst:lst + 1], scalar2=None, op0=ALU.mult), [K("H"), K("PF")], [K("H")])
                    V(lambda e, hr=hr, lst=lst: e.scalar_tensor_tensor(out=H[hr, :], in0=rB[hr, 0:64], scalar=B_["PF"][hr, lst:lst + 1], in1=H[hr, :], op0=ALU.mult, op1=ALU.add), [K("H"), K("PF"), kb], [K("H")])
                PE(lambda e: e.matmul(rC[:, 0:64], lhsT=B_["ArkT"][:], rhs=Vh, start=True, stop=False), [K("ArkT"), K("vt")], [kb])
                PE(lambda e: e.matmul(rC[:, 0:64], lhsT=B_["AnrbT"][:], rhs=B_["Ms"][:], start=False, stop=True), [K("AnrbT"), K("Ms")], [kb])
                V(lambda e, hr=hr: e.tensor_tensor(out=B_["Yt"][:, hr], in0=B_["Yt"][:, hr], in1=rC[:, 0:64], op=ALU.add), [K("Yt"), kb], [K("Yt")])
            for h in range(2):
                head(h)
            p.dma("gpsimd", ry_s[d, t0:t0 + 128, :], B_["Yt"][:], reads=[K("Yt")], writes=["ry_s"])

        def ssd_chunk(d, c):
            t0 = c * 128
            Mk = UT if d == 0 else LT
            last = 127 if d == 0 else 0
            p.dma("gpsimd", sxt[:], ssd_s[t0:t0 + 128, :], reads=["ssd_s"], writes=["sxt"])
            if d == 1:
                p.dma("gpsimd", syf[:], sy_s[t0:t0 + 128, :], reads=["sy_s"], writes=["syf"])
                p.dma("gpsimd", zt[:], cols_s[2 + t0:2 + t0 + 128, 1280:1536], reads=["cols_s"], writes=["zts"])
            sx = sxt[:, 0:256]; sB = sxt[:, 256:384]; sC = sxt[:, 384:512]
            dt_d = sxt[:, 512 + d * 4:516 + d * 4]; a_d = sxt[:, 520 + d * 4:524 + d * 4]
            PE(lambda e: e.transpose(out=pA[:, 0:128], in_=sB, identity=ident), ["sxt", "msk"], ["ps0"])
            PE(lambda e: e.transpose(out=pA[:, 128:256], in_=sC, identity=ident), ["sxt", "msk"], ["ps0"])
            S(lambda e: e.copy(out=BCF[:], in_=pA[:, 0:256]), ["ps0"], ["BCF"])
            if SSDSTOP == 1: return
            PE(lambda e: e.matmul(pSc[:, :128], lhsT=BCF[:, 0:128], rhs=BCF[:, 128:256], start=True, stop=True), ["BCF"], ["ps0"])
            S(lambda e: e.copy(out=scT[:], in_=pSc[:, :128]), ["ps0"], ["scT"])
            PE(lambda e: e.matmul(pAc[:, :4], lhsT=Mk, rhs=a_d, start=True, stop=True), ["sxt", "msk"], ["ps0"])
            S(lambda e: e.copy(out=acol[:], in_=pAc[:, :4]), ["ps0"], ["acol"])
            S(lambda e: e.mul(out=nacol[:], in_=pAc[:, :4], mul=-1.0), ["ps0"], ["nacol"])
            if SSDSTOP == 2: return
            for h in range(4):
                (V if os.environ.get('A_V1') else G)(lambda e, h=h: e.tensor_scalar(out=Rm[:, h, :], in0=Mk, scalar1=a_d[:, h:h + 1], scalar2=None, op0=ALU.mult), ["sxt", "msk"], ["Rm"])
            PE(lambda e: e.matmul(pRow[:, :512], lhsT=ones, rhs=Rm[:].rearrange("p h n -> p (h n)"), start=True, stop=True), ["Rm", "msk"], ["ps1"])
            S(lambda e: e.activation(out=E[:].rearrange("p h n -> p (h n)"), in_=pRow[:, :512], func=AF.Exp), ["ps1"], ["E"])
            for h in range(4):
                S(lambda e, h=h: e.activation(out=dend[:, h:h + 1], in_=pRow[:, h * 128 + last:h * 128 + last + 1], func=AF.Exp, bias=nacol[:, h:h + 1], scale=1.0), ["ps1", "nacol"], ["dend"])
            if SSDSTOP == 3: return
            for h in range(4):
                hs = slice(h * 64, (h + 1) * 64)
                S(lambda e, h=h: e.activation(out=Dm[:], in_=pRow[:, h * 128:(h + 1) * 128], func=AF.Identity, bias=nacol[:, h:h + 1], scale=1.0), ["ps1", "nacol"], ["Dm"])
                G(lambda e: e.tensor_scalar(out=Dm[:], in0=Dm[:], scalar1=0.0, scalar2=None, op0=ALU.min), ["Dm"], ["Dm"])
                S(lambda e: e.activation(out=Dm[:], in_=Dm[:], func=AF.Exp), ["Dm"], ["Dm"])
                G(lambda e: e.tensor_tensor(out=Dm[:], in0=Dm[:], in1=Mk, op=ALU.mult), ["Dm", "msk"], ["Dm"])
                G(lambda e, h=h: e.tensor_tensor(out=M[:, h, :], in0=Dm[:], in1=scT[:], op=ALU.mult), ["Dm", "scT"], ["M"])
                G(lambda e, h=h: e.tensor_tensor(out=CdF[:, h, :], in0=E[:, h, :], in1=BCF[:, 128:256], op=ALU.mult), ["E", "BCF"], ["CdF"])
                G(lambda e, h=h, hs=hs: e.tensor_scalar(out=xdt[:, hs], in0=sx[:, hs], scalar1=dt_d[:, h:h + 1], scalar2=None, op0=ALU.mult), ["sxt"], ["xdt"])
                G(lambda e, h=h, hs=hs: e.tensor_scalar(out=xend[:, hs], in0=xdt[:, hs], scalar1=dend[:, h:h + 1], scalar2=None, op0=ALU.mult), ["xdt", "dend"], ["xend"])
            if SSDSTOP == 4: return
            for h in range(4):
                hs = slice(h * 64, (h + 1) * 64)
                PE(lambda e, h=h, hs=hs: e.matmul(pY[:, hs], lhsT=M[:, h, :], rhs=xdt[:, hs], start=True, stop=False), ["M", "xdt"], ["ps2"])
                PE(lambda e, h=h, hs=hs: e.matmul(pY[:, hs], lhsT=CdF[:, h, :], rhs=H[:, hs], start=False, stop=True), ["CdF", "H"], ["ps2"])
            PE(lambda e: e.matmul(pSt[:, :256], lhsT=sB, rhs=xend[:], start=True, stop=True), ["sxt", "xend"], ["ps2"])
            if SSDSTOP == 5: return
            for h in range(4):
                hs = slice(h * 64, (h + 1) * 64)
                V(lambda e, h=h, hs=hs: e.scalar_tensor_tensor(out=H[:, hs], in0=H[:, hs], scalar=E[:, h, last:last + 1], in1=pSt[:, hs], op0=ALU.mult, op1=ALU.add), ["H", "E", "ps2"], ["H"])
            if SSDSTOP == 6: return
            if d == 0:
                S(lambda e: e.copy(out=yt[:], in_=pY[:, :256]), ["ps2"], ["yt"])
                p.dma("gpsimd", sy_s[t0:t0 + 128, :], yt[:], reads=["yt"], writes=["sy_s"])
            else:
                V(lambda e: e.tensor_tensor(out=yt[:], in0=pY[:, :256], in1=syf[:], op=ALU.add), ["ps2", "syf"], ["yt"])
                G(lambda e: e.tensor_tensor(out=xd[:], in0=sx, in1=rv("dsk"), op=ALU.mult), ["sxt", "rvs"], ["xd"])
                G(lambda e: e.tensor_tensor(out=yt[:], in0=yt[:], in1=xd[:], op=ALU.add), ["yt", "xd"], ["yt"])
                S(lambda e: e.activation(out=szs[:], in_=zt[:], func=AF.Silu), ["zts"], ["szs"])
                G(lambda e: e.tensor_tensor(out=yt[:], in0=yt[:], in1=szs[:], op=ALU.mult), ["yt", "szs"], ["yt"])
                p.dma("gpsimd", yc_o[t0:t0 + 128, :], yt[:], reads=["yt"], writes=["yc"])

        ssd_list = [(0, c) for c in range(NTL)] + [(1, c) for c in range(NTL - 1, -1, -1)]
        V(lambda e: e.memset(H[:], 0.0), [], ["H"])
        def ssd_pair(it):
            for si in (2 * it, 2 * it + 1):
                dd, cc = ssd_list[si]
                if dd == 1 and cc == NTL - 1:
                    V(lambda e: e.memset(H[:], 0.0), [], ["H"])
                ssd_chunk(dd, cc)
        for it in range(NTL):
            chains = []
            if not os.environ.get("A_NOGDN"):
                chains += [p.capture(gdn_chunk, 0, it), p.capture(gdn_chunk, 1, NTL - 1 - it)]
            if not os.environ.get("A_NORWKV"):
                chains += [p.capture(rwkv_block, 0, it), p.capture(rwkv_block, 1, NTL - 1 - it)]
            if not os.environ.get("A_NOSSD"):
                chains += [p.capture(ssd_pair, it)]
            p.emit_interleaved(chains)
    p.barrier()


def phase4(p, T, L):
    nc = p.nc
    V = L["V"]; G = L["G"]; S = L["S"]; PE = L["PE"]; PS = L["PS"]; ident = L["ident"]; rv = L["rv"]
    gy_s, ry_s, cols_s, rpost_s, ya_o, yb_o = (L[k] for k in ["gy_s", "ry_s", "cols_s", "rpost_s", "ya_o", "yb_o"])
    NTL = T // 128
    with contextlib.ExitStack() as sc:
        sb = lambda name, shape, dt=F32: sc.enter_context(nc.sbuf_tensor("s_" + name, list(shape), dt))
        g0 = sb("g0", [128, 128]); g1 = sb("g1", [128, 128]); o = sb("o", [128, 128]); jk = sb("jk", [128, 128]); ss = sb("ss", [128, 1])
        zt = sb("zt4", [128, 128]); ya = sb("ya_t", [128, 128])
        r0 = sb("r0", [128, 128]); r1 = sb("r1", [128, 128]); y = sb("y4", [128, 128]); st = sb("st4", [128, 2]); sq = sb("sq4", [128, 128]); vr = sb("vr4", [128, 2])
        rp = sb("rp4", [128, 258]); yb = sb("yb_t", [128, 128])
        pT, pU = PS[0], PS[1]
        for tt in range(NTL):
            t0 = tt * 128
            p.dma("sync", g0[:], gy_s[0, t0:t0 + 128, :], reads=["gy_s"], writes=["g0"])
            p.dma("sync", g1[:], gy_s[1, t0:t0 + 128, :], reads=["gy_s"], writes=["g1"])
            p.dma("scalar", zt[:], cols_s[2 + t0:2 + t0 + 128, 384:512], reads=["cols_s"], writes=["zt4"])
            p.dma("sync", r0[:], ry_s[0, t0:t0 + 128, :], reads=["ry_s"], writes=["r0"])
            p.dma("sync", r1[:], ry_s[1, t0:t0 + 128, :], reads=["ry_s"], writes=["r1"])
            p.dma("scalar", rp[:], rpost_s[t0:t0 + 128, :], reads=["rpost_s"], writes=["rp4"])
            V(lambda e: e.tensor_tensor(out=o[:], in0=g0[:], in1=g1[:], op=ALU.add), ["g0", "g1"], ["o"])
            V(lambda e: e.scalar_tensor_tensor(out=jk[:], in0=o[:], scalar=1.0, in1=o[:], op0=ALU.mult, op1=ALU.mult, accum_out=ss[:, 0:1]), ["o"], ["jk", "ss"])
            V(lambda e: e.tensor_scalar(out=ss[:], in0=ss[:], scalar1=1.0 / 128, scalar2=1e-6, op0=ALU.mult, op1=ALU.add), ["ss"], ["ss"])
            S(lambda e: e.activation(out=ss[:], in_=ss[:], func=AF.Sqrt), ["ss"], ["ss"])
            V(lambda e: e.reciprocal(out=ss[:], in_=ss[:]), ["ss"], ["ss"])
            V(lambda e: e.scalar_tensor_tensor(out=o[:], in0=o[:], scalar=ss[:, 0:1], in1=rv("gnorm"), op0=ALU.mult, op1=ALU.mult), ["o", "ss", "rvs"], ["o"])
            S(lambda e: e.activation(out=zt[:], in_=zt[:], func=AF.Silu), ["zt4"], ["zt4"])
            V(lambda e: e.tensor_tensor(out=ya[:], in0=o[:], in1=zt[:], op=ALU.mult), ["o", "zt4"], ["ya_t"])
            p.dma("gpsimd", ya_o[t0:t0 + 128, :], ya[:], reads=["ya_t"], writes=["ya"])
            V(lambda e: e.tensor_tensor(out=y[:], in0=r0[:], in1=r1[:], op=ALU.add), ["r0", "r1"], ["y4"])
            V(lambda e: e.tensor_reduce(out=st[:], in_=y[:].rearrange("p (h n) -> p h n", h=2), axis=AX.X, op=ALU.add), ["y4"], ["st4"])
            V(lambda e: e.tensor_scalar(out=st[:], in0=st[:], scalar1=1.0 / 64, scalar2=None, op0=ALU.mult), ["st4"], ["st4"])
            for h in range(2):
                hs = slice(h * 64, (h + 1) * 64)
                V(lambda e, h=h, hs=hs: e.tensor_scalar(out=y[:, hs], in0=y[:, hs], scalar1=st[:, h:h + 1], scalar2=None, op0=ALU.subtract), ["y4", "st4"], ["y4"])
            S(lambda e: e.activation(out=sq[:], in_=y[:], func=AF.Square), ["y4"], ["sq4"])
            V(lambda e: e.tensor_reduce(out=vr[:], in_=sq[:].rearrange("p (h n) -> p h n", h=2), axis=AX.X, op=ALU.add), ["sq4"], ["vr4"])
            V(lambda e: e.tensor_scalar(out=vr[:], in0=vr[:], scalar1=1.0 / 64, scalar2=64e-5, op0=ALU.mult, op1=ALU.add), ["vr4"], ["vr4"])
            S(lambda e: e.activation(out=vr[:], in_=vr[:], func=AF.Sqrt), ["vr4"], ["vr4"])
            V(lambda e: e.reciprocal(out=vr[:], in_=vr[:]), ["vr4"], ["vr4"])
            for h in range(2):
                hs = slice(h * 64, (h + 1) * 64)
                V(lambda e, h=h, hs=hs: e.tensor_scalar(out=y[:, hs], in0=y[:, hs], scalar1=vr[:, h:h + 1], scalar2=None, op0=ALU.mult), ["y4", "vr4"], ["y4"])
            V(lambda e: e.tensor_tensor(out=y[:], in0=y[:], in1=rv("lng"), op=ALU.mult), ["y4", "rvs"], ["y4"])
            V(lambda e: e.tensor_tensor(out=y[:], in0=y[:], in1=rv("lnb"), op=ALU.add), ["y4", "rvs"], ["y4"])
            for h in range(2):
                hs = slice(h * 64, (h + 1) * 64)
                V(lambda e, h=h, hs=hs: e.scalar_tensor_tensor(out=y[:, hs], in0=rp[:, hs], scalar=rp[:, 256 + h:257 + h], in1=y[:, hs], op0=ALU.mult, op1=ALU.add), ["y4", "rp4"], ["y4"])
            V(lambda e: e.tensor_tensor(out=yb[:], in0=y[:], in1=rp[:, 128:256], op=ALU.mult), ["y4", "rp4"], ["yb_t"])
            p.dma("gpsimd", yb_o[t0:t0 + 128, :], yb[:], reads=["yb_t"], writes=["yb"])


ALPHA = 4 ** 0.25
NTOK = 2048
T1 = 256
T2 = 512
NE = 32


def ln_fm(p, h, hk, nch, NT, gcol, bcol, ones_mean, sq, pS, pQ, tmp, kS, kQ, eps=1e-5):
    for oc in range(nch):
        p.op("tensor", lambda e, oc=oc: e.matmul(pS[:, :NT], lhsT=ones_mean[:], rhs=h[:, oc, :], start=(oc == 0), stop=(oc == nch - 1)),
             reads=[hk, "ones_mean"], writes=[kS])
    for oc in range(nch):
        s = oc % 2
        p.op("scalar", lambda e, oc=oc, s=s: e.activation(out=sq[:, s, :], in_=h[:, oc, :], func=AF.Square), reads=[hk], writes=[f"sq{s}"])
        p.op("tensor", lambda e, oc=oc, s=s: e.matmul(pQ[:, :NT], lhsT=ones_mean[:], rhs=sq[:, s, :], start=(oc == 0), stop=(oc == nch - 1)),
             reads=[f"sq{s}", "ones_mean"], writes=[kQ])
    mean, rstd, t = tmp["mean"], tmp["rstd"], tmp["t"]
    p.op("scalar", lambda e: e.copy(out=mean[:], in_=pS[:, :NT]), reads=[kS], writes=["ln_mean"])
    p.op("vector", lambda e: e.tensor_tensor(out=t[:], in0=mean[:], in1=mean[:], op=ALU.mult), reads=["ln_mean"], writes=["ln_t"])
    p.op("vector", lambda e: e.tensor_tensor(out=t[:], in0=pQ[:, :NT], in1=t[:], op=ALU.subtract), reads=[kQ, "ln_t"], writes=["ln_t"])
    p.op("vector", lambda e: e.tensor_scalar(out=t[:], in0=t[:], scalar1=eps, scalar2=None, op0=ALU.add), reads=["ln_t"], writes=["ln_t"])
    p.op("scalar", lambda e: e.activation(out=t[:], in_=t[:], func=AF.Sqrt), reads=["ln_t"], writes=["ln_t"])
    p.op("vector", lambda e: e.reciprocal(out=rstd[:], in_=t[:]), reads=["ln_t"], writes=["ln_rstd"])
    for oc in range(nch):
        p.op("vector", lambda e, oc=oc: e.tensor_tensor(out=h[:, oc, :], in0=h[:, oc, :], in1=mean[:], op=ALU.subtract), reads=[hk, "ln_mean"], writes=[hk])
        p.op("vector", lambda e, oc=oc: e.tensor_tensor(out=h[:, oc, :], in0=h[:, oc, :], in1=rstd[:], op=ALU.mult), reads=[hk, "ln_rstd"], writes=[hk])
        p.op("scalar", lambda e, oc=oc: e.activation(out=h[:, oc, :], in_=h[:, oc, :], func=AF.Identity, scale=gcol(oc), bias=bcol(oc)),
             reads=[hk, "vec"], writes=[hk])


def build_B(ntok=NTOK, ne=NE, dump=False):
    p = Prog(); nc = p.nc
    D = 1024
    xT = p.dram("xT", [D, ntok]); yT = p.dram("yT", [2048, ntok]); pT = p.dram("pT", [256, ntok])
    wg = p.dram("wg", [D, 3072]); wb = p.dram("wb", [2048, D]); wo = p.dram("wo", [D, D]); wplg = p.dram("wplg", [D, D])
    wpl = p.dram("wpl", [256, D]); wr = p.dram("wr", [D, 32]); br = p.dram("br", [1, 32])
    wgu = p.dram("wgu", [32, D, 2048]); wd = p.dram("wd", [32, D, D])
    bgu = p.dram("bgu", [128, 32, 16]); bd = p.dram("bd", [32, D]); vec = p.dram("vec", [128, 5, 8]); ident_d = p.dram("ident", [128, 128])
    outT = p.dram("outT", [D, ntok], kind="ExternalOutput")
    x1bf_s = p.dram("x1bf_s", [128, 8, ntok], BF16, kind="Internal")
    acc_s = p.dram("acc_s", [128, 8, ntok], F32, kind="Internal")
    if dump:
        x1_d = p.dram("x1_d", [D, ntok], kind="ExternalOutput")
        gate_d = p.dram("gate_d", [32, ntok], kind="ExternalOutput")

    ident = p.sb("ident", [128, 128]); ones_mean = p.sb("ones_mean", [128, 128]); vecs = p.sb("vecs", [128, 5, 8])
    gateT = p.sb("gateT", [32, ntok]); ones_row = p.sb("ones_row", [1, 128]); brow = p.sb("brow", [1, 32])
    PS = [p.ps(f"ps{i}", [128, 512]) for i in range(8)]
    p.dma("sync", ident[:], ident_d[:, :], writes=["ident"])
    p.dma("sync", vecs[:], vec[:, :, :], writes=["vec"])
    p.dma("sync", brow[:], br[:, :], writes=["brow"])
    p.op("vector", lambda e: e.memset(ones_mean[:], 1.0 / 1024), writes=["ones_mean"])
    p.op("vector", lambda e: e.memset(ones_row[:], 1.0), writes=["ones_row"])

    with contextlib.ExitStack() as sc:
        def sb(name, shape, dt=F32):
            return sc.enter_context(nc.sbuf_tensor("s_" + name, list(shape), dt))
        wg_bf = sb("wg_bf", [128, 8, 3072], BF16); wb_bf = sb("wb_bf", [128, 16, 1024], BF16)
        wo_bf = sb("wo_bf", [128, 8, 1024], BF16); wplg_bf = sb("wplg_bf", [128, 8, 1024], BF16); wpl_bf = sb("wpl_bf", [128, 2, 1024], BF16)
        wr_sb = sb("wr_sb", [128, 8, 32]); ones512 = sb("ones512", [128, 128])
        for kc in range(8):
            p.dma("gpsimd", wg_bf[:, kc, :], wg[kc * 128:(kc + 1) * 128, :], writes=["wg_bf"])
            p.dma("gpsimd", wo_bf[:, kc, :], wo[kc * 128:(kc + 1) * 128, :], writes=["wo_bf"])
            p.dma("gpsimd", wplg_bf[:, kc, :], wplg[kc * 128:(kc + 1) * 128, :], writes=["wplg_bf"])
            p.dma("sync", wr_sb[:, kc, :], wr[kc * 128:(kc + 1) * 128, :], writes=["wr_sb"])
        for kc in range(16):
            p.dma("gpsimd", wb_bf[:, kc, :], wb[kc * 128:(kc + 1) * 128, :], writes=["wb_bf"])
        for kc in range(2):
            p.dma("gpsimd", wpl_bf[:, kc, :], wpl[kc * 128:(kc + 1) * 128, :], writes=["wpl_bf"])
        p.op("vector", lambda e: e.memset(ones512[:], 1.0 / 512), writes=["ones512"])
        xf = sb("xf", [128, 8, T1]); x_bf = sb("x_bf", [128, 8, T1], BF16); uf = sb("uf", [128, 8, T1])
        y_bf = sb("y_bf", [128, 16, T1], BF16); m_bf = sb("m_bf", [128, 8, T1], BF16); sq = sb("sq", [128, 2, T1])
        x1b = sb("x1b", [128, 8, T1], BF16); accb = sb("accb", [128, 8, T1])
        pf = sb("pf", [128, 2, T1], BF16)
        tm = {k: sb("tm_" + k, [128, T1]) for k in ["mean", "rstd", "t", "g", "mf", "t2", "rs"]}
        lg = sb("lg", [128, 32]); top8 = sb("top8", [128, 8]); nmx = sb("nmx", [128, 1]); msk = sb("msk", [128, 32])
        ex = sb("ex", [128, 32]); ssum = sb("ssum", [128, 1]); gt = sb("gt", [128, 32])
        pG, pB, pH, pS, pQ, pR, pT_, pP = PS
        xTv = xT.rearrange("(kc p) n -> p kc n", p=128); yTv = yT.rearrange("(kc p) n -> p kc n", p=128)
        pTv = pT.rearrange("(kc p) n -> p kc n", p=128)
        for t in range(ntok // T1):
            o = t * T1
            p.dma("sync", xf[:], xTv[:, :, o:o + T1], writes=["xf"])
            p.dma("gpsimd", x_bf[:], xTv[:, :, o:o + T1], writes=["x_bf"])
            p.dma("gpsimd", y_bf[:, 0:8, :], yTv[:, 0:8, o:o + T1], writes=["y_bf_a"])
            p.dma("sync", uf[:], yTv[:, 8:16, o:o + T1], writes=["uf"])
            p.dma("gpsimd", pf[:], pTv[:, :, o:o + T1], writes=["pf"])
            for g in range(2):
                for c in range(4):
                    cc = g * 4 + c; s = cc % 2
                    p.op("scalar", lambda e, cc=cc, s=s: e.activation(out=sq[:, s, :], in_=uf[:, cc, :], func=AF.Square), reads=["uf"], writes=[f"sq{s}"])
                    p.op("tensor", lambda e, c=c, s=s: e.matmul(pS[:, :T1], lhsT=ones512[:], rhs=sq[:, s, :], start=(c == 0), stop=(c == 3)),
                         reads=[f"sq{s}", "ones512"], writes=["pS"])
                rs = tm["rs"]
                p.op("vector", lambda e: e.tensor_scalar(out=rs[:], in0=pS[:, :T1], scalar1=1e-5, scalar2=None, op0=ALU.add), reads=["pS"], writes=["rs"])
                p.op("scalar", lambda e: e.activation(out=rs[:], in_=rs[:], func=AF.Sqrt), reads=["rs"], writes=["rs"])
                p.op("vector", lambda e: e.reciprocal(out=rs[:], in_=rs[:]), reads=["rs"], writes=["rs"])
                for c in range(4):
                    cc = g * 4 + c
                    p.op("vector", lambda e, cc=cc: e.scalar_tensor_tensor(out=y_bf[:, 8 + cc, :], in0=uf[:, cc, :], scalar=vecs[:, 4, cc:cc + 1], in1=rs[:], op0=ALU.mult, op1=ALU.mult),
                         reads=["uf", "rs", "vec"], writes=["y_bf_u"])
            brk = [(0, 4), (4, 8), (8, 16)]
            for oc in range(8):
                for b in range(3):
                    c0 = b * 1024 + oc * 128
                    for kc in range(8):
                        p.op("tensor", lambda e, kc=kc, c0=c0: e.matmul(pG[:, :T1], lhsT=wg_bf[:, kc, c0:c0 + 128], rhs=x_bf[:, kc, :], start=(kc == 0), stop=(kc == 7)),
                             reads=["wg_bf", "x_bf"], writes=["pG"])
                    p.op("scalar", lambda e: e.activation(out=tm["g"][:], in_=pG[:, :T1], func=AF.Sigmoid), reads=["pG"], writes=["tm_g"])
                    k0, k1 = brk[b]
                    for kc in range(k0, k1):
                        p.op("tensor", lambda e, kc=kc, k0=k0, k1=k1: e.matmul(pB[:, :T1], lhsT=wb_bf[:, kc, oc * 128:(oc + 1) * 128], rhs=y_bf[:, kc, :], start=(kc == k0), stop=(kc == k1 - 1)),
                             reads=["wb_bf", "y_bf_a", "y_bf_u"], writes=["pB"])
                    if b == 0:
                        p.op("vector", lambda e: e.tensor_tensor(out=tm["mf"][:], in0=tm["g"][:], in1=pB[:, :T1], op=ALU.mult), reads=["tm_g", "pB"], writes=["tm_mf"])
                    else:
                        p.op("vector", lambda e: e.tensor_tensor(out=tm["t2"][:], in0=tm["g"][:], in1=pB[:, :T1], op=ALU.mult), reads=["tm_g", "pB"], writes=["tm_t2"])
                        if b == 1:
                            p.op("vector", lambda e: e.tensor_tensor(out=tm["mf"][:], in0=tm["mf"][:], in1=tm["t2"][:], op=ALU.add), reads=["tm_mf", "tm_t2"], writes=["tm_mf"])
                        else:
                            p.op("vector", lambda e, oc=oc: e.tensor_tensor(out=m_bf[:, oc, :], in0=tm["mf"][:], in1=tm["t2"][:], op=ALU.add), reads=["tm_mf", "tm_t2"], writes=["m_bf"])
            for oc in range(8):
                for kc in range(8):
                    p.op("tensor", lambda e, kc=kc, oc=oc: e.matmul(pH[:, :T1], lhsT=wo_bf[:, kc, oc * 128:(oc + 1) * 128], rhs=m_bf[:, kc, :], start=(kc == 0), stop=(kc == 7)),
                         reads=["wo_bf", "m_bf"], writes=["pH"])
                p.op("vector", lambda e, oc=oc: e.scalar_tensor_tensor(out=xf[:, oc, :], in0=xf[:, oc, :], scalar=ALPHA, in1=pH[:, :T1], op0=ALU.mult, op1=ALU.add),
                     reads=["xf", "pH"], writes=["xf"])
            ln_fm(p, xf, "xf", 8, T1, lambda oc: vecs[:, 0, oc:oc + 1], lambda oc: vecs[:, 1, oc:oc + 1], ones_mean, sq, pS, pQ, tm, "pS", "pQ")
            p.op("scalar", lambda e: e.copy(out=x1b[:], in_=xf[:]), reads=["xf"], writes=["x1b"])
            p.dma("sync", x1bf_s[:, :, o:o + T1], x1b[:], reads=["x1b"], writes=["x1bf_s"])
            if dump:
                p.dma("sync", x1_d.rearrange("(kc p) n -> p kc n", p=128)[:, :, o:o + T1], xf[:], reads=["xf"], writes=["x1_d"])
            for s in range(T1 // 128):
                for kc in range(8):
                    p.op("tensor", lambda e, kc=kc, s=s: e.matmul(pR[:, :32], lhsT=xf[:, kc, s * 128:(s + 1) * 128], rhs=wr_sb[:, kc, :], start=(kc == 0), stop=False),
                         reads=["xf", "wr_sb"], writes=["pR"])
                p.op("tensor", lambda e: e.matmul(pR[:, :32], lhsT=ones_row[:, :], rhs=brow[:, :], start=False, stop=True), reads=["ones_row", "brow"], writes=["pR"])
                p.op("vector", lambda e: e.tensor_copy(out=lg[:], in_=pR[:, :32]), reads=["pR"], writes=["lg"])
                p.op("vector", lambda e: e.max(out=top8[:], in_=lg[:]), reads=["lg"], writes=["top8"])
                p.op("vector", lambda e: e.tensor_scalar(out=nmx[:], in0=top8[:, 0:1], scalar1=-1.0, scalar2=None, op0=ALU.mult), reads=["top8"], writes=["nmx"])
                p.op("vector", lambda e: e.tensor_scalar(out=msk[:], in0=lg[:], scalar1=top8[:, 3:4], scalar2=None, op0=ALU.is_ge), reads=["lg", "top8"], writes=["msk"])
                p.op("scalar", lambda e: e.activation(out=ex[:], in_=lg[:], func=AF.Exp, bias=nmx[:, 0:1], scale=1.0), reads=["lg", "nmx"], writes=["ex"])
                p.op("vector", lambda e: e.scalar_tensor_tensor(out=ex[:], in0=ex[:], scalar=1.0, in1=msk[:], op0=ALU.mult, op1=ALU.mult, accum_out=ssum[:, 0:1]),
                     reads=["ex", "msk"], writes=["ex", "ssum"])
                p.op("vector", lambda e: e.reciprocal(out=ssum[:], in_=ssum[:]), reads=["ssum"], writes=["ssum"])
                p.op("vector", lambda e: e.tensor_scalar(out=gt[:], in0=ex[:], scalar1=ssum[:, 0:1], scalar2=None, op0=ALU.mult), reads=["ex", "ssum"], writes=["gt"])
                p.op("tensor", lambda e: e.transpose(out=pT_[:32, :128], in_=gt[:], identity=ident[:]), reads=["gt", "ident"], writes=["pT"])
                oo = o + s * 128
                p.op("scalar", lambda e, oo=oo: e.copy(out=gateT[:, oo:oo + 128], in_=pT_[:32, :128]), reads=["pT"], writes=["gateT"])
            for oc in range(8):
                for kc in range(2):
                    p.op("tensor", lambda e, kc=kc, oc=oc: e.matmul(pP[:, :T1], lhsT=wpl_bf[:, kc, oc * 128:(oc + 1) * 128], rhs=pf[:, kc, :], start=(kc == 0), stop=(kc == 1)),
                         reads=["wpl_bf", "pf"], writes=["pP"])
                for kc in range(8):
                    p.op("tensor", lambda e, kc=kc, oc=oc: e.matmul(pG[:, :T1], lhsT=wplg_bf[:, kc, oc * 128:(oc + 1) * 128], rhs=x1b[:, kc, :], start=(kc == 0), stop=(kc == 7)),
                         reads=["wplg_bf", "x1b"], writes=["pG"])
                p.op("scalar", lambda e: e.activation(out=tm["g"][:], in_=pG[:, :T1], func=AF.Sigmoid), reads=["pG"], writes=["tm_g"])
                p.op("vector", lambda e: e.tensor_tensor(out=tm["t2"][:], in0=tm["g"][:], in1=pP[:, :T1], op=ALU.mult), reads=["tm_g", "pP"], writes=["tm_t2"])
                p.op("vector", lambda e, oc=oc: e.scalar_tensor_tensor(out=accb[:, oc, :], in0=xf[:, oc, :], scalar=ALPHA, in1=tm["t2"][:], op0=ALU.mult, op1=ALU.add),
                     reads=["xf", "tm_t2"], writes=["accb"])
            p.dma("sync", acc_s[:, :, o:o + T1], accb[:], reads=["accb"], writes=["acc_s"])
    if dump:
        p.dma("sync", gate_d[:, :], gateT[:], reads=["gateT"], writes=["gate_d"])
    p.barrier()

    H = ntok // 2
    with contextlib.ExitStack() as sc:
        def sb(name, shape, dt=F32):
            return sc.enter_context(nc.sbuf_tensor("s_" + name, list(shape), dt))
        x1h = sb("x1h", [128, 8, H], BF16); acc = sb("acc", [128, 8, H])
        wgu_b = [sb(f"wgu_b{i}", [128, 8, 2048], BF16) for i in range(2)]
        wd_b = [sb(f"wd_b{i}", [128, 8, 1024], BF16) for i in range(2)]
        bgu_sb = sb("bgu_sb", [128, 32, 16]); bd_sb = sb("bd_sb", [32, 1024]); ones32 = sb("ones32", [32, 128])
        act = sb("act", [128, 8, T2], BF16); gbc = sb("gbc", [128, T2]); gm = sb("gm", [32, T2])
        glu = sb("glu", [128, T2]); up1 = sb("up1", [128, T2]); sg = sb("sg", [128, T2]); t1 = sb("t1", [128, T2]); t2 = sb("t2", [128, T2])
        sq = sb("sq2", [128, 2, T2]); tm = {k: sb("tn_" + k, [128, T2]) for k in ["mean", "rstd", "t"]}
        p.dma("sync", bgu_sb[:], bgu[:, :, :], writes=["bgu_sb"])
        p.dma("sync", bd_sb[:], bd[:, :], writes=["bd_sb"])
        p.op("vector", lambda e: e.memset(ones32[:], 1.0), writes=["ones32"])
        pGl = [PS[0], PS[1]]; pUp = [PS[2], PS[3]]; pD = [PS[4], PS[5]]; pBC = PS[6]; pS = PS[7]; pQ = PS[6]
        outv = outT.rearrange("(kc p) n -> p kc n", p=128)
        cnt = 0
        for hf in range(2):
            ho = hf * H
            p.dma("sync", x1h[:], x1bf_s[:, :, ho:ho + H], reads=["x1bf_s"], writes=["x1h"])
            p.dma("sync", acc[:], acc_s[:, :, ho:ho + H], reads=["acc_s"], writes=["acc"])
            for tt in range(H // T2):
                to = tt * T2
                for oc in range(8):
                    j = cnt % 2; cnt += 1
                    p.op("tensor", lambda e, oc=oc, j=j, to=to: e.matmul(pD[j][:, :T2], lhsT=bd_sb[:, oc * 128:(oc + 1) * 128], rhs=gateT[:, ho + to:ho + to + T2], start=True, stop=True),
                         reads=["bd_sb", "gateT"], writes=[f"pD{j}"])
                    p.op("vector", lambda e, oc=oc, j=j, to=to: e.tensor_tensor(out=acc[:, oc, to:to + T2], in0=acc[:, oc, to:to + T2], in1=pD[j][:, :T2], op=ALU.add),
                         reads=["acc", f"pD{j}"], writes=["acc"])
            for ex_ in range(ne):
                wb_i = ex_ % 2
                wgs, wds = wgu_b[wb_i], wd_b[wb_i]
                for kc in range(0, 8, 2):
                    p.dma("gpsimd", wgs[:, kc:kc + 2, :], wgu[ex_, kc * 128:(kc + 2) * 128, :].rearrange("(k p) n -> p k n", p=128), writes=[f"wgu{wb_i}"])
                for kc in range(0, 8, 4):
                    p.dma("gpsimd", wds[:, kc:kc + 4, :], wd[ex_, kc * 128:(kc + 4) * 128, :].rearrange("(k p) n -> p k n", p=128), writes=[f"wd{wb_i}"])
                for tt in range(H // T2):
                    to = tt * T2
                    p.op("vector", lambda e, to=to, ex_=ex_: e.tensor_scalar(out=gm[:], in0=gateT[:, ho + to:ho + to + T2], scalar1=ident[0:32, ex_:ex_ + 1], scalar2=None, op0=ALU.mult),
                         reads=["gateT", "ident"], writes=["gm"])
                    p.op("tensor", lambda e: e.matmul(pBC[:, :T2], lhsT=ones32[:], rhs=gm[:], start=True, stop=True), reads=["ones32", "gm"], writes=["pBC"])
                    p.op("scalar", lambda e: e.copy(out=gbc[:], in_=pBC[:, :T2]), reads=["pBC"], writes=["gbc"])
                    for oc in range(8):
                        j = oc % 2
                        for kc in range(8):
                            p.op("tensor", lambda e, kc=kc, oc=oc, j=j, to=to: e.matmul(pGl[j][:, :T2], lhsT=wgs[:, kc, oc * 128:(oc + 1) * 128], rhs=x1h[:, kc, to:to + T2], start=(kc == 0), stop=(kc == 7)),
                                 reads=[f"wgu{wb_i}", "x1h"], writes=[f"pGl{j}"])
                        for kc in range(8):
                            p.op("tensor", lambda e, kc=kc, oc=oc, j=j, to=to: e.matmul(pUp[j][:, :T2], lhsT=wgs[:, kc, 1024 + oc * 128:1024 + (oc + 1) * 128], rhs=x1h[:, kc, to:to + T2], start=(kc == 0), stop=(kc == 7)),
                                 reads=[f"wgu{wb_i}", "x1h"], writes=[f"pUp{j}"])
                        p.op("vector", lambda e, oc=oc, j=j, ex_=ex_: e.tensor_scalar(out=glu[:], in0=pGl[j][:, :T2], scalar1=bgu_sb[:, ex_, oc:oc + 1], scalar2=7.0, op0=ALU.add, op1=ALU.min),
                             reads=[f"pGl{j}", "bgu_sb"], writes=["glu"])
                        p.op("vector", lambda e, oc=oc, j=j, ex_=ex_: e.tensor_scalar(out=up1[:], in0=pUp[j][:, :T2], scalar1=bgu_sb[:, ex_, 8 + oc:9 + oc], scalar2=7.0, op0=ALU.add, op1=ALU.min),
                             reads=[f"pUp{j}", "bgu_sb"], writes=["up1"])
                        p.op("gpsimd", lambda e: e.tensor_scalar(out=up1[:], in0=up1[:], scalar1=-7.0, scalar2=1.0, op0=ALU.max, op1=ALU.add), reads=["up1"], writes=["up1"])
                        p.op("scalar", lambda e: e.activation(out=sg[:], in_=glu[:], func=AF.Sigmoid, scale=1.702), reads=["glu"], writes=["sg"])
                        p.op("gpsimd", lambda e: e.tensor_tensor(out=t2[:], in0=up1[:], in1=gbc[:], op=ALU.mult), reads=["up1", "gbc"], writes=["t2"])
                        p.op("vector", lambda e: e.tensor_tensor(out=t1[:], in0=glu[:], in1=sg[:], op=ALU.mult), reads=["glu", "sg"], writes=["t1"])
                        p.op("vector", lambda e, oc=oc: e.tensor_tensor(out=act[:, oc, :], in0=t1[:], in1=t2[:], op=ALU.mult), reads=["t1", "t2"], writes=["act"])
                    for oc in range(8):
                        j = cnt % 2; cnt += 1
                        for kc in range(8):
                            p.op("tensor", lambda e, kc=kc, oc=oc, j=j: e.matmul(pD[j][:, :T2], lhsT=wds[:, kc, oc * 128:(oc + 1) * 128], rhs=act[:, kc, :], start=(kc == 0), stop=(kc == 7)),
                                 reads=[f"wd{wb_i}", "act"], writes=[f"pD{j}"])
                        p.op("vector", lambda e, oc=oc, j=j, to=to: e.tensor_tensor(out=acc[:, oc, to:to + T2], in0=acc[:, oc, to:to + T2], in1=pD[j][:, :T2], op=ALU.add),
                             reads=["acc", f"pD{j}"], writes=["acc"])
            for tt in range(H // T2):
                to = tt * T2
                hv = acc[:, :, to:to + T2]
                ln_fm(p, hv, "acc", 8, T2, lambda oc: vecs[:, 2, oc:oc + 1], lambda oc: vecs[:, 3, oc:oc + 1], ones_mean, sq, pS, pQ, tm, "pS7", "pBC")
                p.dma("sync", outv[:, :, ho + to:ho + to + T2], hv, reads=["acc"], writes=["outT"])
    p.finish_wait("sync", ["outT"] + (["x1_d", "gate_d"] if dump else []))
    return p.build()


def build_L0(ntok=2048):
    p = Prog(); nc = p.nc
    xT = p.dram("xT", [1024, ntok]); vec = p.dram("vec", [128, 2, 8])
    outT = p.dram("outT", [1024, ntok], kind="ExternalOutput")
    ones_mean = p.sb("ones_mean", [128, 128]); vecs = p.sb("vecs", [128, 2, 8])
    p.dma("sync", vecs[:], vec[:, :, :], writes=["vec"])
    p.op("vector", lambda e: e.memset(ones_mean[:], 1.0 / 1024), writes=["ones_mean"])
    TT = 512
    h = [p.sb(f"h{i}", [128, 8, TT]) for i in range(2)]
    sq = p.sb("sq", [128, 2, TT]); tm = {k: p.sb("tm_" + k, [128, TT]) for k in ["mean", "rstd", "t"]}
    pS = p.ps("pS", [128, 512]); pQ = p.ps("pQ", [128, 512])
    xv = xT.rearrange("(kc p) n -> p kc n", p=128); ov = outT.rearrange("(kc p) n -> p kc n", p=128)
    for t in range(ntok // TT):
        o = t * TT; i = t % 2
        p.dma("sync", h[i][:], xv[:, :, o:o + TT], writes=[f"h{i}"])
        ln_fm(p, h[i], f"h{i}", 8, TT, lambda oc: vecs[:, 0, oc:oc + 1], lambda oc: vecs[:, 1, oc:oc + 1], ones_mean, sq, pS, pQ, tm, "pS", "pQ")
        p.dma("gpsimd", ov[:, :, o:o + TT], h[i][:], reads=[f"h{i}"], writes=["outT"])
    p.finish_wait("sync", ["outT"])
    return p.build()


def host_inputs_B(L, stream, ya, yb, u, z, c):
    sl = slice(c * 2048, (c + 1) * 2048)
    f = lambda a: np.ascontiguousarray(a, dtype=np.float32)
    ycat = np.concatenate([ya[sl], yb[sl], u[sl]], axis=1)
    vec = np.stack([z['ln1_g'][L].reshape(8, 128).T, z['ln1_b'][L].reshape(8, 128).T, z['ln2_g'][L].reshape(8, 128).T,
                    z['ln2_b'][L].reshape(8, 128).T, z['ssd_norm'][L].reshape(8, 128).T], axis=1)
    return {
        "xT": f(stream[sl].T), "yT": f(ycat.T), "pT": f(z['p'][L].reshape(-1, 256)[sl].T),
        "wg": f(z['w_in'][L][:, 6576:]), "wb": f(z['w_branch'][L]), "wo": f(z['w_o'][L]), "wplg": f(z['w_pl_gate'][L]),
        "wpl": f(z['w_pl'][L]), "wr": f(z['w_router'][L]), "br": f(z['b_router'][L][None]),
        "wgu": f(z['w_gu'][L]), "wd": f(z['w_down'][L]), "bgu": f(z['b_gu'][L].reshape(32, 16, 128).transpose(2, 0, 1)),
        "bd": f(z['b_down'][L]), "vec": f(vec), "ident": np.eye(128, dtype=np.float32),
    }


def kernel(**inputs):
    z = {k: np.asarray(v) for k, v in inputs.items()}
    NCORE = 8
    cores = list(range(NCORE))
    xf = z['x'].reshape(-1, 1024).astype(np.float32)
    f = lambda a: np.ascontiguousarray(a, dtype=np.float32)
    vec0 = f(np.stack([z['ln_in_g'].reshape(8, 128).T, z['ln_in_b'].reshape(8, 128).T], axis=1))
    nc0 = build_L0()
    res = run_bass_kernel_spmd(nc0, [{"xT": f(xf[c * 2048:(c + 1) * 2048].T), "vec": vec0} for c in cores], core_ids=cores)
    stream = np.concatenate([r["outT"].T for r in res.results], axis=0)
    for L in range(2):
        ncA = build_A(T=8192)
        imA = [host_inputs_A(z, L, stream[b * 8192:(b + 1) * 8192], j) for b in range(2) for j in range(4)]
        resA = run_bass_kernel_spmd(ncA, imA, core_ids=cores).results
        ya = np.concatenate([np.concatenate([resA[b * 4 + j]["ya"] for j in range(4)], axis=1) for b in range(2)], axis=0)
        yb = np.concatenate([np.concatenate([resA[b * 4 + j]["yb"] for j in range(4)], axis=1) for b in range(2)], axis=0)
        u = np.concatenate([np.concatenate([resA[b * 4 + j]["yc"] for j in range(4)], axis=1) for b in range(2)], axis=0)
        del resA, imA
        ncB = build_B()
        imB = [host_inputs_B(L, stream, ya, yb, u, z, c) for c in cores]
        resB = run_bass_kernel_spmd(ncB, imB, core_ids=cores).results
        stream = np.concatenate([r["outT"].T for r in resB], axis=0)
        del resB, imB
    return np.ascontiguousarray(stream.reshape(2, 8192, 1024), dtype=np.float32)
```

```python
import os
import contextlib, time
import numpy as np
import concourse.bass as bass
import concourse.mybir as mybir
from concourse.bass_utils import run_bass_kernel_spmd

F32 = mybir.dt.float32
BF16 = mybir.dt.bfloat16
I32 = mybir.dt.int32
ALU = mybir.AluOpType
AF = mybir.ActivationFunctionType
AX = mybir.AxisListType

ENG = ["sync", "gpsimd", "scalar", "vector", "tensor"]
NDMASEM = 6
import os as _os
ATTACH = bool(int(_os.environ.get('FW_ATTACH', '1')))


class Prog:
    def __init__(self, immediate=True):
        self.immediate = immediate
        self.nc = bass.Bass("TRN2", target_bir_lowering=False)
        try:
            self.nc.allow_low_precision("bf16 matmul operands with fp32 accumulation")
            self.nc.allow_non_contiguous_dma("strided layouts")
        except Exception as ex:
            print("allow_* failed", ex)
        self.st = contextlib.ExitStack()
        self.ops = {e: [] for e in ENG}
        self.cnt = {}
        self.sems = {}
        self.lastw = {}
        self.reads = {}
        self.seen = {e: {} for e in ENG}
        self.dma_i = {e: 0 for e in ENG}
        self.dma_last = {}
        self.ninstr = 0
        for e in ["gpsimd", "scalar", "vector", "tensor"]:
            self._sem("c_" + e)
        for e in ["sync", "gpsimd", "scalar"]:
            for i in range(NDMASEM):
                self._sem(f"d_{e}_{i}")

    def _sem(self, name):
        self.sems[name] = self.st.enter_context(self.nc.semaphore(name))
        self.cnt[name] = 0

    def dram(self, name, shape, dt=F32, kind="ExternalInput"):
        return self.nc.dram_tensor(name, list(shape), dt, kind=kind).ap()

    def sb(self, name, shape, dt=F32):
        return self.st.enter_context(self.nc.sbuf_tensor("s_" + name, list(shape), dt))

    def ps(self, name, shape, dt=F32):
        return self.st.enter_context(self.nc.psum_tensor("p_" + name, list(shape), dt))

    def _deps(self, eng, reads, writes):
        need = {}
        def add(tok):
            if tok is None:
                return
            s, v = tok
            if need.get(s, 0) < v:
                need[s] = v
        for k in reads:
            add(self.lastw.get(k))
        for k in writes:
            add(self.lastw.get(k))
            for t in self.reads.get(k, ()):
                add(t)
        out = []
        for s, v in need.items():
            if self.seen[eng].get(s, 0) < v:
                self.seen[eng][s] = v
                out.append((s, v))
        return out

    def _commit(self, tok, reads, writes):
        for k in reads:
            self.reads.setdefault(k, []).append(tok)
        for k in writes:
            self.lastw[k] = tok
            self.reads[k] = []

    def capture(self, f, *a):
        self._buf = []
        try:
            f(*a)
        finally:
            buf, self._buf = self._buf, None
        return buf

    def emit_interleaved(self, chains):
        chains = [list(c) for c in chains if c]
        idx = [0] * len(chains)
        live = True
        while live:
            live = False
            for ci, c in enumerate(chains):
                if idx[ci] < len(c):
                    kind, a, kw = c[idx[ci]]; idx[ci] += 1; live = True
                    (self.op if kind == "op" else self.dma)(*a, **kw)

    def op(self, eng, fn, reads=(), writes=()):
        if getattr(self, "_buf", None) is not None:
            self._buf.append(("op", (eng, fn, reads, writes), {})); return None
        psr = [k for k in reads if isinstance(k, str) and k.startswith("ps")]
        if psr:
            reads = [k for k in reads if k not in psr]
            writes = list(writes) + psr
        waits = self._deps(eng, reads, writes)
        s = "c_" + eng
        self.cnt[s] += 1
        tok = (s, self.cnt[s])
        self._commit(tok, reads, writes)
        self._emit(eng, waits, fn, s, 1)
        self.ninstr += 1
        return tok

    def dma(self, eng, out, in_, reads=(), writes=(), **kw):
        if getattr(self, "_buf", None) is not None:
            self._buf.append(("dma", (eng, out, in_, reads, writes), kw)); return None
        slot = self.dma_i[eng] % NDMASEM
        self.dma_i[eng] += 1
        s = f"d_{eng}_{slot}"
        waits = self._deps(eng, reads, writes)
        prev = self.cnt[s]
        if prev > 0 and self.seen[eng].get(s, 0) < prev:
            self.seen[eng][s] = prev
            waits.append((s, prev))
        self.cnt[s] += 16
        tok = (s, self.cnt[s])
        self._commit(tok, reads, writes)
        fn = lambda e, out=out, in_=in_, kw=kw: e.dma_start(out=out, in_=in_, **kw)
        self._emit(eng, waits, fn, s, 16)
        self.ninstr += 1
        return tok

    def coll(self, kind, in_ap, out_ap, groups, reads=(), writes=()):
        eng = "gpsimd"
        slot = self.dma_i[eng] % NDMASEM
        self.dma_i[eng] += 1
        s = f"d_{eng}_{slot}"
        waits = self._deps(eng, reads, writes)
        prev = self.cnt[s]
        if prev > 0 and self.seen[eng].get(s, 0) < prev:
            self.seen[eng][s] = prev
            waits.append((s, prev))
        self.cnt[s] += 16
        tok = (s, self.cnt[s])
        self._commit(tok, reads, writes)
        fn = lambda e: e.collective_compute(kind, ALU.bypass, replica_groups=groups, ins=[in_ap], outs=[out_ap])
        self._emit(eng, waits, fn, s, 16)
        self.ninstr += 1
        return tok

    def barrier(self):
        for eng in ENG:
            waits = []
            for sname, v in self.cnt.items():
                if v > 0 and self.seen[eng].get(sname, 0) < v:
                    self.seen[eng][sname] = v
                    waits.append((sname, v))
            self._emit(eng, waits, None, None, 0)

    def finish_wait(self, eng, keys):
        waits = self._deps(eng, keys, ())
        self._emit(eng, waits, None, None, 0)

    def _emit(self, eng, waits, fn, s, inc):
        if not self.immediate:
            self.ops[eng].append((waits, fn, s, inc)); return
        engobj = getattr(self.nc, eng)
        if fn is None or not ATTACH:
            for (ws, wv) in waits:
                engobj.wait_ge(self.sems[ws], wv)
            if fn is not None:
                fn(engobj).then_inc(self.sems[s], inc)
            return
        for (ws, wv) in waits[1:]:
            engobj.wait_ge(self.sems[ws], wv)
        ins = fn(engobj)
        if waits:
            ins._wait_ge(self.sems[waits[0][0]], waits[0][1])
        ins.then_inc(self.sems[s], inc)

    def build(self):
        if self.immediate:
            self.st.close(); return self.nc
        nc = self.nc
        with nc.Block() as block:
            def mk(e):
                def body(engobj):
                    for waits, fn, s, inc in self.ops[e]:
                        for (ws, wv) in waits:
                            engobj.wait_ge(self.sems[ws], wv)
                        if fn is not None:
                            fn(engobj).then_inc(self.sems[s], inc)
                return body
            block.sync(mk("sync"))
            block.gpsimd(mk("gpsimd"))
            block.scalar(mk("scalar"))
            block.vector(mk("vector"))
            block.tensor(mk("tensor"))
        self.st.close()
        return nc


NCOL = 2060
NEG = -30000.0
RV = {}
_o = 0
for _n, _l in [("gconv", 5 * 384), ("sconv", 5 * 512), ("sconvb", 512), ("mup", 768), ("mun", 768), ("spb", 10), ("alog", 10),
               ("gnorm", 128), ("w0", 256), ("a0", 256), ("kk", 128), ("ka", 128), ("rk", 128), ("lng", 128), ("lnb", 128), ("dsk", 256)]:
    RV[_n] = (_o, _l); _o += _l
NV = _o


def host_inputs_A(z, L, stream_b, j):
    f = lambda a: np.ascontiguousarray(a, dtype=np.float32)
    w_in = z['w_in'][L]
    g = j // 2
    GD0, RW0, SS0 = 0, 2064, 2064 + 1920
    r = lambda a, n: list(range(a, a + n))
    cols = (r(GD0 + j * 128, 128) + r(GD0 + 512 + j * 128, 128) + r(GD0 + 1024 + j * 128, 128) + r(GD0 + 1536 + j * 128, 128)
            + r(RW0 + j * 128, 128) + r(RW0 + 512 + j * 128, 128) + r(RW0 + 1024 + j * 128, 128) + r(RW0 + 1536, 384)
            + r(SS0 + j * 256, 256) + r(SS0 + 1024 + j * 256, 256) + r(SS0 + 2048 + g * 128, 128) + r(SS0 + 2304 + g * 128, 128)
            + [GD0 + 2048 + d * 4 + j for d in range(2)] + [GD0 + 2056 + d * 4 + j for d in range(2)]
            + [SS0 + 2560 + d * 16 + 4 * j + i for d in range(2) for i in range(4)])
    assert len(cols) == NCOL
    rv = np.zeros(NV, np.float32)
    def put(n, a):
        o, l = RV[n]; a = np.asarray(a, np.float32).reshape(-1); assert a.size == l, (n, a.size, l); rv[o:o + l] = a
    qkv_idx = r(j * 128, 128) + r(512 + j * 128, 128) + r(1024 + j * 128, 128)
    xbc_idx = r(j * 256, 256) + r(1024 + g * 128, 128) + r(1280 + g * 128, 128)
    rw_idx = r(j * 128, 128) + r(512 + j * 128, 128) + r(1024 + j * 128, 128) + r(1536, 384)
    put("gconv", z['gdn_conv'][L][:, qkv_idx]); put("sconv", z['ssd_conv'][L][:, xbc_idx]); put("sconvb", z['ssd_conv_b'][L][xbc_idx])
    put("mup", z['rwkv_mu_prev'][L][rw_idx]); put("mun", z['rwkv_mu_next'][L][rw_idx])
    put("spb", np.concatenate([z['gdn_dt_bias'][L][:, j], z['ssd_dt_bias'][L][:, 4 * j:4 * j + 4].reshape(-1)]))
    put("alog", np.concatenate([z['gdn_a_log'][L][:, j], z['ssd_a_log'][L][:, 4 * j:4 * j + 4].reshape(-1)]))
    put("gnorm", z['gdn_norm'][L]); put("w0", z['rwkv_w0'][L][:, j * 128:(j + 1) * 128]); put("a0", z['rwkv_a0'][L][:, j * 128:(j + 1) * 128])
    put("kk", z['rwkv_k_k'][L][j * 128:(j + 1) * 128]); put("ka", z['rwkv_k_a'][L][j * 128:(j + 1) * 128])
    put("rk", z['rwkv_r_k'][L][2 * j:2 * j + 2]); put("lng", z['rwkv_ln_g'][L][j * 128:(j + 1) * 128]); put("lnb", z['rwkv_ln_b'][L][j * 128:(j + 1) * 128])
    put("dsk", np.repeat(z['ssd_d'][L][4 * j:4 * j + 4], 64))
    k = np.arange(128)
    UT = (k[:, None] <= k[None, :]).astype(np.float32); LT = (k[:, None] >= k[None, :]).astype(np.float32)
    blk = (k[:, None] // 64 == k[None, :] // 64).astype(np.float32)
    bdUT = UT * blk; bdLT = LT * blk
    msk = np.stack([UT, LT, np.where(UT > 0, 0.0, NEG), np.where(LT > 0, 0.0, NEG), np.eye(128), np.ones((128, 128)), bdUT, bdLT, -(bdUT - np.eye(128)), -(bdLT - np.eye(128)), bdUT - np.eye(128), bdLT - np.eye(128)], axis=1)
    return {
        "xT": f(stream_b.T), "wc": f(w_in[:, cols]), "rowvec": f(rv[None]),
        "wup": f(z['rwkv_w_up'][L][:, :, j * 128:(j + 1) * 128].reshape(128, 128)),
        "aup": f(z['rwkv_a_up'][L][:, :, j * 128:(j + 1) * 128].reshape(128, 128)),
        "gup": f(z['rwkv_g_up'][L][:, j * 128:(j + 1) * 128]), "msk": f(msk),
    }


def build_A(T=8192, dump=False, NS=16, phases=(1, 2, 3, 4)):
    p = Prog(); nc = p.nc
    NTL = T // 128
    dk = "ExternalOutput" if dump else "Internal"
    xT = p.dram("xT", [1024, T]); wc = p.dram("wc", [1024, NCOL]); rowvec = p.dram("rowvec", [1, NV])
    wup = p.dram("wup", [128, 128]); aup = p.dram("aup", [128, 128]); gup = p.dram("gup", [128, 128]); mskd = p.dram("msk", [128, 12, 128])
    ya_o = p.dram("ya", [T, 128], kind="ExternalOutput"); yb_o = p.dram("yb", [T, 128], kind="ExternalOutput"); yc_o = p.dram("yc", [T, 256], kind="ExternalOutput")
    cols_s = p.dram("cols_s", [T + 4, NCOL], kind=dk)
    gkq_s = p.dram("gkq_s", [T, 2, 128], kind=dk); gsc_s = p.dram("gsc_s", [T, 4], kind=dk); gbvT_s = p.dram("gbvT_s", [2, 128, T], kind=dk)
    rw_s = p.dram("rw_s", [2, 2, T, 5, 64], kind=dk); rvT_s = p.dram("rvT_s", [128, T], kind=dk); rpost_s = p.dram("rpost_s", [T, 258], kind=dk)
    ssd_s = p.dram("ssd_s", [T, 528], kind=dk)
    gy_s = p.dram("gy_s", [2, T, 128], kind=dk); gkqv_s = p.dram("gkqv_s", [T, 3, 128], kind=dk); gbg_s = p.dram("gbg_s", [T, 4], kind=dk); ry_s = p.dram("ry_s", [2, T, 128], kind=dk); sy_s = p.dram("sy_s", [T, 256], kind=dk)

    V = lambda fn, r=(), w=(): p.op("vector", fn, r, w)
    G = lambda fn, r=(), w=(): p.op("gpsimd", fn, r, w)
    S = lambda fn, r=(), w=(): p.op("scalar", fn, r, w)
    PE = lambda fn, r=(), w=(): p.op("tensor", fn, r, w)

    msk = p.sb("msk", [128, 12, 128]); rvs = p.sb("rvs", [128, NV])
    p.dma("sync", msk[:], mskd[:, :, :], writes=["msk"])
    p.dma("sync", rvs[:], rowvec.partition_broadcast(128)[:, 0, :], writes=["rvs"])
    UT, LT, NEGf, NEGb, ident, ones, bdUT, bdLT, nbdUTs, nbdLTs, sbdUT, sbdLT = (msk[:, i, :] for i in range(12))
    def rv(n, a=0, l=None):
        o, ln = RV[n]
        return rvs[:, o + a:o + a + (ln - a if l is None else l)]
    negexp = p.sb("negexp", [128, 10])
    S(lambda e: e.activation(out=negexp[:], in_=rv("alog"), func=AF.Exp), ["rvs"], ["negexp"])
    V(lambda e: e.tensor_scalar(out=negexp[:], in0=negexp[:], scalar1=-1.0, scalar2=None, op0=ALU.mult), ["negexp"], ["negexp"])
    PS = [p.ps(f"ps{i}", [128, 512]) for i in range(8)]

    if 1 in phases:
        with contextlib.ExitStack() as sc:
            sb = lambda name, shape, dt=F32: sc.enter_context(nc.sbuf_tensor("s_" + name, list(shape), dt))
            W_bf = sb("W_bf", [128, 8, 2048], BF16); w_sm = sb("w_sm", [128, 8, 12])
            zt = sb("zt", [2, NCOL])
            xb = [sb(f"xb{i}", [128, 8, 128], BF16) for i in range(2)]; xf = [sb(f"xf{i}", [128, 8, 128]) for i in range(2)]
            ct = [sb(f"ct{i}", [128, NCOL]) for i in range(2)]
            wcv = wc.rearrange("(kc p) n -> p kc n", p=128)
            for kc in range(8):
                p.dma("gpsimd", W_bf[:, kc, :], wc[kc * 128:(kc + 1) * 128, 0:2048], writes=["W_bf"])
            p.dma("sync", w_sm[:], wcv[:, :, 2048:2060], writes=["w_sm"])
            V(lambda e: e.memset(zt[:], 0.0), [], ["zt"])
            p.dma("sync", cols_s[0:2, :], zt[:], reads=["zt"], writes=["cols_pad"])
            p.dma("sync", cols_s[T + 2:T + 4, :], zt[:], reads=["zt"], writes=["cols_pad"])
            xTv = xT.rearrange("(kc p) n -> p kc n", p=128)
            for tt in range(NTL):
                t0 = tt * 128; i = tt % 2
                p.dma("gpsimd", xb[i][:], xTv[:, :, t0:t0 + 128], writes=[f"xb{i}"])
                p.dma("sync", xf[i][:], xTv[:, :, t0:t0 + 128], writes=[f"xf{i}"])
                for gq in range(4):
                    pp = PS[gq]
                    for kc in range(8):
                        PE(lambda e, kc=kc, gq=gq, pp=pp, i=i: e.matmul(pp[:, :], lhsT=xb[i][:, kc, :], rhs=W_bf[:, kc, gq * 512:(gq + 1) * 512], start=(kc == 0), stop=(kc == 7)),
                           [f"xb{i}", "W_bf"], [f"ps{gq}"])
                    if gq % 2 == 0:
                        S(lambda e, gq=gq, pp=pp, i=i: e.copy(out=ct[i][:, gq * 512:(gq + 1) * 512], in_=pp[:, :]), [f"ps{gq}"], [f"ct{i}"])
                    else:
                        V(lambda e, gq=gq, pp=pp, i=i: e.tensor_copy(out=ct[i][:, gq * 512:(gq + 1) * 512], in_=pp[:, :]), [f"ps{gq}"], [f"ct{i}"])
                for kc in range(8):
                    PE(lambda e, kc=kc, i=i: e.matmul(PS[4][:, :12], lhsT=xf[i][:, kc, :], rhs=w_sm[:, kc, :], start=(kc == 0), stop=(kc == 7)),
                       [f"xf{i}", "w_sm"], ["ps4"])
                V(lambda e, i=i: e.tensor_copy(out=ct[i][:, 2048:2060], in_=PS[4][:, :12]), ["ps4"], [f"ct{i}"])
                p.dma("sync", cols_s[2 + t0:2 + t0 + 128, :], ct[i][:], reads=[f"ct{i}"], writes=["cols_s"])
        p.barrier()

    if 2 in phases:
        with contextlib.ExitStack() as sc:
            sb = lambda name, shape, dt=F32: sc.enter_context(nc.sbuf_tensor("s_" + name, list(shape), dt))
            win = [sb(f"win{j}", [128, NCOL]) for j in range(5)]
            wup_sb = sb("wup_sb", [128, 128]); aup_sb = sb("aup_sb", [128, 128]); gup_sb = sb("gup_sb", [128, 128])
            p.dma("sync", wup_sb[:], wup[:, :], writes=["wup_sb"]); p.dma("sync", aup_sb[:], aup[:, :], writes=["aup_sb"]); p.dma("sync", gup_sb[:], gup[:, :], writes=["gup_sb"])
            cacc = sb("cacc", [128, 896]); ctmp = sb("ctmp", [128, 896]); qkv = sb("qkv", [128, 384]); xbc = sb("xbc", [128, 528])
            junk = sb("junk", [128, 128]); ssq = sb("ssq", [128, 4]); kq = sb("kq", [128, 2, 128])
            spx = sb("spx", [128, 10]); spa = sb("spa", [128, 10]); spl = sb("spl", [128, 10]); beta = sb("beta", [128, 2]); gsc = sb("gsc", [128, 4])
            bv = sb("bv", [128, 2, 128]); trs = sb("trs", [128, 128]); bg = sb("bg", [128, 4])
            sh = sb("sh", [128, 768]); d1 = sb("d1", [128, 768]); d2 = sb("d2", [128, 768])
            tw = sb("tw", [128, 128]); twT = sb("twT", [128, 128]); alT = sb("alT", [128, 128]); sgl = sb("sgl", [128, 128]); sgT = sb("sgT", [128, 128])
            RW = [sb(f"RW{d}", [128, 5, 128]) for d in range(2)]; ad = [sb(f"ad{d}", [128, 128]) for d in range(2)]
            wraw = sb("wraw", [128, 128]); kx = sb("kx", [128, 128]); sqk = sb("sqk", [128, 128]); rkk = sb("rkk", [128, 2]); kkn = sb("kkn", [128, 128])
            rkr = sb("rkr", [128, 128]); prod = sb("prod", [128, 128]); bon = sb("bon", [128, 2, 2]); rpost = sb("rpost", [128, 258]); t128 = sb("t128", [128, 128])
            pT1, pT2, pM1, pM2, pT3 = PS[0], PS[1], PS[2], PS[3], PS[4]
            for tt in range(NTL):
                t0 = tt * 128
                for j in range(5):
                    p.dma("sync" if j % 2 == 0 else "scalar", win[j][:], cols_s[t0 + j:t0 + j + 128, :], reads=["cols_s", "cols_pad"], writes=[f"win{j}"])
                cur = win[2]
                for (c0, c1, o0, cname, cw) in [(0, 384, 0, "gconv", 384), (1536, 2048, 384, "sconv", 512)]:
                    for j in range(5):
                        wj = rv(cname, j * cw, cw)
                        if j == 0:
                            V(lambda e, c0=c0, c1=c1, o0=o0, wj=wj, cw=cw: e.tensor_tensor(out=cacc[:, o0:o0 + cw], in0=win[0][:, c0:c1], in1=wj, op=ALU.mult), ["win0", "rvs"], [f"cacc{o0}"])
                        else:
                            G(lambda e, c0=c0, c1=c1, o0=o0, wj=wj, cw=cw, j=j: e.tensor_tensor(out=ctmp[:, o0:o0 + cw], in0=win[j][:, c0:c1], in1=wj, op=ALU.mult), [f"win{j}", "rvs"], [f"ctmp{o0}"])
                            V(lambda e, o0=o0, cw=cw: e.tensor_tensor(out=cacc[:, o0:o0 + cw], in0=cacc[:, o0:o0 + cw], in1=ctmp[:, o0:o0 + cw], op=ALU.add), [f"cacc{o0}", f"ctmp{o0}"], [f"cacc{o0}"])
                V(lambda e: e.tensor_tensor(out=cacc[:, 384:896], in0=cacc[:, 384:896], in1=rv("sconvb"), op=ALU.add), ["cacc384", "rvs"], ["cacc384"])
                S(lambda e: e.activation(out=qkv[:], in_=cacc[:, 0:384], func=AF.Silu), ["cacc0"], ["qkv"])
                S(lambda e: e.activation(out=xbc[:, 0:512], in_=cacc[:, 384:896], func=AF.Silu), ["cacc384"], ["xbc"])
                V(lambda e: e.tensor_tensor(out=spx[:], in0=cur[:, 2050:2060], in1=rv("spb"), op=ALU.add), ["win2", "rvs"], ["spx"])
                S(lambda e: e.activation(out=spa[:], in_=spx[:], func=AF.Abs), ["spx"], ["spa"])
                S(lambda e: e.activation(out=spa[:], in_=spa[:], func=AF.Exp, scale=-1.0), ["spa"], ["spa"])
                S(lambda e: e.activation(out=spl[:], in_=spa[:], func=AF.Ln, bias=1.0), ["spa"], ["spl"])
                V(lambda e: e.tensor_scalar(out=spx[:], in0=spx[:], scalar1=0.0, scalar2=None, op0=ALU.max), ["spx"], ["spx"])
                V(lambda e: e.tensor_tensor(out=spx[:], in0=spx[:], in1=spl[:], op=ALU.add), ["spx", "spl"], ["spx"])
                V(lambda e: e.tensor_tensor(out=spl[:], in0=spx[:], in1=negexp[:], op=ALU.mult), ["spx", "negexp"], ["spl"])
                V(lambda e: e.tensor_copy(out=xbc[:, 512:520], in_=spx[:, 2:10]), ["spx"], ["xbc"])
                V(lambda e: e.tensor_copy(out=xbc[:, 520:528], in_=spl[:, 2:10]), ["spl"], ["xbc"])
                p.dma("gpsimd", ssd_s[t0:t0 + 128, :], xbc[:], reads=["xbc"], writes=["ssd_s"])
                S(lambda e: e.activation(out=beta[:], in_=cur[:, 2048:2050], func=AF.Sigmoid), ["win2"], ["beta"])
                S(lambda e: e.activation(out=gsc[:, 0:2], in_=spl[:, 0:2], func=AF.Exp), ["spl"], ["gsc"])
                V(lambda e: e.scalar_tensor_tensor(out=gsc[:, 2:4], in0=gsc[:, 0:2], scalar=-1.0, in1=beta[:], op0=ALU.mult, op1=ALU.mult), ["gsc", "beta"], ["gsc"])
                for qi in range(2):
                    src = qkv[:, qi * 128:(qi + 1) * 128]
                    V(lambda e, src=src, qi=qi: e.scalar_tensor_tensor(out=junk[:], in0=src, scalar=1.0, in1=src, op0=ALU.mult, op1=ALU.mult, accum_out=ssq[:, qi:qi + 1]), ["qkv"], ["junk", "ssq"])
                V(lambda e: e.tensor_scalar(out=ssq[:, 0:2], in0=ssq[:, 0:2], scalar1=1e-6, scalar2=None, op0=ALU.add), ["ssq"], ["ssq"])
                S(lambda e: e.activation(out=ssq[:, 0:2], in_=ssq[:, 0:2], func=AF.Sqrt), ["ssq"], ["ssq"])
                V(lambda e: e.reciprocal(out=ssq[:, 0:2], in_=ssq[:, 0:2]), ["ssq"], ["ssq"])
                V(lambda e: e.tensor_scalar(out=kq[:, 0, :], in0=qkv[:, 128:256], scalar1=ssq[:, 1:2], scalar2=None, op0=ALU.mult), ["qkv", "ssq"], ["kq"])
                V(lambda e: e.tensor_scalar(out=kq[:, 1, :], in0=qkv[:, 0:128], scalar1=ssq[:, 0:1], scalar2=128 ** -0.5, op0=ALU.mult, op1=ALU.mult), ["qkv", "ssq"], ["kq"])
                p.dma("gpsimd", gkqv_s[t0:t0 + 128, 0:2, :], kq[:], reads=["kq"], writes=["gkqv_s"])
                p.dma("gpsimd", gkqv_s[t0:t0 + 128, 2, :], qkv[:, 256:384], reads=["qkv"], writes=["gkqv_s"])
                G(lambda e: e.tensor_copy(out=bg[:, 0:2], in_=beta[:]), ["beta"], ["bg"])
                G(lambda e: e.tensor_copy(out=bg[:, 2:4], in_=spl[:, 0:2]), ["spl"], ["bg"])
                p.dma("gpsimd", gbg_s[t0:t0 + 128, :], bg[:], reads=["bg"], writes=["gbg_s"])
                c_, pv, nx = cur[:, 512:1280], win[1][:, 512:1280], win[3][:, 512:1280]
                V(lambda e: e.tensor_tensor(out=d1[:], in0=pv, in1=c_, op=ALU.subtract), ["win1", "win2"], ["d1"])
                G(lambda e: e.tensor_tensor(out=d1[:], in0=d1[:], in1=rv("mup"), op=ALU.mult), ["d1", "rvs"], ["d1"])
                V(lambda e: e.tensor_tensor(out=d2[:], in0=nx, in1=c_, op=ALU.subtract), ["win3", "win2"], ["d2"])
                G(lambda e: e.tensor_tensor(out=d2[:], in0=d2[:], in1=rv("mun"), op=ALU.mult), ["d2", "rvs"], ["d2"])
                V(lambda e: e.tensor_tensor(out=sh[:], in0=c_, in1=d1[:], op=ALU.add), ["win2", "d1"], ["sh"])
                V(lambda e: e.tensor_tensor(out=sh[:], in0=sh[:], in1=d2[:], op=ALU.add), ["sh", "d2"], ["sh"])
                r_, k_, v_, wl, al, gl = (sh[:, i * 128:(i + 1) * 128] for i in range(6))
                S(lambda e: e.activation(out=tw[:], in_=wl, func=AF.Tanh), ["sh"], ["tw"])
                PE(lambda e: e.transpose(out=pT1[:, :128], in_=tw[:], identity=ident), ["tw", "msk"], ["ps0"])
                S(lambda e: e.copy(out=twT[:], in_=pT1[:, :128]), ["ps0"], ["twT"])
                PE(lambda e: e.transpose(out=pT2[:, :128], in_=al, identity=ident), ["sh", "msk"], ["ps1"])
                V(lambda e: e.tensor_copy(out=alT[:], in_=pT2[:, :128]), ["ps1"], ["alT"])
                S(lambda e: e.activation(out=sgl[:], in_=gl, func=AF.Sigmoid), ["sh"], ["sgl"])
                PE(lambda e: e.transpose(out=pT3[:, :128], in_=sgl[:], identity=ident), ["sgl", "msk"], ["ps4"])
                V(lambda e: e.tensor_copy(out=sgT[:], in_=pT3[:, :128]), ["ps4"], ["sgT"])
                PE(lambda e: e.matmul(pT3[:, 128:256], lhsT=sgT[:], rhs=gup_sb[:], start=True, stop=True), ["sgT", "gup_sb"], ["ps4"])
                S(lambda e: e.copy(out=rpost[:, 128:256], in_=pT3[:, 128:256]), ["ps4"], ["rpost"])
                V(lambda e: e.tensor_tensor(out=kx[:], in0=k_, in1=rv("kk"), op=ALU.mult), ["sh", "rvs"], ["kx"])
                S(lambda e: e.activation(out=sqk[:], in_=kx[:], func=AF.Square), ["kx"], ["sqk"])
                V(lambda e: e.tensor_reduce(out=rkk[:], in_=sqk[:].rearrange("p (h n) -> p h n", h=2), axis=AX.X, op=ALU.add), ["sqk"], ["rkk"])
                V(lambda e: e.tensor_scalar(out=rkk[:], in0=rkk[:], scalar1=1e-6, scalar2=None, op0=ALU.add), ["rkk"], ["rkk"])
                S(lambda e: e.activation(out=rkk[:], in_=rkk[:], func=AF.Sqrt), ["rkk"], ["rkk"])
                V(lambda e: e.reciprocal(out=rkk[:], in_=rkk[:]), ["rkk"], ["rkk"])
                for h in range(2):
                    V(lambda e, h=h: e.tensor_scalar(out=kkn[:, h * 64:(h + 1) * 64], in0=kx[:, h * 64:(h + 1) * 64], scalar1=rkk[:, h:h + 1], scalar2=None, op0=ALU.mult), ["kx", "rkk"], ["kkn"])
                G(lambda e: e.tensor_tensor(out=rkr[:], in0=r_, in1=rv("rk"), op=ALU.mult), ["sh", "rvs"], ["rkr"])
                for d in range(2):
                    hs = slice(d * 64, (d + 1) * 64)
                    PE(lambda e, hs=hs: e.matmul(pM1[:, :128], lhsT=twT[hs, :], rhs=wup_sb[hs, :], start=True, stop=True), ["twT", "wup_sb"], ["ps2"])
                    V(lambda e, d=d: e.tensor_tensor(out=wraw[:], in0=pM1[:, :128], in1=rv("w0", d * 128, 128), op=ALU.add), ["ps2", "rvs"], ["wraw"])
                    S(lambda e: e.activation(out=wraw[:], in_=wraw[:], func=AF.Sigmoid), ["wraw"], ["wraw"])
                    S(lambda e, d=d: e.activation(out=RW[d][:, 0, :], in_=wraw[:], func=AF.Exp, scale=-0.6065306597126334), ["wraw"], [f"RW{d}"])
                    PE(lambda e, hs=hs: e.matmul(pM2[:, :128], lhsT=alT[hs, :], rhs=aup_sb[hs, :], start=True, stop=True), ["alT", "aup_sb"], ["ps3"])
                    V(lambda e, d=d: e.tensor_tensor(out=ad[d][:], in0=pM2[:, :128], in1=rv("a0", d * 128, 128), op=ALU.add), ["ps3", "rvs"], [f"ad{d}"])
                    S(lambda e, d=d: e.activation(out=ad[d][:], in_=ad[d][:], func=AF.Sigmoid), [f"ad{d}"], [f"ad{d}"])
                    G(lambda e, d=d: e.tensor_copy(out=RW[d][:, 1, :], in_=kkn[:]), ["kkn"], [f"RW{d}"])
                    V(lambda e, d=d: e.scalar_tensor_tensor(out=RW[d][:, 2, :], in0=kkn[:], scalar=-1.0, in1=ad[d][:], op0=ALU.mult, op1=ALU.mult), ["kkn", f"ad{d}"], [f"RW{d}"])
                    V(lambda e, d=d: e.scalar_tensor_tensor(out=t128[:], in0=ad[d][:], scalar=-1.0, in1=rv("ka"), op0=ALU.add, op1=ALU.mult), [f"ad{d}", "rvs"], ["t128"])
                    V(lambda e, d=d: e.scalar_tensor_tensor(out=RW[d][:, 3, :], in0=t128[:], scalar=1.0, in1=k_, op0=ALU.add, op1=ALU.mult), ["t128", "sh"], [f"RW{d}"])
                    G(lambda e, d=d: e.tensor_copy(out=RW[d][:, 4, :], in_=r_), ["sh"], [f"RW{d}"])
                    V(lambda e, d=d: e.tensor_tensor(out=prod[:], in0=rkr[:], in1=RW[d][:, 3, :], op=ALU.mult), ["rkr", f"RW{d}"], ["prod"])
                    V(lambda e, d=d: e.tensor_reduce(out=bon[:, d, :], in_=prod[:].rearrange("p (h n) -> p h n", h=2), axis=AX.X, op=ALU.add), ["prod"], ["bon"])
                    for h in range(2):
                        p.dma("gpsimd", rw_s[d, h, t0:t0 + 128, :, :], RW[d][:, :, h * 64:(h + 1) * 64], reads=[f"RW{d}"], writes=["rw_s"])
                V(lambda e: e.tensor_tensor(out=rpost[:, 256:258], in0=bon[:, 0, :], in1=bon[:, 1, :], op=ALU.add), ["bon"], ["rpost"])
                G(lambda e: e.tensor_copy(out=rpost[:, 0:128], in_=v_), ["sh"], ["rpost"])
                p.dma("gpsimd", rpost_s[t0:t0 + 128, :], rpost[:], reads=["rpost"], writes=["rpost_s"])
                PE(lambda e: e.transpose(out=pT2[:, :128], in_=v_, identity=ident), ["sh", "msk"], ["ps1"])
                V(lambda e: e.tensor_copy(out=t128[:], in_=pT2[:, :128]), ["ps1"], ["t128"])
                p.dma("gpsimd", rvT_s[:, t0:t0 + 128], t128[:], reads=["t128"], writes=["rvT_s"])
        p.barrier()
    fin = ["ya", "yb", "yc"]
    if dump:
        fin += ["cols_s", "gkqv_s", "gbg_s", "rw_s", "rvT_s", "rpost_s", "ssd_s", "gy_s", "ry_s"]
    if 3 in phases:
        phase3(p, T, NS, locals())
    if 4 in phases:
        phase4(p, T, locals())
    p.finish_wait("sync", [k for k in fin if k in p.lastw])
    return p.build()


def phase3(p, T, NS, L):
    SSDSTOP = int(os.environ.get('A_SSDSTOP', '99'))
    GSTOP = int(os.environ.get('A_GSTOP', '99'))
    nc = p.nc
    V = L["V"]; G = L["G"]; S = L["S"]; PE = L["PE"]; PS = L["PS"]
    UT, LT, ident, ones = L["UT"], L["LT"], L["ident"], L["ones"]
    gkqv_s, gbg_s, rw_s, rvT_s, ssd_s, gy_s, ry_s, sy_s, cols_s, yc_o = (L[k] for k in
        ["gkqv_s", "gbg_s", "rw_s", "rvT_s", "ssd_s", "gy_s", "ry_s", "sy_s", "cols_s", "yc_o"])
    bdUT, bdLT, nbdUTs, nbdLTs, sbdUT, sbdLT = (L[k_] for k_ in ["bdUT", "bdLT", "nbdUTs", "nbdLTs", "sbdUT", "sbdLT"])
    rpost_s = L["rpost_s"]
    rv = L["rv"]
    NTL = T // 128; NCH = T // NS
    with contextlib.ExitStack() as sc:
        sb = lambda name, shape, dt=F32: sc.enter_context(nc.sbuf_tensor("s_" + name, list(shape), dt))
        RB = []
        for d in range(2):
            r_ = {}
            for nm, shp in [("rwt", [128, 5, 128]), ("vt", [128, 128]), ("lw", [128, 128]), ("lp", [128, 128]), ("Pt", [128, 128]), ("iP", [128, 128]), ("Pm", [128, 128]),
                            ("rt", [128, 128]), ("kt", [128, 128]), ("nbt", [128, 128]), ("ct", [128, 128]), ("rtF", [128, 128]), ("nbF", [128, 128]), ("cF", [128, 128]), ("PF", [128, 128]),
                            ("X", [128, 128]), ("XT", [128, 128]), ("AckT", [128, 128]), ("ArkT", [128, 128]), ("AnrbT", [128, 128]),
                            ("P0", [128, 128]), ("P1", [128, 128]), ("PT0", [128, 128]), ("PT1", [128, 128]), ("TT", [128, 128]),
                            ("Zs", [128, 64]), ("Ms", [128, 64]), ("Yt", [128, 128]), ("H", [128, 64])]:
                r_[nm] = sb(f"r{d}_{nm}", shp)
            for nm in ["cFh", "nbFh", "ktFh", "rtFh"]:
                for h in range(2):
                    r_[f"{nm}{h}"] = sb(f"r{d}_{nm}{h}", [128, 128])
                    V(lambda e, t_=r_[f"{nm}{h}"]: e.memset(t_[:], 0.0), [], [f"r{d}_{nm}{h}"])
            for nm in ["ktS", "nbS"]:
                for ch in range(2):
                    for h in range(2):
                        r_[f"{nm}{ch}{h}"] = sb(f"r{d}_{nm}{ch}{h}", [128, 128])
                        V(lambda e, t_=r_[f"{nm}{ch}{h}"]: e.memset(t_[:], 0.0), [], [f"r{d}_{nm}{ch}{h}"])
            V(lambda e, r_=r_: e.memset(r_["H"][:], 0.0), [], [f"r{d}_H"])
            V(lambda e, r_=r_: e.memset(r_["Zs"][:], 0.0), [], [f"r{d}_Zs"])
            V(lambda e, r_=r_: e.memset(r_["Ms"][:], 0.0), [], [f"r{d}_Ms"])
            RB.append(r_)
        GB = []
        for d in range(2):
            g_ = {}
            for nm, shp in [("kqv", [128, 3, 128]), ("bg", [128, 4]), ("kF", [128, 128]), ("qF", [128, 128]), ("Gs", [128, 128]), ("QKs", [128, 128]),
                            ("gcc", [128, 1]), ("ngcc", [128, 1]), ("R", [128, 128]), ("grow", [128, 128]), ("egrow", [128, 128]), ("dT", [128, 128]), ("dN", [128, 128]),
                            ("Bd", [128, 128]), ("brow", [128, 128]), ("X", [128, 128]), ("XT", [128, 128]), ("P0", [128, 128]), ("P1", [128, 128]),
                            ("PT0", [128, 128]), ("PT1", [128, 128]), ("TT", [128, 128]), ("vb", [128, 128]), ("kbg", [128, 128]), ("be", [128, 1]), ("eg", [128, 1]),
                            ("u", [128, 128]), ("wF", [128, 128]), ("qdF", [128, 128]), ("QKm", [128, 128]), ("vnew", [128, 128]), ("kdA", [128, 128]), ("kdB", [128, 128]),
                            ("dl", [128, 1]), ("dlA", [128, 1]), ("dlB", [128, 1]), ("S", [128, 128]), ("o", [128, 128]), ("t1", [128, 128]), ("t2", [128, 128])]:
                g_[nm] = sb(f"g{d}_{nm}", shp)
            GB.append(g_)
            V(lambda e, g_=g_: e.memset(g_["S"][:], 0.0), [], [f"g{d}_S"])
            V(lambda e, g_=g_: e.memset(g_["vnew"][:], 0.0), [], [f"g{d}_vnew"])
        sxt = sb("sxt", [128, 528]); BCF = sb("BCF", [128, 256]); scT = sb("scT", [128, 128]); acol = sb("acol", [128, 4]); nacol = sb("nacol", [128, 4])
        Rm = sb("Rm", [128, 4, 128]); E = sb("E", [128, 4, 128]); Dm = sb("Dm", [128, 128]); M = sb("M", [128, 4, 128]); CdF = sb("CdF", [128, 4, 128])
        xdt = sb("xdt", [128, 256]); xend = sb("xend", [128, 256]); dend = sb("dend", [128, 4]); H = sb("H", [128, 256]); yt = sb("yt", [128, 256])
        syf = sb("syf", [128, 256]); zt = sb("zts", [128, 256]); xd = sb("xd", [128, 256]); szs = sb("szs", [128, 256])
        pA, pSc, pAc, pRow, pY, pSt = PS[0][:, 0:256], PS[0][:, 256:384], PS[0][:, 384:512], PS[1], PS[2][:, 0:256], PS[2][:, 256:512]
        print('phase3 sbuf remaining', nc.sbuf_bytes_remaining, flush=True)

        def gdn_chunk(d, c):
            t0 = c * 128
            B_ = GB[d]; K = lambda n: f"g{d}_{n}"
            bank = PS[6 + d]; kb = f"ps{6 + d}"
            regs = [bank[:, i * 128:(i + 1) * 128] for i in range(4)] + [PS[3][:, d * 256:d * 256 + 128], PS[3][:, d * 256 + 128:d * 256 + 256]]
            keys = [kb] * 4 + ["ps3", "ps3"]
            (rA, rB, rC, rD, rE, rF), (kA, kB, kC, kD, kE, kF_) = regs, keys
            fwd = (d == 0)
            Mtri = bdUT if fwd else bdLT
            MaskT = Mtri
            MaskN = bdLT if fwd else bdUT
            nST = nbdUTs if fwd else nbdLTs
            nSN = nbdLTs if fwd else nbdUTs
            selA = bdLT[:, 0:1]; selB = bdUT[:, 127:128]
            hA, hB = slice(0, 64), slice(64, 128)
            if fwd:
                first, second, lastF, lastS = hA, hB, 63, 127
            else:
                first, second, lastF, lastS = hB, hA, 64, 0
            kqv, bg = B_["kqv"], B_["bg"]
            p.dma("sync", kqv[:], gkqv_s[t0:t0 + 128, :, :], reads=["gkqv_s"], writes=[K("kqv")])
            p.dma("sync", bg[:], gbg_s[t0:t0 + 128, :], reads=["gbg_s"], writes=[K("bg")])
            kc, qc, vc = kqv[:, 0, :], kqv[:, 1, :], kqv[:, 2, :]
            beta = bg[:, d:d + 1]; g = bg[:, 2 + d:3 + d]
            kF, qF, Gs, QKs = B_["kF"], B_["qF"], B_["Gs"], B_["QKs"]
            PE(lambda e: e.transpose(out=rA, in_=kc, identity=ident), [K("kqv"), "msk"], [kA])
            S(lambda e: e.copy(out=kF[:], in_=rA), [kA], [K("kF")])
            PE(lambda e: e.transpose(out=rB, in_=qc, identity=ident), [K("kqv"), "msk"], [kB])
            S(lambda e: e.copy(out=qF[:], in_=rB), [kB], [K("qF")])
            PE(lambda e: e.matmul(rC, lhsT=kF[:], rhs=kF[:], start=True, stop=True), [K("kF")], [kC])
            S(lambda e: e.copy(out=Gs[:], in_=rC), [kC], [K("Gs")])
            PE(lambda e: e.matmul(rD, lhsT=kF[:], rhs=qF[:], start=True, stop=True), [K("kF"), K("qF")], [kD])
            S(lambda e: e.copy(out=QKs[:], in_=rD), [kD], [K("QKs")])
            G(lambda e: e.tensor_scalar(out=B_["R"][:], in0=Mtri, scalar1=g, scalar2=None, op0=ALU.mult), [K("bg"), "msk"], [K("R")])
            PE(lambda e: e.matmul(rE, lhsT=ones, rhs=B_["R"][:], start=True, stop=True), [K("R"), "msk"], [kE])
            S(lambda e: e.copy(out=B_["grow"][:], in_=rE), [kE], [K("grow")])
            S(lambda e: e.activation(out=B_["egrow"][:], in_=rE, func=AF.Exp), [kE], [K("egrow")])
            V(lambda e: e.scalar_tensor_tensor(out=B_["t1"][:], in0=B_["grow"][:], scalar=1.0, in1=ident, op0=ALU.mult, op1=ALU.mult, accum_out=B_["gcc"][:, 0:1]), [K("grow"), "msk"], [K("t1"), K("gcc")])
            S(lambda e: e.mul(out=B_["ngcc"][:], in_=B_["gcc"][:], mul=-1.0), [K("gcc")], [K("ngcc")])
            for nm, bias_k, sc, Mk in [("dT", "ngcc", 1.0, MaskT), ("dN", "gcc", -1.0, MaskN)]:
                S(lambda e, nm=nm, bias_k=bias_k, sc=sc: e.activation(out=B_[nm][:], in_=B_["grow"][:], func=AF.Identity, bias=B_[bias_k][:, 0:1], scale=sc), [K("grow"), K(bias_k)], [K(nm)])
                G(lambda e, nm=nm: e.tensor_scalar(out=B_[nm][:], in0=B_[nm][:], scalar1=0.0, scalar2=None, op0=ALU.min), [K(nm)], [K(nm)])
                S(lambda e, nm=nm: e.activation(out=B_[nm][:], in_=B_[nm][:], func=AF.Exp), [K(nm)], [K(nm)])
                G(lambda e, nm=nm, Mk=Mk: e.tensor_tensor(out=B_[nm][:], in0=B_[nm][:], in1=Mk, op=ALU.mult), [K(nm), "msk"], [K(nm)])
            G(lambda e: e.tensor_scalar(out=B_["Bd"][:], in0=ident, scalar1=beta, scalar2=None, op0=ALU.mult), [K("bg"), "msk"], [K("Bd")])
            PE(lambda e: e.matmul(rF, lhsT=ones, rhs=B_["Bd"][:], start=True, stop=True), [K("Bd"), "msk"], [kF_])
            S(lambda e: e.copy(out=B_["brow"][:], in_=rF), [kF_], [K("brow")])
            G(lambda e: e.tensor_tensor(out=B_["t1"][:], in0=Gs[:], in1=B_["dT"][:], op=ALU.mult), [K("Gs"), K("dT"), K("t1")], [K("t1")])
            G(lambda e: e.tensor_tensor(out=B_["t1"][:], in0=B_["t1"][:], in1=B_["brow"][:], op=ALU.mult), [K("t1"), K("brow")], [K("t1")])
            G(lambda e: e.tensor_tensor(out=B_["XT"][:], in0=B_["t1"][:], in1=nST, op=ALU.mult), [K("t1"), "msk"], [K("XT")])
            G(lambda e: e.tensor_tensor(out=B_["t2"][:], in0=Gs[:], in1=B_["dN"][:], op=ALU.mult), [K("Gs"), K("dN")], [K("t2")])
            G(lambda e: e.tensor_scalar(out=B_["t2"][:], in0=B_["t2"][:], scalar1=beta, scalar2=None, op0=ALU.mult), [K("t2"), K("bg")], [K("t2")])
            G(lambda e: e.tensor_tensor(out=B_["X"][:], in0=B_["t2"][:], in1=nSN, op=ALU.mult), [K("t2"), "msk"], [K("X")])
            G(lambda e: e.tensor_tensor(out=B_["TT"][:], in0=B_["XT"][:], in1=ident, op=ALU.add), [K("XT"), "msk"], [K("TT")])
            if GSTOP == 1: return
            Pc, PTc, kP, kPT = B_["X"], B_["XT"], K("X"), K("XT")
            for lv in range(1, 6):
                Pn, kPn = B_[f"P{lv % 2}"], K(f"P{lv % 2}")
                PE(lambda e, Pc=Pc, PTc=PTc: e.matmul(rA, lhsT=PTc[:], rhs=Pc[:], start=True, stop=True), [kP, kPT], [kA])
                S(lambda e, Pn=Pn: e.copy(out=Pn[:], in_=rA), [kA], [kPn])
                if lv < 5:
                    PTn, kPTn = B_[f"PT{lv % 2}"], K(f"PT{lv % 2}")
                    PE(lambda e, Pc=Pc, PTc=PTc: e.matmul(rB, lhsT=Pc[:], rhs=PTc[:], start=True, stop=True), [kP, kPT], [kB])
                    S(lambda e, PTn=PTn: e.copy(out=PTn[:], in_=rB), [kB], [kPTn])
                PE(lambda e, Pn=Pn: e.matmul(rC, lhsT=Pn[:], rhs=B_["TT"][:], start=True, stop=True), [kPn, K("TT")], [kC])
                V(lambda e: e.tensor_tensor(out=B_["TT"][:], in0=B_["TT"][:], in1=rC, op=ALU.add), [K("TT"), kC], [K("TT")])
                Pc, kP = Pn, kPn
                if lv < 5:
                    PTc, kPT = PTn, kPTn
            if GSTOP == 2: return
            S(lambda e: e.activation(out=B_["eg"][:], in_=B_["gcc"][:], func=AF.Exp), [K("gcc")], [K("eg")])
            G(lambda e: e.tensor_tensor(out=B_["be"][:], in0=B_["eg"][:], in1=beta, op=ALU.mult), [K("eg"), K("bg")], [K("be")])
            G(lambda e: e.tensor_scalar(out=B_["vb"][:], in0=vc, scalar1=beta, scalar2=None, op0=ALU.mult), [K("kqv"), K("bg")], [K("vb")])
            G(lambda e: e.tensor_scalar(out=B_["kbg"][:], in0=kc, scalar1=B_["be"][:, 0:1], scalar2=None, op0=ALU.mult), [K("kqv"), K("be")], [K("kbg")])
            PE(lambda e: e.matmul(rD, lhsT=B_["TT"][:], rhs=B_["vb"][:], start=True, stop=True), [K("TT"), K("vb")], [kD])
            S(lambda e: e.copy(out=B_["u"][:], in_=rD), [kD], [K("u")])
            PE(lambda e: e.matmul(rE, lhsT=B_["kbg"][:], rhs=B_["TT"][:], start=True, stop=True), [K("kbg"), K("TT")], [kE])
            S(lambda e: e.copy(out=B_["wF"][:], in_=rE), [kE], [K("wF")])
            G(lambda e: e.tensor_tensor(out=B_["qdF"][:], in0=qF[:], in1=B_["egrow"][:], op=ALU.mult), [K("qF"), K("egrow")], [K("qdF")])
            G(lambda e: e.tensor_tensor(out=B_["QKm"][:], in0=QKs[:], in1=B_["dT"][:], op=ALU.mult), [K("QKs"), K("dT")], [K("QKm")])
            for hs, lst in [(first, lastF), (second, lastS)]:
                S(lambda e, hs=hs, lst=lst: e.activation(out=B_["dl"][hs, :], in_=B_["gcc"][hs, :], func=AF.Exp, bias=B_["grow"][hs, lst:lst + 1], scale=-1.0), [K("gcc"), K("grow")], [K("dl")])
            G(lambda e: e.tensor_tensor(out=B_["dlA"][:], in0=B_["dl"][:], in1=selA, op=ALU.mult), [K("dl"), "msk"], [K("dlA")])
            G(lambda e: e.tensor_tensor(out=B_["dlB"][:], in0=B_["dl"][:], in1=selB, op=ALU.mult), [K("dl"), "msk"], [K("dlB")])
            G(lambda e: e.tensor_scalar(out=B_["kdA"][:], in0=kc, scalar1=B_["dlA"][:, 0:1], scalar2=None, op0=ALU.mult), [K("kqv"), K("dlA")], [K("kdA")])
            G(lambda e: e.tensor_scalar(out=B_["kdB"][:], in0=kc, scalar1=B_["dlB"][:, 0:1], scalar2=None, op0=ALU.mult), [K("kqv"), K("dlB")], [K("kdB")])
            kdF, kdS, kkF, kkS = (B_["kdA"], B_["kdB"], K("kdA"), K("kdB")) if fwd else (B_["kdB"], B_["kdA"], K("kdB"), K("kdA"))
            if GSTOP == 3: return
            Sst = B_["S"]
            for (hs, lst, kd_, kkd, rW, kW, rO, kO) in [(first, lastF, kdF, kkF, rA, kA, rC, kC), (second, lastS, kdS, kkS, rB, kB, rD, kD)]:
                PE(lambda e, rW=rW: e.matmul(rW, lhsT=B_["wF"][:], rhs=Sst[:], start=True, stop=True), [K("wF"), K("S")], [kW])
                V(lambda e, hs=hs, rW=rW: e.tensor_tensor(out=B_["vnew"][hs, :], in0=B_["u"][hs, :], in1=rW[hs, :], op=ALU.subtract), [K("u"), kW], [K("vnew")])
                PE(lambda e, rO=rO: e.matmul(rO, lhsT=B_["qdF"][:], rhs=Sst[:], start=True, stop=True), [K("qdF"), K("S")], [kO])
                S(lambda e, hs=hs, rO=rO: e.copy(out=B_["o"][hs, :], in_=rO[hs, :]), [kO], [K("o")])
                PE(lambda e, kd_=kd_: e.matmul(rF, lhsT=kd_[:], rhs=B_["vnew"][:], start=True, stop=True), [kkd, K("vnew")], [kF_])
                V(lambda e, lst=lst: e.scalar_tensor_tensor(out=Sst[:], in0=Sst[:], scalar=B_["egrow"][:, lst:lst + 1], in1=rF, op0=ALU.mult, op1=ALU.add), [K("S"), K("egrow"), kF_], [K("S")])
            if GSTOP == 5: return
            PE(lambda e: e.matmul(rE, lhsT=B_["QKm"][:], rhs=B_["vnew"][:], start=True, stop=True), [K("QKm"), K("vnew")], [kE])
            V(lambda e: e.tensor_tensor(out=B_["o"][:], in0=B_["o"][:], in1=rE, op=ALU.add), [K("o"), kE], [K("o")])
            p.dma("gpsimd", gy_s[d, t0:t0 + 128, :], B_["o"][:], reads=[K("o")], writes=["gy_s"])

        def rwkv_block(d, c):
            t0 = c * 128
            B_ = RB[d]; K = lambda n: f"r{d}_{n}"
            bank = PS[4 + d]; kb = f"ps{4 + d}"
            rA, rB, rC, rD = (bank[:, i * 128:(i + 1) * 128] for i in range(4))
            fwd = (d == 0)
            Mtri = bdUT if fwd else bdLT
            mS_N = sbdLT if fwd else sbdUT
            mS_T = sbdUT if fwd else sbdLT
            mI_T = bdUT if fwd else bdLT
            selc = [bdLT[:, 0:1], bdUT[:, 127:128]]
            hA, hB = slice(0, 64), slice(64, 128)
            order = [(0, hA, 63), (1, hB, 127)] if fwd else [(1, hB, 64), (0, hA, 0)]
            rwt, vt = B_["rwt"], B_["vt"]
            for h in range(2):
                p.dma("sync", rwt[:, :, h * 64:(h + 1) * 64], rw_s[d, h, t0:t0 + 128, :, :], reads=["rw_s"], writes=[K("rwt")])
            p.dma("sync", vt[:], rpost_s[t0:t0 + 128, 0:128], reads=["rpost_s"], writes=[K("vt")])
            w_, kk_, nkka_, kd_, r_ = (rwt[:, i, :] for i in range(5))
            S(lambda e: e.activation(out=B_["lw"][:], in_=w_, func=AF.Ln), [K("rwt")], [K("lw")])
            PE(lambda e: e.matmul(rA, lhsT=Mtri, rhs=B_["lw"][:], start=True, stop=True), [K("lw"), "msk"], [kb])
            S(lambda e: e.copy(out=B_["lp"][:], in_=rA), [kb], [K("lp")])
            S(lambda e: e.activation(out=B_["Pt"][:], in_=rA, func=AF.Exp), [kb], [K("Pt")])
            S(lambda e: e.activation(out=B_["iP"][:], in_=rA, func=AF.Exp, scale=-1.0), [kb], [K("iP")])
            G(lambda e: e.tensor_tensor(out=B_["Pm"][:], in0=B_["lp"][:], in1=B_["lw"][:], op=ALU.subtract), [K("lp"), K("lw")], [K("Pm")])
            S(lambda e: e.activation(out=B_["Pm"][:], in_=B_["Pm"][:], func=AF.Exp), [K("Pm")], [K("Pm")])
            G(lambda e: e.tensor_tensor(out=B_["rt"][:], in0=r_, in1=B_["Pt"][:], op=ALU.mult), [K("rwt"), K("Pt")], [K("rt")])
            G(lambda e: e.tensor_tensor(out=B_["kt"][:], in0=kd_, in1=B_["iP"][:], op=ALU.mult), [K("rwt"), K("iP")], [K("kt")])
            G(lambda e: e.tensor_tensor(out=B_["nbt"][:], in0=nkka_, in1=B_["iP"][:], op=ALU.mult), [K("rwt"), K("iP")], [K("nbt")])
            G(lambda e: e.tensor_tensor(out=B_["ct"][:], in0=kk_, in1=B_["Pm"][:], op=ALU.mult), [K("rwt"), K("Pm")], [K("ct")])
            for src, full, hm in [("rt", "rtF", "rtFh"), ("nbt", "nbF", "nbFh"), ("ct", "cF", "cFh"), ("kt", None, "ktFh"), ("Pt", "PF", None)]:
                PE(lambda e, src=src: e.transpose(out=rB, in_=B_[src][:], identity=ident), [K(src), "msk"], [kb])
                if full is not None:
                    S(lambda e, full=full: e.copy(out=B_[full][:], in_=rB), [kb], [K(full)])
                if hm is not None:
                    for h, hs in [(0, hA), (1, hB)]:
                        S(lambda e, hm=hm, h=h, hs=hs: e.copy(out=B_[f"{hm}{h}"][hs, :], in_=rB[hs, :]), [kb], [K(f"{hm}{h}")])
            for ch in range(2):
                for h in range(2):
                    cs = slice(h * 64, (h + 1) * 64)
                    G(lambda e, ch=ch, h=h, cs=cs: e.tensor_scalar(out=B_[f"ktS{ch}{h}"][:, cs], in0=B_["kt"][:, cs], scalar1=selc[ch], scalar2=None, op0=ALU.mult), [K("kt"), "msk"], [K(f"ktS{ch}{h}")])
                    G(lambda e, ch=ch, h=h, cs=cs: e.tensor_scalar(out=B_[f"nbS{ch}{h}"][:, cs], in0=B_["nbt"][:, cs], scalar1=selc[ch], scalar2=None, op0=ALU.mult), [K("nbt"), "msk"], [K(f"nbS{ch}{h}")])
            def head(h):
                hr = hA if h == 0 else hB
                cFh, nbFh, ktFh, rtFh = (B_[f"{n}{h}"] for n in ["cFh", "nbFh", "ktFh", "rtFh"])
                kcF, knbF, kktF, krtF = (K(f"{n}{h}") for n in ["cFh", "nbFh", "ktFh", "rtFh"])
                for (nm, l_, kl, r__, kr, mk) in [("X", cFh, kcF, "nbF", K("nbF"), mS_N), ("XT", nbFh, knbF, "cF", K("cF"), mS_T), ("AckT", ktFh, kktF, "cF", K("cF"), mS_T),
                                                   ("ArkT", ktFh, kktF, "rtF", K("rtF"), mI_T), ("AnrbT", nbFh, knbF, "rtF", K("rtF"), mI_T)]:
                    PE(lambda e, l_=l_, r__=r__: e.matmul(rC, lhsT=l_[:], rhs=B_[r__][:], start=True, stop=True), [kl, kr], [kb])
                    V(lambda e, nm=nm, mk=mk: e.tensor_tensor(out=B_[nm][:], in0=rC, in1=mk, op=ALU.mult), [kb, "msk"], [K(nm)])
                G(lambda e: e.tensor_tensor(out=B_["TT"][:], in0=B_["XT"][:], in1=ident, op=ALU.add), [K("XT"), "msk"], [K("TT")])
                Pc, PTc, kP, kPT = B_["X"], B_["XT"], K("X"), K("XT")
                for lv in range(1, 6):
                    Pn, kPn = B_[f"P{lv % 2}"], K(f"P{lv % 2}")
                    PE(lambda e, Pc=Pc, PTc=PTc: e.matmul(rA, lhsT=PTc[:], rhs=Pc[:], start=True, stop=True), [kP, kPT], [kb])
                    S(lambda e, Pn=Pn: e.copy(out=Pn[:], in_=rA), [kb], [kPn])
                    if lv < 5:
                        PTn, kPTn = B_[f"PT{lv % 2}"], K(f"PT{lv % 2}")
                        PE(lambda e, Pc=Pc, PTc=PTc: e.matmul(rB, lhsT=Pc[:], rhs=PTc[:], start=True, stop=True), [kP, kPT], [kb])
                        S(lambda e, PTn=PTn: e.copy(out=PTn[:], in_=rB), [kb], [kPTn])
                    PE(lambda e, Pn=Pn: e.matmul(rC, lhsT=Pn[:], rhs=B_["TT"][:], start=True, stop=True), [kPn, K("TT")], [kb])
                    V(lambda e: e.tensor_tensor(out=B_["TT"][:], in0=B_["TT"][:], in1=rC, op=ALU.add), [K("TT"), kb], [K("TT")])
                    Pc, kP = Pn, kPn
                    if lv < 5:
                        PTc, kPT = PTn, kPTn
                Vh = vt[:, hr]
                H = B_["H"]
                for (ch, hs, lst) in order:
                    PE(lambda e: e.matmul(rD[:, 0:64], lhsT=cFh[:], rhs=H[:], start=True, stop=False), [kcF, K("H")], [kb])
                    PE(lambda e: e.matmul(rD[:, 0:64], lhsT=B_["AckT"][:], rhs=Vh, start=False, stop=True), [K("AckT"), K("vt")], [kb])
                    S(lambda e, hs=hs: e.copy(out=B_["Zs"][hs, :], in_=rD[hs, 0:64]), [kb], [K("Zs")])
                    PE(lambda e: e.matmul(rD[:, 64:128], lhsT=B_["TT"][:], rhs=B_["Zs"][:], start=True, stop=True), [K("TT"), K("Zs")], [kb])
                    S(lambda e, hs=hs: e.copy(out=B_["Ms"][hs, :], in_=rD[hs, 64:128]), [kb], [K("Ms")])
                    PE(lambda e: e.matmul(rA[:, 0:64], lhsT=rtFh[:], rhs=H[:], start=True, stop=True), [krtF, K("H")], [kb])
                    S(lambda e, hs=hs, hr=hr: e.copy(out=B_["Yt"][hs, hr], in_=rA[hs, 0:64]), [kb], [K("Yt")])
                    PE(lambda e, ch=ch, h=h: e.matmul(rB[:, 0:64], lhsT=B_[f"ktS{ch}{h}"][:], rhs=Vh, start=True, stop=False), [K(f"ktS{ch}{h}"), K("vt")], [kb])
                    PE(lambda e, ch=ch, h=h: e.matmul(rB[:, 0:64], lhsT=B_[f"nbS{ch}{h}"][:], rhs=B_["Ms"][:], start=False, stop=True), [K(f"nbS{ch}{h}"), K("Ms")], [kb])
                    V(lambda e, hr=hr, lst=lst: e.tensor_scalar(out=H[hr, :], in0=H[hr, :], scalar1=B_["PF"][hr, lst:lst + 1], scalar2=None, op0=ALU.mult), [K("H"), K("PF")], [K("H")])
                    V(lambda e, hr=hr, lst=lst: e.scalar_tensor_tensor(out=H[hr, :], in0=rB[hr, 0:64], scalar=B_["PF"][hr, lst:lst + 1], in1=H[hr, :], op0=ALU.mult, op1=ALU.add), [K("H"), K("PF"), kb], [K("H")])
                PE(lambda e: e.matmul(rC[:, 0:64], lhsT=B_["ArkT"][:], rhs=Vh, start=True, stop=False), [K("ArkT"), K("vt")], [kb])
                PE(lambda e: e.matmul(rC[:, 0:64], lhsT=B_["AnrbT"][:], rhs=B_["Ms"][:], start=False, stop=True), [K("AnrbT"), K("Ms")], [kb])
                V(lambda e, hr=hr: e.tensor_tensor(out=B_["Yt"][:, hr], in0=B_["Yt"][:, hr], in1=rC[:, 0:64], op=ALU.add), [K("Yt"), kb], [K("Yt")])
            for h in range(2):
                head(h)
            p.dma("gpsimd", ry_s[d, t0:t0 + 128, :], B_["Yt"][:], reads=[K("Yt")], writes=["ry_s"])

        def ssd_chunk(d, c):
            t0 = c * 128
            Mk = UT if d == 0 else LT
            last = 127 if d == 0 else 0
            p.dma("gpsimd", sxt[:], ssd_s[t0:t0 + 128, :], reads=["ssd_s"], writes=["sxt"])
            if d == 1:
                p.dma("gpsimd", syf[:], sy_s[t0:t0 + 128, :], reads=["sy_s"], writes=["syf"])
                p.dma("gpsimd", zt[:], cols_s[2 + t0:2 + t0 + 128, 1280:1536], reads=["cols_s"], writes=["zts"])
            sx = sxt[:, 0:256]; sB = sxt[:, 256:384]; sC = sxt[:, 384:512]
            dt_d = sxt[:, 512 + d * 4:516 + d * 4]; a_d = sxt[:, 520 + d * 4:524 + d * 4]
            PE(lambda e: e.transpose(out=pA[:, 0:128], in_=sB, identity=ident), ["sxt", "msk"], ["ps0"])
            PE(lambda e: e.transpose(out=pA[:, 128:256], in_=sC, identity=ident), ["sxt", "msk"], ["ps0"])
            S(lambda e: e.copy(out=BCF[:], in_=pA[:, 0:256]), ["ps0"], ["BCF"])
            if SSDSTOP == 1: return
            PE(lambda e: e.matmul(pSc[:, :128], lhsT=BCF[:, 0:128], rhs=BCF[:, 128:256], start=True, stop=True), ["BCF"], ["ps0"])
            S(lambda e: e.copy(out=scT[:], in_=pSc[:, :128]), ["ps0"], ["scT"])
            PE(lambda e: e.matmul(pAc[:, :4], lhsT=Mk, rhs=a_d, start=True, stop=True), ["sxt", "msk"], ["ps0"])
            S(lambda e: e.copy(out=acol[:], in_=pAc[:, :4]), ["ps0"], ["acol"])
            S(lambda e: e.mul(out=nacol[:], in_=pAc[:, :4], mul=-1.0), ["ps0"], ["nacol"])
            if SSDSTOP == 2: return
            for h in range(4):
                (V if os.environ.get('A_V1') else G)(lambda e, h=h: e.tensor_scalar(out=Rm[:, h, :], in0=Mk, scalar1=a_d[:, h:h + 1], scalar2=None, op0=ALU.mult), ["sxt", "msk"], ["Rm"])
            PE(lambda e: e.matmul(pRow[:, :512], lhsT=ones, rhs=Rm[:].rearrange("p h n -> p (h n)"), start=True, stop=True), ["Rm", "msk"], ["ps1"])
            S(lambda e: e.activation(out=E[:].rearrange("p h n -> p (h n)"), in_=pRow[:, :512], func=AF.Exp), ["ps1"], ["E"])
            for h in range(4):
                S(lambda e, h=h: e.activation(out=dend[:, h:h + 1], in_=pRow[:, h * 128 + last:h * 128 + last + 1], func=AF.Exp, bias=nacol[:, h:h + 1], scale=1.0), ["ps1", "nacol"], ["dend"])
            if SSDSTOP == 3: return
            for h in range(4):
                hs = slice(h * 64, (h + 1) * 64)
                S(lambda e, h=h: e.activation(out=Dm[:], in_=pRow[:, h * 128:(h + 1) * 128], func=AF.Identity, bias=nacol[:, h:h + 1], scale=1.0), ["ps1", "nacol"], ["Dm"])
                G(lambda e: e.tensor_scalar(out=Dm[:], in0=Dm[:], scalar1=0.0, scalar2=None, op0=ALU.min), ["Dm"], ["Dm"])
                S(lambda e: e.activation(out=Dm[:], in_=Dm[:], func=AF.Exp), ["Dm"], ["Dm"])
                G(lambda e: e.tensor_tensor(out=Dm[:], in0=Dm[:], in1=Mk, op=ALU.mult), ["Dm", "msk"], ["Dm"])
                G(lambda e, h=h: e.tensor_tensor(out=M[:, h, :], in0=Dm[:], in1=scT[:], op=ALU.mult), ["Dm", "scT"], ["M"])
                G(lambda e, h=h: e.tensor_tensor(out=CdF[:, h, :], in0=E[:, h, :], in1=BCF[:, 128:256], op=ALU.mult), ["E", "BCF"], ["CdF"])
                G(lambda e, h=h, hs=hs: e.tensor_scalar(out=xdt[:, hs], in0=sx[:, hs], scalar1=dt_d[:, h:h + 1], scalar2=None, op0=ALU.mult), ["sxt"], ["xdt"])
                G(lambda e, h=h, hs=hs: e.tensor_scalar(out=xend[:, hs], in0=xdt[:, hs], scalar1=dend[:, h:h + 1], scalar2=None, op0=ALU.mult), ["xdt", "dend"], ["xend"])
            if SSDSTOP == 4: return
            for h in range(4):
                hs = slice(h * 64, (h + 1) * 64)
                PE(lambda e, h=h, hs=hs: e.matmul(pY[:, hs], lhsT=M[:, h, :], rhs=xdt[:, hs], start=True, stop=False), ["M", "xdt"], ["ps2"])
                PE(lambda e, h=h, hs=hs: e.matmul(pY[:, hs], lhsT=CdF[:, h, :], rhs=H[:, hs], start=False, stop=True), ["CdF", "H"], ["ps2"])
            PE(lambda e: e.matmul(pSt[:, :256], lhsT=sB, rhs=xend[:], start=True, stop=True), ["sxt", "xend"], ["ps2"])
            if SSDSTOP == 5: return
            for h in range(4):
                hs = slice(h * 64, (h + 1) * 64)
                V(lambda e, h=h, hs=hs: e.scalar_tensor_tensor(out=H[:, hs], in0=H[:, hs], scalar=E[:, h, last:last + 1], in1=pSt[:, hs], op0=ALU.mult, op1=ALU.add), ["H", "E", "ps2"], ["H"])
            if SSDSTOP == 6: return
            if d == 0:
                S(lambda e: e.copy(out=yt[:], in_=pY[:, :256]), ["ps2"], ["yt"])
                p.dma("gpsimd", sy_s[t0:t0 + 128, :], yt[:], reads=["yt"], writes=["sy_s"])
            else:
                V(lambda e: e.tensor_tensor(out=yt[:], in0=pY[:, :256], in1=syf[:], op=ALU.add), ["ps2", "syf"], ["yt"])
                G(lambda e: e.tensor_tensor(out=xd[:], in0=sx, in1=rv("dsk"), op=ALU.mult), ["sxt", "rvs"], ["xd"])
                G(lambda e: e.tensor_tensor(out=yt[:], in0=yt[:], in1=xd[:], op=ALU.add), ["yt", "xd"], ["yt"])
                S(lambda e: e.activation(out=szs[:], in_=zt[:], func=AF.Silu), ["zts"], ["szs"])
                G(lambda e: e.tensor_tensor(out=yt[:], in0=yt[:], in1=szs[:], op=ALU.mult), ["yt", "szs"], ["yt"])
                p.dma("gpsimd", yc_o[t0:t0 + 128, :], yt[:], reads=["yt"], writes=["yc"])

        ssd_list = [(0, c) for c in range(NTL)] + [(1, c) for c in range(NTL - 1, -1, -1)]
        V(lambda e: e.memset(H[:], 0.0), [], ["H"])
        def ssd_pair(it):
            for si in (2 * it, 2 * it + 1):
                dd, cc = ssd_list[si]
                if dd == 1 and cc == NTL - 1:
                    V(lambda e: e.memset(H[:], 0.0), [], ["H"])
                ssd_chunk(dd, cc)
        for it in range(NTL):
            chains = []
            if not os.environ.get("A_NOGDN"):
                chains += [p.capture(gdn_chunk, 0, it), p.capture(gdn_chunk, 1, NTL - 1 - it)]
            if not os.environ.get("A_NORWKV"):
                chains += [p.capture(rwkv_block, 0, it), p.capture(rwkv_block, 1, NTL - 1 - it)]
            if not os.environ.get("A_NOSSD"):
                chains += [p.capture(ssd_pair, it)]
            p.emit_interleaved(chains)
    p.barrier()


def phase4(p, T, L):
    nc = p.nc
    V = L["V"]; G = L["G"]; S = L["S"]; PE = L["PE"]; PS = L["PS"]; ident = L["ident"]; rv = L["rv"]
    gy_s, ry_s, cols_s, rpost_s, ya_o, yb_o = (L[k] for k in ["gy_s", "ry_s", "cols_s", "rpost_s", "ya_o", "yb_o"])
    NTL = T // 128
    with contextlib.ExitStack() as sc:
        sb = lambda name, shape, dt=F32: sc.enter_context(nc.sbuf_tensor("s_" + name, list(shape), dt))
        g0 = sb("g0", [128, 128]); g1 = sb("g1", [128, 128]); o = sb("o", [128, 128]); jk = sb("jk", [128, 128]); ss = sb("ss", [128, 1])
        zt = sb("zt4", [128, 128]); ya = sb("ya_t", [128, 128])
        r0 = sb("r0", [128, 128]); r1 = sb("r1", [128, 128]); y = sb("y4", [128, 128]); st = sb("st4", [128, 2]); sq = sb("sq4", [128, 128]); vr = sb("vr4", [128, 2])
        rp = sb("rp4", [128, 258]); yb = sb("yb_t", [128, 128])
        pT, pU = PS[0], PS[1]
        for tt in range(NTL):
            t0 = tt * 128
            p.dma("sync", g0[:], gy_s[0, t0:t0 + 128, :], reads=["gy_s"], writes=["g0"])
            p.dma("sync", g1[:], gy_s[1, t0:t0 + 128, :], reads=["gy_s"], writes=["g1"])
            p.dma("scalar", zt[:], cols_s[2 + t0:2 + t0 + 128, 384:512], reads=["cols_s"], writes=["zt4"])
            p.dma("sync", r0[:], ry_s[0, t0:t0 + 128, :], reads=["ry_s"], writes=["r0"])
            p.dma("sync", r1[:], ry_s[1, t0:t0 + 128, :], reads=["ry_s"], writes=["r1"])
            p.dma("scalar", rp[:], rpost_s[t0:t0 + 128, :], reads=["rpost_s"], writes=["rp4"])
            V(lambda e: e.tensor_tensor(out=o[:], in0=g0[:], in1=g1[:], op=ALU.add), ["g0", "g1"], ["o"])
            V(lambda e: e.scalar_tensor_tensor(out=jk[:], in0=o[:], scalar=1.0, in1=o[:], op0=ALU.mult, op1=ALU.mult, accum_out=ss[:, 0:1]), ["o"], ["jk", "ss"])
            V(lambda e: e.tensor_scalar(out=ss[:], in0=ss[:], scalar1=1.0 / 128, scalar2=1e-6, op0=ALU.mult, op1=ALU.add), ["ss"], ["ss"])
            S(lambda e: e.activation(out=ss[:], in_=ss[:], func=AF.Sqrt), ["ss"], ["ss"])
            V(lambda e: e.reciprocal(out=ss[:], in_=ss[:]), ["ss"], ["ss"])
            V(lambda e: e.scalar_tensor_tensor(out=o[:], in0=o[:], scalar=ss[:, 0:1], in1=rv("gnorm"), op0=ALU.mult, op1=ALU.mult), ["o", "ss", "rvs"], ["o"])
            S(lambda e: e.activation(out=zt[:], in_=zt[:], func=AF.Silu), ["zt4"], ["zt4"])
            V(lambda e: e.tensor_tensor(out=ya[:], in0=o[:], in1=zt[:], op=ALU.mult), ["o", "zt4"], ["ya_t"])
            p.dma("gpsimd", ya_o[t0:t0 + 128, :], ya[:], reads=["ya_t"], writes=["ya"])
            V(lambda e: e.tensor_tensor(out=y[:], in0=r0[:], in1=r1[:], op=ALU.add), ["r0", "r1"], ["y4"])
            V(lambda e: e.tensor_reduce(out=st[:], in_=y[:].rearrange("p (h n) -> p h n", h=2), axis=AX.X, op=ALU.add), ["y4"], ["st4"])
            V(lambda e: e.tensor_scalar(out=st[:], in0=st[:], scalar1=1.0 / 64, scalar2=None, op0=ALU.mult), ["st4"], ["st4"])
            for h in range(2):
                hs = slice(h * 64, (h + 1) * 64)
                V(lambda e, h=h, hs=hs: e.tensor_scalar(out=y[:, hs], in0=y[:, hs], scalar1=st[:, h:h + 1], scalar2=None, op0=ALU.subtract), ["y4", "st4"], ["y4"])
            S(lambda e: e.activation(out=sq[:], in_=y[:], func=AF.Square), ["y4"], ["sq4"])
            V(lambda e: e.tensor_reduce(out=vr[:], in_=sq[:].rearrange("p (h n) -> p h n", h=2), axis=AX.X, op=ALU.add), ["sq4"], ["vr4"])
            V(lambda e: e.tensor_scalar(out=vr[:], in0=vr[:], scalar1=1.0 / 64, scalar2=64e-5, op0=ALU.mult, op1=ALU.add), ["vr4"], ["vr4"])
            S(lambda e: e.activation(out=vr[:], in_=vr[:], func=AF.Sqrt), ["vr4"], ["vr4"])
            V(lambda e: e.reciprocal(out=vr[:], in_=vr[:]), ["vr4"], ["vr4"])
            for h in range(2):
                hs = slice(h * 64, (h + 1) * 64)
                V(lambda e, h=h, hs=hs: e.tensor_scalar(out=y[:, hs], in0=y[:, hs], scalar1=vr[:, h:h + 1], scalar2=None, op0=ALU.mult), ["y4", "vr4"], ["y4"])
            V(lambda e: e.tensor_tensor(out=y[:], in0=y[:], in1=rv("lng"), op=ALU.mult), ["y4", "rvs"], ["y4"])
            V(lambda e: e.tensor_tensor(out=y[:], in0=y[:], in1=rv("lnb"), op=ALU.add), ["y4", "rvs"], ["y4"])
            for h in range(2):
                hs = slice(h * 64, (h + 1) * 64)
                V(lambda e, h=h, hs=hs: e.scalar_tensor_tensor(out=y[:, hs], in0=rp[:, hs], scalar=rp[:, 256 + h:257 + h], in1=y[:, hs], op0=ALU.mult, op1=ALU.add), ["y4", "rp4"], ["y4"])
            V(lambda e: e.tensor_tensor(out=yb[:], in0=y[:], in1=rp[:, 128:256], op=ALU.mult), ["y4", "rp4"], ["yb_t"])
            p.dma("gpsimd", yb_o[t0:t0 + 128, :], yb[:], reads=["yb_t"], writes=["yb"])


ALPHA = 4 ** 0.25
NTOK = 2048
T1 = 256
T2 = 512
NE = 32


def ln_fm(p, h, hk, nch, NT, gcol, bcol, ones_mean, sq, pS, pQ, tmp, kS, kQ, eps=1e-5):
    for oc in range(nch):
        p.op("tensor", lambda e, oc=oc: e.matmul(pS[:, :NT], lhsT=ones_mean[:], rhs=h[:, oc, :], start=(oc == 0), stop=(oc == nch - 1)),
             reads=[hk, "ones_mean"], writes=[kS])
    sqs = (lambda s_: sq[s_][:]) if isinstance(sq, (list, tuple)) else (lambda s_: sq[:, s_, :])
    for oc in range(nch):
        s = oc % 2
        p.op("scalar", lambda e, oc=oc, s=s: e.activation(out=sqs(s), in_=h[:, oc, :], func=AF.Square), reads=[hk], writes=[f"sq{s}"])
        p.op("tensor", lambda e, oc=oc, s=s: e.matmul(pQ[:, :NT], lhsT=ones_mean[:], rhs=sqs(s), start=(oc == 0), stop=(oc == nch - 1)),
             reads=[f"sq{s}", "ones_mean"], writes=[kQ])
    mean, rstd, t = tmp["mean"], tmp["rstd"], tmp["t"]
    p.op("scalar", lambda e: e.copy(out=mean[:], in_=pS[:, :NT]), reads=[kS], writes=["ln_mean"])
    p.op("vector", lambda e: e.tensor_tensor(out=t[:], in0=mean[:], in1=mean[:], op=ALU.mult), reads=["ln_mean"], writes=["ln_t"])
    p.op("vector", lambda e: e.tensor_tensor(out=t[:], in0=pQ[:, :NT], in1=t[:], op=ALU.subtract), reads=[kQ, "ln_t"], writes=["ln_t"])
    p.op("vector", lambda e: e.tensor_scalar(out=t[:], in0=t[:], scalar1=eps, scalar2=None, op0=ALU.add), reads=["ln_t"], writes=["ln_t"])
    p.op("scalar", lambda e: e.activation(out=t[:], in_=t[:], func=AF.Sqrt), reads=["ln_t"], writes=["ln_t"])
    p.op("vector", lambda e: e.reciprocal(out=rstd[:], in_=t[:]), reads=["ln_t"], writes=["ln_rstd"])
    for oc in range(nch):
        p.op("vector", lambda e, oc=oc: e.tensor_tensor(out=h[:, oc, :], in0=h[:, oc, :], in1=mean[:], op=ALU.subtract), reads=[hk, "ln_mean"], writes=[hk])
        p.op("vector", lambda e, oc=oc: e.tensor_tensor(out=h[:, oc, :], in0=h[:, oc, :], in1=rstd[:], op=ALU.mult), reads=[hk, "ln_rstd"], writes=[hk])
        p.op("scalar", lambda e, oc=oc: e.activation(out=h[:, oc, :], in_=h[:, oc, :], func=AF.Identity, scale=gcol(oc), bias=bcol(oc)),
             reads=[hk, "vec"], writes=[hk])


def build_B(ntok=NTOK, ne=NE, dump=False):
    p = Prog(); nc = p.nc
    D = 1024
    xT = p.dram("xT", [D, ntok]); yT = p.dram("yT", [2048, ntok]); pT = p.dram("pT", [256, ntok])
    wg = p.dram("wg", [D, 3072]); wb = p.dram("wb", [2048, D]); wo = p.dram("wo", [D, D]); wplg = p.dram("wplg", [D, D])
    wpl = p.dram("wpl", [256, D]); wr = p.dram("wr", [D, 32]); br = p.dram("br", [1, 32])
    wgu = p.dram("wgu", [32, D, 2048]); wd = p.dram("wd", [32, D, D])
    bgu = p.dram("bgu", [128, 32, 16]); bd = p.dram("bd", [32, D]); vec = p.dram("vec", [128, 5, 8]); ident_d = p.dram("ident", [128, 128])
    outT = p.dram("outT", [D, ntok], kind="ExternalOutput")
    x1bf_s = p.dram("x1bf_s", [128, 8, ntok], BF16, kind="Internal")
    acc_s = p.dram("acc_s", [128, 8, ntok], F32, kind="Internal")
    if dump:
        x1_d = p.dram("x1_d", [D, ntok], kind="ExternalOutput")
        gate_d = p.dram("gate_d", [32, ntok], kind="ExternalOutput")

    ident = p.sb("ident", [128, 128]); ones_mean = p.sb("ones_mean", [128, 128]); vecs = p.sb("vecs", [128, 5, 8])
    gateT = p.sb("gateT", [32, ntok]); ones_row = p.sb("ones_row", [1, 128]); brow = p.sb("brow", [1, 32])
    PS = [p.ps(f"ps{i}", [128, 512]) for i in range(8)]
    p.dma("sync", ident[:], ident_d[:, :], writes=["ident"])
    p.dma("sync", vecs[:], vec[:, :, :], writes=["vec"])
    p.dma("sync", brow[:], br[:, :], writes=["brow"])
    p.op("vector", lambda e: e.memset(ones_mean[:], 1.0 / 1024), writes=["ones_mean"])
    p.op("vector", lambda e: e.memset(ones_row[:], 1.0), writes=["ones_row"])

    with contextlib.ExitStack() as sc:
        def sb(name, shape, dt=F32):
            return sc.enter_context(nc.sbuf_tensor("s_" + name, list(shape), dt))
        wg_bf = sb("wg_bf", [128, 8, 3072], BF16); wb_bf = sb("wb_bf", [128, 16, 1024], BF16)
        wo_bf = sb("wo_bf", [128, 8, 1024], BF16); wplg_bf = sb("wplg_bf", [128, 8, 1024], BF16); wpl_bf = sb("wpl_bf", [128, 2, 1024], BF16)
        wr_sb = sb("wr_sb", [128, 8, 32]); ones512 = sb("ones512", [128, 128])
        for kc in range(8):
            p.dma("gpsimd", wg_bf[:, kc, :], wg[kc * 128:(kc + 1) * 128, :], writes=["wg_bf"])
            p.dma("gpsimd", wo_bf[:, kc, :], wo[kc * 128:(kc + 1) * 128, :], writes=["wo_bf"])
            p.dma("gpsimd", wplg_bf[:, kc, :], wplg[kc * 128:(kc + 1) * 128, :], writes=["wplg_bf"])
            p.dma("sync", wr_sb[:, kc, :], wr[kc * 128:(kc + 1) * 128, :], writes=["wr_sb"])
        for kc in range(16):
            p.dma("gpsimd", wb_bf[:, kc, :], wb[kc * 128:(kc + 1) * 128, :], writes=["wb_bf"])
        for kc in range(2):
            p.dma("gpsimd", wpl_bf[:, kc, :], wpl[kc * 128:(kc + 1) * 128, :], writes=["wpl_bf"])
        p.op("vector", lambda e: e.memset(ones512[:], 1.0 / 512), writes=["ones512"])
        xf = sb("xf", [128, 8, T1]); x_bf = sb("x_bf", [128, 8, T1], BF16); uf = sb("uf", [128, 8, T1])
        y_bf = sb("y_bf", [128, 16, T1], BF16); m_bf = sb("m_bf", [128, 8, T1], BF16); sq = sb("sq", [128, 2, T1])
        x1b = sb("x1b", [128, 8, T1], BF16); accb = sb("accb", [128, 8, T1])
        pf = sb("pf", [128, 2, T1], BF16)
        tm = {k: sb("tm_" + k, [128, T1]) for k in ["mean", "rstd", "t", "g", "mf", "t2", "rs"]}
        lg = sb("lg", [128, 32]); top8 = sb("top8", [128, 8]); nmx = sb("nmx", [128, 1]); msk = sb("msk", [128, 32])
        ex = sb("ex", [128, 32]); ssum = sb("ssum", [128, 1]); gt = sb("gt", [128, 32])
        pG, pB, pH, pS, pQ, pR, pT_, pP = PS
        xTv = xT.rearrange("(kc p) n -> p kc n", p=128); yTv = yT.rearrange("(kc p) n -> p kc n", p=128)
        pTv = pT.rearrange("(kc p) n -> p kc n", p=128)
        for t in range(ntok // T1):
            o = t * T1
            p.dma("sync", xf[:], xTv[:, :, o:o + T1], writes=["xf"])
            p.dma("gpsimd", x_bf[:], xTv[:, :, o:o + T1], writes=["x_bf"])
            p.dma("gpsimd", y_bf[:, 0:8, :], yTv[:, 0:8, o:o + T1], writes=["y_bf_a"])
            p.dma("sync", uf[:], yTv[:, 8:16, o:o + T1], writes=["uf"])
            p.dma("gpsimd", pf[:], pTv[:, :, o:o + T1], writes=["pf"])
            for g in range(2):
                for c in range(4):
                    cc = g * 4 + c; s = cc % 2
                    p.op("scalar", lambda e, cc=cc, s=s: e.activation(out=sq[:, s, :], in_=uf[:, cc, :], func=AF.Square), reads=["uf"], writes=[f"sq{s}"])
                    p.op("tensor", lambda e, c=c, s=s: e.matmul(pS[:, :T1], lhsT=ones512[:], rhs=sq[:, s, :], start=(c == 0), stop=(c == 3)),
                         reads=[f"sq{s}", "ones512"], writes=["pS"])
                rs = tm["rs"]
                p.op("vector", lambda e: e.tensor_scalar(out=rs[:], in0=pS[:, :T1], scalar1=1e-5, scalar2=None, op0=ALU.add), reads=["pS"], writes=["rs"])
                p.op("scalar", lambda e: e.activation(out=rs[:], in_=rs[:], func=AF.Sqrt), reads=["rs"], writes=["rs"])
                p.op("vector", lambda e: e.reciprocal(out=rs[:], in_=rs[:]), reads=["rs"], writes=["rs"])
                for c in range(4):
                    cc = g * 4 + c
                    p.op("vector", lambda e, cc=cc: e.scalar_tensor_tensor(out=y_bf[:, 8 + cc, :], in0=uf[:, cc, :], scalar=vecs[:, 4, cc:cc + 1], in1=rs[:], op0=ALU.mult, op1=ALU.mult),
                         reads=["uf", "rs", "vec"], writes=["y_bf_u"])
            brk = [(0, 4), (4, 8), (8, 16)]
            for oc in range(8):
                for b in range(3):
                    c0 = b * 1024 + oc * 128
                    for kc in range(8):
                        p.op("tensor", lambda e, kc=kc, c0=c0: e.matmul(pG[:, :T1], lhsT=wg_bf[:, kc, c0:c0 + 128], rhs=x_bf[:, kc, :], start=(kc == 0), stop=(kc == 7)),
                             reads=["wg_bf", "x_bf"], writes=["pG"])
                    p.op("scalar", lambda e: e.activation(out=tm["g"][:], in_=pG[:, :T1], func=AF.Sigmoid), reads=["pG"], writes=["tm_g"])
                    k0, k1 = brk[b]
                    for kc in range(k0, k1):
                        p.op("tensor", lambda e, kc=kc, k0=k0, k1=k1: e.matmul(pB[:, :T1], lhsT=wb_bf[:, kc, oc * 128:(oc + 1) * 128], rhs=y_bf[:, kc, :], start=(kc == k0), stop=(kc == k1 - 1)),
                             reads=["wb_bf", "y_bf_a", "y_bf_u"], writes=["pB"])
                    if b == 0:
                        p.op("vector", lambda e: e.tensor_tensor(out=tm["mf"][:], in0=tm["g"][:], in1=pB[:, :T1], op=ALU.mult), reads=["tm_g", "pB"], writes=["tm_mf"])
                    else:
                        p.op("vector", lambda e: e.tensor_tensor(out=tm["t2"][:], in0=tm["g"][:], in1=pB[:, :T1], op=ALU.mult), reads=["tm_g", "pB"], writes=["tm_t2"])
                        if b == 1:
                            p.op("vector", lambda e: e.tensor_tensor(out=tm["mf"][:], in0=tm["mf"][:], in1=tm["t2"][:], op=ALU.add), reads=["tm_mf", "tm_t2"], writes=["tm_mf"])
                        else:
                            p.op("vector", lambda e, oc=oc: e.tensor_tensor(out=m_bf[:, oc, :], in0=tm["mf"][:], in1=tm["t2"][:], op=ALU.add), reads=["tm_mf", "tm_t2"], writes=["m_bf"])
            for oc in range(8):
                for kc in range(8):
                    p.op("tensor", lambda e, kc=kc, oc=oc: e.matmul(pH[:, :T1], lhsT=wo_bf[:, kc, oc * 128:(oc + 1) * 128], rhs=m_bf[:, kc, :], start=(kc == 0), stop=(kc == 7)),
                         reads=["wo_bf", "m_bf"], writes=["pH"])
                p.op("vector", lambda e, oc=oc: e.scalar_tensor_tensor(out=xf[:, oc, :], in0=xf[:, oc, :], scalar=ALPHA, in1=pH[:, :T1], op0=ALU.mult, op1=ALU.add),
                     reads=["xf", "pH"], writes=["xf"])
            ln_fm(p, xf, "xf", 8, T1, lambda oc: vecs[:, 0, oc:oc + 1], lambda oc: vecs[:, 1, oc:oc + 1], ones_mean, sq, pS, pQ, tm, "pS", "pQ")
            p.op("scalar", lambda e: e.copy(out=x1b[:], in_=xf[:]), reads=["xf"], writes=["x1b"])
            p.dma("sync", x1bf_s[:, :, o:o + T1], x1b[:], reads=["x1b"], writes=["x1bf_s"])
            if dump:
                p.dma("sync", x1_d.rearrange("(kc p) n -> p kc n", p=128)[:, :, o:o + T1], xf[:], reads=["xf"], writes=["x1_d"])
            for s in range(T1 // 128):
                for kc in range(8):
                    p.op("tensor", lambda e, kc=kc, s=s: e.matmul(pR[:, :32], lhsT=xf[:, kc, s * 128:(s + 1) * 128], rhs=wr_sb[:, kc, :], start=(kc == 0), stop=False),
                         reads=["xf", "wr_sb"], writes=["pR"])
                p.op("tensor", lambda e: e.matmul(pR[:, :32], lhsT=ones_row[:, :], rhs=brow[:, :], start=False, stop=True), reads=["ones_row", "brow"], writes=["pR"])
                p.op("vector", lambda e: e.tensor_copy(out=lg[:], in_=pR[:, :32]), reads=["pR"], writes=["lg"])
                p.op("vector", lambda e: e.max(out=top8[:], in_=lg[:]), reads=["lg"], writes=["top8"])
                p.op("vector", lambda e: e.tensor_scalar(out=nmx[:], in0=top8[:, 0:1], scalar1=-1.0, scalar2=None, op0=ALU.mult), reads=["top8"], writes=["nmx"])
                p.op("vector", lambda e: e.tensor_scalar(out=msk[:], in0=lg[:], scalar1=top8[:, 3:4], scalar2=None, op0=ALU.is_ge), reads=["lg", "top8"], writes=["msk"])
                p.op("scalar", lambda e: e.activation(out=ex[:], in_=lg[:], func=AF.Exp, bias=nmx[:, 0:1], scale=1.0), reads=["lg", "nmx"], writes=["ex"])
                p.op("vector", lambda e: e.scalar_tensor_tensor(out=ex[:], in0=ex[:], scalar=1.0, in1=msk[:], op0=ALU.mult, op1=ALU.mult, accum_out=ssum[:, 0:1]),
                     reads=["ex", "msk"], writes=["ex", "ssum"])
                p.op("vector", lambda e: e.reciprocal(out=ssum[:], in_=ssum[:]), reads=["ssum"], writes=["ssum"])
                p.op("vector", lambda e: e.tensor_scalar(out=gt[:], in0=ex[:], scalar1=ssum[:, 0:1], scalar2=None, op0=ALU.mult), reads=["ex", "ssum"], writes=["gt"])
                p.op("tensor", lambda e: e.transpose(out=pT_[:32, :128], in_=gt[:], identity=ident[:]), reads=["gt", "ident"], writes=["pT"])
                oo = o + s * 128
                p.op("scalar", lambda e, oo=oo: e.copy(out=gateT[:, oo:oo + 128], in_=pT_[:32, :128]), reads=["pT"], writes=["gateT"])
            for oc in range(8):
                for kc in range(2):
                    p.op("tensor", lambda e, kc=kc, oc=oc: e.matmul(pP[:, :T1], lhsT=wpl_bf[:, kc, oc * 128:(oc + 1) * 128], rhs=pf[:, kc, :], start=(kc == 0), stop=(kc == 1)),
                         reads=["wpl_bf", "pf"], writes=["pP"])
                for kc in range(8):
                    p.op("tensor", lambda e, kc=kc, oc=oc: e.matmul(pG[:, :T1], lhsT=wplg_bf[:, kc, oc * 128:(oc + 1) * 128], rhs=x1b[:, kc, :], start=(kc == 0), stop=(kc == 7)),
                         reads=["wplg_bf", "x1b"], writes=["pG"])
                p.op("scalar", lambda e: e.activation(out=tm["g"][:], in_=pG[:, :T1], func=AF.Sigmoid), reads=["pG"], writes=["tm_g"])
                p.op("vector", lambda e: e.tensor_tensor(out=tm["t2"][:], in0=tm["g"][:], in1=pP[:, :T1], op=ALU.mult), reads=["tm_g", "pP"], writes=["tm_t2"])
                p.op("vector", lambda e, oc=oc: e.scalar_tensor_tensor(out=accb[:, oc, :], in0=xf[:, oc, :], scalar=ALPHA, in1=tm["t2"][:], op0=ALU.mult, op1=ALU.add),
                     reads=["xf", "tm_t2"], writes=["accb"])
            p.dma("sync", acc_s[:, :, o:o + T1], accb[:], reads=["accb"], writes=["acc_s"])
    if dump:
        p.dma("sync", gate_d[:, :], gateT[:], reads=["gateT"], writes=["gate_d"])
    p.barrier()

    H = ntok // 2
    with contextlib.ExitStack() as sc:
        def sb(name, shape, dt=F32):
            return sc.enter_context(nc.sbuf_tensor("s_" + name, list(shape), dt))
        x1h = sb("x1h", [128, 8, H], BF16); acc = sb("acc", [128, 8, H])
        wgu_b = [sb(f"wgu_b{i}", [128, 8, 2048], BF16) for i in range(2)]
        wd_b = [sb(f"wd_b{i}", [128, 8, 1024], BF16) for i in range(2)]
        bgu_sb = sb("bgu_sb", [128, 32, 16]); bd_sb = sb("bd_sb", [32, 1024]); ones32 = sb("ones32", [32, 128])
        act = [sb(f"act{c}", [128, 8, T2], BF16) for c in range(2)]; gbc = [sb(f"gbc{c}", [128, T2]) for c in range(2)]; gm = [sb(f"gm{c}", [32, T2]) for c in range(2)]
        glu = [sb(f"glu{c}", [128, T2]) for c in range(2)]; up1 = [sb(f"up1{c}", [128, T2]) for c in range(2)]; sg = [sb(f"sg{c}", [128, T2]) for c in range(2)]
        t1 = [sb(f"t1{c}", [128, T2]) for c in range(2)]; t2 = [sb(f"t2{c}", [128, T2]) for c in range(2)]
        sq = [t1[0], t2[0]]; tm = {"mean": glu[0], "rstd": up1[0], "t": sg[0]}
        print('moe sbuf remaining', nc.sbuf_bytes_remaining, flush=True)
        p.dma("sync", bgu_sb[:], bgu[:, :, :], writes=["bgu_sb"])
        p.dma("sync", bd_sb[:], bd[:, :], writes=["bd_sb"])
        p.op("vector", lambda e: e.memset(ones32[:], 1.0), writes=["ones32"])
        pBC = PS[6]; pS = PS[7]; pQ = PS[6]
        outv = outT.rearrange("(kc p) n -> p kc n", p=128)

        def moe_tile(ex_, tt, ho, wgs, wds, wb_i):
            c = tt
            pGl, pUp, pD = PS[3 * c], PS[3 * c + 1], PS[3 * c + 2]
            kGl, kUp, kD = f"pGl{c}", f"pUp{c}", f"pD{c}"
            to = tt * T2
            p.op("vector", lambda e: e.tensor_scalar(out=gm[c][:], in0=gateT[:, ho + to:ho + to + T2], scalar1=ident[0:32, ex_:ex_ + 1], scalar2=None, op0=ALU.mult),
                 reads=["gateT", "ident"], writes=[f"gm{c}"])
            pBCc, kBC = (PS[6], "pBC") if c == 0 else (PS[7], "pS7")
            p.op("tensor", lambda e: e.matmul(pBCc[:, :T2], lhsT=ones32[:], rhs=gm[c][:], start=True, stop=True), reads=["ones32", f"gm{c}"], writes=[kBC])
            p.op("scalar", lambda e: e.copy(out=gbc[c][:], in_=pBCc[:, :T2]), reads=[kBC], writes=[f"gbc{c}"])
            for oc in range(8):
                for kc in range(8):
                    p.op("tensor", lambda e, kc=kc, oc=oc: e.matmul(pGl[:, :T2], lhsT=wgs[:, kc, oc * 128:(oc + 1) * 128], rhs=x1h[:, kc, to:to + T2], start=(kc == 0), stop=(kc == 7)),
                         reads=[f"wgu{wb_i}", "x1h"], writes=[kGl])
                for kc in range(8):
                    p.op("tensor", lambda e, kc=kc, oc=oc: e.matmul(pUp[:, :T2], lhsT=wgs[:, kc, 1024 + oc * 128:1024 + (oc + 1) * 128], rhs=x1h[:, kc, to:to + T2], start=(kc == 0), stop=(kc == 7)),
                         reads=[f"wgu{wb_i}", "x1h"], writes=[kUp])
                p.op("vector", lambda e, oc=oc: e.tensor_scalar(out=glu[c][:], in0=pGl[:, :T2], scalar1=bgu_sb[:, ex_, oc:oc + 1], scalar2=7.0, op0=ALU.add, op1=ALU.min),
                     reads=[kGl, "bgu_sb"], writes=[f"glu{c}"])
                p.op("vector", lambda e, oc=oc: e.tensor_scalar(out=up1[c][:], in0=pUp[:, :T2], scalar1=bgu_sb[:, ex_, 8 + oc:9 + oc], scalar2=7.0, op0=ALU.add, op1=ALU.min),
                     reads=[kUp, "bgu_sb"], writes=[f"up1{c}"])
                p.op("gpsimd", lambda e: e.tensor_scalar(out=up1[c][:], in0=up1[c][:], scalar1=-7.0, scalar2=1.0, op0=ALU.max, op1=ALU.add), reads=[f"up1{c}"], writes=[f"up1{c}"])
                p.op("scalar", lambda e: e.activation(out=sg[c][:], in_=glu[c][:], func=AF.Sigmoid, scale=1.702), reads=[f"glu{c}"], writes=[f"sg{c}"])
                p.op("gpsimd", lambda e: e.tensor_tensor(out=t2[c][:], in0=up1[c][:], in1=gbc[c][:], op=ALU.mult), reads=[f"up1{c}", f"gbc{c}"], writes=[f"t2{c}"])
                p.op("vector", lambda e: e.tensor_tensor(out=t1[c][:], in0=glu[c][:], in1=sg[c][:], op=ALU.mult), reads=[f"glu{c}", f"sg{c}"], writes=[f"t1{c}"])
                p.op("vector", lambda e, oc=oc: e.tensor_tensor(out=act[c][:, oc, :], in0=t1[c][:], in1=t2[c][:], op=ALU.mult), reads=[f"t1{c}", f"t2{c}"], writes=[f"act{c}"])
            for oc in range(8):
                for kc in range(8):
                    p.op("tensor", lambda e, kc=kc, oc=oc: e.matmul(pD[:, :T2], lhsT=wds[:, kc, oc * 128:(oc + 1) * 128], rhs=act[c][:, kc, :], start=(kc == 0), stop=(kc == 7)),
                         reads=[f"wd{wb_i}", f"act{c}"], writes=[kD])
                p.op("vector", lambda e, oc=oc: e.tensor_tensor(out=acc[:, oc, to:to + T2], in0=acc[:, oc, to:to + T2], in1=pD[:, :T2], op=ALU.add),
                     reads=[f"acc{c}", kD], writes=[f"acc{c}"])

        for hf in range(2):
            ho = hf * H
            p.dma("sync", x1h[:], x1bf_s[:, :, ho:ho + H], reads=["x1bf_s"], writes=["x1h"])
            p.dma("sync", acc[:], acc_s[:, :, ho:ho + H], reads=["acc_s"], writes=["acc0", "acc1"])
            for tt in range(H // T2):
                to = tt * T2
                for oc in range(8):
                    pDc = PS[3 * tt + 2]
                    p.op("tensor", lambda e, oc=oc, to=to, pDc=pDc: e.matmul(pDc[:, :T2], lhsT=bd_sb[:, oc * 128:(oc + 1) * 128], rhs=gateT[:, ho + to:ho + to + T2], start=True, stop=True),
                         reads=["bd_sb", "gateT"], writes=[f"pD{tt}"])
                    p.op("vector", lambda e, oc=oc, to=to, pDc=pDc: e.tensor_tensor(out=acc[:, oc, to:to + T2], in0=acc[:, oc, to:to + T2], in1=pDc[:, :T2], op=ALU.add),
                         reads=[f"acc{tt}", f"pD{tt}"], writes=[f"acc{tt}"])
            for ex_ in range(ne):
                wb_i = ex_ % 2
                wgs, wds = wgu_b[wb_i], wd_b[wb_i]
                for kc in (range(0, 8, 2) if not (os.environ.get("B_NODMA") and ex_ >= 2) else []):
                    p.dma("gpsimd", wgs[:, kc:kc + 2, :], wgu[ex_, kc * 128:(kc + 2) * 128, :].rearrange("(k p) n -> p k n", p=128), writes=[f"wgu{wb_i}"])
                for kc in (range(0, 8, 4) if not (os.environ.get("B_NODMA") and ex_ >= 2) else []):
                    p.dma("gpsimd", wds[:, kc:kc + 4, :], wd[ex_, kc * 128:(kc + 4) * 128, :].rearrange("(k p) n -> p k n", p=128), writes=[f"wd{wb_i}"])
                chains = [p.capture(moe_tile, ex_, tt, ho, wgs, wds, wb_i) for tt in range(H // T2)]
                p.emit_interleaved(chains)
            for tt in range(H // T2):
                to = tt * T2
                hv = acc[:, :, to:to + T2]
                ln_fm(p, hv, f"acc{tt}", 8, T2, lambda oc: vecs[:, 2, oc:oc + 1], lambda oc: vecs[:, 3, oc:oc + 1], ones_mean, sq, pS, pQ, tm, "pS7", "pBC")
                p.dma("sync", outv[:, :, ho + to:ho + to + T2], hv, reads=[f"acc{tt}"], writes=["outT"])
            p.barrier()
    p.finish_wait("sync", ["outT"] + (["x1_d", "gate_d"] if dump else []))
    return p.build()


def build_L0(ntok=2048):
    p = Prog(); nc = p.nc
    xT = p.dram("xT", [1024, ntok]); vec = p.dram("vec", [128, 2, 8])
    outT = p.dram("outT", [1024, ntok], kind="ExternalOutput")
    ones_mean = p.sb("ones_mean", [128, 128]); vecs = p.sb("vecs", [128, 2, 8])
    p.dma("sync", vecs[:], vec[:, :, :], writes=["vec"])
    p.op("vector", lambda e: e.memset(ones_mean[:], 1.0 / 1024), writes=["ones_mean"])
    TT = 512
    h = [p.sb(f"h{i}", [128, 8, TT]) for i in range(2)]
    sq = p.sb("sq", [128, 2, TT]); tm = {k: p.sb("tm_" + k, [128, TT]) for k in ["mean", "rstd", "t"]}
    pS = p.ps("pS", [128, 512]); pQ = p.ps("pQ", [128, 512])
    xv = xT.rearrange("(kc p) n -> p kc n", p=128); ov = outT.rearrange("(kc p) n -> p kc n", p=128)
    for t in range(ntok // TT):
        o = t * TT; i = t % 2
        p.dma("sync", h[i][:], xv[:, :, o:o + TT], writes=[f"h{i}"])
        ln_fm(p, h[i], f"h{i}", 8, TT, lambda oc: vecs[:, 0, oc:oc + 1], lambda oc: vecs[:, 1, oc:oc + 1], ones_mean, sq, pS, pQ, tm, "pS", "pQ")
        p.dma("gpsimd", ov[:, :, o:o + TT], h[i][:], reads=[f"h{i}"], writes=["outT"])
    p.finish_wait("sync", ["outT"])
    return p.build()


def host_inputs_B(L, stream, ya, yb, u, z, c):
    sl = slice(c * 2048, (c + 1) * 2048)
    f = lambda a: np.ascontiguousarray(a, dtype=np.float32)
    ycat = np.concatenate([ya[sl], yb[sl], u[sl]], axis=1)
    vec = np.stack([z['ln1_g'][L].reshape(8, 128).T, z['ln1_b'][L].reshape(8, 128).T, z['ln2_g'][L].reshape(8, 128).T,
                    z['ln2_b'][L].reshape(8, 128).T, z['ssd_norm'][L].reshape(8, 128).T], axis=1)
    return {
        "xT": f(stream[sl].T), "yT": f(ycat.T), "pT": f(z['p'][L].reshape(-1, 256)[sl].T),
        "wg": f(z['w_in'][L][:, 6576:]), "wb": f(z['w_branch'][L]), "wo": f(z['w_o'][L]), "wplg": f(z['w_pl_gate'][L]),
        "wpl": f(z['w_pl'][L]), "wr": f(z['w_router'][L]), "br": f(z['b_router'][L][None]),
        "wgu": f(z['w_gu'][L]), "wd": f(z['w_down'][L]), "bgu": f(z['b_gu'][L].reshape(32, 16, 128).transpose(2, 0, 1)),
        "bd": f(z['b_down'][L]), "vec": f(vec), "ident": np.eye(128, dtype=np.float32),
    }


def kernel(**inputs):
    z = {k: np.asarray(v) for k, v in inputs.items()}
    NCORE = 8
    cores = list(range(NCORE))
    xf = z['x'].reshape(-1, 1024).astype(np.float32)
    f = lambda a: np.ascontiguousarray(a, dtype=np.float32)
    vec0 = f(np.stack([z['ln_in_g'].reshape(8, 128).T, z['ln_in_b'].reshape(8, 128).T], axis=1))
    nc0 = build_L0()
    res = run_bass_kernel_spmd(nc0, [{"xT": f(xf[c * 2048:(c + 1) * 2048].T), "vec": vec0} for c in cores], core_ids=cores)
    stream = np.concatenate([r["outT"].T for r in res.results], axis=0)
    for L in range(2):
        ncA = build_A(T=8192)
        imA = [host_inputs_A(z, L, stream[b * 8192:(b + 1) * 8192], j) for b in range(2) for j in range(4)]
        resA = run_bass_kernel_spmd(ncA, imA, core_ids=cores).results
        ya = np.concatenate([np.concatenate([resA[b * 4 + j]["ya"] for j in range(4)], axis=1) for b in range(2)], axis=0)
        yb = np.concatenate([np.concatenate([resA[b * 4 + j]["yb"] for j in range(4)], axis=1) for b in range(2)], axis=0)
        u = np.concatenate([np.concatenate([resA[b * 4 + j]["yc"] for j in range(4)], axis=1) for b in range(2)], axis=0)
        del resA, imA
        ncB = build_B()
        imB = [host_inputs_B(L, stream, ya, yb, u, z, c) for c in cores]
        resB = run_bass_kernel_spmd(ncB, imB, core_ids=cores).results
        stream = np.concatenate([r["outT"].T for r in resB], axis=0)
        del resB, imB
    return np.ascontiguousarray(stream.reshape(2, 8192, 1024), dtype=np.float32)
```

```python
import os
import contextlib, time
import numpy as np
import concourse.bass as bass
import concourse.mybir as mybir
from concourse.bass_utils import run_bass_kernel_spmd

F32 = mybir.dt.float32
BF16 = mybir.dt.bfloat16
I32 = mybir.dt.int32
ALU = mybir.AluOpType
AF = mybir.ActivationFunctionType
AX = mybir.AxisListType

ENG = ["sync", "gpsimd", "scalar", "vector", "tensor"]
NDMASEM = 6
import os as _os
ATTACH = bool(int(_os.environ.get('FW_ATTACH', '1')))


class Prog:
    def __init__(self, immediate=True):
        self.immediate = immediate
        self.nc = bass.Bass("TRN2", target_bir_lowering=False)
        try:
            self.nc.allow_low_precision("bf16 matmul operands with fp32 accumulation")
            self.nc.allow_non_contiguous_dma("strided layouts")
        except Exception as ex:
            print("allow_* failed", ex)
        self.st = contextlib.ExitStack()
        self.ops = {e: [] for e in ENG}
        self.cnt = {}
        self.sems = {}
        self.lastw = {}
        self.reads = {}
        self.seen = {e: {} for e in ENG}
        self.dma_i = {e: 0 for e in ENG}
        self.dma_last = {}
        self.ninstr = 0
        for e in ["gpsimd", "scalar", "vector", "tensor"]:
            self._sem("c_" + e)
        for e in ["sync", "gpsimd", "scalar"]:
            for i in range(NDMASEM):
                self._sem(f"d_{e}_{i}")

    def _sem(self, name):
        self.sems[name] = self.st.enter_context(self.nc.semaphore(name))
        self.cnt[name] = 0

    def dram(self, name, shape, dt=F32, kind="ExternalInput"):
        return self.nc.dram_tensor(name, list(shape), dt, kind=kind).ap()

    def sb(self, name, shape, dt=F32):
        return self.st.enter_context(self.nc.sbuf_tensor("s_" + name, list(shape), dt))

    def ps(self, name, shape, dt=F32):
        return self.st.enter_context(self.nc.psum_tensor("p_" + name, list(shape), dt))

    def _deps(self, eng, reads, writes):
        need = {}
        def add(tok):
            if tok is None:
                return
            s, v = tok
            if need.get(s, 0) < v:
                need[s] = v
        for k in reads:
            add(self.lastw.get(k))
        for k in writes:
            add(self.lastw.get(k))
            for t in self.reads.get(k, ()):
                add(t)
        out = []
        for s, v in need.items():
            if self.seen[eng].get(s, 0) < v:
                self.seen[eng][s] = v
                out.append((s, v))
        return out

    def _commit(self, tok, reads, writes):
        for k in reads:
            self.reads.setdefault(k, []).append(tok)
        for k in writes:
            self.lastw[k] = tok
            self.reads[k] = []

    def capture(self, f, *a):
        self._buf = []
        try:
            f(*a)
        finally:
            buf, self._buf = self._buf, None
        return buf

    def emit_interleaved(self, chains):
        chains = [list(c) for c in chains if c]
        idx = [0] * len(chains)
        live = True
        while live:
            live = False
            for ci, c in enumerate(chains):
                if idx[ci] < len(c):
                    kind, a, kw = c[idx[ci]]; idx[ci] += 1; live = True
                    (self.op if kind == "op" else self.dma)(*a, **kw)

    def emit_balanced(self, chains):
        chains = [list(c) for c in chains if c]
        idx = [0] * len(chains)
        total = sum(len(c) for c in chains)
        for _ in range(total):
            ci = min((i for i in range(len(chains)) if idx[i] < len(chains[i])), key=lambda i: idx[i] / len(chains[i]))
            kind, a, kw = chains[ci][idx[ci]]; idx[ci] += 1
            (self.op if kind == "op" else self.dma)(*a, **kw)

    def op(self, eng, fn, reads=(), writes=()):
        if getattr(self, "_buf", None) is not None:
            self._buf.append(("op", (eng, fn, reads, writes), {})); return None
        psr = [k for k in reads if isinstance(k, str) and k.startswith("ps")]
        if psr:
            reads = [k for k in reads if k not in psr]
            writes = list(writes) + psr
        waits = self._deps(eng, reads, writes)
        s = "c_" + eng
        self.cnt[s] += 1
        tok = (s, self.cnt[s])
        self._commit(tok, reads, writes)
        self._emit(eng, waits, fn, s, 1)
        self.ninstr += 1
        return tok

    def dma(self, eng, out, in_, reads=(), writes=(), **kw):
        if getattr(self, "_buf", None) is not None:
            self._buf.append(("dma", (eng, out, in_, reads, writes), kw)); return None
        slot = self.dma_i[eng] % NDMASEM
        self.dma_i[eng] += 1
        s = f"d_{eng}_{slot}"
        waits = self._deps(eng, reads, writes)
        prev = self.cnt[s]
        if prev > 0 and self.seen[eng].get(s, 0) < prev:
            self.seen[eng][s] = prev
            waits.append((s, prev))
        self.cnt[s] += 16
        tok = (s, self.cnt[s])
        self._commit(tok, reads, writes)
        fn = lambda e, out=out, in_=in_, kw=kw: e.dma_start(out=out, in_=in_, **kw)
        self._emit(eng, waits, fn, s, 16)
        self.ninstr += 1
        return tok

    def coll(self, kind, in_ap, out_ap, groups, reads=(), writes=()):
        eng = "gpsimd"
        slot = self.dma_i[eng] % NDMASEM
        self.dma_i[eng] += 1
        s = f"d_{eng}_{slot}"
        waits = self._deps(eng, reads, writes)
        prev = self.cnt[s]
        if prev > 0 and self.seen[eng].get(s, 0) < prev:
            self.seen[eng][s] = prev
            waits.append((s, prev))
        self.cnt[s] += 16
        tok = (s, self.cnt[s])
        self._commit(tok, reads, writes)
        fn = lambda e: e.collective_compute(kind, ALU.bypass, replica_groups=groups, ins=[in_ap], outs=[out_ap])
        self._emit(eng, waits, fn, s, 16)
        self.ninstr += 1
        return tok

    def barrier(self):
        for eng in ENG:
            waits = []
            for sname, v in self.cnt.items():
                if v > 0 and self.seen[eng].get(sname, 0) < v:
                    self.seen[eng][sname] = v
                    waits.append((sname, v))
            self._emit(eng, waits, None, None, 0)

    def finish_wait(self, eng, keys):
        waits = self._deps(eng, keys, ())
        self._emit(eng, waits, None, None, 0)

    def _emit(self, eng, waits, fn, s, inc):
        if not self.immediate:
            self.ops[eng].append((waits, fn, s, inc)); return
        engobj = getattr(self.nc, eng)
        if fn is None or not ATTACH:
            for (ws, wv) in waits:
                engobj.wait_ge(self.sems[ws], wv)
            if fn is not None:
                fn(engobj).then_inc(self.sems[s], inc)
            return
        for (ws, wv) in waits[1:]:
            engobj.wait_ge(self.sems[ws], wv)
        ins = fn(engobj)
        if waits:
            ins._wait_ge(self.sems[waits[0][0]], waits[0][1])
        ins.then_inc(self.sems[s], inc)

    def build(self):
        if self.immediate:
            self.st.close(); return self.nc
        nc = self.nc
        with nc.Block() as block:
            def mk(e):
                def body(engobj):
                    for waits, fn, s, inc in self.ops[e]:
                        for (ws, wv) in waits:
                            engobj.wait_ge(self.sems[ws], wv)
                        if fn is not None:
                            fn(engobj).then_inc(self.sems[s], inc)
                return body
            block.sync(mk("sync"))
            block.gpsimd(mk("gpsimd"))
            block.scalar(mk("scalar"))
            block.vector(mk("vector"))
            block.tensor(mk("tensor"))
        self.st.close()
        return nc


NCOL = 2060
NEG = -30000.0
RV = {}
_o = 0
for _n, _l in [("gconv", 5 * 384), ("sconv", 5 * 512), ("sconvb", 512), ("mup", 768), ("mun", 768), ("spb", 10), ("alog", 10),
               ("gnorm", 128), ("w0", 256), ("a0", 256), ("kk", 128), ("ka", 128), ("rk", 128), ("lng", 128), ("lnb", 128), ("dsk", 256)]:
    RV[_n] = (_o, _l); _o += _l
NV = _o


def host_inputs_A(z, L, stream_b, j):
    f = lambda a: np.ascontiguousarray(a, dtype=np.float32)
    w_in = z['w_in'][L]
    g = j // 2
    GD0, RW0, SS0 = 0, 2064, 2064 + 1920
    r = lambda a, n: list(range(a, a + n))
    cols = (r(GD0 + j * 128, 128) + r(GD0 + 512 + j * 128, 128) + r(GD0 + 1024 + j * 128, 128) + r(GD0 + 1536 + j * 128, 128)
            + r(RW0 + j * 128, 128) + r(RW0 + 512 + j * 128, 128) + r(RW0 + 1024 + j * 128, 128) + r(RW0 + 1536, 384)
            + r(SS0 + j * 256, 256) + r(SS0 + 1024 + j * 256, 256) + r(SS0 + 2048 + g * 128, 128) + r(SS0 + 2304 + g * 128, 128)
            + [GD0 + 2048 + d * 4 + j for d in range(2)] + [GD0 + 2056 + d * 4 + j for d in range(2)]
            + [SS0 + 2560 + d * 16 + 4 * j + i for d in range(2) for i in range(4)])
    assert len(cols) == NCOL
    rv = np.zeros(NV, np.float32)
    def put(n, a):
        o, l = RV[n]; a = np.asarray(a, np.float32).reshape(-1); assert a.size == l, (n, a.size, l); rv[o:o + l] = a
    qkv_idx = r(j * 128, 128) + r(512 + j * 128, 128) + r(1024 + j * 128, 128)
    xbc_idx = r(j * 256, 256) + r(1024 + g * 128, 128) + r(1280 + g * 128, 128)
    rw_idx = r(j * 128, 128) + r(512 + j * 128, 128) + r(1024 + j * 128, 128) + r(1536, 384)
    put("gconv", z['gdn_conv'][L][:, qkv_idx]); put("sconv", z['ssd_conv'][L][:, xbc_idx]); put("sconvb", z['ssd_conv_b'][L][xbc_idx])
    put("mup", z['rwkv_mu_prev'][L][rw_idx]); put("mun", z['rwkv_mu_next'][L][rw_idx])
    put("spb", np.concatenate([z['gdn_dt_bias'][L][:, j], z['ssd_dt_bias'][L][:, 4 * j:4 * j + 4].reshape(-1)]))
    put("alog", np.concatenate([z['gdn_a_log'][L][:, j], z['ssd_a_log'][L][:, 4 * j:4 * j + 4].reshape(-1)]))
    put("gnorm", z['gdn_norm'][L]); put("w0", z['rwkv_w0'][L][:, j * 128:(j + 1) * 128]); put("a0", z['rwkv_a0'][L][:, j * 128:(j + 1) * 128])
    put("kk", z['rwkv_k_k'][L][j * 128:(j + 1) * 128]); put("ka", z['rwkv_k_a'][L][j * 128:(j + 1) * 128])
    put("rk", z['rwkv_r_k'][L][2 * j:2 * j + 2]); put("lng", z['rwkv_ln_g'][L][j * 128:(j + 1) * 128]); put("lnb", z['rwkv_ln_b'][L][j * 128:(j + 1) * 128])
    put("dsk", np.repeat(z['ssd_d'][L][4 * j:4 * j + 4], 64))
    k = np.arange(128)
    UT = (k[:, None] <= k[None, :]).astype(np.float32); LT = (k[:, None] >= k[None, :]).astype(np.float32)
    blk = (k[:, None] // 64 == k[None, :] // 64).astype(np.float32)
    bdUT = UT * blk; bdLT = LT * blk
    msk = np.stack([UT, LT, np.where(UT > 0, 0.0, NEG), np.where(LT > 0, 0.0, NEG), np.eye(128), np.ones((128, 128)), bdUT, bdLT, -(bdUT - np.eye(128)), -(bdLT - np.eye(128)), bdUT - np.eye(128), bdLT - np.eye(128)], axis=1)
    return {
        "xT": f(stream_b.T), "wc": f(w_in[:, cols]), "rowvec": f(rv[None]),
        "wup": f(z['rwkv_w_up'][L][:, :, j * 128:(j + 1) * 128].reshape(128, 128)),
        "aup": f(z['rwkv_a_up'][L][:, :, j * 128:(j + 1) * 128].reshape(128, 128)),
        "gup": f(z['rwkv_g_up'][L][:, j * 128:(j + 1) * 128]), "msk": f(msk),
    }


def build_A(T=8192, dump=False, NS=16, phases=(1, 2, 3, 4)):
    p = Prog(); nc = p.nc
    NTL = T // 128
    dk = "ExternalOutput" if dump else "Internal"
    xT = p.dram("xT", [1024, T]); wc = p.dram("wc", [1024, NCOL]); rowvec = p.dram("rowvec", [1, NV])
    wup = p.dram("wup", [128, 128]); aup = p.dram("aup", [128, 128]); gup = p.dram("gup", [128, 128]); mskd = p.dram("msk", [128, 12, 128])
    ya_o = p.dram("ya", [T, 128], kind="ExternalOutput"); yb_o = p.dram("yb", [T, 128], kind="ExternalOutput"); yc_o = p.dram("yc", [T, 256], kind="ExternalOutput")
    cols_s = p.dram("cols_s", [T + 4, NCOL], kind=dk)
    gkq_s = p.dram("gkq_s", [T, 2, 128], kind=dk); gsc_s = p.dram("gsc_s", [T, 4], kind=dk); gbvT_s = p.dram("gbvT_s", [2, 128, T], kind=dk)
    rw_s = p.dram("rw_s", [2, 2, T, 5, 64], kind=dk); rvT_s = p.dram("rvT_s", [128, T], kind=dk); rpost_s = p.dram("rpost_s", [T, 258], kind=dk)
    ssd_s = p.dram("ssd_s", [T, 528], kind=dk)
    gy_s = p.dram("gy_s", [2, T, 128], kind=dk); gkqv_s = p.dram("gkqv_s", [T, 3, 128], kind=dk); gbg_s = p.dram("gbg_s", [T, 4], kind=dk); ry_s = p.dram("ry_s", [2, T, 128], kind=dk); sy_s = p.dram("sy_s", [T, 256], kind=dk)

    V = lambda fn, r=(), w=(): p.op("vector", fn, r, w)
    G = lambda fn, r=(), w=(): p.op("gpsimd", fn, r, w)
    S = lambda fn, r=(), w=(): p.op("scalar", fn, r, w)
    PE = lambda fn, r=(), w=(): p.op("tensor", fn, r, w)

    msk = p.sb("msk", [128, 12, 128]); rvs = p.sb("rvs", [128, NV])
    p.dma("sync", msk[:], mskd[:, :, :], writes=["msk"])
    p.dma("sync", rvs[:], rowvec.partition_broadcast(128)[:, 0, :], writes=["rvs"])
    UT, LT, NEGf, NEGb, ident, ones, bdUT, bdLT, nbdUTs, nbdLTs, sbdUT, sbdLT = (msk[:, i, :] for i in range(12))
    def rv(n, a=0, l=None):
        o, ln = RV[n]
        return rvs[:, o + a:o + a + (ln - a if l is None else l)]
    negexp = p.sb("negexp", [128, 10])
    S(lambda e: e.activation(out=negexp[:], in_=rv("alog"), func=AF.Exp), ["rvs"], ["negexp"])
    V(lambda e: e.tensor_scalar(out=negexp[:], in0=negexp[:], scalar1=-1.0, scalar2=None, op0=ALU.mult), ["negexp"], ["negexp"])
    PS = [p.ps(f"ps{i}", [128, 512]) for i in range(8)]

    if 1 in phases:
        with contextlib.ExitStack() as sc:
            sb = lambda name, shape, dt=F32: sc.enter_context(nc.sbuf_tensor("s_" + name, list(shape), dt))
            W_bf = sb("W_bf", [128, 8, 2048], BF16); w_sm = sb("w_sm", [128, 8, 12])
            zt = sb("zt", [2, NCOL])
            xb = [sb(f"xb{i}", [128, 8, 128], BF16) for i in range(2)]; xf = [sb(f"xf{i}", [128, 8, 128]) for i in range(2)]
            ct = [sb(f"ct{i}", [128, NCOL]) for i in range(2)]
            wcv = wc.rearrange("(kc p) n -> p kc n", p=128)
            for kc in range(8):
                p.dma("gpsimd", W_bf[:, kc, :], wc[kc * 128:(kc + 1) * 128, 0:2048], writes=["W_bf"])
            p.dma("sync", w_sm[:], wcv[:, :, 2048:2060], writes=["w_sm"])
            V(lambda e: e.memset(zt[:], 0.0), [], ["zt"])
            p.dma("sync", cols_s[0:2, :], zt[:], reads=["zt"], writes=["cols_pad"])
            p.dma("sync", cols_s[T + 2:T + 4, :], zt[:], reads=["zt"], writes=["cols_pad"])
            xTv = xT.rearrange("(kc p) n -> p kc n", p=128)
            for tt in range(NTL):
                t0 = tt * 128; i = tt % 2
                p.dma("gpsimd", xb[i][:], xTv[:, :, t0:t0 + 128], writes=[f"xb{i}"])
                p.dma("sync", xf[i][:], xTv[:, :, t0:t0 + 128], writes=[f"xf{i}"])
                for gq in range(4):
                    pp = PS[gq]
                    for kc in range(8):
                        PE(lambda e, kc=kc, gq=gq, pp=pp, i=i: e.matmul(pp[:, :], lhsT=xb[i][:, kc, :], rhs=W_bf[:, kc, gq * 512:(gq + 1) * 512], start=(kc == 0), stop=(kc == 7)),
                           [f"xb{i}", "W_bf"], [f"ps{gq}"])
                    if gq % 2 == 0:
                        S(lambda e, gq=gq, pp=pp, i=i: e.copy(out=ct[i][:, gq * 512:(gq + 1) * 512], in_=pp[:, :]), [f"ps{gq}"], [f"ct{i}"])
                    else:
                        V(lambda e, gq=gq, pp=pp, i=i: e.tensor_copy(out=ct[i][:, gq * 512:(gq + 1) * 512], in_=pp[:, :]), [f"ps{gq}"], [f"ct{i}"])
                for kc in range(8):
                    PE(lambda e, kc=kc, i=i: e.matmul(PS[4][:, :12], lhsT=xf[i][:, kc, :], rhs=w_sm[:, kc, :], start=(kc == 0), stop=(kc == 7)),
                       [f"xf{i}", "w_sm"], ["ps4"])
                V(lambda e, i=i: e.tensor_copy(out=ct[i][:, 2048:2060], in_=PS[4][:, :12]), ["ps4"], [f"ct{i}"])
                p.dma("sync", cols_s[2 + t0:2 + t0 + 128, :], ct[i][:], reads=[f"ct{i}"], writes=["cols_s"])
        p.barrier()

    if 2 in phases:
        with contextlib.ExitStack() as sc:
            sb = lambda name, shape, dt=F32: sc.enter_context(nc.sbuf_tensor("s_" + name, list(shape), dt))
            win = [sb(f"win{j}", [128, NCOL]) for j in range(5)]
            wup_sb = sb("wup_sb", [128, 128]); aup_sb = sb("aup_sb", [128, 128]); gup_sb = sb("gup_sb", [128, 128])
            p.dma("sync", wup_sb[:], wup[:, :], writes=["wup_sb"]); p.dma("sync", aup_sb[:], aup[:, :], writes=["aup_sb"]); p.dma("sync", gup_sb[:], gup[:, :], writes=["gup_sb"])
            cacc = sb("cacc", [128, 896]); ctmp = sb("ctmp", [128, 896]); qkv = sb("qkv", [128, 384]); xbc = sb("xbc", [128, 528])
            junk = sb("junk", [128, 128]); ssq = sb("ssq", [128, 4]); kq = sb("kq", [128, 2, 128])
            spx = sb("spx", [128, 10]); spa = sb("spa", [128, 10]); spl = sb("spl", [128, 10]); beta = sb("beta", [128, 2]); gsc = sb("gsc", [128, 4])
            bv = sb("bv", [128, 2, 128]); trs = sb("trs", [128, 128]); bg = sb("bg", [128, 4])
            sh = sb("sh", [128, 768]); d1 = sb("d1", [128, 768]); d2 = sb("d2", [128, 768])
            tw = sb("tw", [128, 128]); twT = sb("twT", [128, 128]); alT = sb("alT", [128, 128]); sgl = sb("sgl", [128, 128]); sgT = sb("sgT", [128, 128])
            RW = [sb(f"RW{d}", [128, 5, 128]) for d in range(2)]; ad = [sb(f"ad{d}", [128, 128]) for d in range(2)]
            wraw = sb("wraw", [128, 128]); kx = sb("kx", [128, 128]); sqk = sb("sqk", [128, 128]); rkk = sb("rkk", [128, 2]); kkn = sb("kkn", [128, 128])
            rkr = sb("rkr", [128, 128]); prod = sb("prod", [128, 128]); bon = sb("bon", [128, 2, 2]); rpost = sb("rpost", [128, 258]); t128 = sb("t128", [128, 128])
            pT1, pT2, pM1, pM2, pT3 = PS[0], PS[1], PS[2], PS[3], PS[4]
            for tt in range(NTL):
                t0 = tt * 128
                for j in range(5):
                    p.dma("sync" if j % 2 == 0 else "scalar", win[j][:], cols_s[t0 + j:t0 + j + 128, :], reads=["cols_s", "cols_pad"], writes=[f"win{j}"])
                cur = win[2]
                for (c0, c1, o0, cname, cw) in [(0, 384, 0, "gconv", 384), (1536, 2048, 384, "sconv", 512)]:
                    for j in range(5):
                        wj = rv(cname, j * cw, cw)
                        if j == 0:
                            V(lambda e, c0=c0, c1=c1, o0=o0, wj=wj, cw=cw: e.tensor_tensor(out=cacc[:, o0:o0 + cw], in0=win[0][:, c0:c1], in1=wj, op=ALU.mult), ["win0", "rvs"], [f"cacc{o0}"])
                        else:
                            G(lambda e, c0=c0, c1=c1, o0=o0, wj=wj, cw=cw, j=j: e.tensor_tensor(out=ctmp[:, o0:o0 + cw], in0=win[j][:, c0:c1], in1=wj, op=ALU.mult), [f"win{j}", "rvs"], [f"ctmp{o0}"])
                            V(lambda e, o0=o0, cw=cw: e.tensor_tensor(out=cacc[:, o0:o0 + cw], in0=cacc[:, o0:o0 + cw], in1=ctmp[:, o0:o0 + cw], op=ALU.add), [f"cacc{o0}", f"ctmp{o0}"], [f"cacc{o0}"])
                V(lambda e: e.tensor_tensor(out=cacc[:, 384:896], in0=cacc[:, 384:896], in1=rv("sconvb"), op=ALU.add), ["cacc384", "rvs"], ["cacc384"])
                S(lambda e: e.activation(out=qkv[:], in_=cacc[:, 0:384], func=AF.Silu), ["cacc0"], ["qkv"])
                S(lambda e: e.activation(out=xbc[:, 0:512], in_=cacc[:, 384:896], func=AF.Silu), ["cacc384"], ["xbc"])
                V(lambda e: e.tensor_tensor(out=spx[:], in0=cur[:, 2050:2060], in1=rv("spb"), op=ALU.add), ["win2", "rvs"], ["spx"])
                S(lambda e: e.activation(out=spa[:], in_=spx[:], func=AF.Abs), ["spx"], ["spa"])
                S(lambda e: e.activation(out=spa[:], in_=spa[:], func=AF.Exp, scale=-1.0), ["spa"], ["spa"])
                S(lambda e: e.activation(out=spl[:], in_=spa[:], func=AF.Ln, bias=1.0), ["spa"], ["spl"])
                V(lambda e: e.tensor_scalar(out=spx[:], in0=spx[:], scalar1=0.0, scalar2=None, op0=ALU.max), ["spx"], ["spx"])
                V(lambda e: e.tensor_tensor(out=spx[:], in0=spx[:], in1=spl[:], op=ALU.add), ["spx", "spl"], ["spx"])
                V(lambda e: e.tensor_tensor(out=spl[:], in0=spx[:], in1=negexp[:], op=ALU.mult), ["spx", "negexp"], ["spl"])
                V(lambda e: e.tensor_copy(out=xbc[:, 512:520], in_=spx[:, 2:10]), ["spx"], ["xbc"])
                V(lambda e: e.tensor_copy(out=xbc[:, 520:528], in_=spl[:, 2:10]), ["spl"], ["xbc"])
                p.dma("gpsimd", ssd_s[t0:t0 + 128, :], xbc[:], reads=["xbc"], writes=["ssd_s"])
                S(lambda e: e.activation(out=beta[:], in_=cur[:, 2048:2050], func=AF.Sigmoid), ["win2"], ["beta"])
                S(lambda e: e.activation(out=gsc[:, 0:2], in_=spl[:, 0:2], func=AF.Exp), ["spl"], ["gsc"])
                V(lambda e: e.scalar_tensor_tensor(out=gsc[:, 2:4], in0=gsc[:, 0:2], scalar=-1.0, in1=beta[:], op0=ALU.mult, op1=ALU.mult), ["gsc", "beta"], ["gsc"])
                for qi in range(2):
                    src = qkv[:, qi * 128:(qi + 1) * 128]
                    V(lambda e, src=src, qi=qi: e.scalar_tensor_tensor(out=junk[:], in0=src, scalar=1.0, in1=src, op0=ALU.mult, op1=ALU.mult, accum_out=ssq[:, qi:qi + 1]), ["qkv"], ["junk", "ssq"])
                V(lambda e: e.tensor_scalar(out=ssq[:, 0:2], in0=ssq[:, 0:2], scalar1=1e-6, scalar2=None, op0=ALU.add), ["ssq"], ["ssq"])
                S(lambda e: e.activation(out=ssq[:, 0:2], in_=ssq[:, 0:2], func=AF.Sqrt), ["ssq"], ["ssq"])
                V(lambda e: e.reciprocal(out=ssq[:, 0:2], in_=ssq[:, 0:2]), ["ssq"], ["ssq"])
                V(lambda e: e.tensor_scalar(out=kq[:, 0, :], in0=qkv[:, 128:256], scalar1=ssq[:, 1:2], scalar2=None, op0=ALU.mult), ["qkv", "ssq"], ["kq"])
                V(lambda e: e.tensor_scalar(out=kq[:, 1, :], in0=qkv[:, 0:128], scalar1=ssq[:, 0:1], scalar2=128 ** -0.5, op0=ALU.mult, op1=ALU.mult), ["qkv", "ssq"], ["kq"])
                p.dma("gpsimd", gkqv_s[t0:t0 + 128, 0:2, :], kq[:], reads=["kq"], writes=["gkqv_s"])
                p.dma("gpsimd", gkqv_s[t0:t0 + 128, 2, :], qkv[:, 256:384], reads=["qkv"], writes=["gkqv_s"])
                G(lambda e: e.tensor_copy(out=bg[:, 0:2], in_=beta[:]), ["beta"], ["bg"])
                G(lambda e: e.tensor_copy(out=bg[:, 2:4], in_=spl[:, 0:2]), ["spl"], ["bg"])
                p.dma("gpsimd", gbg_s[t0:t0 + 128, :], bg[:], reads=["bg"], writes=["gbg_s"])
                c_, pv, nx = cur[:, 512:1280], win[1][:, 512:1280], win[3][:, 512:1280]
                V(lambda e: e.tensor_tensor(out=d1[:], in0=pv, in1=c_, op=ALU.subtract), ["win1", "win2"], ["d1"])
                G(lambda e: e.tensor_tensor(out=d1[:], in0=d1[:], in1=rv("mup"), op=ALU.mult), ["d1", "rvs"], ["d1"])
                V(lambda e: e.tensor_tensor(out=d2[:], in0=nx, in1=c_, op=ALU.subtract), ["win3", "win2"], ["d2"])
                G(lambda e: e.tensor_tensor(out=d2[:], in0=d2[:], in1=rv("mun"), op=ALU.mult), ["d2", "rvs"], ["d2"])
                V(lambda e: e.tensor_tensor(out=sh[:], in0=c_, in1=d1[:], op=ALU.add), ["win2", "d1"], ["sh"])
                V(lambda e: e.tensor_tensor(out=sh[:], in0=sh[:], in1=d2[:], op=ALU.add), ["sh", "d2"], ["sh"])
                r_, k_, v_, wl, al, gl = (sh[:, i * 128:(i + 1) * 128] for i in range(6))
                S(lambda e: e.activation(out=tw[:], in_=wl, func=AF.Tanh), ["sh"], ["tw"])
                PE(lambda e: e.transpose(out=pT1[:, :128], in_=tw[:], identity=ident), ["tw", "msk"], ["ps0"])
                S(lambda e: e.copy(out=twT[:], in_=pT1[:, :128]), ["ps0"], ["twT"])
                PE(lambda e: e.transpose(out=pT2[:, :128], in_=al, identity=ident), ["sh", "msk"], ["ps1"])
                V(lambda e: e.tensor_copy(out=alT[:], in_=pT2[:, :128]), ["ps1"], ["alT"])
                S(lambda e: e.activation(out=sgl[:], in_=gl, func=AF.Sigmoid), ["sh"], ["sgl"])
                PE(lambda e: e.transpose(out=pT3[:, :128], in_=sgl[:], identity=ident), ["sgl", "msk"], ["ps4"])
                V(lambda e: e.tensor_copy(out=sgT[:], in_=pT3[:, :128]), ["ps4"], ["sgT"])
                PE(lambda e: e.matmul(pT3[:, 128:256], lhsT=sgT[:], rhs=gup_sb[:], start=True, stop=True), ["sgT", "gup_sb"], ["ps4"])
                S(lambda e: e.copy(out=rpost[:, 128:256], in_=pT3[:, 128:256]), ["ps4"], ["rpost"])
                V(lambda e: e.tensor_tensor(out=kx[:], in0=k_, in1=rv("kk"), op=ALU.mult), ["sh", "rvs"], ["kx"])
                S(lambda e: e.activation(out=sqk[:], in_=kx[:], func=AF.Square), ["kx"], ["sqk"])
                V(lambda e: e.tensor_reduce(out=rkk[:], in_=sqk[:].rearrange("p (h n) -> p h n", h=2), axis=AX.X, op=ALU.add), ["sqk"], ["rkk"])
                V(lambda e: e.tensor_scalar(out=rkk[:], in0=rkk[:], scalar1=1e-6, scalar2=None, op0=ALU.add), ["rkk"], ["rkk"])
                S(lambda e: e.activation(out=rkk[:], in_=rkk[:], func=AF.Sqrt), ["rkk"], ["rkk"])
                V(lambda e: e.reciprocal(out=rkk[:], in_=rkk[:]), ["rkk"], ["rkk"])
                for h in range(2):
                    V(lambda e, h=h: e.tensor_scalar(out=kkn[:, h * 64:(h + 1) * 64], in0=kx[:, h * 64:(h + 1) * 64], scalar1=rkk[:, h:h + 1], scalar2=None, op0=ALU.mult), ["kx", "rkk"], ["kkn"])
                G(lambda e: e.tensor_tensor(out=rkr[:], in0=r_, in1=rv("rk"), op=ALU.mult), ["sh", "rvs"], ["rkr"])
                for d in range(2):
                    hs = slice(d * 64, (d + 1) * 64)
                    PE(lambda e, hs=hs: e.matmul(pM1[:, :128], lhsT=twT[hs, :], rhs=wup_sb[hs, :], start=True, stop=True), ["twT", "wup_sb"], ["ps2"])
                    V(lambda e, d=d: e.tensor_tensor(out=wraw[:], in0=pM1[:, :128], in1=rv("w0", d * 128, 128), op=ALU.add), ["ps2", "rvs"], ["wraw"])
                    S(lambda e: e.activation(out=wraw[:], in_=wraw[:], func=AF.Sigmoid), ["wraw"], ["wraw"])
                    S(lambda e, d=d: e.activation(out=RW[d][:, 0, :], in_=wraw[:], func=AF.Exp, scale=-0.6065306597126334), ["wraw"], [f"RW{d}"])
                    PE(lambda e, hs=hs: e.matmul(pM2[:, :128], lhsT=alT[hs, :], rhs=aup_sb[hs, :], start=True, stop=True), ["alT", "aup_sb"], ["ps3"])
                    V(lambda e, d=d: e.tensor_tensor(out=ad[d][:], in0=pM2[:, :128], in1=rv("a0", d * 128, 128), op=ALU.add), ["ps3", "rvs"], [f"ad{d}"])
                    S(lambda e, d=d: e.activation(out=ad[d][:], in_=ad[d][:], func=AF.Sigmoid), [f"ad{d}"], [f"ad{d}"])
                    G(lambda e, d=d: e.tensor_copy(out=RW[d][:, 1, :], in_=kkn[:]), ["kkn"], [f"RW{d}"])
                    V(lambda e, d=d: e.scalar_tensor_tensor(out=RW[d][:, 2, :], in0=kkn[:], scalar=-1.0, in1=ad[d][:], op0=ALU.mult, op1=ALU.mult), ["kkn", f"ad{d}"], [f"RW{d}"])
                    V(lambda e, d=d: e.scalar_tensor_tensor(out=t128[:], in0=ad[d][:], scalar=-1.0, in1=rv("ka"), op0=ALU.add, op1=ALU.mult), [f"ad{d}", "rvs"], ["t128"])
                    V(lambda e, d=d: e.scalar_tensor_tensor(out=RW[d][:, 3, :], in0=t128[:], scalar=1.0, in1=k_, op0=ALU.add, op1=ALU.mult), ["t128", "sh"], [f"RW{d}"])
                    G(lambda e, d=d: e.tensor_copy(out=RW[d][:, 4, :], in_=r_), ["sh"], [f"RW{d}"])
                    V(lambda e, d=d: e.tensor_tensor(out=prod[:], in0=rkr[:], in1=RW[d][:, 3, :], op=ALU.mult), ["rkr", f"RW{d}"], ["prod"])
                    V(lambda e, d=d: e.tensor_reduce(out=bon[:, d, :], in_=prod[:].rearrange("p (h n) -> p h n", h=2), axis=AX.X, op=ALU.add), ["prod"], ["bon"])
                    for h in range(2):
                        p.dma("gpsimd", rw_s[d, h, t0:t0 + 128, :, :], RW[d][:, :, h * 64:(h + 1) * 64], reads=[f"RW{d}"], writes=["rw_s"])
                V(lambda e: e.tensor_tensor(out=rpost[:, 256:258], in0=bon[:, 0, :], in1=bon[:, 1, :], op=ALU.add), ["bon"], ["rpost"])
                G(lambda e: e.tensor_copy(out=rpost[:, 0:128], in_=v_), ["sh"], ["rpost"])
                p.dma("gpsimd", rpost_s[t0:t0 + 128, :], rpost[:], reads=["rpost"], writes=["rpost_s"])
                PE(lambda e: e.transpose(out=pT2[:, :128], in_=v_, identity=ident), ["sh", "msk"], ["ps1"])
                V(lambda e: e.tensor_copy(out=t128[:], in_=pT2[:, :128]), ["ps1"], ["t128"])
                p.dma("gpsimd", rvT_s[:, t0:t0 + 128], t128[:], reads=["t128"], writes=["rvT_s"])
        p.barrier()
    fin = ["ya", "yb", "yc"]
    if dump:
        fin += ["cols_s", "gkqv_s", "gbg_s", "rw_s", "rvT_s", "rpost_s", "ssd_s", "gy_s", "ry_s"]
    if 3 in phases:
        phase3(p, T, NS, locals())
    if 4 in phases:
        phase4(p, T, locals())
    p.finish_wait("sync", [k for k in fin if k in p.lastw])
    return p.build()


def phase3(p, T, NS, L):
    SSDSTOP = int(os.environ.get('A_SSDSTOP', '99'))
    GSTOP = int(os.environ.get('A_GSTOP', '99'))
    nc = p.nc
    V = L["V"]; G = L["G"]; S = L["S"]; PE = L["PE"]; PS = L["PS"]
    UT, LT, ident, ones = L["UT"], L["LT"], L["ident"], L["ones"]
    gkqv_s, gbg_s, rw_s, rvT_s, ssd_s, gy_s, ry_s, sy_s, cols_s, yc_o = (L[k] for k in
        ["gkqv_s", "gbg_s", "rw_s", "rvT_s", "ssd_s", "gy_s", "ry_s", "sy_s", "cols_s", "yc_o"])
    bdUT, bdLT, nbdUTs, nbdLTs, sbdUT, sbdLT = (L[k_] for k_ in ["bdUT", "bdLT", "nbdUTs", "nbdLTs", "sbdUT", "sbdLT"])
    rpost_s = L["rpost_s"]
    rv = L["rv"]
    NTL = T // 128; NCH = T // NS
    with contextlib.ExitStack() as sc:
        sb = lambda name, shape, dt=F32: sc.enter_context(nc.sbuf_tensor("s_" + name, list(shape), dt))
        RB = []
        for d in range(2):
            r_ = {}
            for nm, shp in [("rwt", [128, 5, 128]), ("vt", [128, 128]), ("lw", [128, 128]), ("lp", [128, 128]), ("Pt", [128, 128]), ("iP", [128, 128]), ("Pm", [128, 128]),
                            ("rt", [128, 128]), ("kt", [128, 128]), ("nbt", [128, 128]), ("ct", [128, 128]), ("rtF", [128, 128]), ("nbF", [128, 128]), ("cF", [128, 128]), ("PF", [128, 128]),
                            ("X", [128, 128]), ("XT", [128, 128]), ("AckT", [128, 128]), ("ArkT", [128, 128]), ("AnrbT", [128, 128]),
                            ("P0", [128, 128]), ("P1", [128, 128]), ("PT0", [128, 128]), ("PT1", [128, 128]), ("TT", [128, 128]),
                            ("Zs", [128, 64]), ("Ms", [128, 64]), ("Yt", [128, 128]), ("H", [128, 64])]:
                r_[nm] = sb(f"r{d}_{nm}", shp)
            for nm in ["cFh", "nbFh", "ktFh", "rtFh"]:
                for h in range(2):
                    r_[f"{nm}{h}"] = sb(f"r{d}_{nm}{h}", [128, 128])
                    V(lambda e, t_=r_[f"{nm}{h}"]: e.memset(t_[:], 0.0), [], [f"r{d}_{nm}{h}"])
            for nm in ["ktS", "nbS"]:
                for ch in range(2):
                    for h in range(2):
                        r_[f"{nm}{ch}{h}"] = sb(f"r{d}_{nm}{ch}{h}", [128, 128])
                        V(lambda e, t_=r_[f"{nm}{ch}{h}"]: e.memset(t_[:], 0.0), [], [f"r{d}_{nm}{ch}{h}"])
            V(lambda e, r_=r_: e.memset(r_["H"][:], 0.0), [], [f"r{d}_H"])
            V(lambda e, r_=r_: e.memset(r_["Zs"][:], 0.0), [], [f"r{d}_Zs"])
            V(lambda e, r_=r_: e.memset(r_["Ms"][:], 0.0), [], [f"r{d}_Ms"])
            RB.append(r_)
        GB = []
        for d in range(2):
            g_ = {}
            for nm, shp in [("kqv", [128, 3, 128]), ("bg", [128, 4]), ("kF", [128, 128]), ("qF", [128, 128]), ("Gs", [128, 128]), ("QKs", [128, 128]),
                            ("gcc", [128, 1]), ("ngcc", [128, 1]), ("R", [128, 128]), ("grow", [128, 128]), ("egrow", [128, 128]), ("dT", [128, 128]), ("dN", [128, 128]),
                            ("Bd", [128, 128]), ("brow", [128, 128]), ("X", [128, 128]), ("XT", [128, 128]), ("P0", [128, 128]), ("P1", [128, 128]),
                            ("PT0", [128, 128]), ("PT1", [128, 128]), ("TT", [128, 128]), ("vb", [128, 128]), ("kbg", [128, 128]), ("be", [128, 1]), ("eg", [128, 1]),
                            ("u", [128, 128]), ("wF", [128, 128]), ("qdF", [128, 128]), ("QKm", [128, 128]), ("vnew", [128, 128]), ("kdA", [128, 128]), ("kdB", [128, 128]),
                            ("dl", [128, 1]), ("dlA", [128, 1]), ("dlB", [128, 1]), ("S", [128, 128]), ("o", [128, 128]), ("t1", [128, 128]), ("t2", [128, 128])]:
                g_[nm] = sb(f"g{d}_{nm}", shp)
            GB.append(g_)
            V(lambda e, g_=g_: e.memset(g_["S"][:], 0.0), [], [f"g{d}_S"])
            V(lambda e, g_=g_: e.memset(g_["vnew"][:], 0.0), [], [f"g{d}_vnew"])
        sxt = sb("sxt", [128, 528]); BCF = sb("BCF", [128, 256]); scT = sb("scT", [128, 128]); acol = sb("acol", [128, 4]); nacol = sb("nacol", [128, 4])
        Rm = sb("Rm", [128, 4, 128]); E = sb("E", [128, 4, 128]); Dm = sb("Dm", [128, 128]); M = sb("M", [128, 4, 128]); CdF = sb("CdF", [128, 4, 128])
        xdt = sb("xdt", [128, 256]); xend = sb("xend", [128, 256]); dend = sb("dend", [128, 4]); H = sb("H", [128, 256]); yt = sb("yt", [128, 256])
        syf = sb("syf", [128, 256]); zt = sb("zts", [128, 256]); xd = sb("xd", [128, 256]); szs = sb("szs", [128, 256])
        pA, pSc, pAc, pRow, pY, pSt = PS[0][:, 0:256], PS[0][:, 256:384], PS[0][:, 384:512], PS[1], PS[2][:, 0:256], PS[2][:, 256:512]
        print('phase3 sbuf remaining', nc.sbuf_bytes_remaining, flush=True)

        def gdn_chunk(d, c):
            t0 = c * 128
            B_ = GB[d]; K = lambda n: f"g{d}_{n}"
            bank = PS[6 + d]; kb = f"ps{6 + d}"
            regs = [bank[:, i * 128:(i + 1) * 128] for i in range(4)] + [PS[3][:, d * 256:d * 256 + 128], PS[3][:, d * 256 + 128:d * 256 + 256]]
            keys = [kb] * 4 + ["ps3", "ps3"]
            (rA, rB, rC, rD, rE, rF), (kA, kB, kC, kD, kE, kF_) = regs, keys
            fwd = (d == 0)
            Mtri = bdUT if fwd else bdLT
            MaskT = Mtri
            MaskN = bdLT if fwd else bdUT
            nST = nbdUTs if fwd else nbdLTs
            nSN = nbdLTs if fwd else nbdUTs
            selA = bdLT[:, 0:1]; selB = bdUT[:, 127:128]
            hA, hB = slice(0, 64), slice(64, 128)
            if fwd:
                first, second, lastF, lastS = hA, hB, 63, 127
            else:
                first, second, lastF, lastS = hB, hA, 64, 0
            kqv, bg = B_["kqv"], B_["bg"]
            p.dma("sync", kqv[:], gkqv_s[t0:t0 + 128, :, :], reads=["gkqv_s"], writes=[K("kqv")])
            p.dma("sync", bg[:], gbg_s[t0:t0 + 128, :], reads=["gbg_s"], writes=[K("bg")])
            kc, qc, vc = kqv[:, 0, :], kqv[:, 1, :], kqv[:, 2, :]
            beta = bg[:, d:d + 1]; g = bg[:, 2 + d:3 + d]
            kF, qF, Gs, QKs = B_["kF"], B_["qF"], B_["Gs"], B_["QKs"]
            PE(lambda e: e.transpose(out=rA, in_=kc, identity=ident), [K("kqv"), "msk"], [kA])
            S(lambda e: e.copy(out=kF[:], in_=rA), [kA], [K("kF")])
            PE(lambda e: e.transpose(out=rB, in_=qc, identity=ident), [K("kqv"), "msk"], [kB])
            S(lambda e: e.copy(out=qF[:], in_=rB), [kB], [K("qF")])
            PE(lambda e: e.matmul(rC, lhsT=kF[:], rhs=kF[:], start=True, stop=True), [K("kF")], [kC])
            S(lambda e: e.copy(out=Gs[:], in_=rC), [kC], [K("Gs")])
            PE(lambda e: e.matmul(rD, lhsT=kF[:], rhs=qF[:], start=True, stop=True), [K("kF"), K("qF")], [kD])
            S(lambda e: e.copy(out=QKs[:], in_=rD), [kD], [K("QKs")])
            G(lambda e: e.tensor_scalar(out=B_["R"][:], in0=Mtri, scalar1=g, scalar2=None, op0=ALU.mult), [K("bg"), "msk"], [K("R")])
            PE(lambda e: e.matmul(rE, lhsT=ones, rhs=B_["R"][:], start=True, stop=True), [K("R"), "msk"], [kE])
            S(lambda e: e.copy(out=B_["grow"][:], in_=rE), [kE], [K("grow")])
            S(lambda e: e.activation(out=B_["egrow"][:], in_=rE, func=AF.Exp), [kE], [K("egrow")])
            V(lambda e: e.scalar_tensor_tensor(out=B_["t1"][:], in0=B_["grow"][:], scalar=1.0, in1=ident, op0=ALU.mult, op1=ALU.mult, accum_out=B_["gcc"][:, 0:1]), [K("grow"), "msk"], [K("t1"), K("gcc")])
            S(lambda e: e.mul(out=B_["ngcc"][:], in_=B_["gcc"][:], mul=-1.0), [K("gcc")], [K("ngcc")])
            for nm, bias_k, sc, Mk in [("dT", "ngcc", 1.0, MaskT), ("dN", "gcc", -1.0, MaskN)]:
                S(lambda e, nm=nm, bias_k=bias_k, sc=sc: e.activation(out=B_[nm][:], in_=B_["grow"][:], func=AF.Identity, bias=B_[bias_k][:, 0:1], scale=sc), [K("grow"), K(bias_k)], [K(nm)])
                G(lambda e, nm=nm: e.tensor_scalar(out=B_[nm][:], in0=B_[nm][:], scalar1=0.0, scalar2=None, op0=ALU.min), [K(nm)], [K(nm)])
                S(lambda e, nm=nm: e.activation(out=B_[nm][:], in_=B_[nm][:], func=AF.Exp), [K(nm)], [K(nm)])
                G(lambda e, nm=nm, Mk=Mk: e.tensor_tensor(out=B_[nm][:], in0=B_[nm][:], in1=Mk, op=ALU.mult), [K(nm), "msk"], [K(nm)])
            G(lambda e: e.tensor_scalar(out=B_["Bd"][:], in0=ident, scalar1=beta, scalar2=None, op0=ALU.mult), [K("bg"), "msk"], [K("Bd")])
            PE(lambda e: e.matmul(rF, lhsT=ones, rhs=B_["Bd"][:], start=True, stop=True), [K("Bd"), "msk"], [kF_])
            S(lambda e: e.copy(out=B_["brow"][:], in_=rF), [kF_], [K("brow")])
            G(lambda e: e.tensor_tensor(out=B_["t1"][:], in0=Gs[:], in1=B_["dT"][:], op=ALU.mult), [K("Gs"), K("dT"), K("t1")], [K("t1")])
            G(lambda e: e.tensor_tensor(out=B_["t1"][:], in0=B_["t1"][:], in1=B_["brow"][:], op=ALU.mult), [K("t1"), K("brow")], [K("t1")])
            G(lambda e: e.tensor_tensor(out=B_["XT"][:], in0=B_["t1"][:], in1=nST, op=ALU.mult), [K("t1"), "msk"], [K("XT")])
            G(lambda e: e.tensor_tensor(out=B_["t2"][:], in0=Gs[:], in1=B_["dN"][:], op=ALU.mult), [K("Gs"), K("dN")], [K("t2")])
            G(lambda e: e.tensor_scalar(out=B_["t2"][:], in0=B_["t2"][:], scalar1=beta, scalar2=None, op0=ALU.mult), [K("t2"), K("bg")], [K("t2")])
            G(lambda e: e.tensor_tensor(out=B_["X"][:], in0=B_["t2"][:], in1=nSN, op=ALU.mult), [K("t2"), "msk"], [K("X")])
            G(lambda e: e.tensor_tensor(out=B_["TT"][:], in0=B_["XT"][:], in1=ident, op=ALU.add), [K("XT"), "msk"], [K("TT")])
            if GSTOP == 1: return
            Pc, PTc, kP, kPT = B_["X"], B_["XT"], K("X"), K("XT")
            for lv in range(1, 6):
                Pn, kPn = B_[f"P{lv % 2}"], K(f"P{lv % 2}")
                PE(lambda e, Pc=Pc, PTc=PTc: e.matmul(rA, lhsT=PTc[:], rhs=Pc[:], start=True, stop=True), [kP, kPT], [kA])
                S(lambda e, Pn=Pn: e.copy(out=Pn[:], in_=rA), [kA], [kPn])
                if lv < 5:
                    PTn, kPTn = B_[f"PT{lv % 2}"], K(f"PT{lv % 2}")
                    PE(lambda e, Pc=Pc, PTc=PTc: e.matmul(rB, lhsT=Pc[:], rhs=PTc[:], start=True, stop=True), [kP, kPT], [kB])
                    S(lambda e, PTn=PTn: e.copy(out=PTn[:], in_=rB), [kB], [kPTn])
                PE(lambda e, Pn=Pn: e.matmul(rC, lhsT=Pn[:], rhs=B_["TT"][:], start=True, stop=True), [kPn, K("TT")], [kC])
                V(lambda e: e.tensor_tensor(out=B_["TT"][:], in0=B_["TT"][:], in1=rC, op=ALU.add), [K("TT"), kC], [K("TT")])
                Pc, kP = Pn, kPn
                if lv < 5:
                    PTc, kPT = PTn, kPTn
            if GSTOP == 2: return
            S(lambda e: e.activation(out=B_["eg"][:], in_=B_["gcc"][:], func=AF.Exp), [K("gcc")], [K("eg")])
            G(lambda e: e.tensor_tensor(out=B_["be"][:], in0=B_["eg"][:], in1=beta, op=ALU.mult), [K("eg"), K("bg")], [K("be")])
            G(lambda e: e.tensor_scalar(out=B_["vb"][:], in0=vc, scalar1=beta, scalar2=None, op0=ALU.mult), [K("kqv"), K("bg")], [K("vb")])
            G(lambda e: e.tensor_scalar(out=B_["kbg"][:], in0=kc, scalar1=B_["be"][:, 0:1], scalar2=None, op0=ALU.mult), [K("kqv"), K("be")], [K("kbg")])
            PE(lambda e: e.matmul(rD, lhsT=B_["TT"][:], rhs=B_["vb"][:], start=True, stop=True), [K("TT"), K("vb")], [kD])
            S(lambda e: e.copy(out=B_["u"][:], in_=rD), [kD], [K("u")])
            PE(lambda e: e.matmul(rE, lhsT=B_["kbg"][:], rhs=B_["TT"][:], start=True, stop=True), [K("kbg"), K("TT")], [kE])
            S(lambda e: e.copy(out=B_["wF"][:], in_=rE), [kE], [K("wF")])
            G(lambda e: e.tensor_tensor(out=B_["qdF"][:], in0=qF[:], in1=B_["egrow"][:], op=ALU.mult), [K("qF"), K("egrow")], [K("qdF")])
            G(lambda e: e.tensor_tensor(out=B_["QKm"][:], in0=QKs[:], in1=B_["dT"][:], op=ALU.mult), [K("QKs"), K("dT")], [K("QKm")])
            for hs, lst in [(first, lastF), (second, lastS)]:
                S(lambda e, hs=hs, lst=lst: e.activation(out=B_["dl"][hs, :], in_=B_["gcc"][hs, :], func=AF.Exp, bias=B_["grow"][hs, lst:lst + 1], scale=-1.0), [K("gcc"), K("grow")], [K("dl")])
            G(lambda e: e.tensor_tensor(out=B_["dlA"][:], in0=B_["dl"][:], in1=selA, op=ALU.mult), [K("dl"), "msk"], [K("dlA")])
            G(lambda e: e.tensor_tensor(out=B_["dlB"][:], in0=B_["dl"][:], in1=selB, op=ALU.mult), [K("dl"), "msk"], [K("dlB")])
            G(lambda e: e.tensor_scalar(out=B_["kdA"][:], in0=kc, scalar1=B_["dlA"][:, 0:1], scalar2=None, op0=ALU.mult), [K("kqv"), K("dlA")], [K("kdA")])
            G(lambda e: e.tensor_scalar(out=B_["kdB"][:], in0=kc, scalar1=B_["dlB"][:, 0:1], scalar2=None, op0=ALU.mult), [K("kqv"), K("dlB")], [K("kdB")])
            kdF, kdS, kkF, kkS = (B_["kdA"], B_["kdB"], K("kdA"), K("kdB")) if fwd else (B_["kdB"], B_["kdA"], K("kdB"), K("kdA"))
            if GSTOP == 3: return
            Sst = B_["S"]
            for (hs, lst, kd_, kkd, rW, kW, rO, kO) in [(first, lastF, kdF, kkF, rA, kA, rC, kC), (second, lastS, kdS, kkS, rB, kB, rD, kD)]:
                PE(lambda e, rW=rW: e.matmul(rW, lhsT=B_["wF"][:], rhs=Sst[:], start=True, stop=True), [K("wF"), K("S")], [kW])
                V(lambda e, hs=hs, rW=rW: e.tensor_tensor(out=B_["vnew"][hs, :], in0=B_["u"][hs, :], in1=rW[hs, :], op=ALU.subtract), [K("u"), kW], [K("vnew")])
                PE(lambda e, rO=rO: e.matmul(rO, lhsT=B_["qdF"][:], rhs=Sst[:], start=True, stop=True), [K("qdF"), K("S")], [kO])
                S(lambda e, hs=hs, rO=rO: e.copy(out=B_["o"][hs, :], in_=rO[hs, :]), [kO], [K("o")])
                PE(lambda e, kd_=kd_: e.matmul(rF, lhsT=kd_[:], rhs=B_["vnew"][:], start=True, stop=True), [kkd, K("vnew")], [kF_])
                V(lambda e, lst=lst: e.scalar_tensor_tensor(out=Sst[:], in0=Sst[:], scalar=B_["egrow"][:, lst:lst + 1], in1=rF, op0=ALU.mult, op1=ALU.add), [K("S"), K("egrow"), kF_], [K("S")])
            if GSTOP == 5: return
            PE(lambda e: e.matmul(rE, lhsT=B_["QKm"][:], rhs=B_["vnew"][:], start=True, stop=True), [K("QKm"), K("vnew")], [kE])
            V(lambda e: e.tensor_tensor(out=B_["o"][:], in0=B_["o"][:], in1=rE, op=ALU.add), [K("o"), kE], [K("o")])
            p.dma("gpsimd", gy_s[d, t0:t0 + 128, :], B_["o"][:], reads=[K("o")], writes=["gy_s"])

        def rwkv_block(d, c):
            t0 = c * 128
            B_ = RB[d]; K = lambda n: f"r{d}_{n}"
            bank = PS[4 + d]; kb = f"ps{4 + d}"
            rA, rB, rC, rD = (bank[:, i * 128:(i + 1) * 128] for i in range(4))
            fwd = (d == 0)
            Mtri = bdUT if fwd else bdLT
            mS_N = sbdLT if fwd else sbdUT
            mS_T = sbdUT if fwd else sbdLT
            mI_T = bdUT if fwd else bdLT
            selc = [bdLT[:, 0:1], bdUT[:, 127:128]]
            hA, hB = slice(0, 64), slice(64, 128)
            order = [(0, hA, 63), (1, hB, 127)] if fwd else [(1, hB, 64), (0, hA, 0)]
            rwt, vt = B_["rwt"], B_["vt"]
            for h in range(2):
                p.dma("sync", rwt[:, :, h * 64:(h + 1) * 64], rw_s[d, h, t0:t0 + 128, :, :], reads=["rw_s"], writes=[K("rwt")])
            p.dma("sync", vt[:], rpost_s[t0:t0 + 128, 0:128], reads=["rpost_s"], writes=[K("vt")])
            w_, kk_, nkka_, kd_, r_ = (rwt[:, i, :] for i in range(5))
            S(lambda e: e.activation(out=B_["lw"][:], in_=w_, func=AF.Ln), [K("rwt")], [K("lw")])
            PE(lambda e: e.matmul(rA, lhsT=Mtri, rhs=B_["lw"][:], start=True, stop=True), [K("lw"), "msk"], [kb])
            S(lambda e: e.copy(out=B_["lp"][:], in_=rA), [kb], [K("lp")])
            S(lambda e: e.activation(out=B_["Pt"][:], in_=rA, func=AF.Exp), [kb], [K("Pt")])
            S(lambda e: e.activation(out=B_["iP"][:], in_=rA, func=AF.Exp, scale=-1.0), [kb], [K("iP")])
            G(lambda e: e.tensor_tensor(out=B_["Pm"][:], in0=B_["lp"][:], in1=B_["lw"][:], op=ALU.subtract), [K("lp"), K("lw")], [K("Pm")])
            S(lambda e: e.activation(out=B_["Pm"][:], in_=B_["Pm"][:], func=AF.Exp), [K("Pm")], [K("Pm")])
            G(lambda e: e.tensor_tensor(out=B_["rt"][:], in0=r_, in1=B_["Pt"][:], op=ALU.mult), [K("rwt"), K("Pt")], [K("rt")])
            G(lambda e: e.tensor_tensor(out=B_["kt"][:], in0=kd_, in1=B_["iP"][:], op=ALU.mult), [K("rwt"), K("iP")], [K("kt")])
            G(lambda e: e.tensor_tensor(out=B_["nbt"][:], in0=nkka_, in1=B_["iP"][:], op=ALU.mult), [K("rwt"), K("iP")], [K("nbt")])
            G(lambda e: e.tensor_tensor(out=B_["ct"][:], in0=kk_, in1=B_["Pm"][:], op=ALU.mult), [K("rwt"), K("Pm")], [K("ct")])
            for src, full, hm in [("rt", "rtF", "rtFh"), ("nbt", "nbF", "nbFh"), ("ct", "cF", "cFh"), ("kt", None, "ktFh"), ("Pt", "PF", None)]:
                PE(lambda e, src=src: e.transpose(out=rB, in_=B_[src][:], identity=ident), [K(src), "msk"], [kb])
                if full is not None:
                    S(lambda e, full=full: e.copy(out=B_[full][:], in_=rB), [kb], [K(full)])
                if hm is not None:
                    for h, hs in [(0, hA), (1, hB)]:
                        S(lambda e, hm=hm, h=h, hs=hs: e.copy(out=B_[f"{hm}{h}"][hs, :], in_=rB[hs, :]), [kb], [K(f"{hm}{h}")])
            for ch in range(2):
                for h in range(2):
                    cs = slice(h * 64, (h + 1) * 64)
                    G(lambda e, ch=ch, h=h, cs=cs: e.tensor_scalar(out=B_[f"ktS{ch}{h}"][:, cs], in0=B_["kt"][:, cs], scalar1=selc[ch], scalar2=None, op0=ALU.mult), [K("kt"), "msk"], [K(f"ktS{ch}{h}")])
                    G(lambda e, ch=ch, h=h, cs=cs: e.tensor_scalar(out=B_[f"nbS{ch}{h}"][:, cs], in0=B_["nbt"][:, cs], scalar1=selc[ch], scalar2=None, op0=ALU.mult), [K("nbt"), "msk"], [K(f"nbS{ch}{h}")])
            def head(h):
                hr = hA if h == 0 else hB
                cFh, nbFh, ktFh, rtFh = (B_[f"{n}{h}"] for n in ["cFh", "nbFh", "ktFh", "rtFh"])
                kcF, knbF, kktF, krtF = (K(f"{n}{h}") for n in ["cFh", "nbFh", "ktFh", "rtFh"])
                for (nm, l_, kl, r__, kr, mk) in [("X", cFh, kcF, "nbF", K("nbF"), mS_N), ("XT", nbFh, knbF, "cF", K("cF"), mS_T), ("AckT", ktFh, kktF, "cF", K("cF"), mS_T),
                                                   ("ArkT", ktFh, kktF, "rtF", K("rtF"), mI_T), ("AnrbT", nbFh, knbF, "rtF", K("rtF"), mI_T)]:
                    PE(lambda e, l_=l_, r__=r__: e.matmul(rC, lhsT=l_[:], rhs=B_[r__][:], start=True, stop=True), [kl, kr], [kb])
                    V(lambda e, nm=nm, mk=mk: e.tensor_tensor(out=B_[nm][:], in0=rC, in1=mk, op=ALU.mult), [kb, "msk"], [K(nm)])
                G(lambda e: e.tensor_tensor(out=B_["TT"][:], in0=B_["XT"][:], in1=ident, op=ALU.add), [K("XT"), "msk"], [K("TT")])
                Pc, PTc, kP, kPT = B_["X"], B_["XT"], K("X"), K("XT")
                for lv in range(1, 6):
                    Pn, kPn = B_[f"P{lv % 2}"], K(f"P{lv % 2}")
                    PE(lambda e, Pc=Pc, PTc=PTc: e.matmul(rA, lhsT=PTc[:], rhs=Pc[:], start=True, stop=True), [kP, kPT], [kb])
                    S(lambda e, Pn=Pn: e.copy(out=Pn[:], in_=rA), [kb], [kPn])
                    if lv < 5:
                        PTn, kPTn = B_[f"PT{lv % 2}"], K(f"PT{lv % 2}")
                        PE(lambda e, Pc=Pc, PTc=PTc: e.matmul(rB, lhsT=Pc[:], rhs=PTc[:], start=True, stop=True), [kP, kPT], [kb])
                        S(lambda e, PTn=PTn: e.copy(out=PTn[:], in_=rB), [kb], [kPTn])
                    PE(lambda e, Pn=Pn: e.matmul(rC, lhsT=Pn[:], rhs=B_["TT"][:], start=True, stop=True), [kPn, K("TT")], [kb])
                    V(lambda e: e.tensor_tensor(out=B_["TT"][:], in0=B_["TT"][:], in1=rC, op=ALU.add), [K("TT"), kb], [K("TT")])
                    Pc, kP = Pn, kPn
                    if lv < 5:
                        PTc, kPT = PTn, kPTn
                Vh = vt[:, hr]
                H = B_["H"]
                for (ch, hs, lst) in order:
                    PE(lambda e: e.matmul(rD[:, 0:64], lhsT=cFh[:], rhs=H[:], start=True, stop=False), [kcF, K("H")], [kb])
                    PE(lambda e: e.matmul(rD[:, 0:64], lhsT=B_["AckT"][:], rhs=Vh, start=False, stop=True), [K("AckT"), K("vt")], [kb])
                    S(lambda e, hs=hs: e.copy(out=B_["Zs"][hs, :], in_=rD[hs, 0:64]), [kb], [K("Zs")])
                    PE(lambda e: e.matmul(rD[:, 64:128], lhsT=B_["TT"][:], rhs=B_["Zs"][:], start=True, stop=True), [K("TT"), K("Zs")], [kb])
                    S(lambda e, hs=hs: e.copy(out=B_["Ms"][hs, :], in_=rD[hs, 64:128]), [kb], [K("Ms")])
                    PE(lambda e: e.matmul(rA[:, 0:64], lhsT=rtFh[:], rhs=H[:], start=True, stop=True), [krtF, K("H")], [kb])
                    S(lambda e, hs=hs, hr=hr: e.copy(out=B_["Yt"][hs, hr], in_=rA[hs, 0:64]), [kb], [K("Yt")])
                    PE(lambda e, ch=ch, h=h: e.matmul(rB[:, 0:64], lhsT=B_[f"ktS{ch}{h}"][:], rhs=Vh, start=True, stop=False), [K(f"ktS{ch}{h}"), K("vt")], [kb])
                    PE(lambda e, ch=ch, h=h: e.matmul(rB[:, 0:64], lhsT=B_[f"nbS{ch}{h}"][:], rhs=B_["Ms"][:], start=False, stop=True), [K(f"nbS{ch}{h}"), K("Ms")], [kb])
                    V(lambda e, hr=hr, lst=lst: e.tensor_scalar(out=H[hr, :], in0=H[hr, :], scalar1=B_["PF"][hr, lst:lst + 1], scalar2=None, op0=ALU.mult), [K("H"), K("PF")], [K("H")])
                    V(lambda e, hr=hr, lst=lst: e.scalar_tensor_tensor(out=H[hr, :], in0=rB[hr, 0:64], scalar=B_["PF"][hr, lst:lst + 1], in1=H[hr, :], op0=ALU.mult, op1=ALU.add), [K("H"), K("PF"), kb], [K("H")])
                PE(lambda e: e.matmul(rC[:, 0:64], lhsT=B_["ArkT"][:], rhs=Vh, start=True, stop=False), [K("ArkT"), K("vt")], [kb])
                PE(lambda e: e.matmul(rC[:, 0:64], lhsT=B_["AnrbT"][:], rhs=B_["Ms"][:], start=False, stop=True), [K("AnrbT"), K("Ms")], [kb])
                V(lambda e, hr=hr: e.tensor_tensor(out=B_["Yt"][:, hr], in0=B_["Yt"][:, hr], in1=rC[:, 0:64], op=ALU.add), [K("Yt"), kb], [K("Yt")])
            for h in range(2):
                head(h)
            p.dma("gpsimd", ry_s[d, t0:t0 + 128, :], B_["Yt"][:], reads=[K("Yt")], writes=["ry_s"])

        def ssd_chunk(d, c):
            t0 = c * 128
            Mk = UT if d == 0 else LT
            last = 127 if d == 0 else 0
            p.dma("gpsimd", sxt[:], ssd_s[t0:t0 + 128, :], reads=["ssd_s"], writes=["sxt"])
            if d == 1:
                p.dma("gpsimd", syf[:], sy_s[t0:t0 + 128, :], reads=["sy_s"], writes=["syf"])
                p.dma("gpsimd", zt[:], cols_s[2 + t0:2 + t0 + 128, 1280:1536], reads=["cols_s"], writes=["zts"])
            sx = sxt[:, 0:256]; sB = sxt[:, 256:384]; sC = sxt[:, 384:512]
            dt_d = sxt[:, 512 + d * 4:516 + d * 4]; a_d = sxt[:, 520 + d * 4:524 + d * 4]
            PE(lambda e: e.transpose(out=pA[:, 0:128], in_=sB, identity=ident), ["sxt", "msk"], ["ps0"])
            PE(lambda e: e.transpose(out=pA[:, 128:256], in_=sC, identity=ident), ["sxt", "msk"], ["ps0"])
            S(lambda e: e.copy(out=BCF[:], in_=pA[:, 0:256]), ["ps0"], ["BCF"])
            if SSDSTOP == 1: return
            PE(lambda e: e.matmul(pSc[:, :128], lhsT=BCF[:, 0:128], rhs=BCF[:, 128:256], start=True, stop=True), ["BCF"], ["ps0"])
            S(lambda e: e.copy(out=scT[:], in_=pSc[:, :128]), ["ps0"], ["scT"])
            PE(lambda e: e.matmul(pAc[:, :4], lhsT=Mk, rhs=a_d, start=True, stop=True), ["sxt", "msk"], ["ps0"])
            S(lambda e: e.copy(out=acol[:], in_=pAc[:, :4]), ["ps0"], ["acol"])
            S(lambda e: e.mul(out=nacol[:], in_=pAc[:, :4], mul=-1.0), ["ps0"], ["nacol"])
            if SSDSTOP == 2: return
            for h in range(4):
                (V if os.environ.get('A_V1') else G)(lambda e, h=h: e.tensor_scalar(out=Rm[:, h, :], in0=Mk, scalar1=a_d[:, h:h + 1], scalar2=None, op0=ALU.mult), ["sxt", "msk"], ["Rm"])
            PE(lambda e: e.matmul(pRow[:, :512], lhsT=ones, rhs=Rm[:].rearrange("p h n -> p (h n)"), start=True, stop=True), ["Rm", "msk"], ["ps1"])
            S(lambda e: e.activation(out=E[:].rearrange("p h n -> p (h n)"), in_=pRow[:, :512], func=AF.Exp), ["ps1"], ["E"])
            for h in range(4):
                S(lambda e, h=h: e.activation(out=dend[:, h:h + 1], in_=pRow[:, h * 128 + last:h * 128 + last + 1], func=AF.Exp, bias=nacol[:, h:h + 1], scale=1.0), ["ps1", "nacol"], ["dend"])
            if SSDSTOP == 3: return
            for h in range(4):
                hs = slice(h * 64, (h + 1) * 64)
                S(lambda e, h=h: e.activation(out=Dm[:], in_=pRow[:, h * 128:(h + 1) * 128], func=AF.Identity, bias=nacol[:, h:h + 1], scale=1.0), ["ps1", "nacol"], ["Dm"])
                G(lambda e: e.tensor_scalar(out=Dm[:], in0=Dm[:], scalar1=0.0, scalar2=None, op0=ALU.min), ["Dm"], ["Dm"])
                S(lambda e: e.activation(out=Dm[:], in_=Dm[:], func=AF.Exp), ["Dm"], ["Dm"])
                G(lambda e: e.tensor_tensor(out=Dm[:], in0=Dm[:], in1=Mk, op=ALU.mult), ["Dm", "msk"], ["Dm"])
                G(lambda e, h=h: e.tensor_tensor(out=M[:, h, :], in0=Dm[:], in1=scT[:], op=ALU.mult), ["Dm", "scT"], ["M"])
                G(lambda e, h=h: e.tensor_tensor(out=CdF[:, h, :], in0=E[:, h, :], in1=BCF[:, 128:256], op=ALU.mult), ["E", "BCF"], ["CdF"])
                G(lambda e, h=h, hs=hs: e.tensor_scalar(out=xdt[:, hs], in0=sx[:, hs], scalar1=dt_d[:, h:h + 1], scalar2=None, op0=ALU.mult), ["sxt"], ["xdt"])
                G(lambda e, h=h, hs=hs: e.tensor_scalar(out=xend[:, hs], in0=xdt[:, hs], scalar1=dend[:, h:h + 1], scalar2=None, op0=ALU.mult), ["xdt", "dend"], ["xend"])
            if SSDSTOP == 4: return
            for h in range(4):
                hs = slice(h * 64, (h + 1) * 64)
                PE(lambda e, h=h, hs=hs: e.matmul(pY[:, hs], lhsT=M[:, h, :], rhs=xdt[:, hs], start=True, stop=False), ["M", "xdt"], ["ps2"])
                PE(lambda e, h=h, hs=hs: e.matmul(pY[:, hs], lhsT=CdF[:, h, :], rhs=H[:, hs], start=False, stop=True), ["CdF", "H"], ["ps2"])
            PE(lambda e: e.matmul(pSt[:, :256], lhsT=sB, rhs=xend[:], start=True, stop=True), ["sxt", "xend"], ["ps2"])
            if SSDSTOP == 5: return
            for h in range(4):
                hs = slice(h * 64, (h + 1) * 64)
                V(lambda e, h=h, hs=hs: e.scalar_tensor_tensor(out=H[:, hs], in0=H[:, hs], scalar=E[:, h, last:last + 1], in1=pSt[:, hs], op0=ALU.mult, op1=ALU.add), ["H", "E", "ps2"], ["H"])
            if SSDSTOP == 6: return
            if d == 0:
                S(lambda e: e.copy(out=yt[:], in_=pY[:, :256]), ["ps2"], ["yt"])
                p.dma("gpsimd", sy_s[t0:t0 + 128, :], yt[:], reads=["yt"], writes=["sy_s"])
            else:
                V(lambda e: e.tensor_tensor(out=yt[:], in0=pY[:, :256], in1=syf[:], op=ALU.add), ["ps2", "syf"], ["yt"])
                G(lambda e: e.tensor_tensor(out=xd[:], in0=sx, in1=rv("dsk"), op=ALU.mult), ["sxt", "rvs"], ["xd"])
                G(lambda e: e.tensor_tensor(out=yt[:], in0=yt[:], in1=xd[:], op=ALU.add), ["yt", "xd"], ["yt"])
                S(lambda e: e.activation(out=szs[:], in_=zt[:], func=AF.Silu), ["zts"], ["szs"])
                G(lambda e: e.tensor_tensor(out=yt[:], in0=yt[:], in1=szs[:], op=ALU.mult), ["yt", "szs"], ["yt"])
                p.dma("gpsimd", yc_o[t0:t0 + 128, :], yt[:], reads=["yt"], writes=["yc"])

        ssd_list = [(0, c) for c in range(NTL)] + [(1, c) for c in range(NTL - 1, -1, -1)]
        V(lambda e: e.memset(H[:], 0.0), [], ["H"])
        def ssd_pair(it):
            for si in (2 * it, 2 * it + 1):
                dd, cc = ssd_list[si]
                if dd == 1 and cc == NTL - 1:
                    V(lambda e: e.memset(H[:], 0.0), [], ["H"])
                ssd_chunk(dd, cc)
        lists = [[] for _ in range(5)]
        for it in range(NTL):
            if not os.environ.get("A_NOGDN"):
                lists[0] += p.capture(gdn_chunk, 0, it); lists[1] += p.capture(gdn_chunk, 1, NTL - 1 - it)
            if not os.environ.get("A_NORWKV"):
                lists[2] += p.capture(rwkv_block, 0, it); lists[3] += p.capture(rwkv_block, 1, NTL - 1 - it)
            if not os.environ.get("A_NOSSD"):
                lists[4] += p.capture(ssd_pair, it)
        p.emit_balanced(lists)
    p.barrier()


def phase4(p, T, L):
    nc = p.nc
    V = L["V"]; G = L["G"]; S = L["S"]; PE = L["PE"]; PS = L["PS"]; ident = L["ident"]; rv = L["rv"]
    gy_s, ry_s, cols_s, rpost_s, ya_o, yb_o = (L[k] for k in ["gy_s", "ry_s", "cols_s", "rpost_s", "ya_o", "yb_o"])
    NTL = T // 128
    with contextlib.ExitStack() as sc:
        sb = lambda name, shape, dt=F32: sc.enter_context(nc.sbuf_tensor("s_" + name, list(shape), dt))
        g0 = sb("g0", [128, 128]); g1 = sb("g1", [128, 128]); o = sb("o", [128, 128]); jk = sb("jk", [128, 128]); ss = sb("ss", [128, 1])
        zt = sb("zt4", [128, 128]); ya = sb("ya_t", [128, 128])
        r0 = sb("r0", [128, 128]); r1 = sb("r1", [128, 128]); y = sb("y4", [128, 128]); st = sb("st4", [128, 2]); sq = sb("sq4", [128, 128]); vr = sb("vr4", [128, 2])
        rp = sb("rp4", [128, 258]); yb = sb("yb_t", [128, 128])
        pT, pU = PS[0], PS[1]
        for tt in range(NTL):
            t0 = tt * 128
            p.dma("sync", g0[:], gy_s[0, t0:t0 + 128, :], reads=["gy_s"], writes=["g0"])
            p.dma("sync", g1[:], gy_s[1, t0:t0 + 128, :], reads=["gy_s"], writes=["g1"])
            p.dma("scalar", zt[:], cols_s[2 + t0:2 + t0 + 128, 384:512], reads=["cols_s"], writes=["zt4"])
            p.dma("sync", r0[:], ry_s[0, t0:t0 + 128, :], reads=["ry_s"], writes=["r0"])
            p.dma("sync", r1[:], ry_s[1, t0:t0 + 128, :], reads=["ry_s"], writes=["r1"])
            p.dma("scalar", rp[:], rpost_s[t0:t0 + 128, :], reads=["rpost_s"], writes=["rp4"])
            V(lambda e: e.tensor_tensor(out=o[:], in0=g0[:], in1=g1[:], op=ALU.add), ["g0", "g1"], ["o"])
            V(lambda e: e.scalar_tensor_tensor(out=jk[:], in0=o[:], scalar=1.0, in1=o[:], op0=ALU.mult, op1=ALU.mult, accum_out=ss[:, 0:1]), ["o"], ["jk", "ss"])
            V(lambda e: e.tensor_scalar(out=ss[:], in0=ss[:], scalar1=1.0 / 128, scalar2=1e-6, op0=ALU.mult, op1=ALU.add), ["ss"], ["ss"])
            S(lambda e: e.activation(out=ss[:], in_=ss[:], func=AF.Sqrt), ["ss"], ["ss"])
            V(lambda e: e.reciprocal(out=ss[:], in_=ss[:]), ["ss"], ["ss"])
            V(lambda e: e.scalar_tensor_tensor(out=o[:], in0=o[:], scalar=ss[:, 0:1], in1=rv("gnorm"), op0=ALU.mult, op1=ALU.mult), ["o", "ss", "rvs"], ["o"])
            S(lambda e: e.activation(out=zt[:], in_=zt[:], func=AF.Silu), ["zt4"], ["zt4"])
            V(lambda e: e.tensor_tensor(out=ya[:], in0=o[:], in1=zt[:], op=ALU.mult), ["o", "zt4"], ["ya_t"])
            p.dma("gpsimd", ya_o[t0:t0 + 128, :], ya[:], reads=["ya_t"], writes=["ya"])
            V(lambda e: e.tensor_tensor(out=y[:], in0=r0[:], in1=r1[:], op=ALU.add), ["r0", "r1"], ["y4"])
            V(lambda e: e.tensor_reduce(out=st[:], in_=y[:].rearrange("p (h n) -> p h n", h=2), axis=AX.X, op=ALU.add), ["y4"], ["st4"])
            V(lambda e: e.tensor_scalar(out=st[:], in0=st[:], scalar1=1.0 / 64, scalar2=None, op0=ALU.mult), ["st4"], ["st4"])
            for h in range(2):
                hs = slice(h * 64, (h + 1) * 64)
                V(lambda e, h=h, hs=hs: e.tensor_scalar(out=y[:, hs], in0=y[:, hs], scalar1=st[:, h:h + 1], scalar2=None, op0=ALU.subtract), ["y4", "st4"], ["y4"])
            S(lambda e: e.activation(out=sq[:], in_=y[:], func=AF.Square), ["y4"], ["sq4"])
            V(lambda e: e.tensor_reduce(out=vr[:], in_=sq[:].rearrange("p (h n) -> p h n", h=2), axis=AX.X, op=ALU.add), ["sq4"], ["vr4"])
            V(lambda e: e.tensor_scalar(out=vr[:], in0=vr[:], scalar1=1.0 / 64, scalar2=64e-5, op0=ALU.mult, op1=ALU.add), ["vr4"], ["vr4"])
            S(lambda e: e.activation(out=vr[:], in_=vr[:], func=AF.Sqrt), ["vr4"], ["vr4"])
            V(lambda e: e.reciprocal(out=vr[:], in_=vr[:]), ["vr4"], ["vr4"])
            for h in range(2):
                hs = slice(h * 64, (h + 1) * 64)
                V(lambda e, h=h, hs=hs: e.tensor_scalar(out=y[:, hs], in0=y[:, hs], scalar1=vr[:, h:h + 1], scalar2=None, op0=ALU.mult), ["y4", "vr4"], ["y4"])
            V(lambda e: e.tensor_tensor(out=y[:], in0=y[:], in1=rv("lng"), op=ALU.mult), ["y4", "rvs"], ["y4"])
            V(lambda e: e.tensor_tensor(out=y[:], in0=y[:], in1=rv("lnb"), op=ALU.add), ["y4", "rvs"], ["y4"])
            for h in range(2):
                hs = slice(h * 64, (h + 1) * 64)
                V(lambda e, h=h, hs=hs: e.scalar_tensor_tensor(out=y[:, hs], in0=rp[:, hs], scalar=rp[:, 256 + h:257 + h], in1=y[:, hs], op0=ALU.mult, op1=ALU.add), ["y4", "rp4"], ["y4"])
            V(lambda e: e.tensor_tensor(out=yb[:], in0=y[:], in1=rp[:, 128:256], op=ALU.mult), ["y4", "rp4"], ["yb_t"])
            p.dma("gpsimd", yb_o[t0:t0 + 128, :], yb[:], reads=["yb_t"], writes=["yb"])


ALPHA = 4 ** 0.25
NTOK = 2048
T1 = 256
T2 = 512
NE = 32


def ln_fm(p, h, hk, nch, NT, gcol, bcol, ones_mean, sq, pS, pQ, tmp, kS, kQ, eps=1e-5):
    for oc in range(nch):
        p.op("tensor", lambda e, oc=oc: e.matmul(pS[:, :NT], lhsT=ones_mean[:], rhs=h[:, oc, :], start=(oc == 0), stop=(oc == nch - 1)),
             reads=[hk, "ones_mean"], writes=[kS])
    sqs = (lambda s_: sq[s_][:]) if isinstance(sq, (list, tuple)) else (lambda s_: sq[:, s_, :])
    for oc in range(nch):
        s = oc % 2
        p.op("scalar", lambda e, oc=oc, s=s: e.activation(out=sqs(s), in_=h[:, oc, :], func=AF.Square), reads=[hk], writes=[f"sq{s}"])
        p.op("tensor", lambda e, oc=oc, s=s: e.matmul(pQ[:, :NT], lhsT=ones_mean[:], rhs=sqs(s), start=(oc == 0), stop=(oc == nch - 1)),
             reads=[f"sq{s}", "ones_mean"], writes=[kQ])
    mean, rstd, t = tmp["mean"], tmp["rstd"], tmp["t"]
    p.op("scalar", lambda e: e.copy(out=mean[:], in_=pS[:, :NT]), reads=[kS], writes=["ln_mean"])
    p.op("vector", lambda e: e.tensor_tensor(out=t[:], in0=mean[:], in1=mean[:], op=ALU.mult), reads=["ln_mean"], writes=["ln_t"])
    p.op("vector", lambda e: e.tensor_tensor(out=t[:], in0=pQ[:, :NT], in1=t[:], op=ALU.subtract), reads=[kQ, "ln_t"], writes=["ln_t"])
    p.op("vector", lambda e: e.tensor_scalar(out=t[:], in0=t[:], scalar1=eps, scalar2=None, op0=ALU.add), reads=["ln_t"], writes=["ln_t"])
    p.op("scalar", lambda e: e.activation(out=t[:], in_=t[:], func=AF.Sqrt), reads=["ln_t"], writes=["ln_t"])
    p.op("vector", lambda e: e.reciprocal(out=rstd[:], in_=t[:]), reads=["ln_t"], writes=["ln_rstd"])
    for oc in range(nch):
        p.op("vector", lambda e, oc=oc: e.tensor_tensor(out=h[:, oc, :], in0=h[:, oc, :], in1=mean[:], op=ALU.subtract), reads=[hk, "ln_mean"], writes=[hk])
        p.op("vector", lambda e, oc=oc: e.tensor_tensor(out=h[:, oc, :], in0=h[:, oc, :], in1=rstd[:], op=ALU.mult), reads=[hk, "ln_rstd"], writes=[hk])
        p.op("scalar", lambda e, oc=oc: e.activation(out=h[:, oc, :], in_=h[:, oc, :], func=AF.Identity, scale=gcol(oc), bias=bcol(oc)),
             reads=[hk, "vec"], writes=[hk])


def build_B(ntok=NTOK, ne=NE, dump=False):
    p = Prog(); nc = p.nc
    D = 1024
    xT = p.dram("xT", [D, ntok]); yT = p.dram("yT", [2048, ntok]); pT = p.dram("pT", [256, ntok])
    wg = p.dram("wg", [D, 3072]); wb = p.dram("wb", [2048, D]); wo = p.dram("wo", [D, D]); wplg = p.dram("wplg", [D, D])
    wpl = p.dram("wpl", [256, D]); wr = p.dram("wr", [D, 32]); br = p.dram("br", [1, 32])
    wgu = p.dram("wgu", [32, D, 2048]); wd = p.dram("wd", [32, D, D])
    bgu = p.dram("bgu", [128, 32, 16]); bd = p.dram("bd", [32, D]); vec = p.dram("vec", [128, 5, 8]); ident_d = p.dram("ident", [128, 128])
    outT = p.dram("outT", [D, ntok], kind="ExternalOutput")
    x1bf_s = p.dram("x1bf_s", [128, 8, ntok], BF16, kind="Internal")
    acc_s = p.dram("acc_s", [128, 8, ntok], F32, kind="Internal")
    if dump:
        x1_d = p.dram("x1_d", [D, ntok], kind="ExternalOutput")
        gate_d = p.dram("gate_d", [32, ntok], kind="ExternalOutput")

    ident = p.sb("ident", [128, 128]); ones_mean = p.sb("ones_mean", [128, 128]); vecs = p.sb("vecs", [128, 5, 8])
    gateT = p.sb("gateT", [32, ntok]); ones_row = p.sb("ones_row", [1, 128]); brow = p.sb("brow", [1, 32])
    PS = [p.ps(f"ps{i}", [128, 512]) for i in range(8)]
    p.dma("sync", ident[:], ident_d[:, :], writes=["ident"])
    p.dma("sync", vecs[:], vec[:, :, :], writes=["vec"])
    p.dma("sync", brow[:], br[:, :], writes=["brow"])
    p.op("vector", lambda e: e.memset(ones_mean[:], 1.0 / 1024), writes=["ones_mean"])
    p.op("vector", lambda e: e.memset(ones_row[:], 1.0), writes=["ones_row"])

    with contextlib.ExitStack() as sc:
        def sb(name, shape, dt=F32):
            return sc.enter_context(nc.sbuf_tensor("s_" + name, list(shape), dt))
        wg_bf = sb("wg_bf", [128, 8, 3072], BF16); wb_bf = sb("wb_bf", [128, 16, 1024], BF16)
        wo_bf = sb("wo_bf", [128, 8, 1024], BF16); wplg_bf = sb("wplg_bf", [128, 8, 1024], BF16); wpl_bf = sb("wpl_bf", [128, 2, 1024], BF16)
        wr_sb = sb("wr_sb", [128, 8, 32]); ones512 = sb("ones512", [128, 128])
        for kc in range(8):
            p.dma("gpsimd", wg_bf[:, kc, :], wg[kc * 128:(kc + 1) * 128, :], writes=["wg_bf"])
            p.dma("gpsimd", wo_bf[:, kc, :], wo[kc * 128:(kc + 1) * 128, :], writes=["wo_bf"])
            p.dma("gpsimd", wplg_bf[:, kc, :], wplg[kc * 128:(kc + 1) * 128, :], writes=["wplg_bf"])
            p.dma("sync", wr_sb[:, kc, :], wr[kc * 128:(kc + 1) * 128, :], writes=["wr_sb"])
        for kc in range(16):
            p.dma("gpsimd", wb_bf[:, kc, :], wb[kc * 128:(kc + 1) * 128, :], writes=["wb_bf"])
        for kc in range(2):
            p.dma("gpsimd", wpl_bf[:, kc, :], wpl[kc * 128:(kc + 1) * 128, :], writes=["wpl_bf"])
        p.op("vector", lambda e: e.memset(ones512[:], 1.0 / 512), writes=["ones512"])
        xf = sb("xf", [128, 8, T1]); x_bf = sb("x_bf", [128, 8, T1], BF16); uf = sb("uf", [128, 8, T1])
        y_bf = sb("y_bf", [128, 16, T1], BF16); m_bf = sb("m_bf", [128, 8, T1], BF16); sq = sb("sq", [128, 2, T1])
        x1b = sb("x1b", [128, 8, T1], BF16); accb = sb("accb", [128, 8, T1])
        pf = sb("pf", [128, 2, T1], BF16)
        tm = {k: sb("tm_" + k, [128, T1]) for k in ["mean", "rstd", "t", "g", "mf", "t2", "rs"]}
        lg = sb("lg", [128, 32]); top8 = sb("top8", [128, 8]); nmx = sb("nmx", [128, 1]); msk = sb("msk", [128, 32])
        ex = sb("ex", [128, 32]); ssum = sb("ssum", [128, 1]); gt = sb("gt", [128, 32])
        pG, pB, pH, pS, pQ, pR, pT_, pP = PS
        xTv = xT.rearrange("(kc p) n -> p kc n", p=128); yTv = yT.rearrange("(kc p) n -> p kc n", p=128)
        pTv = pT.rearrange("(kc p) n -> p kc n", p=128)
        for t in range(ntok // T1):
            o = t * T1
            p.dma("sync", xf[:], xTv[:, :, o:o + T1], writes=["xf"])
            p.dma("gpsimd", x_bf[:], xTv[:, :, o:o + T1], writes=["x_bf"])
            p.dma("gpsimd", y_bf[:, 0:8, :], yTv[:, 0:8, o:o + T1], writes=["y_bf_a"])
            p.dma("sync", uf[:], yTv[:, 8:16, o:o + T1], writes=["uf"])
            p.dma("gpsimd", pf[:], pTv[:, :, o:o + T1], writes=["pf"])
            for g in range(2):
                for c in range(4):
                    cc = g * 4 + c; s = cc % 2
                    p.op("scalar", lambda e, cc=cc, s=s: e.activation(out=sq[:, s, :], in_=uf[:, cc, :], func=AF.Square), reads=["uf"], writes=[f"sq{s}"])
                    p.op("tensor", lambda e, c=c, s=s: e.matmul(pS[:, :T1], lhsT=ones512[:], rhs=sq[:, s, :], start=(c == 0), stop=(c == 3)),
                         reads=[f"sq{s}", "ones512"], writes=["pS"])
                rs = tm["rs"]
                p.op("vector", lambda e: e.tensor_scalar(out=rs[:], in0=pS[:, :T1], scalar1=1e-5, scalar2=None, op0=ALU.add), reads=["pS"], writes=["rs"])
                p.op("scalar", lambda e: e.activation(out=rs[:], in_=rs[:], func=AF.Sqrt), reads=["rs"], writes=["rs"])
                p.op("vector", lambda e: e.reciprocal(out=rs[:], in_=rs[:]), reads=["rs"], writes=["rs"])
                for c in range(4):
                    cc = g * 4 + c
                    p.op("vector", lambda e, cc=cc: e.scalar_tensor_tensor(out=y_bf[:, 8 + cc, :], in0=uf[:, cc, :], scalar=vecs[:, 4, cc:cc + 1], in1=rs[:], op0=ALU.mult, op1=ALU.mult),
                         reads=["uf", "rs", "vec"], writes=["y_bf_u"])
            brk = [(0, 4), (4, 8), (8, 16)]
            for oc in range(8):
                for b in range(3):
                    c0 = b * 1024 + oc * 128
                    for kc in range(8):
                        p.op("tensor", lambda e, kc=kc, c0=c0: e.matmul(pG[:, :T1], lhsT=wg_bf[:, kc, c0:c0 + 128], rhs=x_bf[:, kc, :], start=(kc == 0), stop=(kc == 7)),
                             reads=["wg_bf", "x_bf"], writes=["pG"])
                    p.op("scalar", lambda e: e.activation(out=tm["g"][:], in_=pG[:, :T1], func=AF.Sigmoid), reads=["pG"], writes=["tm_g"])
                    k0, k1 = brk[b]
                    for kc in range(k0, k1):
                        p.op("tensor", lambda e, kc=kc, k0=k0, k1=k1: e.matmul(pB[:, :T1], lhsT=wb_bf[:, kc, oc * 128:(oc + 1) * 128], rhs=y_bf[:, kc, :], start=(kc == k0), stop=(kc == k1 - 1)),
                             reads=["wb_bf", "y_bf_a", "y_bf_u"], writes=["pB"])
                    if b == 0:
                        p.op("vector", lambda e: e.tensor_tensor(out=tm["mf"][:], in0=tm["g"][:], in1=pB[:, :T1], op=ALU.mult), reads=["tm_g", "pB"], writes=["tm_mf"])
                    else:
                        p.op("vector", lambda e: e.tensor_tensor(out=tm["t2"][:], in0=tm["g"][:], in1=pB[:, :T1], op=ALU.mult), reads=["tm_g", "pB"], writes=["tm_t2"])
                        if b == 1:
                            p.op("vector", lambda e: e.tensor_tensor(out=tm["mf"][:], in0=tm["mf"][:], in1=tm["t2"][:], op=ALU.add), reads=["tm_mf", "tm_t2"], writes=["tm_mf"])
                        else:
                            p.op("vector", lambda e, oc=oc: e.tensor_tensor(out=m_bf[:, oc, :], in0=tm["mf"][:], in1=tm["t2"][:], op=ALU.add), reads=["tm_mf", "tm_t2"], writes=["m_bf"])
            for oc in range(8):
                for kc in range(8):
                    p.op("tensor", lambda e, kc=kc, oc=oc: e.matmul(pH[:, :T1], lhsT=wo_bf[:, kc, oc * 128:(oc + 1) * 128], rhs=m_bf[:, kc, :], start=(kc == 0), stop=(kc == 7)),
                         reads=["wo_bf", "m_bf"], writes=["pH"])
                p.op("vector", lambda e, oc=oc: e.scalar_tensor_tensor(out=xf[:, oc, :], in0=xf[:, oc, :], scalar=ALPHA, in1=pH[:, :T1], op0=ALU.mult, op1=ALU.add),
                     reads=["xf", "pH"], writes=["xf"])
            ln_fm(p, xf, "xf", 8, T1, lambda oc: vecs[:, 0, oc:oc + 1], lambda oc: vecs[:, 1, oc:oc + 1], ones_mean, sq, pS, pQ, tm, "pS", "pQ")
            p.op("scalar", lambda e: e.copy(out=x1b[:], in_=xf[:]), reads=["xf"], writes=["x1b"])
            p.dma("sync", x1bf_s[:, :, o:o + T1], x1b[:], reads=["x1b"], writes=["x1bf_s"])
            if dump:
                p.dma("sync", x1_d.rearrange("(kc p) n -> p kc n", p=128)[:, :, o:o + T1], xf[:], reads=["xf"], writes=["x1_d"])
            for s in range(T1 // 128):
                for kc in range(8):
                    p.op("tensor", lambda e, kc=kc, s=s: e.matmul(pR[:, :32], lhsT=xf[:, kc, s * 128:(s + 1) * 128], rhs=wr_sb[:, kc, :], start=(kc == 0), stop=False),
                         reads=["xf", "wr_sb"], writes=["pR"])
                p.op("tensor", lambda e: e.matmul(pR[:, :32], lhsT=ones_row[:, :], rhs=brow[:, :], start=False, stop=True), reads=["ones_row", "brow"], writes=["pR"])
                p.op("vector", lambda e: e.tensor_copy(out=lg[:], in_=pR[:, :32]), reads=["pR"], writes=["lg"])
                p.op("vector", lambda e: e.max(out=top8[:], in_=lg[:]), reads=["lg"], writes=["top8"])
                p.op("vector", lambda e: e.tensor_scalar(out=nmx[:], in0=top8[:, 0:1], scalar1=-1.0, scalar2=None, op0=ALU.mult), reads=["top8"], writes=["nmx"])
                p.op("vector", lambda e: e.tensor_scalar(out=msk[:], in0=lg[:], scalar1=top8[:, 3:4], scalar2=None, op0=ALU.is_ge), reads=["lg", "top8"], writes=["msk"])
                p.op("scalar", lambda e: e.activation(out=ex[:], in_=lg[:], func=AF.Exp, bias=nmx[:, 0:1], scale=1.0), reads=["lg", "nmx"], writes=["ex"])
                p.op("vector", lambda e: e.scalar_tensor_tensor(out=ex[:], in0=ex[:], scalar=1.0, in1=msk[:], op0=ALU.mult, op1=ALU.mult, accum_out=ssum[:, 0:1]),
                     reads=["ex", "msk"], writes=["ex", "ssum"])
                p.op("vector", lambda e: e.reciprocal(out=ssum[:], in_=ssum[:]), reads=["ssum"], writes=["ssum"])
                p.op("vector", lambda e: e.tensor_scalar(out=gt[:], in0=ex[:], scalar1=ssum[:, 0:1], scalar2=None, op0=ALU.mult), reads=["ex", "ssum"], writes=["gt"])
                p.op("tensor", lambda e: e.transpose(out=pT_[:32, :128], in_=gt[:], identity=ident[:]), reads=["gt", "ident"], writes=["pT"])
                oo = o + s * 128
                p.op("scalar", lambda e, oo=oo: e.copy(out=gateT[:, oo:oo + 128], in_=pT_[:32, :128]), reads=["pT"], writes=["gateT"])
            for oc in range(8):
                for kc in range(2):
                    p.op("tensor", lambda e, kc=kc, oc=oc: e.matmul(pP[:, :T1], lhsT=wpl_bf[:, kc, oc * 128:(oc + 1) * 128], rhs=pf[:, kc, :], start=(kc == 0), stop=(kc == 1)),
                         reads=["wpl_bf", "pf"], writes=["pP"])
                for kc in range(8):
                    p.op("tensor", lambda e, kc=kc, oc=oc: e.matmul(pG[:, :T1], lhsT=wplg_bf[:, kc, oc * 128:(oc + 1) * 128], rhs=x1b[:, kc, :], start=(kc == 0), stop=(kc == 7)),
                         reads=["wplg_bf", "x1b"], writes=["pG"])
                p.op("scalar", lambda e: e.activation(out=tm["g"][:], in_=pG[:, :T1], func=AF.Sigmoid), reads=["pG"], writes=["tm_g"])
                p.op("vector", lambda e: e.tensor_tensor(out=tm["t2"][:], in0=tm["g"][:], in1=pP[:, :T1], op=ALU.mult), reads=["tm_g", "pP"], writes=["tm_t2"])
                p.op("vector", lambda e, oc=oc: e.scalar_tensor_tensor(out=accb[:, oc, :], in0=xf[:, oc, :], scalar=ALPHA, in1=tm["t2"][:], op0=ALU.mult, op1=ALU.add),
                     reads=["xf", "tm_t2"], writes=["accb"])
            p.dma("sync", acc_s[:, :, o:o + T1], accb[:], reads=["accb"], writes=["acc_s"])
    if dump:
        p.dma("sync", gate_d[:, :], gateT[:], reads=["gateT"], writes=["gate_d"])
    p.barrier()

    H = ntok // 2
    with contextlib.ExitStack() as sc:
        def sb(name, shape, dt=F32):
            return sc.enter_context(nc.sbuf_tensor("s_" + name, list(shape), dt))
        x1h = sb("x1h", [128, 8, H], BF16); acc = sb("acc", [128, 8, H])
        wgu_b = [sb(f"wgu_b{i}", [128, 8, 2048], BF16) for i in range(2)]
        wd_b = [sb(f"wd_b{i}", [128, 8, 1024], BF16) for i in range(2)]
        bgu_sb = sb("bgu_sb", [128, 32, 16]); bd_sb = sb("bd_sb", [32, 1024]); ones32 = sb("ones32", [32, 128])
        act = [sb(f"act{c}", [128, 8, T2], BF16) for c in range(2)]; gbc = [sb(f"gbc{c}", [128, T2]) for c in range(2)]; gm = [sb(f"gm{c}", [32, T2]) for c in range(2)]
        glu = [sb(f"glu{c}", [128, T2]) for c in range(2)]; up1 = [sb(f"up1{c}", [128, T2]) for c in range(2)]; sg = [sb(f"sg{c}", [128, T2]) for c in range(2)]
        t1 = [sb(f"t1{c}", [128, T2]) for c in range(2)]; t2 = [sb(f"t2{c}", [128, T2]) for c in range(2)]
        sq = [t1[0], t2[0]]; tm = {"mean": glu[0], "rstd": up1[0], "t": sg[0]}
        print('moe sbuf remaining', nc.sbuf_bytes_remaining, flush=True)
        p.dma("sync", bgu_sb[:], bgu[:, :, :], writes=["bgu_sb"])
        p.dma("sync", bd_sb[:], bd[:, :], writes=["bd_sb"])
        p.op("vector", lambda e: e.memset(ones32[:], 1.0), writes=["ones32"])
        pBC = PS[6]; pS = PS[7]; pQ = PS[6]
        outv = outT.rearrange("(kc p) n -> p kc n", p=128)

        def moe_tile(ex_, tt, ho, wgs, wds, wb_i):
            c = tt
            pGl, pUp, pD = PS[3 * c], PS[3 * c + 1], PS[3 * c + 2]
            kGl, kUp, kD = f"pGl{c}", f"pUp{c}", f"pD{c}"
            to = tt * T2
            p.op("vector", lambda e: e.tensor_scalar(out=gm[c][:], in0=gateT[:, ho + to:ho + to + T2], scalar1=ident[0:32, ex_:ex_ + 1], scalar2=None, op0=ALU.mult),
                 reads=["gateT", "ident"], writes=[f"gm{c}"])
            pBCc, kBC = (PS[6], "pBC") if c == 0 else (PS[7], "pS7")
            p.op("tensor", lambda e: e.matmul(pBCc[:, :T2], lhsT=ones32[:], rhs=gm[c][:], start=True, stop=True), reads=["ones32", f"gm{c}"], writes=[kBC])
            p.op("scalar", lambda e: e.copy(out=gbc[c][:], in_=pBCc[:, :T2]), reads=[kBC], writes=[f"gbc{c}"])
            for oc in range(8):
                for kc in range(8):
                    p.op("tensor", lambda e, kc=kc, oc=oc: e.matmul(pGl[:, :T2], lhsT=wgs[:, kc, oc * 128:(oc + 1) * 128], rhs=x1h[:, kc, to:to + T2], start=(kc == 0), stop=(kc == 7)),
                         reads=[f"wgu{wb_i}", "x1h"], writes=[kGl])
                for kc in range(8):
                    p.op("tensor", lambda e, kc=kc, oc=oc: e.matmul(pUp[:, :T2], lhsT=wgs[:, kc, 1024 + oc * 128:1024 + (oc + 1) * 128], rhs=x1h[:, kc, to:to + T2], start=(kc == 0), stop=(kc == 7)),
                         reads=[f"wgu{wb_i}", "x1h"], writes=[kUp])
                p.op("vector", lambda e, oc=oc: e.tensor_scalar(out=glu[c][:], in0=pGl[:, :T2], scalar1=bgu_sb[:, ex_, oc:oc + 1], scalar2=7.0, op0=ALU.add, op1=ALU.min),
                     reads=[kGl, "bgu_sb"], writes=[f"glu{c}"])
                p.op("vector", lambda e, oc=oc: e.tensor_scalar(out=up1[c][:], in0=pUp[:, :T2], scalar1=bgu_sb[:, ex_, 8 + oc:9 + oc], scalar2=7.0, op0=ALU.add, op1=ALU.min),
                     reads=[kUp, "bgu_sb"], writes=[f"up1{c}"])
                p.op("gpsimd", lambda e: e.tensor_scalar(out=up1[c][:], in0=up1[c][:], scalar1=-7.0, scalar2=1.0, op0=ALU.max, op1=ALU.add), reads=[f"up1{c}"], writes=[f"up1{c}"])
                p.op("scalar", lambda e: e.activation(out=sg[c][:], in_=glu[c][:], func=AF.Sigmoid, scale=1.702), reads=[f"glu{c}"], writes=[f"sg{c}"])
                p.op("gpsimd", lambda e: e.tensor_tensor(out=t2[c][:], in0=up1[c][:], in1=gbc[c][:], op=ALU.mult), reads=[f"up1{c}", f"gbc{c}"], writes=[f"t2{c}"])
                p.op("vector", lambda e: e.tensor_tensor(out=t1[c][:], in0=glu[c][:], in1=sg[c][:], op=ALU.mult), reads=[f"glu{c}", f"sg{c}"], writes=[f"t1{c}"])
                p.op("vector", lambda e, oc=oc: e.tensor_tensor(out=act[c][:, oc, :], in0=t1[c][:], in1=t2[c][:], op=ALU.mult), reads=[f"t1{c}", f"t2{c}"], writes=[f"act{c}"])
            for oc in range(8):
                for kc in range(8):
                    p.op("tensor", lambda e, kc=kc, oc=oc: e.matmul(pD[:, :T2], lhsT=wds[:, kc, oc * 128:(oc + 1) * 128], rhs=act[c][:, kc, :], start=(kc == 0), stop=(kc == 7)),
                         reads=[f"wd{wb_i}", f"act{c}"], writes=[kD])
                p.op("vector", lambda e, oc=oc: e.tensor_tensor(out=acc[:, oc, to:to + T2], in0=acc[:, oc, to:to + T2], in1=pD[:, :T2], op=ALU.add),
                     reads=[f"acc{c}", kD], writes=[f"acc{c}"])

        for hf in range(2):
            ho = hf * H
            p.dma("sync", x1h[:], x1bf_s[:, :, ho:ho + H], reads=["x1bf_s"], writes=["x1h"])
            p.dma("sync", acc[:], acc_s[:, :, ho:ho + H], reads=["acc_s"], writes=["acc0", "acc1"])
            for tt in range(H // T2):
                to = tt * T2
                for oc in range(8):
                    pDc = PS[3 * tt + 2]
                    p.op("tensor", lambda e, oc=oc, to=to, pDc=pDc: e.matmul(pDc[:, :T2], lhsT=bd_sb[:, oc * 128:(oc + 1) * 128], rhs=gateT[:, ho + to:ho + to + T2], start=True, stop=True),
                         reads=["bd_sb", "gateT"], writes=[f"pD{tt}"])
                    p.op("vector", lambda e, oc=oc, to=to, pDc=pDc: e.tensor_tensor(out=acc[:, oc, to:to + T2], in0=acc[:, oc, to:to + T2], in1=pDc[:, :T2], op=ALU.add),
                         reads=[f"acc{tt}", f"pD{tt}"], writes=[f"acc{tt}"])
            for ex_ in range(ne):
                wb_i = ex_ % 2
                wgs, wds = wgu_b[wb_i], wd_b[wb_i]
                for kc in (range(0, 8, 2) if not (os.environ.get("B_NODMA") and ex_ >= 2) else []):
                    p.dma("gpsimd", wgs[:, kc:kc + 2, :], wgu[ex_, kc * 128:(kc + 2) * 128, :].rearrange("(k p) n -> p k n", p=128), writes=[f"wgu{wb_i}"])
                for kc in (range(0, 8, 4) if not (os.environ.get("B_NODMA") and ex_ >= 2) else []):
                    p.dma("gpsimd", wds[:, kc:kc + 4, :], wd[ex_, kc * 128:(kc + 4) * 128, :].rearrange("(k p) n -> p k n", p=128), writes=[f"wd{wb_i}"])
                chains = [p.capture(moe_tile, ex_, tt, ho, wgs, wds, wb_i) for tt in range(H // T2)]
                p.emit_interleaved(chains)
            for tt in range(H // T2):
                to = tt * T2
                hv = acc[:, :, to:to + T2]
                ln_fm(p, hv, f"acc{tt}", 8, T2, lambda oc: vecs[:, 2, oc:oc + 1], lambda oc: vecs[:, 3, oc:oc + 1], ones_mean, sq, pS, pQ, tm, "pS7", "pBC")
                p.dma("sync", outv[:, :, ho + to:ho + to + T2], hv, reads=[f"acc{tt}"], writes=["outT"])
            p.barrier()
    p.finish_wait("sync", ["outT"] + (["x1_d", "gate_d"] if dump else []))
    return p.build()


def build_L0(ntok=2048):
    p = Prog(); nc = p.nc
    xT = p.dram("xT", [1024, ntok]); vec = p.dram("vec", [128, 2, 8])
    outT = p.dram("outT", [1024, ntok], kind="ExternalOutput")
    ones_mean = p.sb("ones_mean", [128, 128]); vecs = p.sb("vecs", [128, 2, 8])
    p.dma("sync", vecs[:], vec[:, :, :], writes=["vec"])
    p.op("vector", lambda e: e.memset(ones_mean[:], 1.0 / 1024), writes=["ones_mean"])
    TT = 512
    h = [p.sb(f"h{i}", [128, 8, TT]) for i in range(2)]
    sq = p.sb("sq", [128, 2, TT]); tm = {k: p.sb("tm_" + k, [128, TT]) for k in ["mean", "rstd", "t"]}
    pS = p.ps("pS", [128, 512]); pQ = p.ps("pQ", [128, 512])
    xv = xT.rearrange("(kc p) n -> p kc n", p=128); ov = outT.rearrange("(kc p) n -> p kc n", p=128)
    for t in range(ntok // TT):
        o = t * TT; i = t % 2
        p.dma("sync", h[i][:], xv[:, :, o:o + TT], writes=[f"h{i}"])
        ln_fm(p, h[i], f"h{i}", 8, TT, lambda oc: vecs[:, 0, oc:oc + 1], lambda oc: vecs[:, 1, oc:oc + 1], ones_mean, sq, pS, pQ, tm, "pS", "pQ")
        p.dma("gpsimd", ov[:, :, o:o + TT], h[i][:], reads=[f"h{i}"], writes=["outT"])
    p.finish_wait("sync", ["outT"])
    return p.build()


def host_inputs_B(L, stream, ya, yb, u, z, c):
    sl = slice(c * 2048, (c + 1) * 2048)
    f = lambda a: np.ascontiguousarray(a, dtype=np.float32)
    ycat = np.concatenate([ya[sl], yb[sl], u[sl]], axis=1)
    vec = np.stack([z['ln1_g'][L].reshape(8, 128).T, z['ln1_b'][L].reshape(8, 128).T, z['ln2_g'][L].reshape(8, 128).T,
                    z['ln2_b'][L].reshape(8, 128).T, z['ssd_norm'][L].reshape(8, 128).T], axis=1)
    return {
        "xT": f(stream[sl].T), "yT": f(ycat.T), "pT": f(z['p'][L].reshape(-1, 256)[sl].T),
        "wg": f(z['w_in'][L][:, 6576:]), "wb": f(z['w_branch'][L]), "wo": f(z['w_o'][L]), "wplg": f(z['w_pl_gate'][L]),
        "wpl": f(z['w_pl'][L]), "wr": f(z['w_router'][L]), "br": f(z['b_router'][L][None]),
        "wgu": f(z['w_gu'][L]), "wd": f(z['w_down'][L]), "bgu": f(z['b_gu'][L].reshape(32, 16, 128).transpose(2, 0, 1)),
        "bd": f(z['b_down'][L]), "vec": f(vec), "ident": np.eye(128, dtype=np.float32),
    }


def kernel(**inputs):
    z = {k: np.asarray(v) for k, v in inputs.items()}
    NCORE = 8
    cores = list(range(NCORE))
    xf = z['x'].reshape(-1, 1024).astype(np.float32)
    f = lambda a: np.ascontiguousarray(a, dtype=np.float32)
    vec0 = f(np.stack([z['ln_in_g'].reshape(8, 128).T, z['ln_in_b'].reshape(8, 128).T], axis=1))
    nc0 = build_L0()
    res = run_bass_kernel_spmd(nc0, [{"xT": f(xf[c * 2048:(c + 1) * 2048].T), "vec": vec0} for c in cores], core_ids=cores)
    stream = np.concatenate([r["outT"].T for r in res.results], axis=0)
    for L in range(2):
        ncA = build_A(T=8192)
        imA = [host_inputs_A(z, L, stream[b * 8192:(b + 1) * 8192], j) for b in range(2) for j in range(4)]
        resA = run_bass_kernel_spmd(ncA, imA, core_ids=cores).results
        ya = np.concatenate([np.concatenate([resA[b * 4 + j]["ya"] for j in range(4)], axis=1) for b in range(2)], axis=0)
        yb = np.concatenate([np.concatenate([resA[b * 4 + j]["yb"] for j in range(4)], axis=1) for b in range(2)], axis=0)
        u = np.concatenate([np.concatenate([resA[b * 4 + j]["yc"] for j in range(4)], axis=1) for b in range(2)], axis=0)
        del resA, imA
        ncB = build_B()
        imB = [host_inputs_B(L, stream, ya, yb, u, z, c) for c in cores]
        resB = run_bass_kernel_spmd(ncB, imB, core_ids=cores).results
        stream = np.concatenate([r["outT"].T for r in resB], axis=0)
        del resB, imB
    return np.ascontiguousarray(stream.reshape(2, 8192, 1024), dtype=np.float32)
```

```python
import os
import contextlib, time
import numpy as np
import concourse.bass as bass
import concourse.mybir as mybir
from concourse.bass_utils import run_bass_kernel_spmd

F32 = mybir.dt.float32
BF16 = mybir.dt.bfloat16
I32 = mybir.dt.int32
ALU = mybir.AluOpType
AF = mybir.ActivationFunctionType
AX = mybir.AxisListType

ENG = ["sync", "gpsimd", "scalar", "vector", "tensor"]
NDMASEM = 6
import os as _os
ATTACH = bool(int(_os.environ.get('FW_ATTACH', '1')))


class Prog:
    def __init__(self, immediate=True):
        self.immediate = immediate
        self.nc = bass.Bass("TRN2", target_bir_lowering=False)
        try:
            self.nc.allow_low_precision("bf16 matmul operands with fp32 accumulation")
            self.nc.allow_non_contiguous_dma("strided layouts")
        except Exception as ex:
            print("allow_* failed", ex)
        self.st = contextlib.ExitStack()
        self.ops = {e: [] for e in ENG}
        self.cnt = {}
        self.sems = {}
        self.lastw = {}
        self.reads = {}
        self.seen = {e: {} for e in ENG}
        self.dma_i = {e: 0 for e in ENG}
        self.dma_last = {}
        self.ninstr = 0
        for e in ["gpsimd", "scalar", "vector", "tensor"]:
            self._sem("c_" + e)
        for e in ["sync", "gpsimd", "scalar"]:
            for i in range(NDMASEM):
                self._sem(f"d_{e}_{i}")

    def _sem(self, name):
        self.sems[name] = self.st.enter_context(self.nc.semaphore(name))
        self.cnt[name] = 0

    def dram(self, name, shape, dt=F32, kind="ExternalInput"):
        return self.nc.dram_tensor(name, list(shape), dt, kind=kind).ap()

    def sb(self, name, shape, dt=F32):
        return self.st.enter_context(self.nc.sbuf_tensor("s_" + name, list(shape), dt))

    def ps(self, name, shape, dt=F32):
        return self.st.enter_context(self.nc.psum_tensor("p_" + name, list(shape), dt))

    def _deps(self, eng, reads, writes):
        need = {}
        def add(tok):
            if tok is None:
                return
            s, v = tok
            if need.get(s, 0) < v:
                need[s] = v
        for k in reads:
            add(self.lastw.get(k))
        for k in writes:
            add(self.lastw.get(k))
            for t in self.reads.get(k, ()):
                add(t)
        out = []
        for s, v in need.items():
            if self.seen[eng].get(s, 0) < v:
                self.seen[eng][s] = v
                out.append((s, v))
        return out

    def _commit(self, tok, reads, writes):
        for k in reads:
            self.reads.setdefault(k, []).append(tok)
        for k in writes:
            self.lastw[k] = tok
            self.reads[k] = []

    def capture(self, f, *a):
        self._buf = []
        try:
            f(*a)
        finally:
            buf, self._buf = self._buf, None
        return buf

    def emit_interleaved(self, chains):
        chains = [list(c) for c in chains if c]
        idx = [0] * len(chains)
        live = True
        while live:
            live = False
            for ci, c in enumerate(chains):
                if idx[ci] < len(c):
                    kind, a, kw = c[idx[ci]]; idx[ci] += 1; live = True
                    (self.op if kind == "op" else self.dma)(*a, **kw)

    def emit_balanced(self, chains):
        chains = [list(c) for c in chains if c]
        idx = [0] * len(chains)
        total = sum(len(c) for c in chains)
        for _ in range(total):
            ci = min((i for i in range(len(chains)) if idx[i] < len(chains[i])), key=lambda i: idx[i] / len(chains[i]))
            kind, a, kw = chains[ci][idx[ci]]; idx[ci] += 1
            (self.op if kind == "op" else self.dma)(*a, **kw)

    def op(self, eng, fn, reads=(), writes=()):
        if getattr(self, "_buf", None) is not None:
            self._buf.append(("op", (eng, fn, reads, writes), {})); return None
        psr = [k for k in reads if isinstance(k, str) and k.startswith("ps")]
        if psr:
            reads = [k for k in reads if k not in psr]
            writes = list(writes) + psr
        waits = self._deps(eng, reads, writes)
        s = "c_" + eng
        self.cnt[s] += 1
        tok = (s, self.cnt[s])
        self._commit(tok, reads, writes)
        self._emit(eng, waits, fn, s, 1)
        self.ninstr += 1
        return tok

    def dma(self, eng, out, in_, reads=(), writes=(), **kw):
        if getattr(self, "_buf", None) is not None:
            self._buf.append(("dma", (eng, out, in_, reads, writes), kw)); return None
        slot = self.dma_i[eng] % NDMASEM
        self.dma_i[eng] += 1
        s = f"d_{eng}_{slot}"
        waits = self._deps(eng, reads, writes)
        prev = self.cnt[s]
        if prev > 0 and self.seen[eng].get(s, 0) < prev:
            self.seen[eng][s] = prev
            waits.append((s, prev))
        self.cnt[s] += 16
        tok = (s, self.cnt[s])
        self._commit(tok, reads, writes)
        fn = lambda e, out=out, in_=in_, kw=kw: e.dma_start(out=out, in_=in_, **kw)
        self._emit(eng, waits, fn, s, 16)
        self.ninstr += 1
        return tok

    def coll(self, kind, in_ap, out_ap, groups, reads=(), writes=()):
        eng = "gpsimd"
        slot = self.dma_i[eng] % NDMASEM
        self.dma_i[eng] += 1
        s = f"d_{eng}_{slot}"
        waits = self._deps(eng, reads, writes)
        prev = self.cnt[s]
        if prev > 0 and self.seen[eng].get(s, 0) < prev:
            self.seen[eng][s] = prev
            waits.append((s, prev))
        self.cnt[s] += 16
        tok = (s, self.cnt[s])
        self._commit(tok, reads, writes)
        fn = lambda e: e.collective_compute(kind, ALU.bypass, replica_groups=groups, ins=[in_ap], outs=[out_ap])
        self._emit(eng, waits, fn, s, 16)
        self.ninstr += 1
        return tok

    def barrier(self):
        for eng in ENG:
            waits = []
            for sname, v in self.cnt.items():
                if v > 0 and self.seen[eng].get(sname, 0) < v:
                    self.seen[eng][sname] = v
                    waits.append((sname, v))
            self._emit(eng, waits, None, None, 0)

    def finish_wait(self, eng, keys):
        waits = self._deps(eng, keys, ())
        self._emit(eng, waits, None, None, 0)

    def _emit(self, eng, waits, fn, s, inc):
        if not self.immediate:
            self.ops[eng].append((waits, fn, s, inc)); return
        engobj = getattr(self.nc, eng)
        if fn is None or not ATTACH:
            for (ws, wv) in waits:
                engobj.wait_ge(self.sems[ws], wv)
            if fn is not None:
                fn(engobj).then_inc(self.sems[s], inc)
            return
        for (ws, wv) in waits[1:]:
            engobj.wait_ge(self.sems[ws], wv)
        ins = fn(engobj)
        if waits:
            ins._wait_ge(self.sems[waits[0][0]], waits[0][1])
        ins.then_inc(self.sems[s], inc)

    def build(self):
        if self.immediate:
            self.st.close(); return self.nc
        nc = self.nc
        with nc.Block() as block:
            def mk(e):
                def body(engobj):
                    for waits, fn, s, inc in self.ops[e]:
                        for (ws, wv) in waits:
                            engobj.wait_ge(self.sems[ws], wv)
                        if fn is not None:
                            fn(engobj).then_inc(self.sems[s], inc)
                return body
            block.sync(mk("sync"))
            block.gpsimd(mk("gpsimd"))
            block.scalar(mk("scalar"))
            block.vector(mk("vector"))
            block.tensor(mk("tensor"))
        self.st.close()
        return nc


NCOL = 2060
NEG = -30000.0
RV = {}
_o = 0
for _n, _l in [("gconv", 5 * 384), ("sconv", 5 * 512), ("sconvb", 512), ("mup", 768), ("mun", 768), ("spb", 10), ("alog", 10),
               ("gnorm", 128), ("w0", 256), ("a0", 256), ("kk", 128), ("ka", 128), ("rk", 128), ("lng", 128), ("lnb", 128), ("dsk", 256)]:
    RV[_n] = (_o, _l); _o += _l
NV = _o


def host_inputs_A(z, L, stream_b, j):
    f = lambda a: np.ascontiguousarray(a, dtype=np.float32)
    w_in = z['w_in'][L]
    g = j // 2
    GD0, RW0, SS0 = 0, 2064, 2064 + 1920
    r = lambda a, n: list(range(a, a + n))
    cols = (r(GD0 + j * 128, 128) + r(GD0 + 512 + j * 128, 128) + r(GD0 + 1024 + j * 128, 128) + r(GD0 + 1536 + j * 128, 128)
            + r(RW0 + j * 128, 128) + r(RW0 + 512 + j * 128, 128) + r(RW0 + 1024 + j * 128, 128) + r(RW0 + 1536, 384)
            + r(SS0 + j * 256, 256) + r(SS0 + 1024 + j * 256, 256) + r(SS0 + 2048 + g * 128, 128) + r(SS0 + 2304 + g * 128, 128)
            + [GD0 + 2048 + d * 4 + j for d in range(2)] + [GD0 + 2056 + d * 4 + j for d in range(2)]
            + [SS0 + 2560 + d * 16 + 4 * j + i for d in range(2) for i in range(4)])
    assert len(cols) == NCOL
    rv = np.zeros(NV, np.float32)
    def put(n, a):
        o, l = RV[n]; a = np.asarray(a, np.float32).reshape(-1); assert a.size == l, (n, a.size, l); rv[o:o + l] = a
    qkv_idx = r(j * 128, 128) + r(512 + j * 128, 128) + r(1024 + j * 128, 128)
    xbc_idx = r(j * 256, 256) + r(1024 + g * 128, 128) + r(1280 + g * 128, 128)
    rw_idx = r(j * 128, 128) + r(512 + j * 128, 128) + r(1024 + j * 128, 128) + r(1536, 384)
    put("gconv", z['gdn_conv'][L][:, qkv_idx]); put("sconv", z['ssd_conv'][L][:, xbc_idx]); put("sconvb", z['ssd_conv_b'][L][xbc_idx])
    put("mup", z['rwkv_mu_prev'][L][rw_idx]); put("mun", z['rwkv_mu_next'][L][rw_idx])
    put("spb", np.concatenate([z['gdn_dt_bias'][L][:, j], z['ssd_dt_bias'][L][:, 4 * j:4 * j + 4].reshape(-1)]))
    put("alog", np.concatenate([z['gdn_a_log'][L][:, j], z['ssd_a_log'][L][:, 4 * j:4 * j + 4].reshape(-1)]))
    put("gnorm", z['gdn_norm'][L]); put("w0", z['rwkv_w0'][L][:, j * 128:(j + 1) * 128]); put("a0", z['rwkv_a0'][L][:, j * 128:(j + 1) * 128])
    put("kk", z['rwkv_k_k'][L][j * 128:(j + 1) * 128]); put("ka", z['rwkv_k_a'][L][j * 128:(j + 1) * 128])
    put("rk", z['rwkv_r_k'][L][2 * j:2 * j + 2]); put("lng", z['rwkv_ln_g'][L][j * 128:(j + 1) * 128]); put("lnb", z['rwkv_ln_b'][L][j * 128:(j + 1) * 128])
    put("dsk", np.repeat(z['ssd_d'][L][4 * j:4 * j + 4], 64))
    k = np.arange(128)
    UT = (k[:, None] <= k[None, :]).astype(np.float32); LT = (k[:, None] >= k[None, :]).astype(np.float32)
    blk = (k[:, None] // 64 == k[None, :] // 64).astype(np.float32)
    bdUT = UT * blk; bdLT = LT * blk
    msk = np.stack([UT, LT, np.where(UT > 0, 0.0, NEG), np.where(LT > 0, 0.0, NEG), np.eye(128), np.ones((128, 128)), bdUT, bdLT, -(bdUT - np.eye(128)), -(bdLT - np.eye(128)), bdUT - np.eye(128), bdLT - np.eye(128)], axis=1)
    return {
        "xT": f(stream_b.T), "wc": f(w_in[:, cols]), "rowvec": f(rv[None]),
        "wup": f(z['rwkv_w_up'][L][:, :, j * 128:(j + 1) * 128].reshape(128, 128)),
        "aup": f(z['rwkv_a_up'][L][:, :, j * 128:(j + 1) * 128].reshape(128, 128)),
        "gup": f(z['rwkv_g_up'][L][:, j * 128:(j + 1) * 128]), "msk": f(msk),
    }


def build_A(T=8192, dump=False, NS=16, phases=(1, 2, 3, 4)):
    p = Prog(); nc = p.nc
    NTL = T // 128
    dk = "ExternalOutput" if dump else "Internal"
    xT = p.dram("xT", [1024, T]); wc = p.dram("wc", [1024, NCOL]); rowvec = p.dram("rowvec", [1, NV])
    wup = p.dram("wup", [128, 128]); aup = p.dram("aup", [128, 128]); gup = p.dram("gup", [128, 128]); mskd = p.dram("msk", [128, 12, 128])
    ya_o = p.dram("ya", [T, 128], kind="ExternalOutput"); yb_o = p.dram("yb", [T, 128], kind="ExternalOutput"); yc_o = p.dram("yc", [T, 256], kind="ExternalOutput")
    cols_s = p.dram("cols_s", [T + 4, NCOL], kind=dk)
    gkq_s = p.dram("gkq_s", [T, 2, 128], kind=dk); gsc_s = p.dram("gsc_s", [T, 4], kind=dk); gbvT_s = p.dram("gbvT_s", [2, 128, T], kind=dk)
    rw_s = p.dram("rw_s", [2, 2, T, 5, 64], kind=dk); rvT_s = p.dram("rvT_s", [128, T], kind=dk); rpost_s = p.dram("rpost_s", [T, 258], kind=dk)
    ssd_s = p.dram("ssd_s", [T, 528], kind=dk)
    gy_s = p.dram("gy_s", [2, T, 128], kind=dk); gkqv_s = p.dram("gkqv_s", [T, 3, 128], kind=dk); gbg_s = p.dram("gbg_s", [T, 4], kind=dk); ry_s = p.dram("ry_s", [2, T, 128], kind=dk); sy_s = p.dram("sy_s", [T, 256], kind=dk)

    V = lambda fn, r=(), w=(): p.op("vector", fn, r, w)
    G = lambda fn, r=(), w=(): p.op("gpsimd", fn, r, w)
    S = lambda fn, r=(), w=(): p.op("scalar", fn, r, w)
    PE = lambda fn, r=(), w=(): p.op("tensor", fn, r, w)

    msk = p.sb("msk", [128, 12, 128]); rvs = p.sb("rvs", [128, NV])
    p.dma("sync", msk[:], mskd[:, :, :], writes=["msk"])
    p.dma("sync", rvs[:], rowvec.partition_broadcast(128)[:, 0, :], writes=["rvs"])
    UT, LT, NEGf, NEGb, ident, ones, bdUT, bdLT, nbdUTs, nbdLTs, sbdUT, sbdLT = (msk[:, i, :] for i in range(12))
    def rv(n, a=0, l=None):
        o, ln = RV[n]
        return rvs[:, o + a:o + a + (ln - a if l is None else l)]
    negexp = p.sb("negexp", [128, 10])
    S(lambda e: e.activation(out=negexp[:], in_=rv("alog"), func=AF.Exp), ["rvs"], ["negexp"])
    V(lambda e: e.tensor_scalar(out=negexp[:], in0=negexp[:], scalar1=-1.0, scalar2=None, op0=ALU.mult), ["negexp"], ["negexp"])
    PS = [p.ps(f"ps{i}", [128, 512]) for i in range(8)]

    if 1 in phases:
        with contextlib.ExitStack() as sc:
            sb = lambda name, shape, dt=F32: sc.enter_context(nc.sbuf_tensor("s_" + name, list(shape), dt))
            W_bf = sb("W_bf", [128, 8, 2048], BF16); w_sm = sb("w_sm", [128, 8, 12])
            zt = sb("zt", [2, NCOL])
            xb = [sb(f"xb{i}", [128, 8, 128], BF16) for i in range(2)]; xf = [sb(f"xf{i}", [128, 8, 128]) for i in range(2)]
            ct = [sb(f"ct{i}", [128, NCOL]) for i in range(2)]
            wcv = wc.rearrange("(kc p) n -> p kc n", p=128)
            for kc in range(8):
                p.dma("gpsimd", W_bf[:, kc, :], wc[kc * 128:(kc + 1) * 128, 0:2048], writes=["W_bf"])
            p.dma("sync", w_sm[:], wcv[:, :, 2048:2060], writes=["w_sm"])
            V(lambda e: e.memset(zt[:], 0.0), [], ["zt"])
            p.dma("sync", cols_s[0:2, :], zt[:], reads=["zt"], writes=["cols_pad"])
            p.dma("sync", cols_s[T + 2:T + 4, :], zt[:], reads=["zt"], writes=["cols_pad"])
            xTv = xT.rearrange("(kc p) n -> p kc n", p=128)
            for tt in range(NTL):
                t0 = tt * 128; i = tt % 2
                p.dma("gpsimd", xb[i][:], xTv[:, :, t0:t0 + 128], writes=[f"xb{i}"])
                p.dma("sync", xf[i][:], xTv[:, :, t0:t0 + 128], writes=[f"xf{i}"])
                for gq in range(4):
                    pp = PS[gq]
                    for kc in range(8):
                        PE(lambda e, kc=kc, gq=gq, pp=pp, i=i: e.matmul(pp[:, :], lhsT=xb[i][:, kc, :], rhs=W_bf[:, kc, gq * 512:(gq + 1) * 512], start=(kc == 0), stop=(kc == 7)),
                           [f"xb{i}", "W_bf"], [f"ps{gq}"])
                    if gq % 2 == 0:
                        S(lambda e, gq=gq, pp=pp, i=i: e.copy(out=ct[i][:, gq * 512:(gq + 1) * 512], in_=pp[:, :]), [f"ps{gq}"], [f"ct{i}"])
                    else:
                        V(lambda e, gq=gq, pp=pp, i=i: e.tensor_copy(out=ct[i][:, gq * 512:(gq + 1) * 512], in_=pp[:, :]), [f"ps{gq}"], [f"ct{i}"])
                for kc in range(8):
                    PE(lambda e, kc=kc, i=i: e.matmul(PS[4][:, :12], lhsT=xf[i][:, kc, :], rhs=w_sm[:, kc, :], start=(kc == 0), stop=(kc == 7)),
                       [f"xf{i}", "w_sm"], ["ps4"])
                V(lambda e, i=i: e.tensor_copy(out=ct[i][:, 2048:2060], in_=PS[4][:, :12]), ["ps4"], [f"ct{i}"])
                p.dma("sync", cols_s[2 + t0:2 + t0 + 128, :], ct[i][:], reads=[f"ct{i}"], writes=["cols_s"])
        p.barrier()

    if 2 in phases:
        with contextlib.ExitStack() as sc:
            sb = lambda name, shape, dt=F32: sc.enter_context(nc.sbuf_tensor("s_" + name, list(shape), dt))
            win = [sb(f"win{j}", [128, NCOL]) for j in range(5)]
            wup_sb = sb("wup_sb", [128, 128]); aup_sb = sb("aup_sb", [128, 128]); gup_sb = sb("gup_sb", [128, 128])
            p.dma("sync", wup_sb[:], wup[:, :], writes=["wup_sb"]); p.dma("sync", aup_sb[:], aup[:, :], writes=["aup_sb"]); p.dma("sync", gup_sb[:], gup[:, :], writes=["gup_sb"])
            cacc = sb("cacc", [128, 896]); ctmp = sb("ctmp", [128, 896]); qkv = sb("qkv", [128, 384]); xbc = sb("xbc", [128, 528])
            junk = sb("junk", [128, 128]); ssq = sb("ssq", [128, 4]); kq = sb("kq", [128, 2, 128])
            spx = sb("spx", [128, 10]); spa = sb("spa", [128, 10]); spl = sb("spl", [128, 10]); beta = sb("beta", [128, 2]); gsc = sb("gsc", [128, 4])
            bv = sb("bv", [128, 2, 128]); trs = sb("trs", [128, 128]); bg = sb("bg", [128, 4])
            sh = sb("sh", [128, 768]); d1 = sb("d1", [128, 768]); d2 = sb("d2", [128, 768])
            tw = sb("tw", [128, 128]); twT = sb("twT", [128, 128]); alT = sb("alT", [128, 128]); sgl = sb("sgl", [128, 128]); sgT = sb("sgT", [128, 128])
            RW = [sb(f"RW{d}", [128, 5, 128]) for d in range(2)]; ad = [sb(f"ad{d}", [128, 128]) for d in range(2)]
            wraw = sb("wraw", [128, 128]); kx = sb("kx", [128, 128]); sqk = sb("sqk", [128, 128]); rkk = sb("rkk", [128, 2]); kkn = sb("kkn", [128, 128])
            rkr = sb("rkr", [128, 128]); prod = sb("prod", [128, 128]); bon = sb("bon", [128, 2, 2]); rpost = sb("rpost", [128, 258]); t128 = sb("t128", [128, 128])
            pT1, pT2, pM1, pM2, pT3 = PS[0], PS[1], PS[2], PS[3], PS[4]
            for tt in range(NTL):
                t0 = tt * 128
                for j in range(5):
                    p.dma("sync" if j % 2 == 0 else "scalar", win[j][:], cols_s[t0 + j:t0 + j + 128, :], reads=["cols_s", "cols_pad"], writes=[f"win{j}"])
                cur = win[2]
                for (c0, c1, o0, cname, cw) in [(0, 384, 0, "gconv", 384), (1536, 2048, 384, "sconv", 512)]:
                    for j in range(5):
                        wj = rv(cname, j * cw, cw)
                        if j == 0:
                            V(lambda e, c0=c0, c1=c1, o0=o0, wj=wj, cw=cw: e.tensor_tensor(out=cacc[:, o0:o0 + cw], in0=win[0][:, c0:c1], in1=wj, op=ALU.mult), ["win0", "rvs"], [f"cacc{o0}"])
                        else:
                            G(lambda e, c0=c0, c1=c1, o0=o0, wj=wj, cw=cw, j=j: e.tensor_tensor(out=ctmp[:, o0:o0 + cw], in0=win[j][:, c0:c1], in1=wj, op=ALU.mult), [f"win{j}", "rvs"], [f"ctmp{o0}"])
                            V(lambda e, o0=o0, cw=cw: e.tensor_tensor(out=cacc[:, o0:o0 + cw], in0=cacc[:, o0:o0 + cw], in1=ctmp[:, o0:o0 + cw], op=ALU.add), [f"cacc{o0}", f"ctmp{o0}"], [f"cacc{o0}"])
                V(lambda e: e.tensor_tensor(out=cacc[:, 384:896], in0=cacc[:, 384:896], in1=rv("sconvb"), op=ALU.add), ["cacc384", "rvs"], ["cacc384"])
                S(lambda e: e.activation(out=qkv[:], in_=cacc[:, 0:384], func=AF.Silu), ["cacc0"], ["qkv"])
                S(lambda e: e.activation(out=xbc[:, 0:512], in_=cacc[:, 384:896], func=AF.Silu), ["cacc384"], ["xbc"])
                V(lambda e: e.tensor_tensor(out=spx[:], in0=cur[:, 2050:2060], in1=rv("spb"), op=ALU.add), ["win2", "rvs"], ["spx"])
                S(lambda e: e.activation(out=spa[:], in_=spx[:], func=AF.Abs), ["spx"], ["spa"])
                S(lambda e: e.activation(out=spa[:], in_=spa[:], func=AF.Exp, scale=-1.0), ["spa"], ["spa"])
                S(lambda e: e.activation(out=spl[:], in_=spa[:], func=AF.Ln, bias=1.0), ["spa"], ["spl"])
                V(lambda e: e.tensor_scalar(out=spx[:], in0=spx[:], scalar1=0.0, scalar2=None, op0=ALU.max), ["spx"], ["spx"])
                V(lambda e: e.tensor_tensor(out=spx[:], in0=spx[:], in1=spl[:], op=ALU.add), ["spx", "spl"], ["spx"])
                V(lambda e: e.tensor_tensor(out=spl[:], in0=spx[:], in1=negexp[:], op=ALU.mult), ["spx", "negexp"], ["spl"])
                V(lambda e: e.tensor_copy(out=xbc[:, 512:520], in_=spx[:, 2:10]), ["spx"], ["xbc"])
                V(lambda e: e.tensor_copy(out=xbc[:, 520:528], in_=spl[:, 2:10]), ["spl"], ["xbc"])
                p.dma("gpsimd", ssd_s[t0:t0 + 128, :], xbc[:], reads=["xbc"], writes=["ssd_s"])
                S(lambda e: e.activation(out=beta[:], in_=cur[:, 2048:2050], func=AF.Sigmoid), ["win2"], ["beta"])
                S(lambda e: e.activation(out=gsc[:, 0:2], in_=spl[:, 0:2], func=AF.Exp), ["spl"], ["gsc"])
                V(lambda e: e.scalar_tensor_tensor(out=gsc[:, 2:4], in0=gsc[:, 0:2], scalar=-1.0, in1=beta[:], op0=ALU.mult, op1=ALU.mult), ["gsc", "beta"], ["gsc"])
                for qi in range(2):
                    src = qkv[:, qi * 128:(qi + 1) * 128]
                    V(lambda e, src=src, qi=qi: e.scalar_tensor_tensor(out=junk[:], in0=src, scalar=1.0, in1=src, op0=ALU.mult, op1=ALU.mult, accum_out=ssq[:, qi:qi + 1]), ["qkv"], ["junk", "ssq"])
                V(lambda e: e.tensor_scalar(out=ssq[:, 0:2], in0=ssq[:, 0:2], scalar1=1e-6, scalar2=None, op0=ALU.add), ["ssq"], ["ssq"])
                S(lambda e: e.activation(out=ssq[:, 0:2], in_=ssq[:, 0:2], func=AF.Sqrt), ["ssq"], ["ssq"])
                V(lambda e: e.reciprocal(out=ssq[:, 0:2], in_=ssq[:, 0:2]), ["ssq"], ["ssq"])
                V(lambda e: e.tensor_scalar(out=kq[:, 0, :], in0=qkv[:, 128:256], scalar1=ssq[:, 1:2], scalar2=None, op0=ALU.mult), ["qkv", "ssq"], ["kq"])
                V(lambda e: e.tensor_scalar(out=kq[:, 1, :], in0=qkv[:, 0:128], scalar1=ssq[:, 0:1], scalar2=128 ** -0.5, op0=ALU.mult, op1=ALU.mult), ["qkv", "ssq"], ["kq"])
                p.dma("gpsimd", gkqv_s[t0:t0 + 128, 0:2, :], kq[:], reads=["kq"], writes=["gkqv_s"])
                p.dma("gpsimd", gkqv_s[t0:t0 + 128, 2, :], qkv[:, 256:384], reads=["qkv"], writes=["gkqv_s"])
                G(lambda e: e.tensor_copy(out=bg[:, 0:2], in_=beta[:]), ["beta"], ["bg"])
                G(lambda e: e.tensor_copy(out=bg[:, 2:4], in_=spl[:, 0:2]), ["spl"], ["bg"])
                p.dma("gpsimd", gbg_s[t0:t0 + 128, :], bg[:], reads=["bg"], writes=["gbg_s"])
                c_, pv, nx = cur[:, 512:1280], win[1][:, 512:1280], win[3][:, 512:1280]
                V(lambda e: e.tensor_tensor(out=d1[:], in0=pv, in1=c_, op=ALU.subtract), ["win1", "win2"], ["d1"])
                G(lambda e: e.tensor_tensor(out=d1[:], in0=d1[:], in1=rv("mup"), op=ALU.mult), ["d1", "rvs"], ["d1"])
                V(lambda e: e.tensor_tensor(out=d2[:], in0=nx, in1=c_, op=ALU.subtract), ["win3", "win2"], ["d2"])
                G(lambda e: e.tensor_tensor(out=d2[:], in0=d2[:], in1=rv("mun"), op=ALU.mult), ["d2", "rvs"], ["d2"])
                V(lambda e: e.tensor_tensor(out=sh[:], in0=c_, in1=d1[:], op=ALU.add), ["win2", "d1"], ["sh"])
                V(lambda e: e.tensor_tensor(out=sh[:], in0=sh[:], in1=d2[:], op=ALU.add), ["sh", "d2"], ["sh"])
                r_, k_, v_, wl, al, gl = (sh[:, i * 128:(i + 1) * 128] for i in range(6))
                S(lambda e: e.activation(out=tw[:], in_=wl, func=AF.Tanh), ["sh"], ["tw"])
                PE(lambda e: e.transpose(out=pT1[:, :128], in_=tw[:], identity=ident), ["tw", "msk"], ["ps0"])
                S(lambda e: e.copy(out=twT[:], in_=pT1[:, :128]), ["ps0"], ["twT"])
                PE(lambda e: e.transpose(out=pT2[:, :128], in_=al, identity=ident), ["sh", "msk"], ["ps1"])
                V(lambda e: e.tensor_copy(out=alT[:], in_=pT2[:, :128]), ["ps1"], ["alT"])
                S(lambda e: e.activation(out=sgl[:], in_=gl, func=AF.Sigmoid), ["sh"], ["sgl"])
                PE(lambda e: e.transpose(out=pT3[:, :128], in_=sgl[:], identity=ident), ["sgl", "msk"], ["ps4"])
                V(lambda e: e.tensor_copy(out=sgT[:], in_=pT3[:, :128]), ["ps4"], ["sgT"])
                PE(lambda e: e.matmul(pT3[:, 128:256], lhsT=sgT[:], rhs=gup_sb[:], start=True, stop=True), ["sgT", "gup_sb"], ["ps4"])
                S(lambda e: e.copy(out=rpost[:, 128:256], in_=pT3[:, 128:256]), ["ps4"], ["rpost"])
                V(lambda e: e.tensor_tensor(out=kx[:], in0=k_, in1=rv("kk"), op=ALU.mult), ["sh", "rvs"], ["kx"])
                S(lambda e: e.activation(out=sqk[:], in_=kx[:], func=AF.Square), ["kx"], ["sqk"])
                V(lambda e: e.tensor_reduce(out=rkk[:], in_=sqk[:].rearrange("p (h n) -> p h n", h=2), axis=AX.X, op=ALU.add), ["sqk"], ["rkk"])
                V(lambda e: e.tensor_scalar(out=rkk[:], in0=rkk[:], scalar1=1e-6, scalar2=None, op0=ALU.add), ["rkk"], ["rkk"])
                S(lambda e: e.activation(out=rkk[:], in_=rkk[:], func=AF.Sqrt), ["rkk"], ["rkk"])
                V(lambda e: e.reciprocal(out=rkk[:], in_=rkk[:]), ["rkk"], ["rkk"])
                for h in range(2):
                    V(lambda e, h=h: e.tensor_scalar(out=kkn[:, h * 64:(h + 1) * 64], in0=kx[:, h * 64:(h + 1) * 64], scalar1=rkk[:, h:h + 1], scalar2=None, op0=ALU.mult), ["kx", "rkk"], ["kkn"])
                G(lambda e: e.tensor_tensor(out=rkr[:], in0=r_, in1=rv("rk"), op=ALU.mult), ["sh", "rvs"], ["rkr"])
                for d in range(2):
                    hs = slice(d * 64, (d + 1) * 64)
                    PE(lambda e, hs=hs: e.matmul(pM1[:, :128], lhsT=twT[hs, :], rhs=wup_sb[hs, :], start=True, stop=True), ["twT", "wup_sb"], ["ps2"])
                    V(lambda e, d=d: e.tensor_tensor(out=wraw[:], in0=pM1[:, :128], in1=rv("w0", d * 128, 128), op=ALU.add), ["ps2", "rvs"], ["wraw"])
                    S(lambda e: e.activation(out=wraw[:], in_=wraw[:], func=AF.Sigmoid), ["wraw"], ["wraw"])
                    S(lambda e, d=d: e.activation(out=RW[d][:, 0, :], in_=wraw[:], func=AF.Exp, scale=-0.6065306597126334), ["wraw"], [f"RW{d}"])
                    PE(lambda e, hs=hs: e.matmul(pM2[:, :128], lhsT=alT[hs, :], rhs=aup_sb[hs, :], start=True, stop=True), ["alT", "aup_sb"], ["ps3"])
                    V(lambda e, d=d: e.tensor_tensor(out=ad[d][:], in0=pM2[:, :128], in1=rv("a0", d * 128, 128), op=ALU.add), ["ps3", "rvs"], [f"ad{d}"])
                    S(lambda e, d=d: e.activation(out=ad[d][:], in_=ad[d][:], func=AF.Sigmoid), [f"ad{d}"], [f"ad{d}"])
                    G(lambda e, d=d: e.tensor_copy(out=RW[d][:, 1, :], in_=kkn[:]), ["kkn"], [f"RW{d}"])
                    V(lambda e, d=d: e.scalar_tensor_tensor(out=RW[d][:, 2, :], in0=kkn[:], scalar=-1.0, in1=ad[d][:], op0=ALU.mult, op1=ALU.mult), ["kkn", f"ad{d}"], [f"RW{d}"])
                    V(lambda e, d=d: e.scalar_tensor_tensor(out=t128[:], in0=ad[d][:], scalar=-1.0, in1=rv("ka"), op0=ALU.add, op1=ALU.mult), [f"ad{d}", "rvs"], ["t128"])
                    V(lambda e, d=d: e.scalar_tensor_tensor(out=RW[d][:, 3, :], in0=t128[:], scalar=1.0, in1=k_, op0=ALU.add, op1=ALU.mult), ["t128", "sh"], [f"RW{d}"])
                    G(lambda e, d=d: e.tensor_copy(out=RW[d][:, 4, :], in_=r_), ["sh"], [f"RW{d}"])
                    V(lambda e, d=d: e.tensor_tensor(out=prod[:], in0=rkr[:], in1=RW[d][:, 3, :], op=ALU.mult), ["rkr", f"RW{d}"], ["prod"])
                    V(lambda e, d=d: e.tensor_reduce(out=bon[:, d, :], in_=prod[:].rearrange("p (h n) -> p h n", h=2), axis=AX.X, op=ALU.add), ["prod"], ["bon"])
                    for h in range(2):
                        p.dma("gpsimd", rw_s[d, h, t0:t0 + 128, :, :], RW[d][:, :, h * 64:(h + 1) * 64], reads=[f"RW{d}"], writes=["rw_s"])
                V(lambda e: e.tensor_tensor(out=rpost[:, 256:258], in0=bon[:, 0, :], in1=bon[:, 1, :], op=ALU.add), ["bon"], ["rpost"])
                G(lambda e: e.tensor_copy(out=rpost[:, 0:128], in_=v_), ["sh"], ["rpost"])
                p.dma("gpsimd", rpost_s[t0:t0 + 128, :], rpost[:], reads=["rpost"], writes=["rpost_s"])
                PE(lambda e: e.transpose(out=pT2[:, :128], in_=v_, identity=ident), ["sh", "msk"], ["ps1"])
                V(lambda e: e.tensor_copy(out=t128[:], in_=pT2[:, :128]), ["ps1"], ["t128"])
                p.dma("gpsimd", rvT_s[:, t0:t0 + 128], t128[:], reads=["t128"], writes=["rvT_s"])
        p.barrier()
    fin = ["ya", "yb", "yc"]
    if dump:
        fin += ["cols_s", "gkqv_s", "gbg_s", "rw_s", "rvT_s", "rpost_s", "ssd_s", "gy_s", "ry_s"]
    if 3 in phases:
        phase3(p, T, NS, locals())
    if 4 in phases:
        phase4(p, T, locals())
    p.finish_wait("sync", [k for k in fin if k in p.lastw])
    return p.build()


def phase3(p, T, NS, L):
    SSDSTOP = int(os.environ.get('A_SSDSTOP', '99'))
    GSTOP = int(os.environ.get('A_GSTOP', '99'))
    nc = p.nc
    V = L["V"]; G = L["G"]; S = L["S"]; PE = L["PE"]; PS = L["PS"]
    UT, LT, ident, ones = L["UT"], L["LT"], L["ident"], L["ones"]
    gkqv_s, gbg_s, rw_s, rvT_s, ssd_s, gy_s, ry_s, sy_s, cols_s, yc_o = (L[k] for k in
        ["gkqv_s", "gbg_s", "rw_s", "rvT_s", "ssd_s", "gy_s", "ry_s", "sy_s", "cols_s", "yc_o"])
    bdUT, bdLT, nbdUTs, nbdLTs, sbdUT, sbdLT = (L[k_] for k_ in ["bdUT", "bdLT", "nbdUTs", "nbdLTs", "sbdUT", "sbdLT"])
    rpost_s = L["rpost_s"]
    rv = L["rv"]
    NTL = T // 128; NCH = T // NS
    with contextlib.ExitStack() as sc:
        sb = lambda name, shape, dt=F32: sc.enter_context(nc.sbuf_tensor("s_" + name, list(shape), dt))
        RB = []
        for d in range(2):
            r_ = {}
            for nm, shp in [("rwt", [128, 5, 128]), ("vt", [128, 128]), ("lw", [128, 128]), ("lp", [128, 128]), ("Pt", [128, 128]), ("iP", [128, 128]), ("Pm", [128, 128]),
                            ("rt", [128, 128]), ("kt", [128, 128]), ("nbt", [128, 128]), ("ct", [128, 128]), ("rtF", [128, 128]), ("nbF", [128, 128]), ("cF", [128, 128]), ("PF", [128, 128]),
                            ("X", [128, 128]), ("XT", [128, 128]), ("AckT", [128, 128]), ("ArkT", [128, 128]), ("AnrbT", [128, 128]),
                            ("P0", [128, 128]), ("P1", [128, 128]), ("PT0", [128, 128]), ("PT1", [128, 128]), ("TT", [128, 128]),
                            ("Zs", [128, 64]), ("Ms", [128, 64]), ("Yt", [128, 128]), ("H", [128, 64])]:
                r_[nm] = sb(f"r{d}_{nm}", shp)
            for nm in ["cFh", "nbFh", "ktFh", "rtFh"]:
                for h in range(2):
                    r_[f"{nm}{h}"] = sb(f"r{d}_{nm}{h}", [128, 128])
                    V(lambda e, t_=r_[f"{nm}{h}"]: e.memset(t_[:], 0.0), [], [f"r{d}_{nm}{h}"])
            for nm in ["ktS", "nbS"]:
                for ch in range(2):
                    for h in range(2):
                        r_[f"{nm}{ch}{h}"] = sb(f"r{d}_{nm}{ch}{h}", [128, 128])
                        V(lambda e, t_=r_[f"{nm}{ch}{h}"]: e.memset(t_[:], 0.0), [], [f"r{d}_{nm}{ch}{h}"])
            V(lambda e, r_=r_: e.memset(r_["H"][:], 0.0), [], [f"r{d}_H"])
            V(lambda e, r_=r_: e.memset(r_["Zs"][:], 0.0), [], [f"r{d}_Zs"])
            V(lambda e, r_=r_: e.memset(r_["Ms"][:], 0.0), [], [f"r{d}_Ms"])
            RB.append(r_)
        GB = []
        for d in range(2):
            g_ = {}
            for nm, shp in [("kqv", [128, 3, 128]), ("bg", [128, 4]), ("kF", [128, 128]), ("qF", [128, 128]), ("Gs", [128, 128]), ("QKs", [128, 128]),
                            ("gcc", [128, 1]), ("ngcc", [128, 1]), ("R", [128, 128]), ("grow", [128, 128]), ("egrow", [128, 128]), ("dT", [128, 128]), ("dN", [128, 128]),
                            ("Bd", [128, 128]), ("brow", [128, 128]), ("X", [128, 128]), ("XT", [128, 128]), ("P0", [128, 128]), ("P1", [128, 128]),
                            ("PT0", [128, 128]), ("PT1", [128, 128]), ("TT", [128, 128]), ("vb", [128, 128]), ("kbg", [128, 128]), ("be", [128, 1]), ("eg", [128, 1]),
                            ("u", [128, 128]), ("wF", [128, 128]), ("qdF", [128, 128]), ("QKm", [128, 128]), ("vnew", [128, 128]), ("kdA", [128, 128]), ("kdB", [128, 128]),
                            ("dl", [128, 1]), ("dlA", [128, 1]), ("dlB", [128, 1]), ("S", [128, 128]), ("o", [128, 128]), ("t1", [128, 128]), ("t2", [128, 128])]:
                g_[nm] = sb(f"g{d}_{nm}", shp)
            GB.append(g_)
            V(lambda e, g_=g_: e.memset(g_["S"][:], 0.0), [], [f"g{d}_S"])
            V(lambda e, g_=g_: e.memset(g_["vnew"][:], 0.0), [], [f"g{d}_vnew"])
        sxt = sb("sxt", [128, 528]); BCF = sb("BCF", [128, 256]); scT = sb("scT", [128, 128]); acol = sb("acol", [128, 4]); nacol = sb("nacol", [128, 4])
        Rm = sb("Rm", [128, 4, 128]); E = sb("E", [128, 4, 128]); Dm = sb("Dm", [128, 128]); M = sb("M", [128, 4, 128]); CdF = sb("CdF", [128, 4, 128])
        xdt = sb("xdt", [128, 256]); xend = sb("xend", [128, 256]); dend = sb("dend", [128, 4]); H = sb("H", [128, 256]); yt = sb("yt", [128, 256])
        syf = sb("syf", [128, 256]); zt = sb("zts", [128, 256]); xd = sb("xd", [128, 256]); szs = sb("szs", [128, 256])
        pA, pSc, pAc, pRow, pY, pSt = PS[0][:, 0:256], PS[0][:, 256:384], PS[0][:, 384:512], PS[1], PS[2][:, 0:256], PS[2][:, 256:512]
        print('phase3 sbuf remaining', nc.sbuf_bytes_remaining, flush=True)

        def gdn_chunk(d, c):
            t0 = c * 128
            B_ = GB[d]; K = lambda n: f"g{d}_{n}"
            bank = PS[6 + d]; kb = f"ps{6 + d}"
            regs = [bank[:, i * 128:(i + 1) * 128] for i in range(4)] + [PS[3][:, d * 256:d * 256 + 128], PS[3][:, d * 256 + 128:d * 256 + 256]]
            keys = [kb] * 4 + ["ps3", "ps3"]
            (rA, rB, rC, rD, rE, rF), (kA, kB, kC, kD, kE, kF_) = regs, keys
            fwd = (d == 0)
            Mtri = bdUT if fwd else bdLT
            MaskT = Mtri
            MaskN = bdLT if fwd else bdUT
            nST = nbdUTs if fwd else nbdLTs
            nSN = nbdLTs if fwd else nbdUTs
            selA = bdLT[:, 0:1]; selB = bdUT[:, 127:128]
            hA, hB = slice(0, 64), slice(64, 128)
            if fwd:
                first, second, lastF, lastS = hA, hB, 63, 127
            else:
                first, second, lastF, lastS = hB, hA, 64, 0
            kqv, bg = B_["kqv"], B_["bg"]
            p.dma("sync", kqv[:], gkqv_s[t0:t0 + 128, :, :], reads=["gkqv_s"], writes=[K("kqv")])
            p.dma("sync", bg[:], gbg_s[t0:t0 + 128, :], reads=["gbg_s"], writes=[K("bg")])
            kc, qc, vc = kqv[:, 0, :], kqv[:, 1, :], kqv[:, 2, :]
            beta = bg[:, d:d + 1]; g = bg[:, 2 + d:3 + d]
            kF, qF, Gs, QKs = B_["kF"], B_["qF"], B_["Gs"], B_["QKs"]
            PE(lambda e: e.transpose(out=rA, in_=kc, identity=ident), [K("kqv"), "msk"], [kA])
            S(lambda e: e.copy(out=kF[:], in_=rA), [kA], [K("kF")])
            PE(lambda e: e.transpose(out=rB, in_=qc, identity=ident), [K("kqv"), "msk"], [kB])
            S(lambda e: e.copy(out=qF[:], in_=rB), [kB], [K("qF")])
            PE(lambda e: e.matmul(rC, lhsT=kF[:], rhs=kF[:], start=True, stop=True), [K("kF")], [kC])
            S(lambda e: e.copy(out=Gs[:], in_=rC), [kC], [K("Gs")])
            PE(lambda e: e.matmul(rD, lhsT=kF[:], rhs=qF[:], start=True, stop=True), [K("kF"), K("qF")], [kD])
            S(lambda e: e.copy(out=QKs[:], in_=rD), [kD], [K("QKs")])
            G(lambda e: e.tensor_scalar(out=B_["R"][:], in0=Mtri, scalar1=g, scalar2=None, op0=ALU.mult), [K("bg"), "msk"], [K("R")])
            PE(lambda e: e.matmul(rE, lhsT=ones, rhs=B_["R"][:], start=True, stop=True), [K("R"), "msk"], [kE])
            S(lambda e: e.copy(out=B_["grow"][:], in_=rE), [kE], [K("grow")])
            S(lambda e: e.activation(out=B_["egrow"][:], in_=rE, func=AF.Exp), [kE], [K("egrow")])
            V(lambda e: e.scalar_tensor_tensor(out=B_["t1"][:], in0=B_["grow"][:], scalar=1.0, in1=ident, op0=ALU.mult, op1=ALU.mult, accum_out=B_["gcc"][:, 0:1]), [K("grow"), "msk"], [K("t1"), K("gcc")])
            S(lambda e: e.mul(out=B_["ngcc"][:], in_=B_["gcc"][:], mul=-1.0), [K("gcc")], [K("ngcc")])
            for nm, bias_k, sc, Mk in [("dT", "ngcc", 1.0, MaskT), ("dN", "gcc", -1.0, MaskN)]:
                S(lambda e, nm=nm, bias_k=bias_k, sc=sc: e.activation(out=B_[nm][:], in_=B_["grow"][:], func=AF.Identity, bias=B_[bias_k][:, 0:1], scale=sc), [K("grow"), K(bias_k)], [K(nm)])
                G(lambda e, nm=nm: e.tensor_scalar(out=B_[nm][:], in0=B_[nm][:], scalar1=0.0, scalar2=None, op0=ALU.min), [K(nm)], [K(nm)])
                S(lambda e, nm=nm: e.activation(out=B_[nm][:], in_=B_[nm][:], func=AF.Exp), [K(nm)], [K(nm)])
                G(lambda e, nm=nm, Mk=Mk: e.tensor_tensor(out=B_[nm][:], in0=B_[nm][:], in1=Mk, op=ALU.mult), [K(nm), "msk"], [K(nm)])
            G(lambda e: e.tensor_scalar(out=B_["Bd"][:], in0=ident, scalar1=beta, scalar2=None, op0=ALU.mult), [K("bg"), "msk"], [K("Bd")])
            PE(lambda e: e.matmul(rF, lhsT=ones, rhs=B_["Bd"][:], start=True, stop=True), [K("Bd"), "msk"], [kF_])
            S(lambda e: e.copy(out=B_["brow"][:], in_=rF), [kF_], [K("brow")])
            G(lambda e: e.tensor_tensor(out=B_["t1"][:], in0=Gs[:], in1=B_["dT"][:], op=ALU.mult), [K("Gs"), K("dT"), K("t1")], [K("t1")])
            G(lambda e: e.tensor_tensor(out=B_["t1"][:], in0=B_["t1"][:], in1=B_["brow"][:], op=ALU.mult), [K("t1"), K("brow")], [K("t1")])
            G(lambda e: e.tensor_tensor(out=B_["XT"][:], in0=B_["t1"][:], in1=nST, op=ALU.mult), [K("t1"), "msk"], [K("XT")])
            G(lambda e: e.tensor_tensor(out=B_["t2"][:], in0=Gs[:], in1=B_["dN"][:], op=ALU.mult), [K("Gs"), K("dN")], [K("t2")])
            G(lambda e: e.tensor_scalar(out=B_["t2"][:], in0=B_["t2"][:], scalar1=beta, scalar2=None, op0=ALU.mult), [K("t2"), K("bg")], [K("t2")])
            G(lambda e: e.tensor_tensor(out=B_["X"][:], in0=B_["t2"][:], in1=nSN, op=ALU.mult), [K("t2"), "msk"], [K("X")])
            G(lambda e: e.tensor_tensor(out=B_["TT"][:], in0=B_["XT"][:], in1=ident, op=ALU.add), [K("XT"), "msk"], [K("TT")])
            if GSTOP == 1: return
            Pc, PTc, kP, kPT = B_["X"], B_["XT"], K("X"), K("XT")
            for lv in range(1, 6):
                Pn, kPn = B_[f"P{lv % 2}"], K(f"P{lv % 2}")
                PE(lambda e, Pc=Pc, PTc=PTc: e.matmul(rA, lhsT=PTc[:], rhs=Pc[:], start=True, stop=True), [kP, kPT], [kA])
                S(lambda e, Pn=Pn: e.copy(out=Pn[:], in_=rA), [kA], [kPn])
                if lv < 5:
                    PTn, kPTn = B_[f"PT{lv % 2}"], K(f"PT{lv % 2}")
                    PE(lambda e, Pc=Pc, PTc=PTc: e.matmul(rB, lhsT=Pc[:], rhs=PTc[:], start=True, stop=True), [kP, kPT], [kB])
                    S(lambda e, PTn=PTn: e.copy(out=PTn[:], in_=rB), [kB], [kPTn])
                PE(lambda e, Pn=Pn: e.matmul(rC, lhsT=Pn[:], rhs=B_["TT"][:], start=True, stop=True), [kPn, K("TT")], [kC])
                V(lambda e: e.tensor_tensor(out=B_["TT"][:], in0=B_["TT"][:], in1=rC, op=ALU.add), [K("TT"), kC], [K("TT")])
                Pc, kP = Pn, kPn
                if lv < 5:
                    PTc, kPT = PTn, kPTn
            if GSTOP == 2: return
            S(lambda e: e.activation(out=B_["eg"][:], in_=B_["gcc"][:], func=AF.Exp), [K("gcc")], [K("eg")])
            G(lambda e: e.tensor_tensor(out=B_["be"][:], in0=B_["eg"][:], in1=beta, op=ALU.mult), [K("eg"), K("bg")], [K("be")])
            G(lambda e: e.tensor_scalar(out=B_["vb"][:], in0=vc, scalar1=beta, scalar2=None, op0=ALU.mult), [K("kqv"), K("bg")], [K("vb")])
            G(lambda e: e.tensor_scalar(out=B_["kbg"][:], in0=kc, scalar1=B_["be"][:, 0:1], scalar2=None, op0=ALU.mult), [K("kqv"), K("be")], [K("kbg")])
            PE(lambda e: e.matmul(rD, lhsT=B_["TT"][:], rhs=B_["vb"][:], start=True, stop=True), [K("TT"), K("vb")], [kD])
            S(lambda e: e.copy(out=B_["u"][:], in_=rD), [kD], [K("u")])
            PE(lambda e: e.matmul(rE, lhsT=B_["kbg"][:], rhs=B_["TT"][:], start=True, stop=True), [K("kbg"), K("TT")], [kE])
            S(lambda e: e.copy(out=B_["wF"][:], in_=rE), [kE], [K("wF")])
            G(lambda e: e.tensor_tensor(out=B_["qdF"][:], in0=qF[:], in1=B_["egrow"][:], op=ALU.mult), [K("qF"), K("egrow")], [K("qdF")])
            G(lambda e: e.tensor_tensor(out=B_["QKm"][:], in0=QKs[:], in1=B_["dT"][:], op=ALU.mult), [K("QKs"), K("dT")], [K("QKm")])
            for hs, lst in [(first, lastF), (second, lastS)]:
                S(lambda e, hs=hs, lst=lst: e.activation(out=B_["dl"][hs, :], in_=B_["gcc"][hs, :], func=AF.Exp, bias=B_["grow"][hs, lst:lst + 1], scale=-1.0), [K("gcc"), K("grow")], [K("dl")])
            G(lambda e: e.tensor_tensor(out=B_["dlA"][:], in0=B_["dl"][:], in1=selA, op=ALU.mult), [K("dl"), "msk"], [K("dlA")])
            G(lambda e: e.tensor_tensor(out=B_["dlB"][:], in0=B_["dl"][:], in1=selB, op=ALU.mult), [K("dl"), "msk"], [K("dlB")])
            G(lambda e: e.tensor_scalar(out=B_["kdA"][:], in0=kc, scalar1=B_["dlA"][:, 0:1], scalar2=None, op0=ALU.mult), [K("kqv"), K("dlA")], [K("kdA")])
            G(lambda e: e.tensor_scalar(out=B_["kdB"][:], in0=kc, scalar1=B_["dlB"][:, 0:1], scalar2=None, op0=ALU.mult), [K("kqv"), K("dlB")], [K("kdB")])
            kdF, kdS, kkF, kkS = (B_["kdA"], B_["kdB"], K("kdA"), K("kdB")) if fwd else (B_["kdB"], B_["kdA"], K("kdB"), K("kdA"))
            if GSTOP == 3: return
            Sst = B_["S"]
            for (hs, lst, kd_, kkd, rW, kW, rO, kO) in [(first, lastF, kdF, kkF, rA, kA, rC, kC), (second, lastS, kdS, kkS, rB, kB, rD, kD)]:
                PE(lambda e, rW=rW: e.matmul(rW, lhsT=B_["wF"][:], rhs=Sst[:], start=True, stop=True), [K("wF"), K("S")], [kW])
                V(lambda e, hs=hs, rW=rW: e.tensor_tensor(out=B_["vnew"][hs, :], in0=B_["u"][hs, :], in1=rW[hs, :], op=ALU.subtract), [K("u"), kW], [K("vnew")])
                PE(lambda e, rO=rO: e.matmul(rO, lhsT=B_["qdF"][:], rhs=Sst[:], start=True, stop=True), [K("qdF"), K("S")], [kO])
                S(lambda e, hs=hs, rO=rO: e.copy(out=B_["o"][hs, :], in_=rO[hs, :]), [kO], [K("o")])
                PE(lambda e, kd_=kd_: e.matmul(rF, lhsT=kd_[:], rhs=B_["vnew"][:], start=True, stop=True), [kkd, K("vnew")], [kF_])
                V(lambda e, lst=lst: e.scalar_tensor_tensor(out=Sst[:], in0=Sst[:], scalar=B_["egrow"][:, lst:lst + 1], in1=rF, op0=ALU.mult, op1=ALU.add), [K("S"), K("egrow"), kF_], [K("S")])
            if GSTOP == 5: return
            PE(lambda e: e.matmul(rE, lhsT=B_["QKm"][:], rhs=B_["vnew"][:], start=True, stop=True), [K("QKm"), K("vnew")], [kE])
            V(lambda e: e.tensor_tensor(out=B_["o"][:], in0=B_["o"][:], in1=rE, op=ALU.add), [K("o"), kE], [K("o")])
            p.dma("gpsimd", gy_s[d, t0:t0 + 128, :], B_["o"][:], reads=[K("o")], writes=["gy_s"])

        def rwkv_block(d, c):
            t0 = c * 128
            B_ = RB[d]; K = lambda n: f"r{d}_{n}"
            bank = PS[4 + d]; kb = f"ps{4 + d}"
            rA, rB, rC, rD = (bank[:, i * 128:(i + 1) * 128] for i in range(4))
            fwd = (d == 0)
            Mtri = bdUT if fwd else bdLT
            mS_N = sbdLT if fwd else sbdUT
            mS_T = sbdUT if fwd else sbdLT
            mI_T = bdUT if fwd else bdLT
            selc = [bdLT[:, 0:1], bdUT[:, 127:128]]
            hA, hB = slice(0, 64), slice(64, 128)
            order = [(0, hA, 63), (1, hB, 127)] if fwd else [(1, hB, 64), (0, hA, 0)]
            rwt, vt = B_["rwt"], B_["vt"]
            for h in range(2):
                p.dma("sync", rwt[:, :, h * 64:(h + 1) * 64], rw_s[d, h, t0:t0 + 128, :, :], reads=["rw_s"], writes=[K("rwt")])
            p.dma("sync", vt[:], rpost_s[t0:t0 + 128, 0:128], reads=["rpost_s"], writes=[K("vt")])
            w_, kk_, nkka_, kd_, r_ = (rwt[:, i, :] for i in range(5))
            S(lambda e: e.activation(out=B_["lw"][:], in_=w_, func=AF.Ln), [K("rwt")], [K("lw")])
            PE(lambda e: e.matmul(rA, lhsT=Mtri, rhs=B_["lw"][:], start=True, stop=True), [K("lw"), "msk"], [kb])
            S(lambda e: e.copy(out=B_["lp"][:], in_=rA), [kb], [K("lp")])
            S(lambda e: e.activation(out=B_["Pt"][:], in_=rA, func=AF.Exp), [kb], [K("Pt")])
            S(lambda e: e.activation(out=B_["iP"][:], in_=rA, func=AF.Exp, scale=-1.0), [kb], [K("iP")])
            G(lambda e: e.tensor_tensor(out=B_["Pm"][:], in0=B_["lp"][:], in1=B_["lw"][:], op=ALU.subtract), [K("lp"), K("lw")], [K("Pm")])
            S(lambda e: e.activation(out=B_["Pm"][:], in_=B_["Pm"][:], func=AF.Exp), [K("Pm")], [K("Pm")])
            G(lambda e: e.tensor_tensor(out=B_["rt"][:], in0=r_, in1=B_["Pt"][:], op=ALU.mult), [K("rwt"), K("Pt")], [K("rt")])
            G(lambda e: e.tensor_tensor(out=B_["kt"][:], in0=kd_, in1=B_["iP"][:], op=ALU.mult), [K("rwt"), K("iP")], [K("kt")])
            G(lambda e: e.tensor_tensor(out=B_["nbt"][:], in0=nkka_, in1=B_["iP"][:], op=ALU.mult), [K("rwt"), K("iP")], [K("nbt")])
            G(lambda e: e.tensor_tensor(out=B_["ct"][:], in0=kk_, in1=B_["Pm"][:], op=ALU.mult), [K("rwt"), K("Pm")], [K("ct")])
            for src, full, hm in [("rt", "rtF", "rtFh"), ("nbt", "nbF", "nbFh"), ("ct", "cF", "cFh"), ("kt", None, "ktFh"), ("Pt", "PF", None)]:
                PE(lambda e, src=src: e.transpose(out=rB, in_=B_[src][:], identity=ident), [K(src), "msk"], [kb])
                if full is not None:
                    S(lambda e, full=full: e.copy(out=B_[full][:], in_=rB), [kb], [K(full)])
                if hm is not None:
                    for h, hs in [(0, hA), (1, hB)]:
                        S(lambda e, hm=hm, h=h, hs=hs: e.copy(out=B_[f"{hm}{h}"][hs, :], in_=rB[hs, :]), [kb], [K(f"{hm}{h}")])
            for ch in range(2):
                for h in range(2):
                    cs = slice(h * 64, (h + 1) * 64)
                    G(lambda e, ch=ch, h=h, cs=cs: e.tensor_scalar(out=B_[f"ktS{ch}{h}"][:, cs], in0=B_["kt"][:, cs], scalar1=selc[ch], scalar2=None, op0=ALU.mult), [K("kt"), "msk"], [K(f"ktS{ch}{h}")])
                    G(lambda e, ch=ch, h=h, cs=cs: e.tensor_scalar(out=B_[f"nbS{ch}{h}"][:, cs], in0=B_["nbt"][:, cs], scalar1=selc[ch], scalar2=None, op0=ALU.mult), [K("nbt"), "msk"], [K(f"nbS{ch}{h}")])
            def head(h):
                hr = hA if h == 0 else hB
                cFh, nbFh, ktFh, rtFh = (B_[f"{n}{h}"] for n in ["cFh", "nbFh", "ktFh", "rtFh"])
                kcF, knbF, kktF, krtF = (K(f"{n}{h}") for n in ["cFh", "nbFh", "ktFh", "rtFh"])
                for (nm, l_, kl, r__, kr, mk) in [("X", cFh, kcF, "nbF", K("nbF"), mS_N), ("XT", nbFh, knbF, "cF", K("cF"), mS_T), ("AckT", ktFh, kktF, "cF", K("cF"), mS_T),
                                                   ("ArkT", ktFh, kktF, "rtF", K("rtF"), mI_T), ("AnrbT", nbFh, knbF, "rtF", K("rtF"), mI_T)]:
                    PE(lambda e, l_=l_, r__=r__: e.matmul(rC, lhsT=l_[:], rhs=B_[r__][:], start=True, stop=True), [kl, kr], [kb])
                    V(lambda e, nm=nm, mk=mk: e.tensor_tensor(out=B_[nm][:], in0=rC, in1=mk, op=ALU.mult), [kb, "msk"], [K(nm)])
                G(lambda e: e.tensor_tensor(out=B_["TT"][:], in0=B_["XT"][:], in1=ident, op=ALU.add), [K("XT"), "msk"], [K("TT")])
                Pc, PTc, kP, kPT = B_["X"], B_["XT"], K("X"), K("XT")
                for lv in range(1, 6):
                    Pn, kPn = B_[f"P{lv % 2}"], K(f"P{lv % 2}")
                    PE(lambda e, Pc=Pc, PTc=PTc: e.matmul(rA, lhsT=PTc[:], rhs=Pc[:], start=True, stop=True), [kP, kPT], [kb])
                    S(lambda e, Pn=Pn: e.copy(out=Pn[:], in_=rA), [kb], [kPn])
                    if lv < 5:
                        PTn, kPTn = B_[f"PT{lv % 2}"], K(f"PT{lv % 2}")
                        PE(lambda e, Pc=Pc, PTc=PTc: e.matmul(rB, lhsT=Pc[:], rhs=PTc[:], start=True, stop=True), [kP, kPT], [kb])
                        S(lambda e, PTn=PTn: e.copy(out=PTn[:], in_=rB), [kb], [kPTn])
                    PE(lambda e, Pn=Pn: e.matmul(rC, lhsT=Pn[:], rhs=B_["TT"][:], start=True, stop=True), [kPn, K("TT")], [kb])
                    V(lambda e: e.tensor_tensor(out=B_["TT"][:], in0=B_["TT"][:], in1=rC, op=ALU.add), [K("TT"), kb], [K("TT")])
                    Pc, kP = Pn, kPn
                    if lv < 5:
                        PTc, kPT = PTn, kPTn
                Vh = vt[:, hr]
                H = B_["H"]
                for (ch, hs, lst) in order:
                    PE(lambda e: e.matmul(rD[:, 0:64], lhsT=cFh[:], rhs=H[:], start=True, stop=False), [kcF, K("H")], [kb])
                    PE(lambda e: e.matmul(rD[:, 0:64], lhsT=B_["AckT"][:], rhs=Vh, start=False, stop=True), [K("AckT"), K("vt")], [kb])
                    S(lambda e, hs=hs: e.copy(out=B_["Zs"][hs, :], in_=rD[hs, 0:64]), [kb], [K("Zs")])
                    PE(lambda e: e.matmul(rD[:, 64:128], lhsT=B_["TT"][:], rhs=B_["Zs"][:], start=True, stop=True), [K("TT"), K("Zs")], [kb])
                    S(lambda e, hs=hs: e.copy(out=B_["Ms"][hs, :], in_=rD[hs, 64:128]), [kb], [K("Ms")])
                    PE(lambda e: e.matmul(rA[:, 0:64], lhsT=rtFh[:], rhs=H[:], start=True, stop=True), [krtF, K("H")], [kb])
                    S(lambda e, hs=hs, hr=hr: e.copy(out=B_["Yt"][hs, hr], in_=rA[hs, 0:64]), [kb], [K("Yt")])
                    PE(lambda e, ch=ch, h=h: e.matmul(rB[:, 0:64], lhsT=B_[f"ktS{ch}{h}"][:], rhs=Vh, start=True, stop=False), [K(f"ktS{ch}{h}"), K("vt")], [kb])
                    PE(lambda e, ch=ch, h=h: e.matmul(rB[:, 0:64], lhsT=B_[f"nbS{ch}{h}"][:], rhs=B_["Ms"][:], start=False, stop=True), [K(f"nbS{ch}{h}"), K("Ms")], [kb])
                    V(lambda e, hr=hr, lst=lst: e.tensor_scalar(out=H[hr, :], in0=H[hr, :], scalar1=B_["PF"][hr, lst:lst + 1], scalar2=None, op0=ALU.mult), [K("H"), K("PF")], [K("H")])
                    V(lambda e, hr=hr, lst=lst: e.scalar_tensor_tensor(out=H[hr, :], in0=rB[hr, 0:64], scalar=B_["PF"][hr, lst:lst + 1], in1=H[hr, :], op0=ALU.mult, op1=ALU.add), [K("H"), K("PF"), kb], [K("H")])
                PE(lambda e: e.matmul(rC[:, 0:64], lhsT=B_["ArkT"][:], rhs=Vh, start=True, stop=False), [K("ArkT"), K("vt")], [kb])
                PE(lambda e: e.matmul(rC[:, 0:64], lhsT=B_["AnrbT"][:], rhs=B_["Ms"][:], start=False, stop=True), [K("AnrbT"), K("Ms")], [kb])
                V(lambda e, hr=hr: e.tensor_tensor(out=B_["Yt"][:, hr], in0=B_["Yt"][:, hr], in1=rC[:, 0:64], op=ALU.add), [K("Yt"), kb], [K("Yt")])
            for h in range(2):
                head(h)
            p.dma("gpsimd", ry_s[d, t0:t0 + 128, :], B_["Yt"][:], reads=[K("Yt")], writes=["ry_s"])

        def ssd_chunk(d, c):
            t0 = c * 128
            Mk = UT if d == 0 else LT
            last = 127 if d == 0 else 0
            p.dma("gpsimd", sxt[:], ssd_s[t0:t0 + 128, :], reads=["ssd_s"], writes=["sxt"])
            if d == 1:
                p.dma("gpsimd", syf[:], sy_s[t0:t0 + 128, :], reads=["sy_s"], writes=["syf"])
                p.dma("gpsimd", zt[:], cols_s[2 + t0:2 + t0 + 128, 1280:1536], reads=["cols_s"], writes=["zts"])
            sx = sxt[:, 0:256]; sB = sxt[:, 256:384]; sC = sxt[:, 384:512]
            dt_d = sxt[:, 512 + d * 4:516 + d * 4]; a_d = sxt[:, 520 + d * 4:524 + d * 4]
            PE(lambda e: e.transpose(out=pA[:, 0:128], in_=sB, identity=ident), ["sxt", "msk"], ["ps0"])
            PE(lambda e: e.transpose(out=pA[:, 128:256], in_=sC, identity=ident), ["sxt", "msk"], ["ps0"])
            S(lambda e: e.copy(out=BCF[:], in_=pA[:, 0:256]), ["ps0"], ["BCF"])
            if SSDSTOP == 1: return
            PE(lambda e: e.matmul(pSc[:, :128], lhsT=BCF[:, 0:128], rhs=BCF[:, 128:256], start=True, stop=True), ["BCF"], ["ps0"])
            S(lambda e: e.copy(out=scT[:], in_=pSc[:, :128]), ["ps0"], ["scT"])
            PE(lambda e: e.matmul(pAc[:, :4], lhsT=Mk, rhs=a_d, start=True, stop=True), ["sxt", "msk"], ["ps0"])
            S(lambda e: e.copy(out=acol[:], in_=pAc[:, :4]), ["ps0"], ["acol"])
            S(lambda e: e.mul(out=nacol[:], in_=pAc[:, :4], mul=-1.0), ["ps0"], ["nacol"])
            if SSDSTOP == 2: return
            for h in range(4):
                (V if os.environ.get('A_V1') else G)(lambda e, h=h: e.tensor_scalar(out=Rm[:, h, :], in0=Mk, scalar1=a_d[:, h:h + 1], scalar2=None, op0=ALU.mult), ["sxt", "msk"], ["Rm"])
            PE(lambda e: e.matmul(pRow[:, :512], lhsT=ones, rhs=Rm[:].rearrange("p h n -> p (h n)"), start=True, stop=True), ["Rm", "msk"], ["ps1"])
            S(lambda e: e.activation(out=E[:].rearrange("p h n -> p (h n)"), in_=pRow[:, :512], func=AF.Exp), ["ps1"], ["E"])
            for h in range(4):
                S(lambda e, h=h: e.activation(out=dend[:, h:h + 1], in_=pRow[:, h * 128 + last:h * 128 + last + 1], func=AF.Exp, bias=nacol[:, h:h + 1], scale=1.0), ["ps1", "nacol"], ["dend"])
            if SSDSTOP == 3: return
            for h in range(4):
                hs = slice(h * 64, (h + 1) * 64)
                S(lambda e, h=h: e.activation(out=Dm[:], in_=pRow[:, h * 128:(h + 1) * 128], func=AF.Identity, bias=nacol[:, h:h + 1], scale=1.0), ["ps1", "nacol"], ["Dm"])
                G(lambda e: e.tensor_scalar(out=Dm[:], in0=Dm[:], scalar1=0.0, scalar2=None, op0=ALU.min), ["Dm"], ["Dm"])
                S(lambda e: e.activation(out=Dm[:], in_=Dm[:], func=AF.Exp), ["Dm"], ["Dm"])
                G(lambda e: e.tensor_tensor(out=Dm[:], in0=Dm[:], in1=Mk, op=ALU.mult), ["Dm", "msk"], ["Dm"])
                G(lambda e, h=h: e.tensor_tensor(out=M[:, h, :], in0=Dm[:], in1=scT[:], op=ALU.mult), ["Dm", "scT"], ["M"])
                G(lambda e, h=h: e.tensor_tensor(out=CdF[:, h, :], in0=E[:, h, :], in1=BCF[:, 128:256], op=ALU.mult), ["E", "BCF"], ["CdF"])
                G(lambda e, h=h, hs=hs: e.tensor_scalar(out=xdt[:, hs], in0=sx[:, hs], scalar1=dt_d[:, h:h + 1], scalar2=None, op0=ALU.mult), ["sxt"], ["xdt"])
                G(lambda e, h=h, hs=hs: e.tensor_scalar(out=xend[:, hs], in0=xdt[:, hs], scalar1=dend[:, h:h + 1], scalar2=None, op0=ALU.mult), ["xdt", "dend"], ["xend"])
            if SSDSTOP == 4: return
            for h in range(4):
                hs = slice(h * 64, (h + 1) * 64)
                PE(lambda e, h=h, hs=hs: e.matmul(pY[:, hs], lhsT=M[:, h, :], rhs=xdt[:, hs], start=True, stop=False), ["M", "xdt"], ["ps2"])
                PE(lambda e, h=h, hs=hs: e.matmul(pY[:, hs], lhsT=CdF[:, h, :], rhs=H[:, hs], start=False, stop=True), ["CdF", "H"], ["ps2"])
            PE(lambda e: e.matmul(pSt[:, :256], lhsT=sB, rhs=xend[:], start=True, stop=True), ["sxt", "xend"], ["ps2"])
            if SSDSTOP == 5: return
            for h in range(4):
                hs = slice(h * 64, (h + 1) * 64)
                V(lambda e, h=h, hs=hs: e.scalar_tensor_tensor(out=H[:, hs], in0=H[:, hs], scalar=E[:, h, last:last + 1], in1=pSt[:, hs], op0=ALU.mult, op1=ALU.add), ["H", "E", "ps2"], ["H"])
            if SSDSTOP == 6: return
            if d == 0:
                S(lambda e: e.copy(out=yt[:], in_=pY[:, :256]), ["ps2"], ["yt"])
                p.dma("gpsimd", sy_s[t0:t0 + 128, :], yt[:], reads=["yt"], writes=["sy_s"])
            else:
                V(lambda e: e.tensor_tensor(out=yt[:], in0=pY[:, :256], in1=syf[:], op=ALU.add), ["ps2", "syf"], ["yt"])
                G(lambda e: e.tensor_tensor(out=xd[:], in0=sx, in1=rv("dsk"), op=ALU.mult), ["sxt", "rvs"], ["xd"])
                G(lambda e: e.tensor_tensor(out=yt[:], in0=yt[:], in1=xd[:], op=ALU.add), ["yt", "xd"], ["yt"])
                S(lambda e: e.activation(out=szs[:], in_=zt[:], func=AF.Silu), ["zts"], ["szs"])
                G(lambda e: e.tensor_tensor(out=yt[:], in0=yt[:], in1=szs[:], op=ALU.mult), ["yt", "szs"], ["yt"])
                p.dma("gpsimd", yc_o[t0:t0 + 128, :], yt[:], reads=["yt"], writes=["yc"])

        ssd_list = [(0, c) for c in range(NTL)] + [(1, c) for c in range(NTL - 1, -1, -1)]
        V(lambda e: e.memset(H[:], 0.0), [], ["H"])
        def ssd_pair(it):
            for si in (2 * it, 2 * it + 1):
                dd, cc = ssd_list[si]
                if dd == 1 and cc == NTL - 1:
                    V(lambda e: e.memset(H[:], 0.0), [], ["H"])
                ssd_chunk(dd, cc)
        lists = [[] for _ in range(5)]
        for it in range(NTL):
            if not os.environ.get("A_NOGDN"):
                lists[0] += p.capture(gdn_chunk, 0, it); lists[1] += p.capture(gdn_chunk, 1, NTL - 1 - it)
            if not os.environ.get("A_NORWKV"):
                lists[2] += p.capture(rwkv_block, 0, it); lists[3] += p.capture(rwkv_block, 1, NTL - 1 - it)
            if not os.environ.get("A_NOSSD"):
                lists[4] += p.capture(ssd_pair, it)
        p.emit_balanced(lists)
    p.barrier()


def phase4(p, T, L):
    nc = p.nc
    V = L["V"]; G = L["G"]; S = L["S"]; PE = L["PE"]; PS = L["PS"]; ident = L["ident"]; rv = L["rv"]
    gy_s, ry_s, cols_s, rpost_s, ya_o, yb_o = (L[k] for k in ["gy_s", "ry_s", "cols_s", "rpost_s", "ya_o", "yb_o"])
    NTL = T // 128
    with contextlib.ExitStack() as sc:
        sb = lambda name, shape, dt=F32: sc.enter_context(nc.sbuf_tensor("s_" + name, list(shape), dt))
        g0 = sb("g0", [128, 128]); g1 = sb("g1", [128, 128]); o = sb("o", [128, 128]); jk = sb("jk", [128, 128]); ss = sb("ss", [128, 1])
        zt = sb("zt4", [128, 128]); ya = sb("ya_t", [128, 128])
        r0 = sb("r0", [128, 128]); r1 = sb("r1", [128, 128]); y = sb("y4", [128, 128]); st = sb("st4", [128, 2]); sq = sb("sq4", [128, 128]); vr = sb("vr4", [128, 2])
        rp = sb("rp4", [128, 258]); yb = sb("yb_t", [128, 128])
        pT, pU = PS[0], PS[1]
        for tt in range(NTL):
            t0 = tt * 128
            p.dma("sync", g0[:], gy_s[0, t0:t0 + 128, :], reads=["gy_s"], writes=["g0"])
            p.dma("sync", g1[:], gy_s[1, t0:t0 + 128, :], reads=["gy_s"], writes=["g1"])
            p.dma("scalar", zt[:], cols_s[2 + t0:2 + t0 + 128, 384:512], reads=["cols_s"], writes=["zt4"])
            p.dma("sync", r0[:], ry_s[0, t0:t0 + 128, :], reads=["ry_s"], writes=["r0"])
            p.dma("sync", r1[:], ry_s[1, t0:t0 + 128, :], reads=["ry_s"], writes=["r1"])
            p.dma("scalar", rp[:], rpost_s[t0:t0 + 128, :], reads=["rpost_s"], writes=["rp4"])
            V(lambda e: e.tensor_tensor(out=o[:], in0=g0[:], in1=g1[:], op=ALU.add), ["g0", "g1"], ["o"])
            V(lambda e: e.scalar_tensor_tensor(out=jk[:], in0=o[:], scalar=1.0, in1=o[:], op0=ALU.mult, op1=ALU.mult, accum_out=ss[:, 0:1]), ["o"], ["jk", "ss"])
            V(lambda e: e.tensor_scalar(out=ss[:], in0=ss[:], scalar1=1.0 / 128, scalar2=1e-6, op0=ALU.mult, op1=ALU.add), ["ss"], ["ss"])
            S(lambda e: e.activation(out=ss[:], in_=ss[:], func=AF.Sqrt), ["ss"], ["ss"])
            V(lambda e: e.reciprocal(out=ss[:], in_=ss[:]), ["ss"], ["ss"])
            V(lambda e: e.scalar_tensor_tensor(out=o[:], in0=o[:], scalar=ss[:, 0:1], in1=rv("gnorm"), op0=ALU.mult, op1=ALU.mult), ["o", "ss", "rvs"], ["o"])
            S(lambda e: e.activation(out=zt[:], in_=zt[:], func=AF.Silu), ["zt4"], ["zt4"])
            V(lambda e: e.tensor_tensor(out=ya[:], in0=o[:], in1=zt[:], op=ALU.mult), ["o", "zt4"], ["ya_t"])
            p.dma("gpsimd", ya_o[t0:t0 + 128, :], ya[:], reads=["ya_t"], writes=["ya"])
            V(lambda e: e.tensor_tensor(out=y[:], in0=r0[:], in1=r1[:], op=ALU.add), ["r0", "r1"], ["y4"])
            V(lambda e: e.tensor_reduce(out=st[:], in_=y[:].rearrange("p (h n) -> p h n", h=2), axis=AX.X, op=ALU.add), ["y4"], ["st4"])
            V(lambda e: e.tensor_scalar(out=st[:], in0=st[:], scalar1=1.0 / 64, scalar2=None, op0=ALU.mult), ["st4"], ["st4"])
            for h in range(2):
                hs = slice(h * 64, (h + 1) * 64)
                V(lambda e, h=h, hs=hs: e.tensor_scalar(out=y[:, hs], in0=y[:, hs], scalar1=st[:, h:h + 1], scalar2=None, op0=ALU.subtract), ["y4", "st4"], ["y4"])
            S(lambda e: e.activation(out=sq[:], in_=y[:], func=AF.Square), ["y4"], ["sq4"])
            V(lambda e: e.tensor_reduce(out=vr[:], in_=sq[:].rearrange("p (h n) -> p h n", h=2), axis=AX.X, op=ALU.add), ["sq4"], ["vr4"])
            V(lambda e: e.tensor_scalar(out=vr[:], in0=vr[:], scalar1=1.0 / 64, scalar2=64e-5, op0=ALU.mult, op1=ALU.add), ["vr4"], ["vr4"])
            S(lambda e: e.activation(out=vr[:], in_=vr[:], func=AF.Sqrt), ["vr4"], ["vr4"])
            V(lambda e: e.reciprocal(out=vr[:], in_=vr[:]), ["vr4"], ["vr4"])
            for h in range(2):
                hs = slice(h * 64, (h + 1) * 64)
                V(lambda e, h=h, hs=hs: e.tensor_scalar(out=y[:, hs], in0=y[:, hs], scalar1=vr[:, h:h + 1], scalar2=None, op0=ALU.mult), ["y4", "vr4"], ["y4"])
            V(lambda e: e.tensor_tensor(out=y[:], in0=y[:], in1=rv("lng"), op=ALU.mult), ["y4", "rvs"], ["y4"])
            V(lambda e: e.tensor_tensor(out=y[:], in0=y[:], in1=rv("lnb"), op=ALU.add), ["y4", "rvs"], ["y4"])
            for h in range(2):
                hs = slice(h * 64, (h + 1) * 64)
                V(lambda e, h=h, hs=hs: e.scalar_tensor_tensor(out=y[:, hs], in0=rp[:, hs], scalar=rp[:, 256 + h:257 + h], in1=y[:, hs], op0=ALU.mult, op1=ALU.add), ["y4", "rp4"], ["y4"])
            V(lambda e: e.tensor_tensor(out=yb[:], in0=y[:], in1=rp[:, 128:256], op=ALU.mult), ["y4", "rp4"], ["yb_t"])
            p.dma("gpsimd", yb_o[t0:t0 + 128, :], yb[:], reads=["yb_t"], writes=["yb"])


ALPHA = 4 ** 0.25
NTOK = 2048
T1 = 256
T2 = 512
NE = 32


def ln_fm(p, h, hk, nch, NT, gcol, bcol, ones_mean, sq, pS, pQ, tmp, kS, kQ, eps=1e-5):
    for oc in range(nch):
        p.op("tensor", lambda e, oc=oc: e.matmul(pS[:, :NT], lhsT=ones_mean[:], rhs=h[:, oc, :], start=(oc == 0), stop=(oc == nch - 1)),
             reads=[hk, "ones_mean"], writes=[kS])
    sqs = (lambda s_: sq[s_][:]) if isinstance(sq, (list, tuple)) else (lambda s_: sq[:, s_, :])
    for oc in range(nch):
        s = oc % 2
        p.op("scalar", lambda e, oc=oc, s=s: e.activation(out=sqs(s), in_=h[:, oc, :], func=AF.Square), reads=[hk], writes=[f"sq{s}"])
        p.op("tensor", lambda e, oc=oc, s=s: e.matmul(pQ[:, :NT], lhsT=ones_mean[:], rhs=sqs(s), start=(oc == 0), stop=(oc == nch - 1)),
             reads=[f"sq{s}", "ones_mean"], writes=[kQ])
    mean, rstd, t = tmp["mean"], tmp["rstd"], tmp["t"]
    p.op("scalar", lambda e: e.copy(out=mean[:], in_=pS[:, :NT]), reads=[kS], writes=["ln_mean"])
    p.op("vector", lambda e: e.tensor_tensor(out=t[:], in0=mean[:], in1=mean[:], op=ALU.mult), reads=["ln_mean"], writes=["ln_t"])
    p.op("vector", lambda e: e.tensor_tensor(out=t[:], in0=pQ[:, :NT], in1=t[:], op=ALU.subtract), reads=[kQ, "ln_t"], writes=["ln_t"])
    p.op("vector", lambda e: e.tensor_scalar(out=t[:], in0=t[:], scalar1=eps, scalar2=None, op0=ALU.add), reads=["ln_t"], writes=["ln_t"])
    p.op("scalar", lambda e: e.activation(out=t[:], in_=t[:], func=AF.Sqrt), reads=["ln_t"], writes=["ln_t"])
    p.op("vector", lambda e: e.reciprocal(out=rstd[:], in_=t[:]), reads=["ln_t"], writes=["ln_rstd"])
    for oc in range(nch):
        p.op("vector", lambda e, oc=oc: e.tensor_tensor(out=h[:, oc, :], in0=h[:, oc, :], in1=mean[:], op=ALU.subtract), reads=[hk, "ln_mean"], writes=[hk])
        p.op("vector", lambda e, oc=oc: e.tensor_tensor(out=h[:, oc, :], in0=h[:, oc, :], in1=rstd[:], op=ALU.mult), reads=[hk, "ln_rstd"], writes=[hk])
        p.op("scalar", lambda e, oc=oc: e.activation(out=h[:, oc, :], in_=h[:, oc, :], func=AF.Identity, scale=gcol(oc), bias=bcol(oc)),
             reads=[hk, "vec"], writes=[hk])


def build_B(ntok=NTOK, ne=NE, dump=False):
    p = Prog(); nc = p.nc
    D = 1024
    xT = p.dram("xT", [D, ntok]); yT = p.dram("yT", [2048, ntok]); pT = p.dram("pT", [256, ntok])
    wg = p.dram("wg", [D, 3072]); wb = p.dram("wb", [2048, D]); wo = p.dram("wo", [D, D]); wplg = p.dram("wplg", [D, D])
    wpl = p.dram("wpl", [256, D]); wr = p.dram("wr", [D, 32]); br = p.dram("br", [1, 32])
    wgu = p.dram("wgu", [32, D, 2048]); wd = p.dram("wd", [32, D, D])
    bgu = p.dram("bgu", [128, 32, 16]); bd = p.dram("bd", [32, D]); vec = p.dram("vec", [128, 5, 8]); ident_d = p.dram("ident", [128, 128])
    outT = p.dram("outT", [D, ntok], kind="ExternalOutput")
    x1bf_s = p.dram("x1bf_s", [128, 8, ntok], BF16, kind="Internal")
    acc_s = p.dram("acc_s", [128, 8, ntok], F32, kind="Internal")
    if dump:
        x1_d = p.dram("x1_d", [D, ntok], kind="ExternalOutput")
        gate_d = p.dram("gate_d", [32, ntok], kind="ExternalOutput")

    ident = p.sb("ident", [128, 128]); ones_mean = p.sb("ones_mean", [128, 128]); vecs = p.sb("vecs", [128, 5, 8])
    gateT = p.sb("gateT", [32, ntok]); ones_row = p.sb("ones_row", [1, 128]); brow = p.sb("brow", [1, 32])
    PS = [p.ps(f"ps{i}", [128, 512]) for i in range(8)]
    p.dma("sync", ident[:], ident_d[:, :], writes=["ident"])
    p.dma("sync", vecs[:], vec[:, :, :], writes=["vec"])
    p.dma("sync", brow[:], br[:, :], writes=["brow"])
    p.op("vector", lambda e: e.memset(ones_mean[:], 1.0 / 1024), writes=["ones_mean"])
    p.op("vector", lambda e: e.memset(ones_row[:], 1.0), writes=["ones_row"])

    with contextlib.ExitStack() as sc:
        def sb(name, shape, dt=F32):
            return sc.enter_context(nc.sbuf_tensor("s_" + name, list(shape), dt))
        wg_bf = sb("wg_bf", [128, 8, 3072], BF16); wb_bf = sb("wb_bf", [128, 16, 1024], BF16)
        wo_bf = sb("wo_bf", [128, 8, 1024], BF16); wplg_bf = sb("wplg_bf", [128, 8, 1024], BF16); wpl_bf = sb("wpl_bf", [128, 2, 1024], BF16)
        wr_sb = sb("wr_sb", [128, 8, 32]); ones512 = sb("ones512", [128, 128])
        for kc in range(8):
            p.dma("gpsimd", wg_bf[:, kc, :], wg[kc * 128:(kc + 1) * 128, :], writes=["wg_bf"])
            p.dma("gpsimd", wo_bf[:, kc, :], wo[kc * 128:(kc + 1) * 128, :], writes=["wo_bf"])
            p.dma("gpsimd", wplg_bf[:, kc, :], wplg[kc * 128:(kc + 1) * 128, :], writes=["wplg_bf"])
            p.dma("sync", wr_sb[:, kc, :], wr[kc * 128:(kc + 1) * 128, :], writes=["wr_sb"])
        for kc in range(16):
            p.dma("gpsimd", wb_bf[:, kc, :], wb[kc * 128:(kc + 1) * 128, :], writes=["wb_bf"])
        for kc in range(2):
            p.dma("gpsimd", wpl_bf[:, kc, :], wpl[kc * 128:(kc + 1) * 128, :], writes=["wpl_bf"])
        p.op("vector", lambda e: e.memset(ones512[:], 1.0 / 512), writes=["ones512"])
        xf = sb("xf", [128, 8, T1]); x_bf = sb("x_bf", [128, 8, T1], BF16); uf = sb("uf", [128, 8, T1])
        y_bf = sb("y_bf", [128, 16, T1], BF16); m_bf = sb("m_bf", [128, 8, T1], BF16); sq = sb("sq", [128, 2, T1])
        x1b = sb("x1b", [128, 8, T1], BF16); accb = sb("accb", [128, 8, T1])
        pf = sb("pf", [128, 2, T1], BF16)
        tm = {k: sb("tm_" + k, [128, T1]) for k in ["mean", "rstd", "t", "g", "mf", "t2", "rs"]}
        lg = sb("lg", [128, 32]); top8 = sb("top8", [128, 8]); nmx = sb("nmx", [128, 1]); msk = sb("msk", [128, 32])
        ex = sb("ex", [128, 32]); ssum = sb("ssum", [128, 1]); gt = sb("gt", [128, 32])
        pG, pB, pH, pS, pQ, pR, pT_, pP = PS
        xTv = xT.rearrange("(kc p) n -> p kc n", p=128); yTv = yT.rearrange("(kc p) n -> p kc n", p=128)
        pTv = pT.rearrange("(kc p) n -> p kc n", p=128)
        for t in range(ntok // T1):
            o = t * T1
            p.dma("sync", xf[:], xTv[:, :, o:o + T1], writes=["xf"])
            p.dma("gpsimd", x_bf[:], xTv[:, :, o:o + T1], writes=["x_bf"])
            p.dma("gpsimd", y_bf[:, 0:8, :], yTv[:, 0:8, o:o + T1], writes=["y_bf_a"])
            p.dma("sync", uf[:], yTv[:, 8:16, o:o + T1], writes=["uf"])
            p.dma("gpsimd", pf[:], pTv[:, :, o:o + T1], writes=["pf"])
            for g in range(2):
                for c in range(4):
                    cc = g * 4 + c; s = cc % 2
                    p.op("scalar", lambda e, cc=cc, s=s: e.activation(out=sq[:, s, :], in_=uf[:, cc, :], func=AF.Square), reads=["uf"], writes=[f"sq{s}"])
                    p.op("tensor", lambda e, c=c, s=s: e.matmul(pS[:, :T1], lhsT=ones512[:], rhs=sq[:, s, :], start=(c == 0), stop=(c == 3)),
                         reads=[f"sq{s}", "ones512"], writes=["pS"])
                rs = tm["rs"]
                p.op("vector", lambda e: e.tensor_scalar(out=rs[:], in0=pS[:, :T1], scalar1=1e-5, scalar2=None, op0=ALU.add), reads=["pS"], writes=["rs"])
                p.op("scalar", lambda e: e.activation(out=rs[:], in_=rs[:], func=AF.Sqrt), reads=["rs"], writes=["rs"])
                p.op("vector", lambda e: e.reciprocal(out=rs[:], in_=rs[:]), reads=["rs"], writes=["rs"])
                for c in range(4):
                    cc = g * 4 + c
                    p.op("vector", lambda e, cc=cc: e.scalar_tensor_tensor(out=y_bf[:, 8 + cc, :], in0=uf[:, cc, :], scalar=vecs[:, 4, cc:cc + 1], in1=rs[:], op0=ALU.mult, op1=ALU.mult),
                         reads=["uf", "rs", "vec"], writes=["y_bf_u"])
            brk = [(0, 4), (4, 8), (8, 16)]
            for oc in range(8):
                for b in range(3):
                    c0 = b * 1024 + oc * 128
                    for kc in range(8):
                        p.op("tensor", lambda e, kc=kc, c0=c0: e.matmul(pG[:, :T1], lhsT=wg_bf[:, kc, c0:c0 + 128], rhs=x_bf[:, kc, :], start=(kc == 0), stop=(kc == 7)),
                             reads=["wg_bf", "x_bf"], writes=["pG"])
                    p.op("scalar", lambda e: e.activation(out=tm["g"][:], in_=pG[:, :T1], func=AF.Sigmoid), reads=["pG"], writes=["tm_g"])
                    k0, k1 = brk[b]
                    for kc in range(k0, k1):
                        p.op("tensor", lambda e, kc=kc, k0=k0, k1=k1: e.matmul(pB[:, :T1], lhsT=wb_bf[:, kc, oc * 128:(oc + 1) * 128], rhs=y_bf[:, kc, :], start=(kc == k0), stop=(kc == k1 - 1)),
                             reads=["wb_bf", "y_bf_a", "y_bf_u"], writes=["pB"])
                    if b == 0:
                        p.op("vector", lambda e: e.tensor_tensor(out=tm["mf"][:], in0=tm["g"][:], in1=pB[:, :T1], op=ALU.mult), reads=["tm_g", "pB"], writes=["tm_mf"])
                    else:
                        p.op("vector", lambda e: e.tensor_tensor(out=tm["t2"][:], in0=tm["g"][:], in1=pB[:, :T1], op=ALU.mult), reads=["tm_g", "pB"], writes=["tm_t2"])
                        if b == 1:
                            p.op("vector", lambda e: e.tensor_tensor(out=tm["mf"][:], in0=tm["mf"][:], in1=tm["t2"][:], op=ALU.add), reads=["tm_mf", "tm_t2"], writes=["tm_mf"])
                        else:
                            p.op("vector", lambda e, oc=oc: e.tensor_tensor(out=m_bf[:, oc, :], in0=tm["mf"][:], in1=tm["t2"][:], op=ALU.add), reads=["tm_mf", "tm_t2"], writes=["m_bf"])
            for oc in range(8):
                for kc in range(8):
                    p.op("tensor", lambda e, kc=kc, oc=oc: e.matmul(pH[:, :T1], lhsT=wo_bf[:, kc, oc * 128:(oc + 1) * 128], rhs=m_bf[:, kc, :], start=(kc == 0), stop=(kc == 7)),
                         reads=["wo_bf", "m_bf"], writes=["pH"])
                p.op("vector", lambda e, oc=oc: e.scalar_tensor_tensor(out=xf[:, oc, :], in0=xf[:, oc, :], scalar=ALPHA, in1=pH[:, :T1], op0=ALU.mult, op1=ALU.add),
                     reads=["xf", "pH"], writes=["xf"])
            ln_fm(p, xf, "xf", 8, T1, lambda oc: vecs[:, 0, oc:oc + 1], lambda oc: vecs[:, 1, oc:oc + 1], ones_mean, sq, pS, pQ, tm, "pS", "pQ")
            p.op("scalar", lambda e: e.copy(out=x1b[:], in_=xf[:]), reads=["xf"], writes=["x1b"])
            p.dma("sync", x1bf_s[:, :, o:o + T1], x1b[:], reads=["x1b"], writes=["x1bf_s"])
            if dump:
                p.dma("sync", x1_d.rearrange("(kc p) n -> p kc n", p=128)[:, :, o:o + T1], xf[:], reads=["xf"], writes=["x1_d"])
            for s in range(T1 // 128):
                for kc in range(8):
                    p.op("tensor", lambda e, kc=kc, s=s: e.matmul(pR[:, :32], lhsT=xf[:, kc, s * 128:(s + 1) * 128], rhs=wr_sb[:, kc, :], start=(kc == 0), stop=False),
                         reads=["xf", "wr_sb"], writes=["pR"])
                p.op("tensor", lambda e: e.matmul(pR[:, :32], lhsT=ones_row[:, :], rhs=brow[:, :], start=False, stop=True), reads=["ones_row", "brow"], writes=["pR"])
                p.op("vector", lambda e: e.tensor_copy(out=lg[:], in_=pR[:, :32]), reads=["pR"], writes=["lg"])
                p.op("vector", lambda e: e.max(out=top8[:], in_=lg[:]), reads=["lg"], writes=["top8"])
                p.op("vector", lambda e: e.tensor_scalar(out=nmx[:], in0=top8[:, 0:1], scalar1=-1.0, scalar2=None, op0=ALU.mult), reads=["top8"], writes=["nmx"])
                p.op("vector", lambda e: e.tensor_scalar(out=msk[:], in0=lg[:], scalar1=top8[:, 3:4], scalar2=None, op0=ALU.is_ge), reads=["lg", "top8"], writes=["msk"])
                p.op("scalar", lambda e: e.activation(out=ex[:], in_=lg[:], func=AF.Exp, bias=nmx[:, 0:1], scale=1.0), reads=["lg", "nmx"], writes=["ex"])
                p.op("vector", lambda e: e.scalar_tensor_tensor(out=ex[:], in0=ex[:], scalar=1.0, in1=msk[:], op0=ALU.mult, op1=ALU.mult, accum_out=ssum[:, 0:1]),
                     reads=["ex", "msk"], writes=["ex", "ssum"])
                p.op("vector", lambda e: e.reciprocal(out=ssum[:], in_=ssum[:]), reads=["ssum"], writes=["ssum"])
                p.op("vector", lambda e: e.tensor_scalar(out=gt[:], in0=ex[:], scalar1=ssum[:, 0:1], scalar2=None, op0=ALU.mult), reads=["ex", "ssum"], writes=["gt"])
                p.op("tensor", lambda e: e.transpose(out=pT_[:32, :128], in_=gt[:], identity=ident[:]), reads=["gt", "ident"], writes=["pT"])
                oo = o + s * 128
                p.op("scalar", lambda e, oo=oo: e.copy(out=gateT[:, oo:oo + 128], in_=pT_[:32, :128]), reads=["pT"], writes=["gateT"])
            for oc in range(8):
                for kc in range(2):
                    p.op("tensor", lambda e, kc=kc, oc=oc: e.matmul(pP[:, :T1], lhsT=wpl_bf[:, kc, oc * 128:(oc + 1) * 128], rhs=pf[:, kc, :], start=(kc == 0), stop=(kc == 1)),
                         reads=["wpl_bf", "pf"], writes=["pP"])
                for kc in range(8):
                    p.op("tensor", lambda e, kc=kc, oc=oc: e.matmul(pG[:, :T1], lhsT=wplg_bf[:, kc, oc * 128:(oc + 1) * 128], rhs=x1b[:, kc, :], start=(kc == 0), stop=(kc == 7)),
                         reads=["wplg_bf", "x1b"], writes=["pG"])
                p.op("scalar", lambda e: e.activation(out=tm["g"][:], in_=pG[:, :T1], func=AF.Sigmoid), reads=["pG"], writes=["tm_g"])
                p.op("vector", lambda e: e.tensor_tensor(out=tm["t2"][:], in0=tm["g"][:], in1=pP[:, :T1], op=ALU.mult), reads=["tm_g", "pP"], writes=["tm_t2"])
                p.op("vector", lambda e, oc=oc: e.scalar_tensor_tensor(out=accb[:, oc, :], in0=xf[:, oc, :], scalar=ALPHA, in1=tm["t2"][:], op0=ALU.mult, op1=ALU.add),
                     reads=["xf", "tm_t2"], writes=["accb"])
            p.dma("sync", acc_s[:, :, o:o + T1], accb[:], reads=["accb"], writes=["acc_s"])
    if dump:
        p.dma("sync", gate_d[:, :], gateT[:], reads=["gateT"], writes=["gate_d"])
    p.barrier()

    H = ntok // 2
    with contextlib.ExitStack() as sc:
        def sb(name, shape, dt=F32):
            return sc.enter_context(nc.sbuf_tensor("s_" + name, list(shape), dt))
        x1h = sb("x1h", [128, 8, H], BF16); acc = sb("acc", [128, 8, H])
        wgu_b = [sb(f"wgu_b{i}", [128, 8, 2048], BF16) for i in range(2)]
        wd_b = [sb(f"wd_b{i}", [128, 8, 1024], BF16) for i in range(2)]
        bgu_sb = sb("bgu_sb", [128, 32, 16]); bd_sb = sb("bd_sb", [32, 1024]); ones32 = sb("ones32", [32, 128])
        act = [sb(f"act{c}", [128, 8, T2], BF16) for c in range(2)]; gbc = [sb(f"gbc{c}", [128, T2]) for c in range(2)]; gm = [sb(f"gm{c}", [32, T2]) for c in range(2)]
        glu = [sb(f"glu{c}", [128, T2]) for c in range(2)]; up1 = [sb(f"up1{c}", [128, T2]) for c in range(2)]; sg = [sb(f"sg{c}", [128, T2]) for c in range(2)]
        t1 = [sb(f"t1{c}", [128, T2]) for c in range(2)]; t2 = [sb(f"t2{c}", [128, T2]) for c in range(2)]
        sq = [t1[0], t2[0]]; tm = {"mean": glu[0], "rstd": up1[0], "t": sg[0]}
        print('moe sbuf remaining', nc.sbuf_bytes_remaining, flush=True)
        p.dma("sync", bgu_sb[:], bgu[:, :, :], writes=["bgu_sb"])
        p.dma("sync", bd_sb[:], bd[:, :], writes=["bd_sb"])
        p.op("vector", lambda e: e.memset(ones32[:], 1.0), writes=["ones32"])
        pBC = PS[6]; pS = PS[7]; pQ = PS[6]
        outv = outT.rearrange("(kc p) n -> p kc n", p=128)

        def moe_tile(ex_, tt, ho, wgs, wds, wb_i):
            c = tt
            pGl, pUp, pD = PS[3 * c], PS[3 * c + 1], PS[3 * c + 2]
            kGl, kUp, kD = f"pGl{c}", f"pUp{c}", f"pD{c}"
            to = tt * T2
            p.op("vector", lambda e: e.tensor_scalar(out=gm[c][:], in0=gateT[:, ho + to:ho + to + T2], scalar1=ident[0:32, ex_:ex_ + 1], scalar2=None, op0=ALU.mult),
                 reads=["gateT", "ident"], writes=[f"gm{c}"])
            pBCc, kBC = (PS[6], "pBC") if c == 0 else (PS[7], "pS7")
            p.op("tensor", lambda e: e.matmul(pBCc[:, :T2], lhsT=ones32[:], rhs=gm[c][:], start=True, stop=True), reads=["ones32", f"gm{c}"], writes=[kBC])
            p.op("scalar", lambda e: e.copy(out=gbc[c][:], in_=pBCc[:, :T2]), reads=[kBC], writes=[f"gbc{c}"])
            for oc in range(8):
                for kc in range(8):
                    p.op("tensor", lambda e, kc=kc, oc=oc: e.matmul(pGl[:, :T2], lhsT=wgs[:, kc, oc * 128:(oc + 1) * 128], rhs=x1h[:, kc, to:to + T2], start=(kc == 0), stop=(kc == 7)),
                         reads=[f"wgu{wb_i}", "x1h"], writes=[kGl])
                for kc in range(8):
                    p.op("tensor", lambda e, kc=kc, oc=oc: e.matmul(pUp[:, :T2], lhsT=wgs[:, kc, 1024 + oc * 128:1024 + (oc + 1) * 128], rhs=x1h[:, kc, to:to + T2], start=(kc == 0), stop=(kc == 7)),
                         reads=[f"wgu{wb_i}", "x1h"], writes=[kUp])
                p.op("vector", lambda e, oc=oc: e.tensor_scalar(out=glu[c][:], in0=pGl[:, :T2], scalar1=bgu_sb[:, ex_, oc:oc + 1], scalar2=7.0, op0=ALU.add, op1=ALU.min),
                     reads=[kGl, "bgu_sb"], writes=[f"glu{c}"])
                p.op("vector", lambda e, oc=oc: e.tensor_scalar(out=up1[c][:], in0=pUp[:, :T2], scalar1=bgu_sb[:, ex_, 8 + oc:9 + oc], scalar2=7.0, op0=ALU.add, op1=ALU.min),
                     reads=[kUp, "bgu_sb"], writes=[f"up1{c}"])
                p.op("vector", lambda e: e.tensor_scalar(out=up1[c][:], in0=up1[c][:], scalar1=-7.0, scalar2=1.0, op0=ALU.max, op1=ALU.add), reads=[f"up1{c}"], writes=[f"up1{c}"])
                p.op("scalar", lambda e: e.activation(out=sg[c][:], in_=glu[c][:], func=AF.Sigmoid, scale=1.702), reads=[f"glu{c}"], writes=[f"sg{c}"])
                p.op("vector", lambda e: e.tensor_tensor(out=t2[c][:], in0=up1[c][:], in1=gbc[c][:], op=ALU.mult), reads=[f"up1{c}", f"gbc{c}"], writes=[f"t2{c}"])
                p.op("vector", lambda e: e.tensor_tensor(out=t1[c][:], in0=glu[c][:], in1=sg[c][:], op=ALU.mult), reads=[f"glu{c}", f"sg{c}"], writes=[f"t1{c}"])
                p.op("vector", lambda e, oc=oc: e.tensor_tensor(out=act[c][:, oc, :], in0=t1[c][:], in1=t2[c][:], op=ALU.mult), reads=[f"t1{c}", f"t2{c}"], writes=[f"act{c}"])
            for oc in range(8):
                for kc in range(8):
                    p.op("tensor", lambda e, kc=kc, oc=oc: e.matmul(pD[:, :T2], lhsT=wds[:, kc, oc * 128:(oc + 1) * 128], rhs=act[c][:, kc, :], start=(kc == 0), stop=(kc == 7)),
                         reads=[f"wd{wb_i}", f"act{c}"], writes=[kD])
                p.op("vector", lambda e, oc=oc: e.tensor_tensor(out=acc[:, oc, to:to + T2], in0=acc[:, oc, to:to + T2], in1=pD[:, :T2], op=ALU.add),
                     reads=[f"acc{c}", kD], writes=[f"acc{c}"])

        for hf in range(2):
            ho = hf * H
            p.dma("sync", x1h[:], x1bf_s[:, :, ho:ho + H], reads=["x1bf_s"], writes=["x1h"])
            p.dma("sync", acc[:], acc_s[:, :, ho:ho + H], reads=["acc_s"], writes=["acc0", "acc1"])
            for tt in range(H // T2):
                to = tt * T2
                for oc in range(8):
                    pDc = PS[3 * tt + 2]
                    p.op("tensor", lambda e, oc=oc, to=to, pDc=pDc: e.matmul(pDc[:, :T2], lhsT=bd_sb[:, oc * 128:(oc + 1) * 128], rhs=gateT[:, ho + to:ho + to + T2], start=True, stop=True),
                         reads=["bd_sb", "gateT"], writes=[f"pD{tt}"])
                    p.op("vector", lambda e, oc=oc, to=to, pDc=pDc: e.tensor_tensor(out=acc[:, oc, to:to + T2], in0=acc[:, oc, to:to + T2], in1=pDc[:, :T2], op=ALU.add),
                         reads=[f"acc{tt}", f"pD{tt}"], writes=[f"acc{tt}"])
            for ex_ in range(ne):
                wb_i = ex_ % 2
                wgs, wds = wgu_b[wb_i], wd_b[wb_i]
                for kc in (range(0, 8, 2) if not (os.environ.get("B_NODMA") and ex_ >= 2) else []):
                    p.dma("gpsimd", wgs[:, kc:kc + 2, :], wgu[ex_, kc * 128:(kc + 2) * 128, :].rearrange("(k p) n -> p k n", p=128), writes=[f"wgu{wb_i}"])
                for kc in (range(0, 8, 4) if not (os.environ.get("B_NODMA") and ex_ >= 2) else []):
                    p.dma("gpsimd", wds[:, kc:kc + 4, :], wd[ex_, kc * 128:(kc + 4) * 128, :].rearrange("(k p) n -> p k n", p=128), writes=[f"wd{wb_i}"])
                chains = [p.capture(moe_tile, ex_, tt, ho, wgs, wds, wb_i) for tt in range(H // T2)]
                p.emit_interleaved(chains)
            for tt in range(H // T2):
                to = tt * T2
                hv = acc[:, :, to:to + T2]
                ln_fm(p, hv, f"acc{tt}", 8, T2, lambda oc: vecs[:, 2, oc:oc + 1], lambda oc: vecs[:, 3, oc:oc + 1], ones_mean, sq, pS, pQ, tm, "pS7", "pBC")
                p.dma("sync", outv[:, :, ho + to:ho + to + T2], hv, reads=[f"acc{tt}"], writes=["outT"])
            p.barrier()
    p.finish_wait("sync", ["outT"] + (["x1_d", "gate_d"] if dump else []))
    return p.build()


def build_L0(ntok=2048):
    p = Prog(); nc = p.nc
    xT = p.dram("xT", [1024, ntok]); vec = p.dram("vec", [128, 2, 8])
    outT = p.dram("outT", [1024, ntok], kind="ExternalOutput")
    ones_mean = p.sb("ones_mean", [128, 128]); vecs = p.sb("vecs", [128, 2, 8])
    p.dma("sync", vecs[:], vec[:, :, :], writes=["vec"])
    p.op("vector", lambda e: e.memset(ones_mean[:], 1.0 / 1024), writes=["ones_mean"])
    TT = 512
    h = [p.sb(f"h{i}", [128, 8, TT]) for i in range(2)]
    sq = p.sb("sq", [128, 2, TT]); tm = {k: p.sb("tm_" + k, [128, TT]) for k in ["mean", "rstd", "t"]}
    pS = p.ps("pS", [128, 512]); pQ = p.ps("pQ", [128, 512])
    xv = xT.rearrange("(kc p) n -> p kc n", p=128); ov = outT.rearrange("(kc p) n -> p kc n", p=128)
    for t in range(ntok // TT):
        o = t * TT; i = t % 2
        p.dma("sync", h[i][:], xv[:, :, o:o + TT], writes=[f"h{i}"])
        ln_fm(p, h[i], f"h{i}", 8, TT, lambda oc: vecs[:, 0, oc:oc + 1], lambda oc: vecs[:, 1, oc:oc + 1], ones_mean, sq, pS, pQ, tm, "pS", "pQ")
        p.dma("gpsimd", ov[:, :, o:o + TT], h[i][:], reads=[f"h{i}"], writes=["outT"])
    p.finish_wait("sync", ["outT"])
    return p.build()


def host_inputs_B(L, stream, ya, yb, u, z, c):
    sl = slice(c * 2048, (c + 1) * 2048)
    f = lambda a: np.ascontiguousarray(a, dtype=np.float32)
    ycat = np.concatenate([ya[sl], yb[sl], u[sl]], axis=1)
    vec = np.stack([z['ln1_g'][L].reshape(8, 128).T, z['ln1_b'][L].reshape(8, 128).T, z['ln2_g'][L].reshape(8, 128).T,
                    z['ln2_b'][L].reshape(8, 128).T, z['ssd_norm'][L].reshape(8, 128).T], axis=1)
    return {
        "xT": f(stream[sl].T), "yT": f(ycat.T), "pT": f(z['p'][L].reshape(-1, 256)[sl].T),
        "wg": f(z['w_in'][L][:, 6576:]), "wb": f(z['w_branch'][L]), "wo": f(z['w_o'][L]), "wplg": f(z['w_pl_gate'][L]),
        "wpl": f(z['w_pl'][L]), "wr": f(z['w_router'][L]), "br": f(z['b_router'][L][None]),
        "wgu": f(z['w_gu'][L]), "wd": f(z['w_down'][L]), "bgu": f(z['b_gu'][L].reshape(32, 16, 128).transpose(2, 0, 1)),
        "bd": f(z['b_down'][L]), "vec": f(vec), "ident": np.eye(128, dtype=np.float32),
    }


def kernel(**inputs):
    z = {k: np.asarray(v) for k, v in inputs.items()}
    NCORE = 8
    cores = list(range(NCORE))
    xf = z['x'].reshape(-1, 1024).astype(np.float32)
    f = lambda a: np.ascontiguousarray(a, dtype=np.float32)
    vec0 = f(np.stack([z['ln_in_g'].reshape(8, 128).T, z['ln_in_b'].reshape(8, 128).T], axis=1))
    nc0 = build_L0()
    res = run_bass_kernel_spmd(nc0, [{"xT": f(xf[c * 2048:(c + 1) * 2048].T), "vec": vec0} for c in cores], core_ids=cores)
    stream = np.concatenate([r["outT"].T for r in res.results], axis=0)
    for L in range(2):
        ncA = build_A(T=8192)
        imA = [host_inputs_A(z, L, stream[b * 8192:(b + 1) * 8192], j) for b in range(2) for j in range(4)]
        resA = run_bass_kernel_spmd(ncA, imA, core_ids=cores).results
        ya = np.concatenate([np.concatenate([resA[b * 4 + j]["ya"] for j in range(4)], axis=1) for b in range(2)], axis=0)
        yb = np.concatenate([np.concatenate([resA[b * 4 + j]["yb"] for j in range(4)], axis=1) for b in range(2)], axis=0)
        u = np.concatenate([np.concatenate([resA[b * 4 + j]["yc"] for j in range(4)], axis=1) for b in range(2)], axis=0)
        del resA, imA
        ncB = build_B()
        imB = [host_inputs_B(L, stream, ya, yb, u, z, c) for c in cores]
        resB = run_bass_kernel_spmd(ncB, imB, core_ids=cores).results
        stream = np.concatenate([r["outT"].T for r in resB], axis=0)
        del resB, imB
    return np.ascontiguousarray(stream.reshape(2, 8192, 1024), dtype=np.float32)
```

```python
import os
import contextlib, time
import numpy as np
import concourse.bass as bass
import concourse.mybir as mybir
from concourse.bass_utils import run_bass_kernel_spmd

F32 = mybir.dt.float32
BF16 = mybir.dt.bfloat16
I32 = mybir.dt.int32
ALU = mybir.AluOpType
AF = mybir.ActivationFunctionType
AX = mybir.AxisListType

ENG = ["sync", "gpsimd", "scalar", "vector", "tensor"]
NDMASEM = 6
import os as _os
ATTACH = bool(int(_os.environ.get('FW_ATTACH', '1')))


class Prog:
    def __init__(self, immediate=True):
        self.immediate = immediate
        self.nc = bass.Bass("TRN2", target_bir_lowering=False)
        try:
            self.nc.allow_low_precision("bf16 matmul operands with fp32 accumulation")
            self.nc.allow_non_contiguous_dma("strided layouts")
        except Exception as ex:
            print("allow_* failed", ex)
        self.st = contextlib.ExitStack()
        self.ops = {e: [] for e in ENG}
        self.cnt = {}
        self.sems = {}
        self.lastw = {}
        self.reads = {}
        self.seen = {e: {} for e in ENG}
        self.dma_i = {e: 0 for e in ENG}
        self.dma_last = {}
        self.ninstr = 0
        for e in ["gpsimd", "scalar", "vector", "tensor"]:
            self._sem("c_" + e)
        for e in ["sync", "gpsimd", "scalar"]:
            for i in range(NDMASEM):
                self._sem(f"d_{e}_{i}")

    def _sem(self, name):
        self.sems[name] = self.st.enter_context(self.nc.semaphore(name))
        self.cnt[name] = 0

    def dram(self, name, shape, dt=F32, kind="ExternalInput"):
        return self.nc.dram_tensor(name, list(shape), dt, kind=kind).ap()

    def sb(self, name, shape, dt=F32):
        return self.st.enter_context(self.nc.sbuf_tensor("s_" + name, list(shape), dt))

    def ps(self, name, shape, dt=F32):
        return self.st.enter_context(self.nc.psum_tensor("p_" + name, list(shape), dt))

    def _deps(self, eng, reads, writes):
        need = {}
        def add(tok):
            if tok is None:
                return
            s, v = tok
            if need.get(s, 0) < v:
                need[s] = v
        for k in reads:
            add(self.lastw.get(k))
        for k in writes:
            add(self.lastw.get(k))
            for t in self.reads.get(k, ()):
                add(t)
        out = []
        for s, v in need.items():
            if self.seen[eng].get(s, 0) < v:
                self.seen[eng][s] = v
                out.append((s, v))
        return out

    def _commit(self, tok, reads, writes):
        for k in reads:
            self.reads.setdefault(k, []).append(tok)
        for k in writes:
            self.lastw[k] = tok
            self.reads[k] = []

    def capture(self, f, *a):
        self._buf = []
        try:
            f(*a)
        finally:
            buf, self._buf = self._buf, None
        return buf

    def emit_interleaved(self, chains):
        chains = [list(c) for c in chains if c]
        idx = [0] * len(chains)
        live = True
        while live:
            live = False
            for ci, c in enumerate(chains):
                if idx[ci] < len(c):
                    kind, a, kw = c[idx[ci]]; idx[ci] += 1; live = True
                    (self.op if kind == "op" else self.dma)(*a, **kw)

    def emit_balanced(self, chains):
        chains = [list(c) for c in chains if c]
        idx = [0] * len(chains)
        total = sum(len(c) for c in chains)
        for _ in range(total):
            ci = min((i for i in range(len(chains)) if idx[i] < len(chains[i])), key=lambda i: idx[i] / len(chains[i]))
            kind, a, kw = chains[ci][idx[ci]]; idx[ci] += 1
            (self.op if kind == "op" else self.dma)(*a, **kw)

    def op(self, eng, fn, reads=(), writes=()):
        if getattr(self, "_buf", None) is not None:
            self._buf.append(("op", (eng, fn, reads, writes), {})); return None
        psr = [k for k in reads if isinstance(k, str) and k.startswith("ps")]
        if psr:
            reads = [k for k in reads if k not in psr]
            writes = list(writes) + psr
        waits = self._deps(eng, reads, writes)
        s = "c_" + eng
        self.cnt[s] += 1
        tok = (s, self.cnt[s])
        self._commit(tok, reads, writes)
        self._emit(eng, waits, fn, s, 1)
        self.ninstr += 1
        return tok

    def dma(self, eng, out, in_, reads=(), writes=(), **kw):
        if getattr(self, "_buf", None) is not None:
            self._buf.append(("dma", (eng, out, in_, reads, writes), kw)); return None
        slot = self.dma_i[eng] % NDMASEM
        self.dma_i[eng] += 1
        s = f"d_{eng}_{slot}"
        waits = self._deps(eng, reads, writes)
        prev = self.cnt[s]
        if prev > 0 and self.seen[eng].get(s, 0) < prev:
            self.seen[eng][s] = prev
            waits.append((s, prev))
        self.cnt[s] += 16
        tok = (s, self.cnt[s])
        self._commit(tok, reads, writes)
        fn = lambda e, out=out, in_=in_, kw=kw: e.dma_start(out=out, in_=in_, **kw)
        self._emit(eng, waits, fn, s, 16)
        self.ninstr += 1
        return tok

    def coll(self, kind, in_ap, out_ap, groups, reads=(), writes=()):
        eng = "gpsimd"
        slot = self.dma_i[eng] % NDMASEM
        self.dma_i[eng] += 1
        s = f"d_{eng}_{slot}"
        waits = self._deps(eng, reads, writes)
        prev = self.cnt[s]
        if prev > 0 and self.seen[eng].get(s, 0) < prev:
            self.seen[eng][s] = prev
            waits.append((s, prev))
        self.cnt[s] += 16
        tok = (s, self.cnt[s])
        self._commit(tok, reads, writes)
        fn = lambda e: e.collective_compute(kind, ALU.bypass, replica_groups=groups, ins=[in_ap], outs=[out_ap])
        self._emit(eng, waits, fn, s, 16)
        self.ninstr += 1
        return tok

    def barrier(self):
        for eng in ENG:
            waits = []
            for sname, v in self.cnt.items():
                if v > 0 and self.seen[eng].get(sname, 0) < v:
                    self.seen[eng][sname] = v
                    waits.append((sname, v))
            self._emit(eng, waits, None, None, 0)

    def finish_wait(self, eng, keys):
        waits = self._deps(eng, keys, ())
        self._emit(eng, waits, None, None, 0)

    def _emit(self, eng, waits, fn, s, inc):
        if not self.immediate:
            self.ops[eng].append((waits, fn, s, inc)); return
        engobj = getattr(self.nc, eng)
        if fn is None or not ATTACH:
            for (ws, wv) in waits:
                engobj.wait_ge(self.sems[ws], wv)
            if fn is not None:
                fn(engobj).then_inc(self.sems[s], inc)
            return
        for (ws, wv) in waits[1:]:
            engobj.wait_ge(self.sems[ws], wv)
        ins = fn(engobj)
        if waits:
            ins._wait_ge(self.sems[waits[0][0]], waits[0][1])
        ins.then_inc(self.sems[s], inc)

    def build(self):
        if self.immediate:
            self.st.close(); return self.nc
        nc = self.nc
        with nc.Block() as block:
            def mk(e):
                def body(engobj):
                    for waits, fn, s, inc in self.ops[e]:
                        for (ws, wv) in waits:
                            engobj.wait_ge(self.sems[ws], wv)
                        if fn is not None:
                            fn(engobj).then_inc(self.sems[s], inc)
                return body
            block.sync(mk("sync"))
            block.gpsimd(mk("gpsimd"))
            block.scalar(mk("scalar"))
            block.vector(mk("vector"))
            block.tensor(mk("tensor"))
        self.st.close()
        return nc


NCOL = 2060
NEG = -30000.0
RV = {}
_o = 0
for _n, _l in [("gconv", 5 * 384), ("sconv", 5 * 512), ("sconvb", 512), ("mup", 768), ("mun", 768), ("spb", 10), ("alog", 10),
               ("gnorm", 128), ("w0", 256), ("a0", 256), ("kk", 128), ("ka", 128), ("rk", 128), ("lng", 128), ("lnb", 128), ("dsk", 256)]:
    RV[_n] = (_o, _l); _o += _l
NV = _o


def host_inputs_A(z, L, stream_b, j):
    f = lambda a: np.ascontiguousarray(a, dtype=np.float32)
    w_in = z['w_in'][L]
    g = j // 2
    GD0, RW0, SS0 = 0, 2064, 2064 + 1920
    r = lambda a, n: list(range(a, a + n))
    cols = (r(GD0 + j * 128, 128) + r(GD0 + 512 + j * 128, 128) + r(GD0 + 1024 + j * 128, 128) + r(GD0 + 1536 + j * 128, 128)
            + r(RW0 + j * 128, 128) + r(RW0 + 512 + j * 128, 128) + r(RW0 + 1024 + j * 128, 128) + r(RW0 + 1536, 384)
            + r(SS0 + j * 256, 256) + r(SS0 + 1024 + j * 256, 256) + r(SS0 + 2048 + g * 128, 128) + r(SS0 + 2304 + g * 128, 128)
            + [GD0 + 2048 + d * 4 + j for d in range(2)] + [GD0 + 2056 + d * 4 + j for d in range(2)]
            + [SS0 + 2560 + d * 16 + 4 * j + i for d in range(2) for i in range(4)])
    assert len(cols) == NCOL
    rv = np.zeros(NV, np.float32)
    def put(n, a):
        o, l = RV[n]; a = np.asarray(a, np.float32).reshape(-1); assert a.size == l, (n, a.size, l); rv[o:o + l] = a
    qkv_idx = r(j * 128, 128) + r(512 + j * 128, 128) + r(1024 + j * 128, 128)
    xbc_idx = r(j * 256, 256) + r(1024 + g * 128, 128) + r(1280 + g * 128, 128)
    rw_idx = r(j * 128, 128) + r(512 + j * 128, 128) + r(1024 + j * 128, 128) + r(1536, 384)
    put("gconv", z['gdn_conv'][L][:, qkv_idx]); put("sconv", z['ssd_conv'][L][:, xbc_idx]); put("sconvb", z['ssd_conv_b'][L][xbc_idx])
    put("mup", z['rwkv_mu_prev'][L][rw_idx]); put("mun", z['rwkv_mu_next'][L][rw_idx])
    put("spb", np.concatenate([z['gdn_dt_bias'][L][:, j], z['ssd_dt_bias'][L][:, 4 * j:4 * j + 4].reshape(-1)]))
    put("alog", np.concatenate([z['gdn_a_log'][L][:, j], z['ssd_a_log'][L][:, 4 * j:4 * j + 4].reshape(-1)]))
    put("gnorm", z['gdn_norm'][L]); put("w0", z['rwkv_w0'][L][:, j * 128:(j + 1) * 128]); put("a0", z['rwkv_a0'][L][:, j * 128:(j + 1) * 128])
    put("kk", z['rwkv_k_k'][L][j * 128:(j + 1) * 128]); put("ka", z['rwkv_k_a'][L][j * 128:(j + 1) * 128])
    put("rk", z['rwkv_r_k'][L][2 * j:2 * j + 2]); put("lng", z['rwkv_ln_g'][L][j * 128:(j + 1) * 128]); put("lnb", z['rwkv_ln_b'][L][j * 128:(j + 1) * 128])
    put("dsk", np.repeat(z['ssd_d'][L][4 * j:4 * j + 4], 64))
    k = np.arange(128)
    UT = (k[:, None] <= k[None, :]).astype(np.float32); LT = (k[:, None] >= k[None, :]).astype(np.float32)
    blk = (k[:, None] // 64 == k[None, :] // 64).astype(np.float32)
    bdUT = UT * blk; bdLT = LT * blk
    msk = np.stack([UT, LT, np.where(UT > 0, 0.0, NEG), np.where(LT > 0, 0.0, NEG), np.eye(128), np.ones((128, 128)), bdUT, bdLT, -(bdUT - np.eye(128)), -(bdLT - np.eye(128)), bdUT - np.eye(128), bdLT - np.eye(128)], axis=1)
    return {
        "xT": f(stream_b.T), "wc": f(w_in[:, cols]), "rowvec": f(rv[None]),
        "wup": f(z['rwkv_w_up'][L][:, :, j * 128:(j + 1) * 128].reshape(128, 128)),
        "aup": f(z['rwkv_a_up'][L][:, :, j * 128:(j + 1) * 128].reshape(128, 128)),
        "gup": f(z['rwkv_g_up'][L][:, j * 128:(j + 1) * 128]), "msk": f(msk),
    }


def build_A(T=8192, dump=False, NS=16, phases=(1, 2, 3, 4)):
    p = Prog(); nc = p.nc
    NTL = T // 128
    dk = "ExternalOutput" if dump else "Internal"
    xT = p.dram("xT", [1024, T]); wc = p.dram("wc", [1024, NCOL]); rowvec = p.dram("rowvec", [1, NV])
    wup = p.dram("wup", [128, 128]); aup = p.dram("aup", [128, 128]); gup = p.dram("gup", [128, 128]); mskd = p.dram("msk", [128, 12, 128])
    ya_o = p.dram("ya", [T, 128], kind="ExternalOutput"); yb_o = p.dram("yb", [T, 128], kind="ExternalOutput"); yc_o = p.dram("yc", [T, 256], kind="ExternalOutput")
    cols_s = p.dram("cols_s", [T + 4, NCOL], kind=dk)
    gkq_s = p.dram("gkq_s", [T, 2, 128], kind=dk); gsc_s = p.dram("gsc_s", [T, 4], kind=dk); gbvT_s = p.dram("gbvT_s", [2, 128, T], kind=dk)
    rw_s = p.dram("rw_s", [2, 2, T, 5, 64], kind=dk); rvT_s = p.dram("rvT_s", [128, T], kind=dk); rpost_s = p.dram("rpost_s", [T, 258], kind=dk)
    ssd_s = p.dram("ssd_s", [T, 528], kind=dk)
    gy_s = p.dram("gy_s", [2, T, 128], kind=dk); gkqv_s = p.dram("gkqv_s", [T, 3, 128], kind=dk); gbg_s = p.dram("gbg_s", [T, 4], kind=dk); ry_s = p.dram("ry_s", [2, T, 128], kind=dk); sy_s = p.dram("sy_s", [T, 256], kind=dk)

    V = lambda fn, r=(), w=(): p.op("vector", fn, r, w)
    G = lambda fn, r=(), w=(): p.op("gpsimd", fn, r, w)
    S = lambda fn, r=(), w=(): p.op("scalar", fn, r, w)
    PE = lambda fn, r=(), w=(): p.op("tensor", fn, r, w)

    msk = p.sb("msk", [128, 12, 128]); rvs = p.sb("rvs", [128, NV])
    p.dma("sync", msk[:], mskd[:, :, :], writes=["msk"])
    p.dma("sync", rvs[:], rowvec.partition_broadcast(128)[:, 0, :], writes=["rvs"])
    UT, LT, NEGf, NEGb, ident, ones, bdUT, bdLT, nbdUTs, nbdLTs, sbdUT, sbdLT = (msk[:, i, :] for i in range(12))
    def rv(n, a=0, l=None):
        o, ln = RV[n]
        return rvs[:, o + a:o + a + (ln - a if l is None else l)]
    negexp = p.sb("negexp", [128, 10])
    S(lambda e: e.activation(out=negexp[:], in_=rv("alog"), func=AF.Exp), ["rvs"], ["negexp"])
    V(lambda e: e.tensor_scalar(out=negexp[:], in0=negexp[:], scalar1=-1.0, scalar2=None, op0=ALU.mult), ["negexp"], ["negexp"])
    PS = [p.ps(f"ps{i}", [128, 512]) for i in range(8)]

    if 1 in phases:
        with contextlib.ExitStack() as sc:
            sb = lambda name, shape, dt=F32: sc.enter_context(nc.sbuf_tensor("s_" + name, list(shape), dt))
            W_bf = sb("W_bf", [128, 8, 2048], BF16); w_sm = sb("w_sm", [128, 8, 12])
            zt = sb("zt", [2, NCOL])
            xb = [sb(f"xb{i}", [128, 8, 128], BF16) for i in range(2)]; xf = [sb(f"xf{i}", [128, 8, 128]) for i in range(2)]
            ct = [sb(f"ct{i}", [128, NCOL]) for i in range(2)]
            wcv = wc.rearrange("(kc p) n -> p kc n", p=128)
            for kc in range(8):
                p.dma("gpsimd", W_bf[:, kc, :], wc[kc * 128:(kc + 1) * 128, 0:2048], writes=["W_bf"])
            p.dma("sync", w_sm[:], wcv[:, :, 2048:2060], writes=["w_sm"])
            V(lambda e: e.memset(zt[:], 0.0), [], ["zt"])
            p.dma("sync", cols_s[0:2, :], zt[:], reads=["zt"], writes=["cols_pad"])
            p.dma("sync", cols_s[T + 2:T + 4, :], zt[:], reads=["zt"], writes=["cols_pad"])
            xTv = xT.rearrange("(kc p) n -> p kc n", p=128)
            for tt in range(NTL):
                t0 = tt * 128; i = tt % 2
                p.dma("gpsimd", xb[i][:], xTv[:, :, t0:t0 + 128], writes=[f"xb{i}"])
                p.dma("sync", xf[i][:], xTv[:, :, t0:t0 + 128], writes=[f"xf{i}"])
                for gq in range(4):
                    pp = PS[gq]
                    for kc in range(8):
                        PE(lambda e, kc=kc, gq=gq, pp=pp, i=i: e.matmul(pp[:, :], lhsT=xb[i][:, kc, :], rhs=W_bf[:, kc, gq * 512:(gq + 1) * 512], start=(kc == 0), stop=(kc == 7)),
                           [f"xb{i}", "W_bf"], [f"ps{gq}"])
                    if gq % 2 == 0:
                        S(lambda e, gq=gq, pp=pp, i=i: e.copy(out=ct[i][:, gq * 512:(gq + 1) * 512], in_=pp[:, :]), [f"ps{gq}"], [f"ct{i}"])
                    else:
                        V(lambda e, gq=gq, pp=pp, i=i: e.tensor_copy(out=ct[i][:, gq * 512:(gq + 1) * 512], in_=pp[:, :]), [f"ps{gq}"], [f"ct{i}"])
                for kc in range(8):
                    PE(lambda e, kc=kc, i=i: e.matmul(PS[4][:, :12], lhsT=xf[i][:, kc, :], rhs=w_sm[:, kc, :], start=(kc == 0), stop=(kc == 7)),
                       [f"xf{i}", "w_sm"], ["ps4"])
                V(lambda e, i=i: e.tensor_copy(out=ct[i][:, 2048:2060], in_=PS[4][:, :12]), ["ps4"], [f"ct{i}"])
                p.dma("sync", cols_s[2 + t0:2 + t0 + 128, :], ct[i][:], reads=[f"ct{i}"], writes=["cols_s"])
        p.barrier()

    if 2 in phases:
        with contextlib.ExitStack() as sc:
            sb = lambda name, shape, dt=F32: sc.enter_context(nc.sbuf_tensor("s_" + name, list(shape), dt))
            win = [sb(f"win{j}", [128, NCOL]) for j in range(5)]
            wup_sb = sb("wup_sb", [128, 128]); aup_sb = sb("aup_sb", [128, 128]); gup_sb = sb("gup_sb", [128, 128])
            p.dma("sync", wup_sb[:], wup[:, :], writes=["wup_sb"]); p.dma("sync", aup_sb[:], aup[:, :], writes=["aup_sb"]); p.dma("sync", gup_sb[:], gup[:, :], writes=["gup_sb"])
            cacc = sb("cacc", [128, 896]); ctmp = sb("ctmp", [128, 896]); qkv = sb("qkv", [128, 384]); xbc = sb("xbc", [128, 528])
            junk = sb("junk", [128, 128]); ssq = sb("ssq", [128, 4]); kq = sb("kq", [128, 2, 128])
            spx = sb("spx", [128, 10]); spa = sb("spa", [128, 10]); spl = sb("spl", [128, 10]); beta = sb("beta", [128, 2]); gsc = sb("gsc", [128, 4])
            bv = sb("bv", [128, 2, 128]); trs = sb("trs", [128, 128]); bg = sb("bg", [128, 4])
            sh = sb("sh", [128, 768]); d1 = sb("d1", [128, 768]); d2 = sb("d2", [128, 768])
            tw = sb("tw", [128, 128]); twT = sb("twT", [128, 128]); alT = sb("alT", [128, 128]); sgl = sb("sgl", [128, 128]); sgT = sb("sgT", [128, 128])
            RW = [sb(f"RW{d}", [128, 5, 128]) for d in range(2)]; ad = [sb(f"ad{d}", [128, 128]) for d in range(2)]
            wraw = sb("wraw", [128, 128]); kx = sb("kx", [128, 128]); sqk = sb("sqk", [128, 128]); rkk = sb("rkk", [128, 2]); kkn = sb("kkn", [128, 128])
            rkr = sb("rkr", [128, 128]); prod = sb("prod", [128, 128]); bon = sb("bon", [128, 2, 2]); rpost = sb("rpost", [128, 258]); t128 = sb("t128", [128, 128])
            pT1, pT2, pM1, pM2, pT3 = PS[0], PS[1], PS[2], PS[3], PS[4]
            for tt in range(NTL):
                t0 = tt * 128
                for j in range(5):
                    p.dma("sync" if j % 2 == 0 else "scalar", win[j][:], cols_s[t0 + j:t0 + j + 128, :], reads=["cols_s", "cols_pad"], writes=[f"win{j}"])
                cur = win[2]
                for (c0, c1, o0, cname, cw) in [(0, 384, 0, "gconv", 384), (1536, 2048, 384, "sconv", 512)]:
                    for j in range(5):
                        wj = rv(cname, j * cw, cw)
                        if j == 0:
                            V(lambda e, c0=c0, c1=c1, o0=o0, wj=wj, cw=cw: e.tensor_tensor(out=cacc[:, o0:o0 + cw], in0=win[0][:, c0:c1], in1=wj, op=ALU.mult), ["win0", "rvs"], [f"cacc{o0}"])
                        else:
                            V(lambda e, c0=c0, c1=c1, o0=o0, wj=wj, cw=cw, j=j: e.tensor_tensor(out=ctmp[:, o0:o0 + cw], in0=win[j][:, c0:c1], in1=wj, op=ALU.mult), [f"win{j}", "rvs"], [f"ctmp{o0}"])
                            V(lambda e, o0=o0, cw=cw: e.tensor_tensor(out=cacc[:, o0:o0 + cw], in0=cacc[:, o0:o0 + cw], in1=ctmp[:, o0:o0 + cw], op=ALU.add), [f"cacc{o0}", f"ctmp{o0}"], [f"cacc{o0}"])
                V(lambda e: e.tensor_tensor(out=cacc[:, 384:896], in0=cacc[:, 384:896], in1=rv("sconvb"), op=ALU.add), ["cacc384", "rvs"], ["cacc384"])
                S(lambda e: e.activation(out=qkv[:], in_=cacc[:, 0:384], func=AF.Silu), ["cacc0"], ["qkv"])
                S(lambda e: e.activation(out=xbc[:, 0:512], in_=cacc[:, 384:896], func=AF.Silu), ["cacc384"], ["xbc"])
                V(lambda e: e.tensor_tensor(out=spx[:], in0=cur[:, 2050:2060], in1=rv("spb"), op=ALU.add), ["win2", "rvs"], ["spx"])
                S(lambda e: e.activation(out=spa[:], in_=spx[:], func=AF.Abs), ["spx"], ["spa"])
                S(lambda e: e.activation(out=spa[:], in_=spa[:], func=AF.Exp, scale=-1.0), ["spa"], ["spa"])
                S(lambda e: e.activation(out=spl[:], in_=spa[:], func=AF.Ln, bias=1.0), ["spa"], ["spl"])
                V(lambda e: e.tensor_scalar(out=spx[:], in0=spx[:], scalar1=0.0, scalar2=None, op0=ALU.max), ["spx"], ["spx"])
                V(lambda e: e.tensor_tensor(out=spx[:], in0=spx[:], in1=spl[:], op=ALU.add), ["spx", "spl"], ["spx"])
                V(lambda e: e.tensor_tensor(out=spl[:], in0=spx[:], in1=negexp[:], op=ALU.mult), ["spx", "negexp"], ["spl"])
                V(lambda e: e.tensor_copy(out=xbc[:, 512:520], in_=spx[:, 2:10]), ["spx"], ["xbc"])
                V(lambda e: e.tensor_copy(out=xbc[:, 520:528], in_=spl[:, 2:10]), ["spl"], ["xbc"])
                p.dma("sync", ssd_s[t0:t0 + 128, :], xbc[:], reads=["xbc"], writes=["ssd_s"])
                S(lambda e: e.activation(out=beta[:], in_=cur[:, 2048:2050], func=AF.Sigmoid), ["win2"], ["beta"])
                S(lambda e: e.activation(out=gsc[:, 0:2], in_=spl[:, 0:2], func=AF.Exp), ["spl"], ["gsc"])
                V(lambda e: e.scalar_tensor_tensor(out=gsc[:, 2:4], in0=gsc[:, 0:2], scalar=-1.0, in1=beta[:], op0=ALU.mult, op1=ALU.mult), ["gsc", "beta"], ["gsc"])
                for qi in range(2):
                    src = qkv[:, qi * 128:(qi + 1) * 128]
                    V(lambda e, src=src, qi=qi: e.scalar_tensor_tensor(out=junk[:], in0=src, scalar=1.0, in1=src, op0=ALU.mult, op1=ALU.mult, accum_out=ssq[:, qi:qi + 1]), ["qkv"], ["junk", "ssq"])
                V(lambda e: e.tensor_scalar(out=ssq[:, 0:2], in0=ssq[:, 0:2], scalar1=1e-6, scalar2=None, op0=ALU.add), ["ssq"], ["ssq"])
                S(lambda e: e.activation(out=ssq[:, 0:2], in_=ssq[:, 0:2], func=AF.Sqrt), ["ssq"], ["ssq"])
                V(lambda e: e.reciprocal(out=ssq[:, 0:2], in_=ssq[:, 0:2]), ["ssq"], ["ssq"])
                V(lambda e: e.tensor_scalar(out=kq[:, 0, :], in0=qkv[:, 128:256], scalar1=ssq[:, 1:2], scalar2=None, op0=ALU.mult), ["qkv", "ssq"], ["kq"])
                V(lambda e: e.tensor_scalar(out=kq[:, 1, :], in0=qkv[:, 0:128], scalar1=ssq[:, 0:1], scalar2=128 ** -0.5, op0=ALU.mult, op1=ALU.mult), ["qkv", "ssq"], ["kq"])
                p.dma("sync", gkqv_s[t0:t0 + 128, 0:2, :], kq[:], reads=["kq"], writes=["gkqv_s"])
                p.dma("sync", gkqv_s[t0:t0 + 128, 2, :], qkv[:, 256:384], reads=["qkv"], writes=["gkqv_s"])
                V(lambda e: e.tensor_copy(out=bg[:, 0:2], in_=beta[:]), ["beta"], ["bg"])
                V(lambda e: e.tensor_copy(out=bg[:, 2:4], in_=spl[:, 0:2]), ["spl"], ["bg"])
                p.dma("sync", gbg_s[t0:t0 + 128, :], bg[:], reads=["bg"], writes=["gbg_s"])
                c_, pv, nx = cur[:, 512:1280], win[1][:, 512:1280], win[3][:, 512:1280]
                V(lambda e: e.tensor_tensor(out=d1[:], in0=pv, in1=c_, op=ALU.subtract), ["win1", "win2"], ["d1"])
                V(lambda e: e.tensor_tensor(out=d1[:], in0=d1[:], in1=rv("mup"), op=ALU.mult), ["d1", "rvs"], ["d1"])
                V(lambda e: e.tensor_tensor(out=d2[:], in0=nx, in1=c_, op=ALU.subtract), ["win3", "win2"], ["d2"])
                V(lambda e: e.tensor_tensor(out=d2[:], in0=d2[:], in1=rv("mun"), op=ALU.mult), ["d2", "rvs"], ["d2"])
                V(lambda e: e.tensor_tensor(out=sh[:], in0=c_, in1=d1[:], op=ALU.add), ["win2", "d1"], ["sh"])
                V(lambda e: e.tensor_tensor(out=sh[:], in0=sh[:], in1=d2[:], op=ALU.add), ["sh", "d2"], ["sh"])
                r_, k_, v_, wl, al, gl = (sh[:, i * 128:(i + 1) * 128] for i in range(6))
                S(lambda e: e.activation(out=tw[:], in_=wl, func=AF.Tanh), ["sh"], ["tw"])
                PE(lambda e: e.transpose(out=pT1[:, :128], in_=tw[:], identity=ident), ["tw", "msk"], ["ps0"])
                S(lambda e: e.copy(out=twT[:], in_=pT1[:, :128]), ["ps0"], ["twT"])
                PE(lambda e: e.transpose(out=pT2[:, :128], in_=al, identity=ident), ["sh", "msk"], ["ps1"])
                V(lambda e: e.tensor_copy(out=alT[:], in_=pT2[:, :128]), ["ps1"], ["alT"])
                S(lambda e: e.activation(out=sgl[:], in_=gl, func=AF.Sigmoid), ["sh"], ["sgl"])
                PE(lambda e: e.transpose(out=pT3[:, :128], in_=sgl[:], identity=ident), ["sgl", "msk"], ["ps4"])
                V(lambda e: e.tensor_copy(out=sgT[:], in_=pT3[:, :128]), ["ps4"], ["sgT"])
                PE(lambda e: e.matmul(pT3[:, 128:256], lhsT=sgT[:], rhs=gup_sb[:], start=True, stop=True), ["sgT", "gup_sb"], ["ps4"])
                S(lambda e: e.copy(out=rpost[:, 128:256], in_=pT3[:, 128:256]), ["ps4"], ["rpost"])
                V(lambda e: e.tensor_tensor(out=kx[:], in0=k_, in1=rv("kk"), op=ALU.mult), ["sh", "rvs"], ["kx"])
                S(lambda e: e.activation(out=sqk[:], in_=kx[:], func=AF.Square), ["kx"], ["sqk"])
                V(lambda e: e.tensor_reduce(out=rkk[:], in_=sqk[:].rearrange("p (h n) -> p h n", h=2), axis=AX.X, op=ALU.add), ["sqk"], ["rkk"])
                V(lambda e: e.tensor_scalar(out=rkk[:], in0=rkk[:], scalar1=1e-6, scalar2=None, op0=ALU.add), ["rkk"], ["rkk"])
                S(lambda e: e.activation(out=rkk[:], in_=rkk[:], func=AF.Sqrt), ["rkk"], ["rkk"])
                V(lambda e: e.reciprocal(out=rkk[:], in_=rkk[:]), ["rkk"], ["rkk"])
                for h in range(2):
                    V(lambda e, h=h: e.tensor_scalar(out=kkn[:, h * 64:(h + 1) * 64], in0=kx[:, h * 64:(h + 1) * 64], scalar1=rkk[:, h:h + 1], scalar2=None, op0=ALU.mult), ["kx", "rkk"], ["kkn"])
                V(lambda e: e.tensor_tensor(out=rkr[:], in0=r_, in1=rv("rk"), op=ALU.mult), ["sh", "rvs"], ["rkr"])
                for d in range(2):
                    hs = slice(d * 64, (d + 1) * 64)
                    PE(lambda e, hs=hs: e.matmul(pM1[:, :128], lhsT=twT[hs, :], rhs=wup_sb[hs, :], start=True, stop=True), ["twT", "wup_sb"], ["ps2"])
                    V(lambda e, d=d: e.tensor_tensor(out=wraw[:], in0=pM1[:, :128], in1=rv("w0", d * 128, 128), op=ALU.add), ["ps2", "rvs"], ["wraw"])
                    S(lambda e: e.activation(out=wraw[:], in_=wraw[:], func=AF.Sigmoid), ["wraw"], ["wraw"])
                    S(lambda e, d=d: e.activation(out=RW[d][:, 0, :], in_=wraw[:], func=AF.Exp, scale=-0.6065306597126334), ["wraw"], [f"RW{d}"])
                    PE(lambda e, hs=hs: e.matmul(pM2[:, :128], lhsT=alT[hs, :], rhs=aup_sb[hs, :], start=True, stop=True), ["alT", "aup_sb"], ["ps3"])
                    V(lambda e, d=d: e.tensor_tensor(out=ad[d][:], in0=pM2[:, :128], in1=rv("a0", d * 128, 128), op=ALU.add), ["ps3", "rvs"], [f"ad{d}"])
                    S(lambda e, d=d: e.activation(out=ad[d][:], in_=ad[d][:], func=AF.Sigmoid), [f"ad{d}"], [f"ad{d}"])
                    V(lambda e, d=d: e.tensor_copy(out=RW[d][:, 1, :], in_=kkn[:]), ["kkn"], [f"RW{d}"])
                    V(lambda e, d=d: e.scalar_tensor_tensor(out=RW[d][:, 2, :], in0=kkn[:], scalar=-1.0, in1=ad[d][:], op0=ALU.mult, op1=ALU.mult), ["kkn", f"ad{d}"], [f"RW{d}"])
                    V(lambda e, d=d: e.scalar_tensor_tensor(out=t128[:], in0=ad[d][:], scalar=-1.0, in1=rv("ka"), op0=ALU.add, op1=ALU.mult), [f"ad{d}", "rvs"], ["t128"])
                    V(lambda e, d=d: e.scalar_tensor_tensor(out=RW[d][:, 3, :], in0=t128[:], scalar=1.0, in1=k_, op0=ALU.add, op1=ALU.mult), ["t128", "sh"], [f"RW{d}"])
                    V(lambda e, d=d: e.tensor_copy(out=RW[d][:, 4, :], in_=r_), ["sh"], [f"RW{d}"])
                    V(lambda e, d=d: e.tensor_tensor(out=prod[:], in0=rkr[:], in1=RW[d][:, 3, :], op=ALU.mult), ["rkr", f"RW{d}"], ["prod"])
                    V(lambda e, d=d: e.tensor_reduce(out=bon[:, d, :], in_=prod[:].rearrange("p (h n) -> p h n", h=2), axis=AX.X, op=ALU.add), ["prod"], ["bon"])
                    for h in range(2):
                        p.dma("sync", rw_s[d, h, t0:t0 + 128, :, :], RW[d][:, :, h * 64:(h + 1) * 64], reads=[f"RW{d}"], writes=["rw_s"])
                V(lambda e: e.tensor_tensor(out=rpost[:, 256:258], in0=bon[:, 0, :], in1=bon[:, 1, :], op=ALU.add), ["bon"], ["rpost"])
                V(lambda e: e.tensor_copy(out=rpost[:, 0:128], in_=v_), ["sh"], ["rpost"])
                p.dma("sync", rpost_s[t0:t0 + 128, :], rpost[:], reads=["rpost"], writes=["rpost_s"])
                PE(lambda e: e.transpose(out=pT2[:, :128], in_=v_, identity=ident), ["sh", "msk"], ["ps1"])
                V(lambda e: e.tensor_copy(out=t128[:], in_=pT2[:, :128]), ["ps1"], ["t128"])
                p.dma("sync", rvT_s[:, t0:t0 + 128], t128[:], reads=["t128"], writes=["rvT_s"])
        p.barrier()
    fin = ["ya", "yb", "yc"]
    if dump:
        fin += ["cols_s", "gkqv_s", "gbg_s", "rw_s", "rvT_s", "rpost_s", "ssd_s", "gy_s", "ry_s"]
    if 3 in phases:
        phase3(p, T, NS, locals())
    if 4 in phases:
        phase4(p, T, locals())
    p.finish_wait("sync", [k for k in fin if k in p.lastw])
    return p.build()


def phase3(p, T, NS, L):
    SSDSTOP = int(os.environ.get('A_SSDSTOP', '99'))
    GSTOP = int(os.environ.get('A_GSTOP', '99'))
    nc = p.nc
    V = L["V"]; G = L["G"]; S = L["S"]; PE = L["PE"]; PS = L["PS"]
    UT, LT, ident, ones = L["UT"], L["LT"], L["ident"], L["ones"]
    gkqv_s, gbg_s, rw_s, rvT_s, ssd_s, gy_s, ry_s, sy_s, cols_s, yc_o = (L[k] for k in
        ["gkqv_s", "gbg_s", "rw_s", "rvT_s", "ssd_s", "gy_s", "ry_s", "sy_s", "cols_s", "yc_o"])
    bdUT, bdLT, nbdUTs, nbdLTs, sbdUT, sbdLT = (L[k_] for k_ in ["bdUT", "bdLT", "nbdUTs", "nbdLTs", "sbdUT", "sbdLT"])
    rpost_s = L["rpost_s"]
    rv = L["rv"]
    NTL = T // 128; NCH = T // NS
    with contextlib.ExitStack() as sc:
        sb = lambda name, shape, dt=F32: sc.enter_context(nc.sbuf_tensor("s_" + name, list(shape), dt))
        RB = []
        for d in range(2):
            r_ = {}
            for nm, shp in [("rwt", [128, 5, 128]), ("vt", [128, 128]), ("lw", [128, 128]), ("lp", [128, 128]), ("Pt", [128, 128]), ("iP", [128, 128]), ("Pm", [128, 128]),
                            ("rt", [128, 128]), ("kt", [128, 128]), ("nbt", [128, 128]), ("ct", [128, 128]), ("rtF", [128, 128]), ("nbF", [128, 128]), ("cF", [128, 128]), ("PF", [128, 128]),
                            ("X", [128, 128]), ("XT", [128, 128]), ("AckT", [128, 128]), ("ArkT", [128, 128]), ("AnrbT", [128, 128]),
                            ("P0", [128, 128]), ("P1", [128, 128]), ("PT0", [128, 128]), ("PT1", [128, 128]), ("TT", [128, 128]),
                            ("Zs", [128, 64]), ("Ms", [128, 64]), ("Yt", [128, 128]), ("H", [128, 64])]:
                r_[nm] = sb(f"r{d}_{nm}", shp)
            for nm in ["cFh", "nbFh", "ktFh", "rtFh"]:
                for h in range(2):
                    r_[f"{nm}{h}"] = sb(f"r{d}_{nm}{h}", [128, 128])
                    V(lambda e, t_=r_[f"{nm}{h}"]: e.memset(t_[:], 0.0), [], [f"r{d}_{nm}{h}"])
            for nm in ["ktS", "nbS"]:
                for ch in range(2):
                    for h in range(2):
                        r_[f"{nm}{ch}{h}"] = sb(f"r{d}_{nm}{ch}{h}", [128, 128])
                        V(lambda e, t_=r_[f"{nm}{ch}{h}"]: e.memset(t_[:], 0.0), [], [f"r{d}_{nm}{ch}{h}"])
            V(lambda e, r_=r_: e.memset(r_["H"][:], 0.0), [], [f"r{d}_H"])
            V(lambda e, r_=r_: e.memset(r_["Zs"][:], 0.0), [], [f"r{d}_Zs"])
            V(lambda e, r_=r_: e.memset(r_["Ms"][:], 0.0), [], [f"r{d}_Ms"])
            RB.append(r_)
        GB = []
        for d in range(2):
            g_ = {}
            for nm, shp in [("kqv", [128, 3, 128]), ("bg", [128, 4]), ("kF", [128, 128]), ("qF", [128, 128]), ("Gs", [128, 128]), ("QKs", [128, 128]),
                            ("gcc", [128, 1]), ("ngcc", [128, 1]), ("R", [128, 128]), ("grow", [128, 128]), ("egrow", [128, 128]), ("dT", [128, 128]), ("dN", [128, 128]),
                            ("Bd", [128, 128]), ("brow", [128, 128]), ("X", [128, 128]), ("XT", [128, 128]), ("P0", [128, 128]), ("P1", [128, 128]),
                            ("PT0", [128, 128]), ("PT1", [128, 128]), ("TT", [128, 128]), ("vb", [128, 128]), ("kbg", [128, 128]), ("be", [128, 1]), ("eg", [128, 1]),
                            ("u", [128, 128]), ("wF", [128, 128]), ("qdF", [128, 128]), ("QKm", [128, 128]), ("vnew", [128, 128]), ("kdA", [128, 128]), ("kdB", [128, 128]),
                            ("dl", [128, 1]), ("dlA", [128, 1]), ("dlB", [128, 1]), ("S", [128, 128]), ("o", [128, 128]), ("t1", [128, 128]), ("t2", [128, 128])]:
                g_[nm] = sb(f"g{d}_{nm}", shp)
            GB.append(g_)
            V(lambda e, g_=g_: e.memset(g_["S"][:], 0.0), [], [f"g{d}_S"])
            V(lambda e, g_=g_: e.memset(g_["vnew"][:], 0.0), [], [f"g{d}_vnew"])
        sxt = sb("sxt", [128, 528]); BCF = sb("BCF", [128, 256]); scT = sb("scT", [128, 128]); acol = sb("acol", [128, 4]); nacol = sb("nacol", [128, 4])
        Rm = sb("Rm", [128, 4, 128]); E = sb("E", [128, 4, 128]); Dm = sb("Dm", [128, 128]); M = sb("M", [128, 4, 128]); CdF = sb("CdF", [128, 4, 128])
        xdt = sb("xdt", [128, 256]); xend = sb("xend", [128, 256]); dend = sb("dend", [128, 4]); H = sb("H", [128, 256]); yt = sb("yt", [128, 256])
        syf = sb("syf", [128, 256]); zt = sb("zts", [128, 256]); xd = sb("xd", [128, 256]); szs = sb("szs", [128, 256])
        pA, pSc, pAc, pRow, pY, pSt = PS[0][:, 0:256], PS[0][:, 256:384], PS[0][:, 384:512], PS[1], PS[2][:, 0:256], PS[2][:, 256:512]
        print('phase3 sbuf remaining', nc.sbuf_bytes_remaining, flush=True)

        def gdn_chunk(d, c):
            t0 = c * 128
            B_ = GB[d]; K = lambda n: f"g{d}_{n}"
            bank = PS[6 + d]; kb = f"ps{6 + d}"
            regs = [bank[:, i * 128:(i + 1) * 128] for i in range(4)] + [PS[3][:, d * 256:d * 256 + 128], PS[3][:, d * 256 + 128:d * 256 + 256]]
            keys = [kb] * 4 + ["ps3", "ps3"]
            (rA, rB, rC, rD, rE, rF), (kA, kB, kC, kD, kE, kF_) = regs, keys
            fwd = (d == 0)
            Mtri = bdUT if fwd else bdLT
            MaskT = Mtri
            MaskN = bdLT if fwd else bdUT
            nST = nbdUTs if fwd else nbdLTs
            nSN = nbdLTs if fwd else nbdUTs
            selA = bdLT[:, 0:1]; selB = bdUT[:, 127:128]
            hA, hB = slice(0, 64), slice(64, 128)
            if fwd:
                first, second, lastF, lastS = hA, hB, 63, 127
            else:
                first, second, lastF, lastS = hB, hA, 64, 0
            kqv, bg = B_["kqv"], B_["bg"]
            p.dma("sync", kqv[:], gkqv_s[t0:t0 + 128, :, :], reads=["gkqv_s"], writes=[K("kqv")])
            p.dma("sync", bg[:], gbg_s[t0:t0 + 128, :], reads=["gbg_s"], writes=[K("bg")])
            kc, qc, vc = kqv[:, 0, :], kqv[:, 1, :], kqv[:, 2, :]
            beta = bg[:, d:d + 1]; g = bg[:, 2 + d:3 + d]
            kF, qF, Gs, QKs = B_["kF"], B_["qF"], B_["Gs"], B_["QKs"]
            PE(lambda e: e.transpose(out=rA, in_=kc, identity=ident), [K("kqv"), "msk"], [kA])
            S(lambda e: e.copy(out=kF[:], in_=rA), [kA], [K("kF")])
            PE(lambda e: e.transpose(out=rB, in_=qc, identity=ident), [K("kqv"), "msk"], [kB])
            S(lambda e: e.copy(out=qF[:], in_=rB), [kB], [K("qF")])
            PE(lambda e: e.matmul(rC, lhsT=kF[:], rhs=kF[:], start=True, stop=True), [K("kF")], [kC])
            S(lambda e: e.copy(out=Gs[:], in_=rC), [kC], [K("Gs")])
            PE(lambda e: e.matmul(rD, lhsT=kF[:], rhs=qF[:], start=True, stop=True), [K("kF"), K("qF")], [kD])
            S(lambda e: e.copy(out=QKs[:], in_=rD), [kD], [K("QKs")])
            V(lambda e: e.tensor_scalar(out=B_["R"][:], in0=Mtri, scalar1=g, scalar2=None, op0=ALU.mult), [K("bg"), "msk"], [K("R")])
            PE(lambda e: e.matmul(rE, lhsT=ones, rhs=B_["R"][:], start=True, stop=True), [K("R"), "msk"], [kE])
            S(lambda e: e.copy(out=B_["grow"][:], in_=rE), [kE], [K("grow")])
            S(lambda e: e.activation(out=B_["egrow"][:], in_=rE, func=AF.Exp), [kE], [K("egrow")])
            V(lambda e: e.scalar_tensor_tensor(out=B_["t1"][:], in0=B_["grow"][:], scalar=1.0, in1=ident, op0=ALU.mult, op1=ALU.mult, accum_out=B_["gcc"][:, 0:1]), [K("grow"), "msk"], [K("t1"), K("gcc")])
            S(lambda e: e.mul(out=B_["ngcc"][:], in_=B_["gcc"][:], mul=-1.0), [K("gcc")], [K("ngcc")])
            for nm, bias_k, sc, Mk in [("dT", "ngcc", 1.0, MaskT), ("dN", "gcc", -1.0, MaskN)]:
                S(lambda e, nm=nm, bias_k=bias_k, sc=sc: e.activation(out=B_[nm][:], in_=B_["grow"][:], func=AF.Identity, bias=B_[bias_k][:, 0:1], scale=sc), [K("grow"), K(bias_k)], [K(nm)])
                V(lambda e, nm=nm: e.tensor_scalar(out=B_[nm][:], in0=B_[nm][:], scalar1=0.0, scalar2=None, op0=ALU.min), [K(nm)], [K(nm)])
                S(lambda e, nm=nm: e.activation(out=B_[nm][:], in_=B_[nm][:], func=AF.Exp), [K(nm)], [K(nm)])
                V(lambda e, nm=nm, Mk=Mk: e.tensor_tensor(out=B_[nm][:], in0=B_[nm][:], in1=Mk, op=ALU.mult), [K(nm), "msk"], [K(nm)])
            V(lambda e: e.tensor_scalar(out=B_["Bd"][:], in0=ident, scalar1=beta, scalar2=None, op0=ALU.mult), [K("bg"), "msk"], [K("Bd")])
            PE(lambda e: e.matmul(rF, lhsT=ones, rhs=B_["Bd"][:], start=True, stop=True), [K("Bd"), "msk"], [kF_])
            S(lambda e: e.copy(out=B_["brow"][:], in_=rF), [kF_], [K("brow")])
            V(lambda e: e.tensor_tensor(out=B_["t1"][:], in0=Gs[:], in1=B_["dT"][:], op=ALU.mult), [K("Gs"), K("dT"), K("t1")], [K("t1")])
            V(lambda e: e.tensor_tensor(out=B_["t1"][:], in0=B_["t1"][:], in1=B_["brow"][:], op=ALU.mult), [K("t1"), K("brow")], [K("t1")])
            V(lambda e: e.tensor_tensor(out=B_["XT"][:], in0=B_["t1"][:], in1=nST, op=ALU.mult), [K("t1"), "msk"], [K("XT")])
            V(lambda e: e.tensor_tensor(out=B_["t2"][:], in0=Gs[:], in1=B_["dN"][:], op=ALU.mult), [K("Gs"), K("dN")], [K("t2")])
            V(lambda e: e.tensor_scalar(out=B_["t2"][:], in0=B_["t2"][:], scalar1=beta, scalar2=None, op0=ALU.mult), [K("t2"), K("bg")], [K("t2")])
            V(lambda e: e.tensor_tensor(out=B_["X"][:], in0=B_["t2"][:], in1=nSN, op=ALU.mult), [K("t2"), "msk"], [K("X")])
            V(lambda e: e.tensor_tensor(out=B_["TT"][:], in0=B_["XT"][:], in1=ident, op=ALU.add), [K("XT"), "msk"], [K("TT")])
            if GSTOP == 1: return
            Pc, PTc, kP, kPT = B_["X"], B_["XT"], K("X"), K("XT")
            for lv in range(1, 6):
                Pn, kPn = B_[f"P{lv % 2}"], K(f"P{lv % 2}")
                PE(lambda e, Pc=Pc, PTc=PTc: e.matmul(rA, lhsT=PTc[:], rhs=Pc[:], start=True, stop=True), [kP, kPT], [kA])
                S(lambda e, Pn=Pn: e.copy(out=Pn[:], in_=rA), [kA], [kPn])
                if lv < 5:
                    PTn, kPTn = B_[f"PT{lv % 2}"], K(f"PT{lv % 2}")
                    PE(lambda e, Pc=Pc, PTc=PTc: e.matmul(rB, lhsT=Pc[:], rhs=PTc[:], start=True, stop=True), [kP, kPT], [kB])
                    S(lambda e, PTn=PTn: e.copy(out=PTn[:], in_=rB), [kB], [kPTn])
                PE(lambda e, Pn=Pn: e.matmul(rC, lhsT=Pn[:], rhs=B_["TT"][:], start=True, stop=True), [kPn, K("TT")], [kC])
                V(lambda e: e.tensor_tensor(out=B_["TT"][:], in0=B_["TT"][:], in1=rC, op=ALU.add), [K("TT"), kC], [K("TT")])
                Pc, kP = Pn, kPn
                if lv < 5:
                    PTc, kPT = PTn, kPTn
            if GSTOP == 2: return
            S(lambda e: e.activation(out=B_["eg"][:], in_=B_["gcc"][:], func=AF.Exp), [K("gcc")], [K("eg")])
            V(lambda e: e.tensor_tensor(out=B_["be"][:], in0=B_["eg"][:], in1=beta, op=ALU.mult), [K("eg"), K("bg")], [K("be")])
            V(lambda e: e.tensor_scalar(out=B_["vb"][:], in0=vc, scalar1=beta, scalar2=None, op0=ALU.mult), [K("kqv"), K("bg")], [K("vb")])
            V(lambda e: e.tensor_scalar(out=B_["kbg"][:], in0=kc, scalar1=B_["be"][:, 0:1], scalar2=None, op0=ALU.mult), [K("kqv"), K("be")], [K("kbg")])
            PE(lambda e: e.matmul(rD, lhsT=B_["TT"][:], rhs=B_["vb"][:], start=True, stop=True), [K("TT"), K("vb")], [kD])
            S(lambda e: e.copy(out=B_["u"][:], in_=rD), [kD], [K("u")])
            PE(lambda e: e.matmul(rE, lhsT=B_["kbg"][:], rhs=B_["TT"][:], start=True, stop=True), [K("kbg"), K("TT")], [kE])
            S(lambda e: e.copy(out=B_["wF"][:], in_=rE), [kE], [K("wF")])
            V(lambda e: e.tensor_tensor(out=B_["qdF"][:], in0=qF[:], in1=B_["egrow"][:], op=ALU.mult), [K("qF"), K("egrow")], [K("qdF")])
            V(lambda e: e.tensor_tensor(out=B_["QKm"][:], in0=QKs[:], in1=B_["dT"][:], op=ALU.mult), [K("QKs"), K("dT")], [K("QKm")])
            for hs, lst in [(first, lastF), (second, lastS)]:
                S(lambda e, hs=hs, lst=lst: e.activation(out=B_["dl"][hs, :], in_=B_["gcc"][hs, :], func=AF.Exp, bias=B_["grow"][hs, lst:lst + 1], scale=-1.0), [K("gcc"), K("grow")], [K("dl")])
            V(lambda e: e.tensor_tensor(out=B_["dlA"][:], in0=B_["dl"][:], in1=selA, op=ALU.mult), [K("dl"), "msk"], [K("dlA")])
            V(lambda e: e.tensor_tensor(out=B_["dlB"][:], in0=B_["dl"][:], in1=selB, op=ALU.mult), [K("dl"), "msk"], [K("dlB")])
            V(lambda e: e.tensor_scalar(out=B_["kdA"][:], in0=kc, scalar1=B_["dlA"][:, 0:1], scalar2=None, op0=ALU.mult), [K("kqv"), K("dlA")], [K("kdA")])
            V(lambda e: e.tensor_scalar(out=B_["kdB"][:], in0=kc, scalar1=B_["dlB"][:, 0:1], scalar2=None, op0=ALU.mult), [K("kqv"), K("dlB")], [K("kdB")])
            kdF, kdS, kkF, kkS = (B_["kdA"], B_["kdB"], K("kdA"), K("kdB")) if fwd else (B_["kdB"], B_["kdA"], K("kdB"), K("kdA"))
            if GSTOP == 3: return
            Sst = B_["S"]
            for (hs, lst, kd_, kkd, rW, kW, rO, kO) in [(first, lastF, kdF, kkF, rA, kA, rC, kC), (second, lastS, kdS, kkS, rB, kB, rD, kD)]:
                PE(lambda e, rW=rW: e.matmul(rW, lhsT=B_["wF"][:], rhs=Sst[:], start=True, stop=True), [K("wF"), K("S")], [kW])
                V(lambda e, hs=hs, rW=rW: e.tensor_tensor(out=B_["vnew"][hs, :], in0=B_["u"][hs, :], in1=rW[hs, :], op=ALU.subtract), [K("u"), kW], [K("vnew")])
                PE(lambda e, rO=rO: e.matmul(rO, lhsT=B_["qdF"][:], rhs=Sst[:], start=True, stop=True), [K("qdF"), K("S")], [kO])
                S(lambda e, hs=hs, rO=rO: e.copy(out=B_["o"][hs, :], in_=rO[hs, :]), [kO], [K("o")])
                PE(lambda e, kd_=kd_: e.matmul(rF, lhsT=kd_[:], rhs=B_["vnew"][:], start=True, stop=True), [kkd, K("vnew")], [kF_])
                V(lambda e, lst=lst: e.scalar_tensor_tensor(out=Sst[:], in0=Sst[:], scalar=B_["egrow"][:, lst:lst + 1], in1=rF, op0=ALU.mult, op1=ALU.add), [K("S"), K("egrow"), kF_], [K("S")])
            if GSTOP == 5: return
            PE(lambda e: e.matmul(rE, lhsT=B_["QKm"][:], rhs=B_["vnew"][:], start=True, stop=True), [K("QKm"), K("vnew")], [kE])
            V(lambda e: e.tensor_tensor(out=B_["o"][:], in0=B_["o"][:], in1=rE, op=ALU.add), [K("o"), kE], [K("o")])
            p.dma("sync", gy_s[d, t0:t0 + 128, :], B_["o"][:], reads=[K("o")], writes=["gy_s"])

        def rwkv_block(d, c):
            t0 = c * 128
            B_ = RB[d]; K = lambda n: f"r{d}_{n}"
            bank = PS[4 + d]; kb = f"ps{4 + d}"
            rA, rB, rC, rD = (bank[:, i * 128:(i + 1) * 128] for i in range(4))
            fwd = (d == 0)
            Mtri = bdUT if fwd else bdLT
            mS_N = sbdLT if fwd else sbdUT
            mS_T = sbdUT if fwd else sbdLT
            mI_T = bdUT if fwd else bdLT
            selc = [bdLT[:, 0:1], bdUT[:, 127:128]]
            hA, hB = slice(0, 64), slice(64, 128)
            order = [(0, hA, 63), (1, hB, 127)] if fwd else [(1, hB, 64), (0, hA, 0)]
            rwt, vt = B_["rwt"], B_["vt"]
            for h in range(2):
                p.dma("sync", rwt[:, :, h * 64:(h + 1) * 64], rw_s[d, h, t0:t0 + 128, :, :], reads=["rw_s"], writes=[K("rwt")])
            p.dma("sync", vt[:], rpost_s[t0:t0 + 128, 0:128], reads=["rpost_s"], writes=[K("vt")])
            w_, kk_, nkka_, kd_, r_ = (rwt[:, i, :] for i in range(5))
            S(lambda e: e.activation(out=B_["lw"][:], in_=w_, func=AF.Ln), [K("rwt")], [K("lw")])
            PE(lambda e: e.matmul(rA, lhsT=Mtri, rhs=B_["lw"][:], start=True, stop=True), [K("lw"), "msk"], [kb])
            S(lambda e: e.copy(out=B_["lp"][:], in_=rA), [kb], [K("lp")])
            S(lambda e: e.activation(out=B_["Pt"][:], in_=rA, func=AF.Exp), [kb], [K("Pt")])
            S(lambda e: e.activation(out=B_["iP"][:], in_=rA, func=AF.Exp, scale=-1.0), [kb], [K("iP")])
            V(lambda e: e.tensor_tensor(out=B_["Pm"][:], in0=B_["lp"][:], in1=B_["lw"][:], op=ALU.subtract), [K("lp"), K("lw")], [K("Pm")])
            S(lambda e: e.activation(out=B_["Pm"][:], in_=B_["Pm"][:], func=AF.Exp), [K("Pm")], [K("Pm")])
            V(lambda e: e.tensor_tensor(out=B_["rt"][:], in0=r_, in1=B_["Pt"][:], op=ALU.mult), [K("rwt"), K("Pt")], [K("rt")])
            V(lambda e: e.tensor_tensor(out=B_["kt"][:], in0=kd_, in1=B_["iP"][:], op=ALU.mult), [K("rwt"), K("iP")], [K("kt")])
            V(lambda e: e.tensor_tensor(out=B_["nbt"][:], in0=nkka_, in1=B_["iP"][:], op=ALU.mult), [K("rwt"), K("iP")], [K("nbt")])
            V(lambda e: e.tensor_tensor(out=B_["ct"][:], in0=kk_, in1=B_["Pm"][:], op=ALU.mult), [K("rwt"), K("Pm")], [K("ct")])
            for src, full, hm in [("rt", "rtF", "rtFh"), ("nbt", "nbF", "nbFh"), ("ct", "cF", "cFh"), ("kt", None, "ktFh"), ("Pt", "PF", None)]:
                PE(lambda e, src=src: e.transpose(out=rB, in_=B_[src][:], identity=ident), [K(src), "msk"], [kb])
                if full is not None:
                    S(lambda e, full=full: e.copy(out=B_[full][:], in_=rB), [kb], [K(full)])
                if hm is not None:
                    for h, hs in [(0, hA), (1, hB)]:
                        S(lambda e, hm=hm, h=h, hs=hs: e.copy(out=B_[f"{hm}{h}"][hs, :], in_=rB[hs, :]), [kb], [K(f"{hm}{h}")])
            for ch in range(2):
                for h in range(2):
                    cs = slice(h * 64, (h + 1) * 64)
                    V(lambda e, ch=ch, h=h, cs=cs: e.tensor_scalar(out=B_[f"ktS{ch}{h}"][:, cs], in0=B_["kt"][:, cs], scalar1=selc[ch], scalar2=None, op0=ALU.mult), [K("kt"), "msk"], [K(f"ktS{ch}{h}")])
                    V(lambda e, ch=ch, h=h, cs=cs: e.tensor_scalar(out=B_[f"nbS{ch}{h}"][:, cs], in0=B_["nbt"][:, cs], scalar1=selc[ch], scalar2=None, op0=ALU.mult), [K("nbt"), "msk"], [K(f"nbS{ch}{h}")])
            def head(h):
                hr = hA if h == 0 else hB
                cFh, nbFh, ktFh, rtFh = (B_[f"{n}{h}"] for n in ["cFh", "nbFh", "ktFh", "rtFh"])
                kcF, knbF, kktF, krtF = (K(f"{n}{h}") for n in ["cFh", "nbFh", "ktFh", "rtFh"])
                for (nm, l_, kl, r__, kr, mk) in [("X", cFh, kcF, "nbF", K("nbF"), mS_N), ("XT", nbFh, knbF, "cF", K("cF"), mS_T), ("AckT", ktFh, kktF, "cF", K("cF"), mS_T),
                                                   ("ArkT", ktFh, kktF, "rtF", K("rtF"), mI_T), ("AnrbT", nbFh, knbF, "rtF", K("rtF"), mI_T)]:
                    PE(lambda e, l_=l_, r__=r__: e.matmul(rC, lhsT=l_[:], rhs=B_[r__][:], start=True, stop=True), [kl, kr], [kb])
                    V(lambda e, nm=nm, mk=mk: e.tensor_tensor(out=B_[nm][:], in0=rC, in1=mk, op=ALU.mult), [kb, "msk"], [K(nm)])
                V(lambda e: e.tensor_tensor(out=B_["TT"][:], in0=B_["XT"][:], in1=ident, op=ALU.add), [K("XT"), "msk"], [K("TT")])
                Pc, PTc, kP, kPT = B_["X"], B_["XT"], K("X"), K("XT")
                for lv in range(1, 6):
                    Pn, kPn = B_[f"P{lv % 2}"], K(f"P{lv % 2}")
                    PE(lambda e, Pc=Pc, PTc=PTc: e.matmul(rA, lhsT=PTc[:], rhs=Pc[:], start=True, stop=True), [kP, kPT], [kb])
                    S(lambda e, Pn=Pn: e.copy(out=Pn[:], in_=rA), [kb], [kPn])
                    if lv < 5:
                        PTn, kPTn = B_[f"PT{lv % 2}"], K(f"PT{lv % 2}")
                        PE(lambda e, Pc=Pc, PTc=PTc: e.matmul(rB, lhsT=Pc[:], rhs=PTc[:], start=True, stop=True), [kP, kPT], [kb])
                        S(lambda e, PTn=PTn: e.copy(out=PTn[:], in_=rB), [kb], [kPTn])
                    PE(lambda e, Pn=Pn: e.matmul(rC, lhsT=Pn[:], rhs=B_["TT"][:], start=True, stop=True), [kPn, K("TT")], [kb])
                    V(lambda e: e.tensor_tensor(out=B_["TT"][:], in0=B_["TT"][:], in1=rC, op=ALU.add), [K("TT"), kb], [K("TT")])
                    Pc, kP = Pn, kPn
                    if lv < 5:
                        PTc, kPT = PTn, kPTn
                Vh = vt[:, hr]
                H = B_["H"]
                for (ch, hs, lst) in order:
                    PE(lambda e: e.matmul(rD[:, 0:64], lhsT=cFh[:], rhs=H[:], start=True, stop=False), [kcF, K("H")], [kb])
                    PE(lambda e: e.matmul(rD[:, 0:64], lhsT=B_["AckT"][:], rhs=Vh, start=False, stop=True), [K("AckT"), K("vt")], [kb])
                    S(lambda e, hs=hs: e.copy(out=B_["Zs"][hs, :], in_=rD[hs, 0:64]), [kb], [K("Zs")])
                    PE(lambda e: e.matmul(rD[:, 64:128], lhsT=B_["TT"][:], rhs=B_["Zs"][:], start=True, stop=True), [K("TT"), K("Zs")], [kb])
                    S(lambda e, hs=hs: e.copy(out=B_["Ms"][hs, :], in_=rD[hs, 64:128]), [kb], [K("Ms")])
                    PE(lambda e: e.matmul(rA[:, 0:64], lhsT=rtFh[:], rhs=H[:], start=True, stop=True), [krtF, K("H")], [kb])
                    S(lambda e, hs=hs, hr=hr: e.copy(out=B_["Yt"][hs, hr], in_=rA[hs, 0:64]), [kb], [K("Yt")])
                    PE(lambda e, ch=ch, h=h: e.matmul(rB[:, 0:64], lhsT=B_[f"ktS{ch}{h}"][:], rhs=Vh, start=True, stop=False), [K(f"ktS{ch}{h}"), K("vt")], [kb])
                    PE(lambda e, ch=ch, h=h: e.matmul(rB[:, 0:64], lhsT=B_[f"nbS{ch}{h}"][:], rhs=B_["Ms"][:], start=False, stop=True), [K(f"nbS{ch}{h}"), K("Ms")], [kb])
                    V(lambda e, hr=hr, lst=lst: e.tensor_scalar(out=H[hr, :], in0=H[hr, :], scalar1=B_["PF"][hr, lst:lst + 1], scalar2=None, op0=ALU.mult), [K("H"), K("PF")], [K("H")])
                    V(lambda e, hr=hr, lst=lst: e.scalar_tensor_tensor(out=H[hr, :], in0=rB[hr, 0:64], scalar=B_["PF"][hr, lst:lst + 1], in1=H[hr, :], op0=ALU.mult, op1=ALU.add), [K("H"), K("PF"), kb], [K("H")])
                PE(lambda e: e.matmul(rC[:, 0:64], lhsT=B_["ArkT"][:], rhs=Vh, start=True, stop=False), [K("ArkT"), K("vt")], [kb])
                PE(lambda e: e.matmul(rC[:, 0:64], lhsT=B_["AnrbT"][:], rhs=B_["Ms"][:], start=False, stop=True), [K("AnrbT"), K("Ms")], [kb])
                V(lambda e, hr=hr: e.tensor_tensor(out=B_["Yt"][:, hr], in0=B_["Yt"][:, hr], in1=rC[:, 0:64], op=ALU.add), [K("Yt"), kb], [K("Yt")])
            for h in range(2):
                head(h)
            p.dma("sync", ry_s[d, t0:t0 + 128, :], B_["Yt"][:], reads=[K("Yt")], writes=["ry_s"])

        def ssd_chunk(d, c):
            t0 = c * 128
            Mk = UT if d == 0 else LT
            last = 127 if d == 0 else 0
            p.dma("sync", sxt[:], ssd_s[t0:t0 + 128, :], reads=["ssd_s"], writes=["sxt"])
            if d == 1:
                p.dma("sync", syf[:], sy_s[t0:t0 + 128, :], reads=["sy_s"], writes=["syf"])
                p.dma("sync", zt[:], cols_s[2 + t0:2 + t0 + 128, 1280:1536], reads=["cols_s"], writes=["zts"])
            sx = sxt[:, 0:256]; sB = sxt[:, 256:384]; sC = sxt[:, 384:512]
            dt_d = sxt[:, 512 + d * 4:516 + d * 4]; a_d = sxt[:, 520 + d * 4:524 + d * 4]
            PE(lambda e: e.transpose(out=pA[:, 0:128], in_=sB, identity=ident), ["sxt", "msk"], ["ps0"])
            PE(lambda e: e.transpose(out=pA[:, 128:256], in_=sC, identity=ident), ["sxt", "msk"], ["ps0"])
            S(lambda e: e.copy(out=BCF[:], in_=pA[:, 0:256]), ["ps0"], ["BCF"])
            if SSDSTOP == 1: return
            PE(lambda e: e.matmul(pSc[:, :128], lhsT=BCF[:, 0:128], rhs=BCF[:, 128:256], start=True, stop=True), ["BCF"], ["ps0"])
            S(lambda e: e.copy(out=scT[:], in_=pSc[:, :128]), ["ps0"], ["scT"])
            PE(lambda e: e.matmul(pAc[:, :4], lhsT=Mk, rhs=a_d, start=True, stop=True), ["sxt", "msk"], ["ps0"])
            S(lambda e: e.copy(out=acol[:], in_=pAc[:, :4]), ["ps0"], ["acol"])
            S(lambda e: e.mul(out=nacol[:], in_=pAc[:, :4], mul=-1.0), ["ps0"], ["nacol"])
            if SSDSTOP == 2: return
            for h in range(4):
                (V if os.environ.get('A_V1') else G)(lambda e, h=h: e.tensor_scalar(out=Rm[:, h, :], in0=Mk, scalar1=a_d[:, h:h + 1], scalar2=None, op0=ALU.mult), ["sxt", "msk"], ["Rm"])
            PE(lambda e: e.matmul(pRow[:, :512], lhsT=ones, rhs=Rm[:].rearrange("p h n -> p (h n)"), start=True, stop=True), ["Rm", "msk"], ["ps1"])
            S(lambda e: e.activation(out=E[:].rearrange("p h n -> p (h n)"), in_=pRow[:, :512], func=AF.Exp), ["ps1"], ["E"])
            for h in range(4):
                S(lambda e, h=h: e.activation(out=dend[:, h:h + 1], in_=pRow[:, h * 128 + last:h * 128 + last + 1], func=AF.Exp, bias=nacol[:, h:h + 1], scale=1.0), ["ps1", "nacol"], ["dend"])
            if SSDSTOP == 3: return
            for h in range(4):
                hs = slice(h * 64, (h + 1) * 64)
                S(lambda e, h=h: e.activation(out=Dm[:], in_=pRow[:, h * 128:(h + 1) * 128], func=AF.Identity, bias=nacol[:, h:h + 1], scale=1.0), ["ps1", "nacol"], ["Dm"])
                V(lambda e: e.tensor_scalar(out=Dm[:], in0=Dm[:], scalar1=0.0, scalar2=None, op0=ALU.min), ["Dm"], ["Dm"])
                S(lambda e: e.activation(out=Dm[:], in_=Dm[:], func=AF.Exp), ["Dm"], ["Dm"])
                V(lambda e: e.tensor_tensor(out=Dm[:], in0=Dm[:], in1=Mk, op=ALU.mult), ["Dm", "msk"], ["Dm"])
                V(lambda e, h=h: e.tensor_tensor(out=M[:, h, :], in0=Dm[:], in1=scT[:], op=ALU.mult), ["Dm", "scT"], ["M"])
                V(lambda e, h=h: e.tensor_tensor(out=CdF[:, h, :], in0=E[:, h, :], in1=BCF[:, 128:256], op=ALU.mult), ["E", "BCF"], ["CdF"])
                V(lambda e, h=h, hs=hs: e.tensor_scalar(out=xdt[:, hs], in0=sx[:, hs], scalar1=dt_d[:, h:h + 1], scalar2=None, op0=ALU.mult), ["sxt"], ["xdt"])
                V(lambda e, h=h, hs=hs: e.tensor_scalar(out=xend[:, hs], in0=xdt[:, hs], scalar1=dend[:, h:h + 1], scalar2=None, op0=ALU.mult), ["xdt", "dend"], ["xend"])
            if SSDSTOP == 4: return
            for h in range(4):
                hs = slice(h * 64, (h + 1) * 64)
                PE(lambda e, h=h, hs=hs: e.matmul(pY[:, hs], lhsT=M[:, h, :], rhs=xdt[:, hs], start=True, stop=False), ["M", "xdt"], ["ps2"])
                PE(lambda e, h=h, hs=hs: e.matmul(pY[:, hs], lhsT=CdF[:, h, :], rhs=H[:, hs], start=False, stop=True), ["CdF", "H"], ["ps2"])
            PE(lambda e: e.matmul(pSt[:, :256], lhsT=sB, rhs=xend[:], start=True, stop=True), ["sxt", "xend"], ["ps2"])
            if SSDSTOP == 5: return
            for h in range(4):
                hs = slice(h * 64, (h + 1) * 64)
                V(lambda e, h=h, hs=hs: e.scalar_tensor_tensor(out=H[:, hs], in0=H[:, hs], scalar=E[:, h, last:last + 1], in1=pSt[:, hs], op0=ALU.mult, op1=ALU.add), ["H", "E", "ps2"], ["H"])
            if SSDSTOP == 6: return
            if d == 0:
                S(lambda e: e.copy(out=yt[:], in_=pY[:, :256]), ["ps2"], ["yt"])
                p.dma("sync", sy_s[t0:t0 + 128, :], yt[:], reads=["yt"], writes=["sy_s"])
            else:
                V(lambda e: e.tensor_tensor(out=yt[:], in0=pY[:, :256], in1=syf[:], op=ALU.add), ["ps2", "syf"], ["yt"])
                V(lambda e: e.tensor_tensor(out=xd[:], in0=sx, in1=rv("dsk"), op=ALU.mult), ["sxt", "rvs"], ["xd"])
                V(lambda e: e.tensor_tensor(out=yt[:], in0=yt[:], in1=xd[:], op=ALU.add), ["yt", "xd"], ["yt"])
                S(lambda e: e.activation(out=szs[:], in_=zt[:], func=AF.Silu), ["zts"], ["szs"])
                V(lambda e: e.tensor_tensor(out=yt[:], in0=yt[:], in1=szs[:], op=ALU.mult), ["yt", "szs"], ["yt"])
                p.dma("sync", yc_o[t0:t0 + 128, :], yt[:], reads=["yt"], writes=["yc"])

        ssd_list = [(0, c) for c in range(NTL)] + [(1, c) for c in range(NTL - 1, -1, -1)]
        V(lambda e: e.memset(H[:], 0.0), [], ["H"])
        def ssd_pair(it):
            for si in (2 * it, 2 * it + 1):
                dd, cc = ssd_list[si]
                if dd == 1 and cc == NTL - 1:
                    V(lambda e: e.memset(H[:], 0.0), [], ["H"])
                ssd_chunk(dd, cc)
        lists = [[] for _ in range(5)]
        for it in range(NTL):
            if not os.environ.get("A_NOGDN"):
                lists[0] += p.capture(gdn_chunk, 0, it); lists[1] += p.capture(gdn_chunk, 1, NTL - 1 - it)
            if not os.environ.get("A_NORWKV"):
                lists[2] += p.capture(rwkv_block, 0, it); lists[3] += p.capture(rwkv_block, 1, NTL - 1 - it)
            if not os.environ.get("A_NOSSD"):
                lists[4] += p.capture(ssd_pair, it)
        p.emit_balanced(lists)
    p.barrier()


def phase4(p, T, L):
    nc = p.nc
    V = L["V"]; G = L["G"]; S = L["S"]; PE = L["PE"]; PS = L["PS"]; ident = L["ident"]; rv = L["rv"]
    gy_s, ry_s, cols_s, rpost_s, ya_o, yb_o = (L[k] for k in ["gy_s", "ry_s", "cols_s", "rpost_s", "ya_o", "yb_o"])
    NTL = T // 128
    with contextlib.ExitStack() as sc:
        sb = lambda name, shape, dt=F32: sc.enter_context(nc.sbuf_tensor("s_" + name, list(shape), dt))
        g0 = sb("g0", [128, 128]); g1 = sb("g1", [128, 128]); o = sb("o", [128, 128]); jk = sb("jk", [128, 128]); ss = sb("ss", [128, 1])
        zt = sb("zt4", [128, 128]); ya = sb("ya_t", [128, 128])
        r0 = sb("r0", [128, 128]); r1 = sb("r1", [128, 128]); y = sb("y4", [128, 128]); st = sb("st4", [128, 2]); sq = sb("sq4", [128, 128]); vr = sb("vr4", [128, 2])
        rp = sb("rp4", [128, 258]); yb = sb("yb_t", [128, 128])
        pT, pU = PS[0], PS[1]
        for tt in range(NTL):
            t0 = tt * 128
            p.dma("sync", g0[:], gy_s[0, t0:t0 + 128, :], reads=["gy_s"], writes=["g0"])
            p.dma("sync", g1[:], gy_s[1, t0:t0 + 128, :], reads=["gy_s"], writes=["g1"])
            p.dma("scalar", zt[:], cols_s[2 + t0:2 + t0 + 128, 384:512], reads=["cols_s"], writes=["zt4"])
            p.dma("sync", r0[:], ry_s[0, t0:t0 + 128, :], reads=["ry_s"], writes=["r0"])
            p.dma("sync", r1[:], ry_s[1, t0:t0 + 128, :], reads=["ry_s"], writes=["r1"])
            p.dma("scalar", rp[:], rpost_s[t0:t0 + 128, :], reads=["rpost_s"], writes=["rp4"])
            V(lambda e: e.tensor_tensor(out=o[:], in0=g0[:], in1=g1[:], op=ALU.add), ["g0", "g1"], ["o"])
            V(lambda e: e.scalar_tensor_tensor(out=jk[:], in0=o[:], scalar=1.0, in1=o[:], op0=ALU.mult, op1=ALU.mult, accum_out=ss[:, 0:1]), ["o"], ["jk", "ss"])
            V(lambda e: e.tensor_scalar(out=ss[:], in0=ss[:], scalar1=1.0 / 128, scalar2=1e-6, op0=ALU.mult, op1=ALU.add), ["ss"], ["ss"])
            S(lambda e: e.activation(out=ss[:], in_=ss[:], func=AF.Sqrt), ["ss"], ["ss"])
            V(lambda e: e.reciprocal(out=ss[:], in_=ss[:]), ["ss"], ["ss"])
            V(lambda e: e.scalar_tensor_tensor(out=o[:], in0=o[:], scalar=ss[:, 0:1], in1=rv("gnorm"), op0=ALU.mult, op1=ALU.mult), ["o", "ss", "rvs"], ["o"])
            S(lambda e: e.activation(out=zt[:], in_=zt[:], func=AF.Silu), ["zt4"], ["zt4"])
            V(lambda e: e.tensor_tensor(out=ya[:], in0=o[:], in1=zt[:], op=ALU.mult), ["o", "zt4"], ["ya_t"])
            p.dma("sync", ya_o[t0:t0 + 128, :], ya[:], reads=["ya_t"], writes=["ya"])
            V(lambda e: e.tensor_tensor(out=y[:], in0=r0[:], in1=r1[:], op=ALU.add), ["r0", "r1"], ["y4"])
            V(lambda e: e.tensor_reduce(out=st[:], in_=y[:].rearrange("p (h n) -> p h n", h=2), axis=AX.X, op=ALU.add), ["y4"], ["st4"])
            V(lambda e: e.tensor_scalar(out=st[:], in0=st[:], scalar1=1.0 / 64, scalar2=None, op0=ALU.mult), ["st4"], ["st4"])
            for h in range(2):
                hs = slice(h * 64, (h + 1) * 64)
                V(lambda e, h=h, hs=hs: e.tensor_scalar(out=y[:, hs], in0=y[:, hs], scalar1=st[:, h:h + 1], scalar2=None, op0=ALU.subtract), ["y4", "st4"], ["y4"])
            S(lambda e: e.activation(out=sq[:], in_=y[:], func=AF.Square), ["y4"], ["sq4"])
            V(lambda e: e.tensor_reduce(out=vr[:], in_=sq[:].rearrange("p (h n) -> p h n", h=2), axis=AX.X, op=ALU.add), ["sq4"], ["vr4"])
            V(lambda e: e.tensor_scalar(out=vr[:], in0=vr[:], scalar1=1.0 / 64, scalar2=64e-5, op0=ALU.mult, op1=ALU.add), ["vr4"], ["vr4"])
            S(lambda e: e.activation(out=vr[:], in_=vr[:], func=AF.Sqrt), ["vr4"], ["vr4"])
            V(lambda e: e.reciprocal(out=vr[:], in_=vr[:]), ["vr4"], ["vr4"])
            for h in range(2):
                hs = slice(h * 64, (h + 1) * 64)
                V(lambda e, h=h, hs=hs: e.tensor_scalar(out=y[:, hs], in0=y[:, hs], scalar1=vr[:, h:h + 1], scalar2=None, op0=ALU.mult), ["y4", "vr4"], ["y4"])
            V(lambda e: e.tensor_tensor(out=y[:], in0=y[:], in1=rv("lng"), op=ALU.mult), ["y4", "rvs"], ["y4"])
            V(lambda e: e.tensor_tensor(out=y[:], in0=y[:], in1=rv("lnb"), op=ALU.add), ["y4", "rvs"], ["y4"])
            for h in range(2):
                hs = slice(h * 64, (h + 1) * 64)
                V(lambda e, h=h, hs=hs: e.scalar_tensor_tensor(out=y[:, hs], in0=rp[:, hs], scalar=rp[:, 256 + h:257 + h], in1=y[:, hs], op0=ALU.mult, op1=ALU.add), ["y4", "rp4"], ["y4"])
            V(lambda e: e.tensor_tensor(out=yb[:], in0=y[:], in1=rp[:, 128:256], op=ALU.mult), ["y4", "rp4"], ["yb_t"])
            p.dma("sync", yb_o[t0:t0 + 128, :], yb[:], reads=["yb_t"], writes=["yb"])


ALPHA = 4 ** 0.25
NTOK = 2048
T1 = 256
T2 = 512
NE = 32


def ln_fm(p, h, hk, nch, NT, gcol, bcol, ones_mean, sq, pS, pQ, tmp, kS, kQ, eps=1e-5):
    for oc in range(nch):
        p.op("tensor", lambda e, oc=oc: e.matmul(pS[:, :NT], lhsT=ones_mean[:], rhs=h[:, oc, :], start=(oc == 0), stop=(oc == nch - 1)),
             reads=[hk, "ones_mean"], writes=[kS])
    sqs = (lambda s_: sq[s_][:]) if isinstance(sq, (list, tuple)) else (lambda s_: sq[:, s_, :])
    for oc in range(nch):
        s = oc % 2
        p.op("scalar", lambda e, oc=oc, s=s: e.activation(out=sqs(s), in_=h[:, oc, :], func=AF.Square), reads=[hk], writes=[f"sq{s}"])
        p.op("tensor", lambda e, oc=oc, s=s: e.matmul(pQ[:, :NT], lhsT=ones_mean[:], rhs=sqs(s), start=(oc == 0), stop=(oc == nch - 1)),
             reads=[f"sq{s}", "ones_mean"], writes=[kQ])
    mean, rstd, t = tmp["mean"], tmp["rstd"], tmp["t"]
    p.op("scalar", lambda e: e.copy(out=mean[:], in_=pS[:, :NT]), reads=[kS], writes=["ln_mean"])
    p.op("vector", lambda e: e.tensor_tensor(out=t[:], in0=mean[:], in1=mean[:], op=ALU.mult), reads=["ln_mean"], writes=["ln_t"])
    p.op("vector", lambda e: e.tensor_tensor(out=t[:], in0=pQ[:, :NT], in1=t[:], op=ALU.subtract), reads=[kQ, "ln_t"], writes=["ln_t"])
    p.op("vector", lambda e: e.tensor_scalar(out=t[:], in0=t[:], scalar1=eps, scalar2=None, op0=ALU.add), reads=["ln_t"], writes=["ln_t"])
    p.op("scalar", lambda e: e.activation(out=t[:], in_=t[:], func=AF.Sqrt), reads=["ln_t"], writes=["ln_t"])
    p.op("vector", lambda e: e.reciprocal(out=rstd[:], in_=t[:]), reads=["ln_t"], writes=["ln_rstd"])
    for oc in range(nch):
        p.op("vector", lambda e, oc=oc: e.tensor_tensor(out=h[:, oc, :], in0=h[:, oc, :], in1=mean[:], op=ALU.subtract), reads=[hk, "ln_mean"], writes=[hk])
        p.op("vector", lambda e, oc=oc: e.tensor_tensor(out=h[:, oc, :], in0=h[:, oc, :], in1=rstd[:], op=ALU.mult), reads=[hk, "ln_rstd"], writes=[hk])
        p.op("scalar", lambda e, oc=oc: e.activation(out=h[:, oc, :], in_=h[:, oc, :], func=AF.Identity, scale=gcol(oc), bias=bcol(oc)),
             reads=[hk, "vec"], writes=[hk])


def build_B(ntok=NTOK, ne=NE, dump=False):
    p = Prog(); nc = p.nc
    D = 1024
    xT = p.dram("xT", [D, ntok]); yT = p.dram("yT", [2048, ntok]); pT = p.dram("pT", [256, ntok])
    wg = p.dram("wg", [D, 3072]); wb = p.dram("wb", [2048, D]); wo = p.dram("wo", [D, D]); wplg = p.dram("wplg", [D, D])
    wpl = p.dram("wpl", [256, D]); wr = p.dram("wr", [D, 32]); br = p.dram("br", [1, 32])
    wgu = p.dram("wgu", [32, D, 2048]); wd = p.dram("wd", [32, D, D])
    bgu = p.dram("bgu", [128, 32, 16]); bd = p.dram("bd", [32, D]); vec = p.dram("vec", [128, 5, 8]); ident_d = p.dram("ident", [128, 128])
    outT = p.dram("outT", [D, ntok], kind="ExternalOutput")
    x1bf_s = p.dram("x1bf_s", [128, 8, ntok], BF16, kind="Internal")
    acc_s = p.dram("acc_s", [128, 8, ntok], F32, kind="Internal")
    if dump:
        x1_d = p.dram("x1_d", [D, ntok], kind="ExternalOutput")
        gate_d = p.dram("gate_d", [32, ntok], kind="ExternalOutput")

    ident = p.sb("ident", [128, 128]); ones_mean = p.sb("ones_mean", [128, 128]); vecs = p.sb("vecs", [128, 5, 8])
    gateT = p.sb("gateT", [32, ntok]); ones_row = p.sb("ones_row", [1, 128]); brow = p.sb("brow", [1, 32])
    PS = [p.ps(f"ps{i}", [128, 512]) for i in range(8)]
    p.dma("sync", ident[:], ident_d[:, :], writes=["ident"])
    p.dma("sync", vecs[:], vec[:, :, :], writes=["vec"])
    p.dma("sync", brow[:], br[:, :], writes=["brow"])
    p.op("vector", lambda e: e.memset(ones_mean[:], 1.0 / 1024), writes=["ones_mean"])
    p.op("vector", lambda e: e.memset(ones_row[:], 1.0), writes=["ones_row"])

    with contextlib.ExitStack() as sc:
        def sb(name, shape, dt=F32):
            return sc.enter_context(nc.sbuf_tensor("s_" + name, list(shape), dt))
        wg_bf = sb("wg_bf", [128, 8, 3072], BF16); wb_bf = sb("wb_bf", [128, 16, 1024], BF16)
        wo_bf = sb("wo_bf", [128, 8, 1024], BF16); wplg_bf = sb("wplg_bf", [128, 8, 1024], BF16); wpl_bf = sb("wpl_bf", [128, 2, 1024], BF16)
        wr_sb = sb("wr_sb", [128, 8, 32]); ones512 = sb("ones512", [128, 128])
        for kc in range(8):
            p.dma("gpsimd", wg_bf[:, kc, :], wg[kc * 128:(kc + 1) * 128, :], writes=["wg_bf"])
            p.dma("gpsimd", wo_bf[:, kc, :], wo[kc * 128:(kc + 1) * 128, :], writes=["wo_bf"])
            p.dma("gpsimd", wplg_bf[:, kc, :], wplg[kc * 128:(kc + 1) * 128, :], writes=["wplg_bf"])
            p.dma("sync", wr_sb[:, kc, :], wr[kc * 128:(kc + 1) * 128, :], writes=["wr_sb"])
        for kc in range(16):
            p.dma("gpsimd", wb_bf[:, kc, :], wb[kc * 128:(kc + 1) * 128, :], writes=["wb_bf"])
        for kc in range(2):
            p.dma("gpsimd", wpl_bf[:, kc, :], wpl[kc * 128:(kc + 1) * 128, :], writes=["wpl_bf"])
        p.op("vector", lambda e: e.memset(ones512[:], 1.0 / 512), writes=["ones512"])
        xf = sb("xf", [128, 8, T1]); x_bf = sb("x_bf", [128, 8, T1], BF16); uf = sb("uf", [128, 8, T1])
        y_bf = sb("y_bf", [128, 16, T1], BF16); m_bf = sb("m_bf", [128, 8, T1], BF16); sq = sb("sq", [128, 2, T1])
        x1b = sb("x1b", [128, 8, T1], BF16); accb = sb("accb", [128, 8, T1])
        pf = sb("pf", [128, 2, T1], BF16)
        tm = {k: sb("tm_" + k, [128, T1]) for k in ["mean", "rstd", "t", "g", "mf", "t2", "rs"]}
        lg = sb("lg", [128, 32]); top8 = sb("top8", [128, 8]); nmx = sb("nmx", [128, 1]); msk = sb("msk", [128, 32])
        ex = sb("ex", [128, 32]); ssum = sb("ssum", [128, 1]); gt = sb("gt", [128, 32])
        pG, pB, pH, pS, pQ, pR, pT_, pP = PS
        xTv = xT.rearrange("(kc p) n -> p kc n", p=128); yTv = yT.rearrange("(kc p) n -> p kc n", p=128)
        pTv = pT.rearrange("(kc p) n -> p kc n", p=128)
        for t in range(ntok // T1):
            o = t * T1
            p.dma("sync", xf[:], xTv[:, :, o:o + T1], writes=["xf"])
            p.dma("gpsimd", x_bf[:], xTv[:, :, o:o + T1], writes=["x_bf"])
            p.dma("gpsimd", y_bf[:, 0:8, :], yTv[:, 0:8, o:o + T1], writes=["y_bf_a"])
            p.dma("sync", uf[:], yTv[:, 8:16, o:o + T1], writes=["uf"])
            p.dma("gpsimd", pf[:], pTv[:, :, o:o + T1], writes=["pf"])
            for g in range(2):
                for c in range(4):
                    cc = g * 4 + c; s = cc % 2
                    p.op("scalar", lambda e, cc=cc, s=s: e.activation(out=sq[:, s, :], in_=uf[:, cc, :], func=AF.Square), reads=["uf"], writes=[f"sq{s}"])
                    p.op("tensor", lambda e, c=c, s=s: e.matmul(pS[:, :T1], lhsT=ones512[:], rhs=sq[:, s, :], start=(c == 0), stop=(c == 3)),
                         reads=[f"sq{s}", "ones512"], writes=["pS"])
                rs = tm["rs"]
                p.op("vector", lambda e: e.tensor_scalar(out=rs[:], in0=pS[:, :T1], scalar1=1e-5, scalar2=None, op0=ALU.add), reads=["pS"], writes=["rs"])
                p.op("scalar", lambda e: e.activation(out=rs[:], in_=rs[:], func=AF.Sqrt), reads=["rs"], writes=["rs"])
                p.op("vector", lambda e: e.reciprocal(out=rs[:], in_=rs[:]), reads=["rs"], writes=["rs"])
                for c in range(4):
                    cc = g * 4 + c
                    p.op("vector", lambda e, cc=cc: e.scalar_tensor_tensor(out=y_bf[:, 8 + cc, :], in0=uf[:, cc, :], scalar=vecs[:, 4, cc:cc + 1], in1=rs[:], op0=ALU.mult, op1=ALU.mult),
                         reads=["uf", "rs", "vec"], writes=["y_bf_u"])
            brk = [(0, 4), (4, 8), (8, 16)]
            for oc in range(8):
                for b in range(3):
                    c0 = b * 1024 + oc * 128
                    for kc in range(8):
                        p.op("tensor", lambda e, kc=kc, c0=c0: e.matmul(pG[:, :T1], lhsT=wg_bf[:, kc, c0:c0 + 128], rhs=x_bf[:, kc, :], start=(kc == 0), stop=(kc == 7)),
                             reads=["wg_bf", "x_bf"], writes=["pG"])
                    p.op("scalar", lambda e: e.activation(out=tm["g"][:], in_=pG[:, :T1], func=AF.Sigmoid), reads=["pG"], writes=["tm_g"])
                    k0, k1 = brk[b]
                    for kc in range(k0, k1):
                        p.op("tensor", lambda e, kc=kc, k0=k0, k1=k1: e.matmul(pB[:, :T1], lhsT=wb_bf[:, kc, oc * 128:(oc + 1) * 128], rhs=y_bf[:, kc, :], start=(kc == k0), stop=(kc == k1 - 1)),
                             reads=["wb_bf", "y_bf_a", "y_bf_u"], writes=["pB"])
                    if b == 0:
                        p.op("vector", lambda e: e.tensor_tensor(out=tm["mf"][:], in0=tm["g"][:], in1=pB[:, :T1], op=ALU.mult), reads=["tm_g", "pB"], writes=["tm_mf"])
                    else:
                        p.op("vector", lambda e: e.tensor_tensor(out=tm["t2"][:], in0=tm["g"][:], in1=pB[:, :T1], op=ALU.mult), reads=["tm_g", "pB"], writes=["tm_t2"])
                        if b == 1:
                            p.op("vector", lambda e: e.tensor_tensor(out=tm["mf"][:], in0=tm["mf"][:], in1=tm["t2"][:], op=ALU.add), reads=["tm_mf", "tm_t2"], writes=["tm_mf"])
                        else:
                            p.op("vector", lambda e, oc=oc: e.tensor_tensor(out=m_bf[:, oc, :], in0=tm["mf"][:], in1=tm["t2"][:], op=ALU.add), reads=["tm_mf", "tm_t2"], writes=["m_bf"])
            for oc in range(8):
                for kc in range(8):
                    p.op("tensor", lambda e, kc=kc, oc=oc: e.matmul(pH[:, :T1], lhsT=wo_bf[:, kc, oc * 128:(oc + 1) * 128], rhs=m_bf[:, kc, :], start=(kc == 0), stop=(kc == 7)),
                         reads=["wo_bf", "m_bf"], writes=["pH"])
                p.op("vector", lambda e, oc=oc: e.scalar_tensor_tensor(out=xf[:, oc, :], in0=xf[:, oc, :], scalar=ALPHA, in1=pH[:, :T1], op0=ALU.mult, op1=ALU.add),
                     reads=["xf", "pH"], writes=["xf"])
            ln_fm(p, xf, "xf", 8, T1, lambda oc: vecs[:, 0, oc:oc + 1], lambda oc: vecs[:, 1, oc:oc + 1], ones_mean, sq, pS, pQ, tm, "pS", "pQ")
            p.op("scalar", lambda e: e.copy(out=x1b[:], in_=xf[:]), reads=["xf"], writes=["x1b"])
            p.dma("sync", x1bf_s[:, :, o:o + T1], x1b[:], reads=["x1b"], writes=["x1bf_s"])
            if dump:
                p.dma("sync", x1_d.rearrange("(kc p) n -> p kc n", p=128)[:, :, o:o + T1], xf[:], reads=["xf"], writes=["x1_d"])
            for s in range(T1 // 128):
                for kc in range(8):
                    p.op("tensor", lambda e, kc=kc, s=s: e.matmul(pR[:, :32], lhsT=xf[:, kc, s * 128:(s + 1) * 128], rhs=wr_sb[:, kc, :], start=(kc == 0), stop=False),
                         reads=["xf", "wr_sb"], writes=["pR"])
                p.op("tensor", lambda e: e.matmul(pR[:, :32], lhsT=ones_row[:, :], rhs=brow[:, :], start=False, stop=True), reads=["ones_row", "brow"], writes=["pR"])
                p.op("vector", lambda e: e.tensor_copy(out=lg[:], in_=pR[:, :32]), reads=["pR"], writes=["lg"])
                p.op("vector", lambda e: e.max(out=top8[:], in_=lg[:]), reads=["lg"], writes=["top8"])
                p.op("vector", lambda e: e.tensor_scalar(out=nmx[:], in0=top8[:, 0:1], scalar1=-1.0, scalar2=None, op0=ALU.mult), reads=["top8"], writes=["nmx"])
                p.op("vector", lambda e: e.tensor_scalar(out=msk[:], in0=lg[:], scalar1=top8[:, 3:4], scalar2=None, op0=ALU.is_ge), reads=["lg", "top8"], writes=["msk"])
                p.op("scalar", lambda e: e.activation(out=ex[:], in_=lg[:], func=AF.Exp, bias=nmx[:, 0:1], scale=1.0), reads=["lg", "nmx"], writes=["ex"])
                p.op("vector", lambda e: e.scalar_tensor_tensor(out=ex[:], in0=ex[:], scalar=1.0, in1=msk[:], op0=ALU.mult, op1=ALU.mult, accum_out=ssum[:, 0:1]),
                     reads=["ex", "msk"], writes=["ex", "ssum"])
                p.op("vector", lambda e: e.reciprocal(out=ssum[:], in_=ssum[:]), reads=["ssum"], writes=["ssum"])
                p.op("vector", lambda e: e.tensor_scalar(out=gt[:], in0=ex[:], scalar1=ssum[:, 0:1], scalar2=None, op0=ALU.mult), reads=["ex", "ssum"], writes=["gt"])
                p.op("tensor", lambda e: e.transpose(out=pT_[:32, :128], in_=gt[:], identity=ident[:]), reads=["gt", "ident"], writes=["pT"])
                oo = o + s * 128
                p.op("scalar", lambda e, oo=oo: e.copy(out=gateT[:, oo:oo + 128], in_=pT_[:32, :128]), reads=["pT"], writes=["gateT"])
            for oc in range(8):
                for kc in range(2):
                    p.op("tensor", lambda e, kc=kc, oc=oc: e.matmul(pP[:, :T1], lhsT=wpl_bf[:, kc, oc * 128:(oc + 1) * 128], rhs=pf[:, kc, :], start=(kc == 0), stop=(kc == 1)),
                         reads=["wpl_bf", "pf"], writes=["pP"])
                for kc in range(8):
                    p.op("tensor", lambda e, kc=kc, oc=oc: e.matmul(pG[:, :T1], lhsT=wplg_bf[:, kc, oc * 128:(oc + 1) * 128], rhs=x1b[:, kc, :], start=(kc == 0), stop=(kc == 7)),
                         reads=["wplg_bf", "x1b"], writes=["pG"])
                p.op("scalar", lambda e: e.activation(out=tm["g"][:], in_=pG[:, :T1], func=AF.Sigmoid), reads=["pG"], writes=["tm_g"])
                p.op("vector", lambda e: e.tensor_tensor(out=tm["t2"][:], in0=tm["g"][:], in1=pP[:, :T1], op=ALU.mult), reads=["tm_g", "pP"], writes=["tm_t2"])
                p.op("vector", lambda e, oc=oc: e.scalar_tensor_tensor(out=accb[:, oc, :], in0=xf[:, oc, :], scalar=ALPHA, in1=tm["t2"][:], op0=ALU.mult, op1=ALU.add),
                     reads=["xf", "tm_t2"], writes=["accb"])
            p.dma("sync", acc_s[:, :, o:o + T1], accb[:], reads=["accb"], writes=["acc_s"])
    if dump:
        p.dma("sync", gate_d[:, :], gateT[:], reads=["gateT"], writes=["gate_d"])
    p.barrier()

    H = ntok // 2
    with contextlib.ExitStack() as sc:
        def sb(name, shape, dt=F32):
            return sc.enter_context(nc.sbuf_tensor("s_" + name, list(shape), dt))
        x1h = sb("x1h", [128, 8, H], BF16); acc = sb("acc", [128, 8, H])
        wgu_b = [sb(f"wgu_b{i}", [128, 8, 2048], BF16) for i in range(2)]
        wd_b = [sb(f"wd_b{i}", [128, 8, 1024], BF16) for i in range(2)]
        bgu_sb = sb("bgu_sb", [128, 32, 16]); bd_sb = sb("bd_sb", [32, 1024]); ones32 = sb("ones32", [32, 128])
        act = [sb(f"act{c}", [128, 8, T2], BF16) for c in range(2)]; gbc = [sb(f"gbc{c}", [128, T2]) for c in range(2)]; gm = [sb(f"gm{c}", [32, T2]) for c in range(2)]
        glu = [sb(f"glu{c}", [128, T2]) for c in range(2)]; up1 = [sb(f"up1{c}", [128, T2]) for c in range(2)]; sg = [sb(f"sg{c}", [128, T2]) for c in range(2)]
        t1 = [sb(f"t1{c}", [128, T2]) for c in range(2)]; t2 = [sb(f"t2{c}", [128, T2]) for c in range(2)]
        sq = [t1[0], t2[0]]; tm = {"mean": glu[0], "rstd": up1[0], "t": sg[0]}
        print('moe sbuf remaining', nc.sbuf_bytes_remaining, flush=True)
        p.dma("sync", bgu_sb[:], bgu[:, :, :], writes=["bgu_sb"])
        p.dma("sync", bd_sb[:], bd[:, :], writes=["bd_sb"])
        p.op("vector", lambda e: e.memset(ones32[:], 1.0), writes=["ones32"])
        pBC = PS[6]; pS = PS[7]; pQ = PS[6]
        outv = outT.rearrange("(kc p) n -> p kc n", p=128)

        def moe_tile(ex_, tt, ho, wgs, wds, wb_i):
            c = tt
            pGl, pUp, pD = PS[3 * c], PS[3 * c + 1], PS[3 * c + 2]
            kGl, kUp, kD = f"pGl{c}", f"pUp{c}", f"pD{c}"
            to = tt * T2
            p.op("vector", lambda e: e.tensor_scalar(out=gm[c][:], in0=gateT[:, ho + to:ho + to + T2], scalar1=ident[0:32, ex_:ex_ + 1], scalar2=None, op0=ALU.mult),
                 reads=["gateT", "ident"], writes=[f"gm{c}"])
            pBCc, kBC = (PS[6], "pBC") if c == 0 else (PS[7], "pS7")
            p.op("tensor", lambda e: e.matmul(pBCc[:, :T2], lhsT=ones32[:], rhs=gm[c][:], start=True, stop=True), reads=["ones32", f"gm{c}"], writes=[kBC])
            p.op("scalar", lambda e: e.copy(out=gbc[c][:], in_=pBCc[:, :T2]), reads=[kBC], writes=[f"gbc{c}"])
            for oc in range(8):
                for kc in range(8):
                    p.op("tensor", lambda e, kc=kc, oc=oc: e.matmul(pGl[:, :T2], lhsT=wgs[:, kc, oc * 128:(oc + 1) * 128], rhs=x1h[:, kc, to:to + T2], start=(kc == 0), stop=(kc == 7)),
                         reads=[f"wgu{wb_i}", "x1h"], writes=[kGl])
                for kc in range(8):
                    p.op("tensor", lambda e, kc=kc, oc=oc: e.matmul(pUp[:, :T2], lhsT=wgs[:, kc, 1024 + oc * 128:1024 + (oc + 1) * 128], rhs=x1h[:, kc, to:to + T2], start=(kc == 0), stop=(kc == 7)),
                         reads=[f"wgu{wb_i}", "x1h"], writes=[kUp])
                p.op("vector", lambda e, oc=oc: e.tensor_scalar(out=glu[c][:], in0=pGl[:, :T2], scalar1=bgu_sb[:, ex_, oc:oc + 1], scalar2=7.0, op0=ALU.add, op1=ALU.min),
                     reads=[kGl, "bgu_sb"], writes=[f"glu{c}"])
                p.op("vector", lambda e, oc=oc: e.tensor_scalar(out=up1[c][:], in0=pUp[:, :T2], scalar1=bgu_sb[:, ex_, 8 + oc:9 + oc], scalar2=7.0, op0=ALU.add, op1=ALU.min),
                     reads=[kUp, "bgu_sb"], writes=[f"up1{c}"])
                p.op("vector", lambda e: e.tensor_scalar(out=up1[c][:], in0=up1[c][:], scalar1=-7.0, scalar2=1.0, op0=ALU.max, op1=ALU.add), reads=[f"up1{c}"], writes=[f"up1{c}"])
                p.op("scalar", lambda e: e.activation(out=sg[c][:], in_=glu[c][:], func=AF.Sigmoid, scale=1.702), reads=[f"glu{c}"], writes=[f"sg{c}"])
                p.op("vector", lambda e: e.tensor_tensor(out=t2[c][:], in0=up1[c][:], in1=gbc[c][:], op=ALU.mult), reads=[f"up1{c}", f"gbc{c}"], writes=[f"t2{c}"])
                p.op("vector", lambda e: e.tensor_tensor(out=t1[c][:], in0=glu[c][:], in1=sg[c][:], op=ALU.mult), reads=[f"glu{c}", f"sg{c}"], writes=[f"t1{c}"])
                p.op("vector", lambda e, oc=oc: e.tensor_tensor(out=act[c][:, oc, :], in0=t1[c][:], in1=t2[c][:], op=ALU.mult), reads=[f"t1{c}", f"t2{c}"], writes=[f"act{c}"])
            for oc in range(8):
                for kc in range(8):
                    p.op("tensor", lambda e, kc=kc, oc=oc: e.matmul(pD[:, :T2], lhsT=wds[:, kc, oc * 128:(oc + 1) * 128], rhs=act[c][:, kc, :], start=(kc == 0), stop=(kc == 7)),
                         reads=[f"wd{wb_i}", f"act{c}"], writes=[kD])
                p.op("vector", lambda e, oc=oc: e.tensor_tensor(out=acc[:, oc, to:to + T2], in0=acc[:, oc, to:to + T2], in1=pD[:, :T2], op=ALU.add),
                     reads=[f"acc{c}", kD], writes=[f"acc{c}"])

        for hf in range(2):
            ho = hf * H
            p.dma("sync", x1h[:], x1bf_s[:, :, ho:ho + H], reads=["x1bf_s"], writes=["x1h"])
            p.dma("sync", acc[:], acc_s[:, :, ho:ho + H], reads=["acc_s"], writes=["acc0", "acc1"])
            for tt in range(H // T2):
                to = tt * T2
                for oc in range(8):
                    pDc = PS[3 * tt + 2]
                    p.op("tensor", lambda e, oc=oc, to=to, pDc=pDc: e.matmul(pDc[:, :T2], lhsT=bd_sb[:, oc * 128:(oc + 1) * 128], rhs=gateT[:, ho + to:ho + to + T2], start=True, stop=True),
                         reads=["bd_sb", "gateT"], writes=[f"pD{tt}"])
                    p.op("vector", lambda e, oc=oc, to=to, pDc=pDc: e.tensor_tensor(out=acc[:, oc, to:to + T2], in0=acc[:, oc, to:to + T2], in1=pDc[:, :T2], op=ALU.add),
                         reads=[f"acc{tt}", f"pD{tt}"], writes=[f"acc{tt}"])
            for ex_ in range(ne):
                wb_i = ex_ % 2
                wgs, wds = wgu_b[wb_i], wd_b[wb_i]
                for kc in (range(0, 8, 2) if not (os.environ.get("B_NODMA") and ex_ >= 2) else []):
                    p.dma("gpsimd", wgs[:, kc:kc + 2, :], wgu[ex_, kc * 128:(kc + 2) * 128, :].rearrange("(k p) n -> p k n", p=128), writes=[f"wgu{wb_i}"])
                for kc in (range(0, 8, 4) if not (os.environ.get("B_NODMA") and ex_ >= 2) else []):
                    p.dma("gpsimd", wds[:, kc:kc + 4, :], wd[ex_, kc * 128:(kc + 4) * 128, :].rearrange("(k p) n -> p k n", p=128), writes=[f"wd{wb_i}"])
                chains = [p.capture(moe_tile, ex_, tt, ho, wgs, wds, wb_i) for tt in range(H // T2)]
                p.emit_interleaved(chains)
            for tt in range(H // T2):
                to = tt * T2
                hv = acc[:, :, to:to + T2]
                ln_fm(p, hv, f"acc{tt}", 8, T2, lambda oc: vecs[:, 2, oc:oc + 1], lambda oc: vecs[:, 3, oc:oc + 1], ones_mean, sq, pS, pQ, tm, "pS7", "pBC")
                p.dma("sync", outv[:, :, ho + to:ho + to + T2], hv, reads=[f"acc{tt}"], writes=["outT"])
            p.barrier()
    p.finish_wait("sync", ["outT"] + (["x1_d", "gate_d"] if dump else []))
    return p.build()


def build_L0(ntok=2048):
    p = Prog(); nc = p.nc
    xT = p.dram("xT", [1024, ntok]); vec = p.dram("vec", [128, 2, 8])
    outT = p.dram("outT", [1024, ntok], kind="ExternalOutput")
    ones_mean = p.sb("ones_mean", [128, 128]); vecs = p.sb("vecs", [128, 2, 8])
    p.dma("sync", vecs[:], vec[:, :, :], writes=["vec"])
    p.op("vector", lambda e: e.memset(ones_mean[:], 1.0 / 1024), writes=["ones_mean"])
    TT = 512
    h = [p.sb(f"h{i}", [128, 8, TT]) for i in range(2)]
    sq = p.sb("sq", [128, 2, TT]); tm = {k: p.sb("tm_" + k, [128, TT]) for k in ["mean", "rstd", "t"]}
    pS = p.ps("pS", [128, 512]); pQ = p.ps("pQ", [128, 512])
    xv = xT.rearrange("(kc p) n -> p kc n", p=128); ov = outT.rearrange("(kc p) n -> p kc n", p=128)
    for t in range(ntok // TT):
        o = t * TT; i = t % 2
        p.dma("sync", h[i][:], xv[:, :, o:o + TT], writes=[f"h{i}"])
        ln_fm(p, h[i], f"h{i}", 8, TT, lambda oc: vecs[:, 0, oc:oc + 1], lambda oc: vecs[:, 1, oc:oc + 1], ones_mean, sq, pS, pQ, tm, "pS", "pQ")
        p.dma("gpsimd", ov[:, :, o:o + TT], h[i][:], reads=[f"h{i}"], writes=["outT"])
    p.finish_wait("sync", ["outT"])
    return p.build()


def host_inputs_B(L, stream, ya, yb, u, z, c):
    sl = slice(c * 2048, (c + 1) * 2048)
    f = lambda a: np.ascontiguousarray(a, dtype=np.float32)
    ycat = np.concatenate([ya[sl], yb[sl], u[sl]], axis=1)
    vec = np.stack([z['ln1_g'][L].reshape(8, 128).T, z['ln1_b'][L].reshape(8, 128).T, z['ln2_g'][L].reshape(8, 128).T,
                    z['ln2_b'][L].reshape(8, 128).T, z['ssd_norm'][L].reshape(8, 128).T], axis=1)
    return {
        "xT": f(stream[sl].T), "yT": f(ycat.T), "pT": f(z['p'][L].reshape(-1, 256)[sl].T),
        "wg": f(z['w_in'][L][:, 6576:]), "wb": f(z['w_branch'][L]), "wo": f(z['w_o'][L]), "wplg": f(z['w_pl_gate'][L]),
        "wpl": f(z['w_pl'][L]), "wr": f(z['w_router'][L]), "br": f(z['b_router'][L][None]),
        "wgu": f(z['w_gu'][L]), "wd": f(z['w_down'][L]), "bgu": f(z['b_gu'][L].reshape(32, 16, 128).transpose(2, 0, 1)),
        "bd": f(z['b_down'][L]), "vec": f(vec), "ident": np.eye(128, dtype=np.float32),
    }


def kernel(**inputs):
    z = {k: np.asarray(v) for k, v in inputs.items()}
    NCORE = 8
    cores = list(range(NCORE))
    xf = z['x'].reshape(-1, 1024).astype(np.float32)
    f = lambda a: np.ascontiguousarray(a, dtype=np.float32)
    vec0 = f(np.stack([z['ln_in_g'].reshape(8, 128).T, z['ln_in_b'].reshape(8, 128).T], axis=1))
    nc0 = build_L0()
    res = run_bass_kernel_spmd(nc0, [{"xT": f(xf[c * 2048:(c + 1) * 2048].T), "vec": vec0} for c in cores], core_ids=cores)
    stream = np.concatenate([r["outT"].T for r in res.results], axis=0)
    for L in range(2):
        ncA = build_A(T=8192)
        imA = [host_inputs_A(z, L, stream[b * 8192:(b + 1) * 8192], j) for b in range(2) for j in range(4)]
        resA = run_bass_kernel_spmd(ncA, imA, core_ids=cores).results
        ya = np.concatenate([np.concatenate([resA[b * 4 + j]["ya"] for j in range(4)], axis=1) for b in range(2)], axis=0)
        yb = np.concatenate([np.concatenate([resA[b * 4 + j]["yb"] for j in range(4)], axis=1) for b in range(2)], axis=0)
        u = np.concatenate([np.concatenate([resA[b * 4 + j]["yc"] for j in range(4)], axis=1) for b in range(2)], axis=0)
        del resA, imA
        ncB = build_B()
        imB = [host_inputs_B(L, stream, ya, yb, u, z, c) for c in cores]
        resB = run_bass_kernel_spmd(ncB, imB, core_ids=cores).results
        stream = np.concatenate([r["outT"].T for r in resB], axis=0)
        del resB, imB
    return np.ascontiguousarray(stream.reshape(2, 8192, 1024), dtype=np.float32)
```
